# Optimizing a Trainium2 kernel written in Bass

```python
import jax
import jax.numpy as jnp
from jax import lax
import numpy as np

D_MODEL = 1024
BATCH = 2
SEQ = 16384
DEPTH = 4

GRID_W = 64
CTX_LEN = 256
HEAD_DIM = 64
ROPE_BASE = 10000.0
QBLK = 128
NORM_EPS = 1e-6
NEG_INF = -1e30
A_HEADS = 8
A_KV_HEADS = 2
A_WINDOW = 128
B_HEADS = 8
NA_ROWS = 8
NA_COLS = 16
C_HEADS = 8
C_Q_RANK = 768
C_KV_RANK = 256
C_NOPE = 64
C_ROPE = 32
C_V = 64
D_HEADS = 8
D_KV_HEADS = 2
N_EXPERTS = 32
TOP_K = 4
D_EXPERT = 1024
SWIGLU_LIMIT = 7.0
SWIGLU_ALPHA = 1.702
MOE_BLK = 256
N_EVEN = (DEPTH + 1) // 2
N_ODD = DEPTH // 2
AB_IN = 2304
AB_OUT = 1024
CD_IN = 1824
CD_OUT = 1024

kernel_name = 'hybrid_diffusion_trunk'


def _rms_norm(x, g):
    xf = x.astype(jnp.float32)
    y = xf * lax.rsqrt(jnp.mean(xf * xf, axis=-1, keepdims=True) + NORM_EPS)
    return (y * g.astype(jnp.float32)).astype(x.dtype)


def _modulate(x, g, shift, scale):
    return _rms_norm(x, g) * (1 + scale) + shift


def _axial_rope(n_tokens, dim, dtype):
    t = jnp.arange(n_tokens, dtype=jnp.int32)
    row = (t // GRID_W).astype(jnp.float32)
    col = (t % GRID_W).astype(jnp.float32)
    quarter = dim // 4
    inv_freq = ROPE_BASE ** (-jnp.arange(quarter, dtype=jnp.float32) / quarter)
    ang = jnp.concatenate([row[:, None] * inv_freq, col[:, None] * inv_freq], axis=-1)
    return (jnp.cos(ang)[:, None, :].astype(dtype), jnp.sin(ang)[:, None, :].astype(dtype))


def _apply_rope(x, rope):
    cos, sin = rope
    half = x.shape[-1] // 2
    x1, x2 = x[..., :half], x[..., half:]
    return jnp.concatenate([x1 * cos - x2 * sin, x1 * sin + x2 * cos], axis=-1)


def _group(q, n_groups):
    return q.reshape(q.shape[:2] + (n_groups, q.shape[2] // n_groups, q.shape[3]))


def _scores(q, k):
    return jnp.einsum('bqgrd,bkgd->bgrqk', q, k).astype(jnp.float32)


def _mix(p, v):
    return jnp.einsum('bgrqk,bkgd->bqgrd', p.astype(v.dtype), v)


def _sink_column(sink, like):
    return jnp.broadcast_to(sink.astype(jnp.float32)[None, :, :, None, None], like.shape[:-1] + (1,))


def _dense_attention(q, k, v, sink=None):
    s = _scores(q, k) * (q.shape[-1] ** -0.5)
    if sink is None:
        return _mix(jax.nn.softmax(s, axis=-1), v)
    p = jax.nn.softmax(jnp.concatenate([s, _sink_column(sink, s)], axis=-1), axis=-1)
    return _mix(p[..., :-1], v)


def _to_blocks(q):
    b, t = q.shape[:2]
    return jnp.swapaxes(q.reshape((b, t // QBLK, QBLK) + q.shape[2:]), 0, 1)


def _from_blocks(o):
    o = jnp.swapaxes(o, 0, 1)
    return o.reshape(o.shape[0], o.shape[1] * o.shape[2], -1)


def _blocked_dense_attention(q, k, v):
    return _from_blocks(lax.map(lambda qb: _dense_attention(qb, k, v), _to_blocks(q)))


def _window_sink_attention(q, k, v, kc, vc, sink):
    n = q.shape[1]
    span = QBLK + 2 * A_WINDOW
    pad = ((0, 0), (A_WINDOW, A_WINDOW), (0, 0), (0, 0))
    kp, vp = jnp.pad(k, pad), jnp.pad(v, pad)
    scale = q.shape[-1] ** -0.5
    n_ctx = kc.shape[1]

    def block(args):
        i, qb = args
        start = i * QBLK
        kb = lax.dynamic_slice_in_dim(kp, start, span, axis=1)
        vb = lax.dynamic_slice_in_dim(vp, start, span, axis=1)
        qpos = start + jnp.arange(QBLK)
        kpos = start - A_WINDOW + jnp.arange(span)
        ok = (jnp.abs(qpos[:, None] - kpos[None, :]) <= A_WINDOW) & ((kpos >= 0) & (kpos < n))[None, :]
        s_loc = jnp.where(ok, _scores(qb, kb) * scale, NEG_INF)
        s_ctx = _scores(qb, kc) * scale
        p = jax.nn.softmax(jnp.concatenate([s_loc, s_ctx, _sink_column(sink, s_ctx)], axis=-1), axis=-1)
        return _mix(p[..., :span], vb) + _mix(p[..., span:span + n_ctx], vc)

    out = lax.map(block, (jnp.arange(n // QBLK), _to_blocks(q)))
    return _from_blocks(out)


def _neighbourhood_attention(q, k, v, kc, vc, rpb, rows):
    b, n, h, d = q.shape
    kh = min(NA_ROWS, rows)
    kw = NA_COLS
    scale = d ** -0.5
    kg = k.reshape(b, rows, GRID_W, h, d)
    vg = v.reshape(b, rows, GRID_W, h, d)
    qr = jnp.swapaxes(q.reshape(b, rows, GRID_W, h, d), 0, 1)
    cols = jnp.arange(GRID_W)
    col_idx = jnp.clip(cols - kw // 2, 0, GRID_W - kw)[:, None] + jnp.arange(kw)[None, :]
    dc = col_idx - cols[:, None] + (NA_COLS - 1)
    rpb_f = rpb.astype(jnp.float32)

    def row_block(args):
        r, qb = args
        rs = jnp.clip(r - kh // 2, 0, rows - kh)
        kr = lax.dynamic_slice_in_dim(kg, rs, kh, axis=1)[:, :, col_idx]
        vr = lax.dynamic_slice_in_dim(vg, rs, kh, axis=1)[:, :, col_idx]
        dr = rs + jnp.arange(kh) - r + (NA_ROWS - 1)
        bias = rpb_f[:, dr[None, :, None], dc[:, None, :]]
        s_loc = jnp.einsum('bchd,bicjhd->bhcij', qb, kr).astype(jnp.float32) * scale + bias[None]
        s_loc = s_loc.reshape(b, h, GRID_W, kh * kw)
        s_ctx = jnp.einsum('bchd,bkhd->bhck', qb, kc).astype(jnp.float32) * scale
        p = jax.nn.softmax(jnp.concatenate([s_loc, s_ctx], axis=-1), axis=-1)
        p_loc = p[..., :kh * kw].reshape(b, h, GRID_W, kh, kw).astype(v.dtype)
        p_ctx = p[..., kh * kw:].astype(v.dtype)
        return (jnp.einsum('bhcij,bicjhd->bchd', p_loc, vr)
                + jnp.einsum('bhck,bkhd->bchd', p_ctx, vc))

    out = lax.map(row_block, (jnp.arange(rows), qr))
    return jnp.swapaxes(out, 0, 1).reshape(b, n, h * d)


def _mixer_ab(hc, hl, w_in, w_out, sink, rpb, rope, rows, need_ctx):
    d = HEAD_DIM
    splits = [A_HEADS * d, (A_HEADS + A_KV_HEADS) * d, (A_HEADS + 2 * A_KV_HEADS) * d,
              (A_HEADS + 2 * A_KV_HEADS + B_HEADS) * d, (A_HEADS + 2 * A_KV_HEADS + 2 * B_HEADS) * d]

    def project(h):
        b, t = h.shape[:2]
        qa, ka, va, qb, kb, vb = jnp.split(h @ w_in, splits, axis=-1)
        return (qa.reshape(b, t, A_HEADS, d), ka.reshape(b, t, A_KV_HEADS, d), va.reshape(b, t, A_KV_HEADS, d),
                qb.reshape(b, t, B_HEADS, d), kb.reshape(b, t, B_HEADS, d), vb.reshape(b, t, B_HEADS, d))

    qa_c, ka_c, va_c, qb_c, kb_c, vb_c = project(hc)
    qa, ka, va, qb, kb, vb = project(hl)
    sink_gr = sink.reshape(A_KV_HEADS, A_HEADS // A_KV_HEADS)
    ya = _window_sink_attention(_group(_apply_rope(qa, rope), A_KV_HEADS), _apply_rope(ka, rope), va,
                                ka_c, va_c, sink_gr)
    yb = _neighbourhood_attention(qb, kb, vb, kb_c, vb_c, rpb, rows)
    yl = jnp.concatenate([ya, yb], axis=-1) @ w_out
    if not need_ctx:
        return None, yl
    b, t = hc.shape[:2]
    ya_c = _dense_attention(_group(qa_c, A_KV_HEADS), ka_c, va_c, sink_gr).reshape(b, t, -1)
    yb_c = _dense_attention(qb_c[:, :, :, None], kb_c, vb_c).reshape(b, t, -1)
    return jnp.concatenate([ya_c, yb_c], axis=-1) @ w_out, yl


def _mixer_cd(hc, hl, w_in, q_norm, w_q_b, kv_norm, w_kv_b, dq_norm, dk_norm, w_out, rope_mla, rope_head,
              need_ctx):
    d = HEAD_DIM
    base = C_Q_RANK + C_KV_RANK + C_ROPE
    splits = [C_Q_RANK, C_Q_RANK + C_KV_RANK, base, base + D_HEADS * d, base + (D_HEADS + D_KV_HEADS) * d]

    def project(h, positioned):
        b, t = h.shape[:2]
        cq, ckv, k_rope, qd, kd, vd = jnp.split(h @ w_in, splits, axis=-1)
        q = (_rms_norm(cq, q_norm) @ w_q_b).reshape(b, t, C_HEADS, C_NOPE + C_ROPE)
        kv = (_rms_norm(ckv, kv_norm) @ w_kv_b).reshape(b, t, C_HEADS, C_NOPE + C_V)
        q_nope, q_rope = q[..., :C_NOPE], q[..., C_NOPE:]
        k_nope, v_mla = kv[..., :C_NOPE], kv[..., C_NOPE:]
        k_rope = k_rope[:, :, None, :]
        qd = _rms_norm(qd.reshape(b, t, D_HEADS, d), dq_norm)
        kd = _rms_norm(kd.reshape(b, t, D_KV_HEADS, d), dk_norm)
        vd = vd.reshape(b, t, D_KV_HEADS, d)
        if positioned:
            q_rope, k_rope = _apply_rope(q_rope, rope_mla), _apply_rope(k_rope, rope_mla)
            qd, kd = _apply_rope(qd, rope_head), _apply_rope(kd, rope_head)
        q_mla = jnp.concatenate([q_nope, q_rope], axis=-1)[:, :, :, None]
        k_mla = jnp.concatenate([k_nope, jnp.broadcast_to(k_rope, (b, t, C_HEADS, C_ROPE))], axis=-1)
        return q_mla, k_mla, v_mla, _group(qd, D_KV_HEADS), kd, vd

    qc_c, kc_c, vc_c, qd_c, kd_c, vd_c = project(hc, False)
    qc, kc, vc, qd, kd, vd = project(hl, True)
    yc = _blocked_dense_attention(qc, jnp.concatenate([kc_c, kc], axis=1), jnp.concatenate([vc_c, vc], axis=1))
    yd = _blocked_dense_attention(qd, jnp.concatenate([kd_c, kd], axis=1), jnp.concatenate([vd_c, vd], axis=1))
    yl = jnp.concatenate([yc, yd], axis=-1) @ w_out
    if not need_ctx:
        return None, yl
    b, t = hc.shape[:2]
    yc_c = _dense_attention(qc_c, kc_c, vc_c).reshape(b, t, -1)
    yd_c = _dense_attention(qd_c, kd_c, vd_c).reshape(b, t, -1)
    return jnp.concatenate([yc_c, yd_c], axis=-1) @ w_out, yl


def _clamped_swiglu(u):
    glu, lin = u[..., :D_EXPERT], u[..., D_EXPERT:]
    glu = jnp.minimum(glu, SWIGLU_LIMIT)
    lin = jnp.clip(lin, -SWIGLU_LIMIT, SWIGLU_LIMIT)
    return glu * jax.nn.sigmoid(SWIGLU_ALPHA * glu) * (lin + 1)


def _moe(h, router_w, router_b, w_in, b_in, w_out, b_out):
    n, dm = h.shape
    nk = n * TOP_K
    logits = (h @ router_w).astype(jnp.float32) + router_b.astype(jnp.float32)
    top_val, top_idx = lax.top_k(logits, TOP_K)
    gate = jax.nn.softmax(top_val, axis=-1).astype(h.dtype)
    expert = top_idx.reshape(-1)
    token = jnp.arange(nk, dtype=jnp.int32) // TOP_K
    order = jnp.argsort(expert)
    e_sorted = expert[order]
    sizes = jnp.bincount(expert, length=N_EXPERTS)
    padded = (sizes + MOE_BLK - 1) // MOE_BLK * MOE_BLK
    starts = jnp.cumsum(sizes) - sizes
    pends = jnp.cumsum(padded)
    pstarts = pends - padded
    dest = pstarts[e_sorted] + jnp.arange(nk, dtype=jnp.int32) - starts[e_sorted]
    n_blocks = -(-(nk + N_EXPERTS * (MOE_BLK - 1)) // MOE_BLK)
    tok_buf = jnp.full((n_blocks * MOE_BLK,), n, jnp.int32).at[dest].set(token[order])
    gate_buf = jnp.zeros((n_blocks * MOE_BLK,), h.dtype).at[dest].set(gate.reshape(-1)[order])
    blk_expert = jnp.minimum(jnp.searchsorted(pends, jnp.arange(n_blocks, dtype=jnp.int32) * MOE_BLK,
                                              side='right'), N_EXPERTS - 1)
    h_pad = jnp.concatenate([h, jnp.zeros((1, dm), h.dtype)], axis=0)

    def step(acc, xs):
        idx, g, e = xs
        u = h_pad[idx] @ w_in[e] + b_in[e]
        out = _clamped_swiglu(u) @ w_out[e] + b_out[e]
        return acc.at[idx].add(out * g[:, None]), None

    acc, _ = lax.scan(step, jnp.zeros((n + 1, dm), h.dtype),
                      (tok_buf.reshape(n_blocks, MOE_BLK), gate_buf.reshape(n_blocks, MOE_BLK), blk_expert))
    return acc[:n]


def setup_inputs(seed: int = 0) -> dict:
    key = jax.random.key(seed)
    ks = iter(jax.random.split(key, 32))
    dm = D_MODEL

    def normal(shape, scale):
        return jax.random.normal(next(ks), shape, jnp.float32) * scale

    return {
        'x': normal((BATCH, SEQ, dm), 1.0),
        'c': normal((BATCH, dm), 1.0),
        'ctx': normal((BATCH, CTX_LEN, dm), 1.0),
        'c_ctx': normal((dm,), 1.0),
        'mod_w': normal((DEPTH, dm, 6 * dm), 0.5 * dm ** -0.5),
        'mod_b': normal((DEPTH, 6 * dm), 0.02),
        'norm_mix': 1.0 + normal((DEPTH, dm), 0.05),
        'norm_ffn': 1.0 + normal((DEPTH, dm), 0.05),
        'ab_w_in': normal((N_EVEN, dm, AB_IN), dm ** -0.5),
        'ab_w_out': normal((N_EVEN, AB_OUT, dm), AB_OUT ** -0.5),
        'a_sink': normal((N_EVEN, A_HEADS), 0.5),
        'b_rpb': normal((N_EVEN, B_HEADS, 2 * NA_ROWS - 1, 2 * NA_COLS - 1), 0.1),
        'cd_w_in': normal((N_ODD, dm, CD_IN), dm ** -0.5),
        'c_q_norm': 1.0 + normal((N_ODD, C_Q_RANK), 0.05),
        'c_w_q_b': normal((N_ODD, C_Q_RANK, C_HEADS * (C_NOPE + C_ROPE)), C_Q_RANK ** -0.5),
        'c_kv_norm': 1.0 + normal((N_ODD, C_KV_RANK), 0.05),
        'c_w_kv_b': normal((N_ODD, C_KV_RANK, C_HEADS * (C_NOPE + C_V)), C_KV_RANK ** -0.5),
        'd_q_norm': 1.0 + normal((N_ODD, HEAD_DIM), 0.05),
        'd_k_norm': 1.0 + normal((N_ODD, HEAD_DIM), 0.05),
        'cd_w_out': normal((N_ODD, CD_OUT, dm), CD_OUT ** -0.5),
        'router_w': normal((DEPTH, dm, N_EXPERTS), dm ** -0.5),
        'router_b': normal((DEPTH, N_EXPERTS), 0.01),
        'exp_w_in': normal((DEPTH, N_EXPERTS, dm, 2 * D_EXPERT), dm ** -0.5),
        'exp_b_in': normal((DEPTH, N_EXPERTS, 2 * D_EXPERT), 0.02),
        'exp_w_out': normal((DEPTH, N_EXPERTS, D_EXPERT, dm), D_EXPERT ** -0.5),
        'exp_b_out': normal((DEPTH, N_EXPERTS, dm), 0.02),
        'final_norm': 1.0 + normal((dm,), 0.05),
    }


def reference(x, c, ctx, c_ctx, mod_w, mod_b, norm_mix, norm_ffn, ab_w_in, ab_w_out, a_sink, b_rpb,
              cd_w_in, c_q_norm, c_w_q_b, c_kv_norm, c_w_kv_b, d_q_norm, d_k_norm, cd_w_out,
              router_w, router_b, exp_w_in, exp_b_in, exp_w_out, exp_b_out, final_norm):
    b, t, dm = x.shape
    n_ctx = ctx.shape[1]
    rows = t // GRID_W
    rope_head = _axial_rope(t, HEAD_DIM, x.dtype)
    rope_mla = _axial_rope(t, C_ROPE, x.dtype)
    cond_lat = jax.nn.silu(c)[:, None, :]
    cond_ctx = jax.nn.silu(c_ctx)
    xc = ctx
    for layer in range(DEPTH):
        need_ctx = layer < DEPTH - 1
        i = layer // 2
        mod_lat = jnp.split(cond_lat @ mod_w[layer] + mod_b[layer], 6, axis=-1)
        mod_ctx = jnp.split(cond_ctx @ mod_w[layer] + mod_b[layer], 6, axis=-1)
        hl = _modulate(x, norm_mix[layer], mod_lat[0], mod_lat[1])
        hc = _modulate(xc, norm_mix[layer], mod_ctx[0], mod_ctx[1])
        if layer % 2 == 0:
            yc, yl = _mixer_ab(hc, hl, ab_w_in[i], ab_w_out[i], a_sink[i], b_rpb[i], rope_head, rows, need_ctx)
        else:
            yc, yl = _mixer_cd(hc, hl, cd_w_in[i], c_q_norm[i], c_w_q_b[i], c_kv_norm[i], c_w_kv_b[i],
                               d_q_norm[i], d_k_norm[i], cd_w_out[i], rope_mla, rope_head, need_ctx)
        x = x + mod_lat[2] * yl
        hl = _modulate(x, norm_ffn[layer], mod_lat[3], mod_lat[4])
        moe_w = (router_w[layer], router_b[layer], exp_w_in[layer], exp_b_in[layer], exp_w_out[layer],
                 exp_b_out[layer])
        if need_ctx:
            xc = xc + mod_ctx[2] * yc
            hc = _modulate(xc, norm_ffn[layer], mod_ctx[3], mod_ctx[4])
            y = _moe(jnp.concatenate([hc.reshape(-1, dm), hl.reshape(-1, dm)], axis=0), *moe_w)
            xc = xc + mod_ctx[5] * y[:b * n_ctx].reshape(b, n_ctx, dm)
            x = x + mod_lat[5] * y[b * n_ctx:].reshape(b, t, dm)
        else:
            x = x + mod_lat[5] * _moe(hl.reshape(-1, dm), *moe_w).reshape(b, t, dm)
    return _rms_norm(x, final_norm)
```

```python
import contextlib
import numpy as np
import ml_dtypes
import concourse.bass as bass
import concourse.mybir as mybir
from concourse.bass_utils import run_bass_kernel_spmd

F32 = mybir.dt.float32
BF16 = mybir.dt.bfloat16
AF = mybir.ActivationFunctionType
ALU = mybir.AluOpType
AX = mybir.AxisListType

NCORES = 8
DM = 1024
BATCH = 2
SEQ = 16384
DEPTH = 4
GRID_W = 64
CTX = 256
LAT_PC = SEQ // 4
NT = CTX + LAT_PC
NTOK = CTX + SEQ
NKB = NTOK // 128
EPS = 1e-6
NEXP = 32
SAME_ENGINE_SYNC = True


class Buf:
    __slots__ = ("name", "writers", "readers")

    def __init__(self, name):
        self.name = name
        self.writers = {}
        self.readers = {}


class _Rec:
    def __init__(self):
        self.call = None

    def __getattr__(self, m):
        def f(*a, **kw):
            self.call = (m, a, kw)
            return self
        return f


class Sched:
    ENG = ("pe", "act", "dve", "pool", "sp")

    def __init__(self, nc, stack, ndma_sems=12):
        self.nc = nc
        self.prog = {e: [] for e in self.ENG}
        self.sem = {e: stack.enter_context(nc.semaphore("s_" + e)) for e in self.ENG}
        self.cnt = {e: 0 for e in self.ENG}
        self.waited = {e: {} for e in self.ENG}
        self.dq = {}
        for q in ("sp", "pool"):
            sems = [stack.enter_context(nc.semaphore(f"d_{q}{i}")) for i in range(ndma_sems)]
            self.dq[q] = {"sems": sems, "n": 0}
        self.semkey = {}
        self.ccs = []
        self.ccsem = None
        self.final = []
        self.ninst = 0

    def _key(self, sem):
        k = id(sem)
        self.semkey[k] = sem
        return k

    def _wait(self, eng, deps):
        w = self.waited[eng]
        for k, v in deps.items():
            if w.get(k, 0) >= v:
                continue
            w[k] = v
            sem = self.semkey[k]
            self.prog[eng].append(lambda e, sem=sem, v=v: e.wait_ge(sem, v))

    def _deps(self, eng, reads, writes):
        deps = {}
        own = self._key(self.sem[eng])

        def add(d):
            for k, v in d.items():
                if k == own and (eng == "pe" or not SAME_ENGINE_SYNC):
                    continue
                if deps.get(k, 0) < v:
                    deps[k] = v
        for b in reads:
            add(b.writers)
        for b in writes:
            add(b.writers)
            for k, v in b.readers.items():
                if k != own and deps.get(k, 0) < v:
                    deps[k] = v
        return deps

    def _mark(self, tok, reads, writes):
        k, v = tok
        for b in reads:
            if b.readers.get(k, 0) < v:
                b.readers[k] = v
        for b in writes:
            if b.readers:
                b.readers = {}
                b.writers = {}
            b.writers[k] = v

    def op(self, eng, fn, reads=(), writes=()):
        deps = self._deps(eng, reads, writes)
        self._wait(eng, deps)
        self.cnt[eng] += 1
        n = self.cnt[eng]
        sem = self.sem[eng]
        r = _Rec()
        fn(r)
        m, a, kw = r.call
        self.prog[eng].append(lambda e, m=m, a=a, kw=kw, sem=sem: getattr(e, m)(*a, **kw).then_inc(sem, 1))
        self._mark((self._key(sem), n), reads, writes)
        self.ninst += 1

    def cc(self, stack, fn, scratch, reads=(), writes=()):
        deps = self._deps("pool", reads, writes)
        self._wait("pool", deps)
        if self.ccsem is None:
            self.ccsem = stack.enter_context(self.nc.semaphore("ccsem"))
        sem = self.ccsem
        r = _Rec()
        fn(r)
        m, a, kw = r.call
        self.prog["pool"].append(lambda e, m=m, a=a, kw=kw, sem=sem: getattr(e, m)(*a, **kw).then_inc(sem, 1))
        self.ccs.append(sem)
        n = len(self.ccs)
        self.prog["pool"].append(lambda e, sem=sem, n=n: e.wait_ge(sem, n))
        self.op("pool", lambda e: e.memset(scratch, 0.0), reads=reads, writes=writes)

    def dma(self, q, out, in_, reads=(), writes=(), final=False):
        deps = self._deps(q, reads, writes)
        d = self.dq[q]
        i = d["n"]
        d["n"] += 1
        sems = d["sems"]
        sem = sems[i % len(sems)]
        rnd = i // len(sems)
        k = self._key(sem)
        if rnd > 0:
            deps[k] = max(deps.get(k, 0), 16 * rnd)
        self._wait(q, deps)
        self.prog[q].append(lambda e, o=out, a=in_, sem=sem: e.dma_start(out=o, in_=a).then_inc(sem, 16))
        tok = (k, 16 * (rnd + 1))
        self._mark(tok, reads, writes)
        if final:
            self.final.append(tok)
        self.ninst += 1

    def barrier(self):
        deps = {}
        for e in self.ENG:
            if self.cnt[e]:
                deps[self._key(self.sem[e])] = self.cnt[e]
        for q in self.dq.values():
            n = q["n"]
            L = len(q["sems"])
            for j, sem in enumerate(q["sems"]):
                uses = (n - j + L - 1) // L if n > j else 0
                if uses:
                    deps[self._key(sem)] = 16 * uses
        for e in self.ENG:
            own = self._key(self.sem[e])
            self._wait(e, {kk: v for kk, v in deps.items() if kk != own})

    def emit(self):
        nc = self.nc
        fin = {}
        for k, v in self.final:
            fin[k] = max(fin.get(k, 0), v)
        for q in self.dq.values():
            n = q["n"]
            for j, sem in enumerate(q["sems"]):
                uses = (n - j + len(q["sems"]) - 1) // len(q["sems"]) if n > j else 0
                if uses:
                    fin[self._key(sem)] = max(fin.get(self._key(sem), 0), 16 * uses)
        self._wait("sp", fin)
        prog = self.prog
        with nc.Block() as block:
            @block.tensor
            def _(e):
                for f in prog["pe"]:
                    f(e)

            @block.scalar
            def _(e):
                for f in prog["act"]:
                    f(e)

            @block.vector
            def _(e):
                for f in prog["dve"]:
                    f(e)

            @block.gpsimd
            def _(e):
                for f in prog["pool"]:
                    f(e)

            @block.sync
            def _(e):
                for f in prog["sp"]:
                    f(e)


class K:
    def __init__(self, name):
        self.nc = bass.Bass("TRN2", target_bir_lowering=False, name=name)
        self.stack = contextlib.ExitStack()
        self.S = Sched(self.nc, self.stack)
        self.nbuf = 0
        self.io = {}
        self.fused = False
        self.phase = 0

    def dram(self, name, shape, dt, kind):
        if name in self.io:
            return self.io[name]
        if self.fused and self.phase > 0:
            name = f"{name}_ph{self.phase}"
        return self.nc.dram_tensor(name, list(shape), dt, kind=kind).ap()

    def begin_phase(self, io):
        self.phase += 1
        self.io = io
        self.saved = self.stack
        self.stack = contextlib.ExitStack()

    def end_phase(self):
        self.S.barrier()
        self.stack.close()
        self.stack = self.saved
        self.io = {}

    def sb(self, shape, dt, name=None):
        self.nbuf += 1
        name = f"{name or 't'}_{self.nbuf}"
        t = self.stack.enter_context(self.nc.sbuf_tensor(name, list(shape), dt))
        return t, Buf(name)

    def ps(self, shape, dt, name=None):
        self.nbuf += 1
        name = f"{name or 'p'}_{self.nbuf}"
        t = self.stack.enter_context(self.nc.psum_tensor(name, list(shape), dt))
        return t, Buf(name)

    def finish(self):
        self.S.emit()
        self.stack.close()
        return self.nc


class Ring:
    def __init__(self, items):
        self.items = items
        self.i = 0

    def next(self):
        it = self.items[self.i % len(self.items)]
        self.i += 1
        return it


def token_tiles():
    tiles = [(0, CTX, True)]
    for i in range(LAT_PC // 512):
        tiles.append((CTX + 512 * i, 512, False))
    return tiles


def emit_mod_vectors(k, S, modw_d, modb_sb, modb_b, cond_sb, cond_b, nvec, out_sb, out_b, wring):
    ps_t, ps_b = k.ps([128, 4, 2], F32, "modps")
    for v in range(nvec):
        for hf in range(2):
            w_sb, w_b = wring.next()
            c0 = v * 1024 + hf * 512
            S.dma("sp", w_sb[:], modw_d[:, c0:c0 + 512].rearrange("(kc p) f -> p kc f", p=128), writes=[w_b])
            for j in range(4):
                for kc in range(8):
                    S.op("pe", lambda e, j=j, kc=kc, w_sb=w_sb: e.matmul(ps_t[:, j, :], lhsT=w_sb[:, kc, j * 128:(j + 1) * 128],
                                                                      rhs=cond_sb[:, kc, :], start=(kc == 0), stop=(kc == 7)),
                         reads=[w_b, cond_b], writes=[ps_b])
            for c in range(2):
                S.op("dve", lambda e, v=v, c=c, hf=hf: e.tensor_tensor(out=out_sb[:, v, hf * 4:hf * 4 + 4, c], in0=ps_t[:, :, c],
                                                                     in1=modb_sb[:, v * 8 + hf * 4:v * 8 + hf * 4 + 4], op=ALU.add),
                     reads=[ps_b, modb_b], writes=[out_b])


def emit_norm_tile(k, S, x_sb, x_b, T, onesb, ones_b, sq_sb, sq_b, ss_ps, ss_b, rstd_sb, rstd_b, eps_sb, eps_b):
    S.op("act", lambda e: e.activation(out=sq_sb[:, :, :T], in_=x_sb[:, :, :T], func=AF.Square), reads=[x_b], writes=[sq_b])
    for kc in range(8):
        S.op("pe", lambda e, kc=kc: e.matmul(ss_ps[:, :T], lhsT=onesb[:, :], rhs=sq_sb[:, kc, :T], start=(kc == 0), stop=(kc == 7)),
             reads=[sq_b, ones_b], writes=[ss_b])
    S.op("act", lambda e: e.activation(out=rstd_sb[:, :T], in_=ss_ps[:, :T], func=AF.Sqrt, scale=1.0 / DM, bias=eps_sb[:, 0:1]),
         reads=[ss_b, eps_b], writes=[rstd_b])
    S.op("dve", lambda e: e.reciprocal(out=rstd_sb[:, :T], in_=rstd_sb[:, :T]), reads=[rstd_b], writes=[rstd_b])


def load_consts(k, S, ident_d, need_f32_ident=False):
    identb, identb_b = k.sb([128, 128], BF16, "identb")
    S.dma("pool", identb[:], ident_d[:, :], writes=[identb_b])
    onesb, ones_b = k.sb([128, 128], BF16, "onesb")
    S.op("dve", lambda e: e.memset(onesb[:], 1.0), writes=[ones_b])
    eps_sb, eps_b = k.sb([128, 1], F32, "eps")
    S.op("dve", lambda e: e.memset(eps_sb[:], EPS), writes=[eps_b])
    return identb, identb_b, onesb, ones_b, eps_sb, eps_b


def build_p1(odd, k=None, io=None):
    own = k is None
    if own:
        k = K("p1o" if odd else "p1e")
    else:
        k.begin_phase(io)
    S = k.S
    xT = k.dram("xT", [DM, NT], F32, "ExternalInput")
    cond = k.dram("cond", [128, 8, 2], F32, "ExternalInput")
    modw = k.dram("modw", [DM, 2048], F32, "ExternalInput")
    modb = k.dram("modb", [128, 16], F32, "ExternalInput")
    gain = k.dram("gain", [128, 8], F32, "ExternalInput")
    ropeC = k.dram("ropeC", [128, NT], F32, "ExternalInput")
    ropeS = k.dram("ropeS", [128, NT], F32, "ExternalInput")
    if not odd:
        NW = 2304 + 640
        w_d = k.dram("w", [DM, NW], F32, "ExternalInput")
        fm_out = k.dram("fm", [1664, NT], BF16, "ExternalOutput")
        tm_out = k.dram("tm", [NT, 640], BF16, "ExternalOutput")
    else:
        NW = 1824 + 32 + 640
        w_d = k.dram("w", [DM, NW], F32, "ExternalInput")
        wqb_d = k.dram("wqb", [768, 1536], F32, "ExternalInput")
        wkvb_d = k.dram("wkvb", [256, 1024], F32, "ExternalInput")
        qn_d = k.dram("qn", [128, 6], F32, "ExternalInput")
        kvn_d = k.dram("kvn", [128, 2], F32, "ExternalInput")
        dgC = k.dram("dgq", [128, 4], F32, "ExternalInput")
        ropeC2 = k.dram("ropeC2", [96, NT], F32, "ExternalInput")
        ropeS2 = k.dram("ropeS2", [96, NT], F32, "ExternalInput")
        blk_d = k.dram("blk", [128, 128], F32, "ExternalInput")
        ropeC3 = k.dram("ropeC3", [32, NT], F32, "ExternalInput")
        ropeS3 = k.dram("ropeS3", [32, NT], F32, "ExternalInput")
        fm_out = k.dram("fm", [768 + 512 + 32 + 512 + 128, NT], BF16, "ExternalOutput")
        tm_out = k.dram("tm", [NT, 640], BF16, "ExternalOutput")
    ident_d = k.dram("ident", [128, 128], F32, "ExternalInput")

    identb, identb_b, onesb, ones_b, eps_sb, eps_b = load_consts(k, S, ident_d)

    w_sb, w_b = k.sb([128, 8, NW], BF16, "w_sb")
    for kc in range(8):
        for c0 in range(0, NW, 1024):
            c1 = min(NW, c0 + 1024)
            S.dma("pool", w_sb[:, kc, c0:c1], w_d[kc * 128:(kc + 1) * 128, c0:c1], writes=[w_b])
    if odd:
        wqb_sb, wqb_b = k.sb([128, 6, 1536], BF16, "wqb_sb")
        for kc in range(6):
            for c0 in (0, 768):
                S.dma("pool", wqb_sb[:, kc, c0:c0 + 768], wqb_d[kc * 128:(kc + 1) * 128, c0:c0 + 768], writes=[wqb_b])
        wkvb_sb, wkvb_b = k.sb([128, 2, 1024], BF16, "wkvb_sb")
        for kc in range(2):
            S.dma("pool", wkvb_sb[:, kc, :], wkvb_d[kc * 128:(kc + 1) * 128, :], writes=[wkvb_b])
        lg_sb, lg_b = k.sb([128, 8], F32, "lg_sb")
        S.dma("sp", lg_sb[:, 0:6], qn_d[:, :], writes=[lg_b])
        S.dma("sp", lg_sb[:, 6:8], kvn_d[:, :], writes=[lg_b])
        dg_sb, dg_b = k.sb([128, 4], F32, "dg_sb")
        S.dma("sp", dg_sb[:], dgC[:, :], writes=[dg_b])
        blk_sb, blk_b = k.sb([128, 128], BF16, "blk_sb")
        S.dma("pool", blk_sb[:], blk_d[:, :], writes=[blk_b])

    cond_sb, cond_b = k.sb([128, 8, 2], F32, "cond_sb")
    S.dma("sp", cond_sb[:], cond[:, :, :], writes=[cond_b])
    S.op("act", lambda e: e.activation(out=cond_sb[:], in_=cond_sb[:], func=AF.Silu), reads=[cond_b], writes=[cond_b])
    modb_sb, modb_b = k.sb([128, 16], F32, "modb_sb")
    S.dma("sp", modb_sb[:], modb[:, :], writes=[modb_b])
    gain_sb, gain_b = k.sb([128, 8], F32, "gain_sb")
    S.dma("sp", gain_sb[:], gain[:, :], writes=[gain_b])
    mv_sb, mv_b = k.sb([128, 2, 8, 2], F32, "mv_sb")
    wm, wm_b = k.sb([128, 8, 512], F32, "modw_sb")
    emit_mod_vectors(k, S, modw, modb_sb, modb_b, cond_sb, cond_b, 2, mv_sb, mv_b, Ring([(wm, wm_b)]))
    A_sb, A_b = k.sb([128, 8, 2], F32, "A_sb")
    for c in range(2):
        S.op("dve", lambda e, c=c: e.scalar_tensor_tensor(out=A_sb[:, :, c], in0=mv_sb[:, 1, :, c], scalar=1.0, in1=gain_sb[:, :], op0=ALU.add, op1=ALU.mult),
             reads=[mv_b, gain_b], writes=[A_b])

    x_sb, x_b = k.sb([128, 8, 512], F32, "x_sb")
    sq_sb, sq_b = k.sb([128, 8, 512], BF16, "sq_sb")
    rstd_sb, rstd_b = k.sb([128, 512], F32, "rstd_sb")
    t_sb, t_b = k.sb([128, 512], F32, "t_sb")
    h_sb, h_b = k.sb([128, 8, 512], BF16, "h_sb")
    rc_sb, rc_b = k.sb([128, 512], F32, "rc_sb")
    rs_sb, rs_b = k.sb([128, 512], F32, "rs_sb")
    ss_ps, ss_b = k.ps([128, 512], F32, "ss_ps")
    pring = Ring([k.ps([128, 512], F32, f"pp{i}") for i in range(5)])
    oring = Ring([k.sb([128, 512], BF16, f"ob{i}") for i in range(3)])
    u1_sb, u1_b = k.sb([128, 512], F32, "u1")
    u2_sb, u2_b = k.sb([128, 512], F32, "u2")
    vo_ring = Ring([k.sb([128, 640], BF16, f"vo{i}") for i in range(2)])
    if odd:
        rc2_sb, rc2_b = k.sb([96, 512], F32, "rc2_sb")
        rs2_sb, rs2_b = k.sb([96, 512], F32, "rs2_sb")
        rc3_sb, rc3_b = k.sb([32, 512], F32, "rc3_sb")
        rs3_sb, rs3_b = k.sb([32, 512], F32, "rs3_sb")
        cq_sb, cq_b = k.sb([128, 8, 512], F32, "cq_sb")
        cn_sb, cn_b = k.sb([128, 8, 512], BF16, "cn_sb")
        rq_sb, rq_b = k.sb([128, 512], F32, "rq_sb")
        rkv_sb, rkv_b = k.sb([128, 512], F32, "rkv_sb")
        nrm_sb, nrm_b = k.sb([128, 512], F32, "nrm_sb")

    def mm_fm(ps, ps_b, col0, ncols, T, rhs_sb=None, rhs_b=None, wsb=None, wb=None, nk=8):
        rhs_sb = h_sb if rhs_sb is None else rhs_sb
        rhs_b = h_b if rhs_b is None else rhs_b
        wsb = w_sb if wsb is None else wsb
        wb = w_b if wb is None else wb
        for kc in range(nk):
            S.op("pe", lambda e, kc=kc: e.matmul(ps[:ncols, :T], lhsT=wsb[:, kc, col0:col0 + ncols], rhs=rhs_sb[:, kc, :T],
                                                start=(kc == 0), stop=(kc == nk - 1)), reads=[wb, rhs_b], writes=[ps_b])

    def store_fm(src_fn, src_bufs, row0, nrows, t0, T, eng="act"):
        o_sb, o_b = oring.next()
        if eng == "act":
            S.op("act", lambda e: e.copy(out=o_sb[:nrows, :T], in_=src_fn()), reads=src_bufs, writes=[o_b])
        S.dma("sp", fm_out[row0:row0 + nrows, t0:t0 + T], o_sb[:nrows, :T], reads=[o_b])

    def rope_store(psA, psA_b, psB, psB_b, nrows, row0, t0, T, C, C_b, Sn, Sn_b, norm=None):
        o_sb, o_b = oring.next()
        S.op("dve", lambda e: e.tensor_tensor(out=u1_sb[:nrows, :T], in0=psA[:nrows, :T], in1=C[:nrows, :T], op=ALU.mult), reads=[psA_b, C_b], writes=[u1_b])
        S.op("dve", lambda e: e.tensor_tensor(out=u2_sb[:nrows, :T], in0=psB[:nrows, :T], in1=Sn[:nrows, :T], op=ALU.mult), reads=[psB_b, Sn_b], writes=[u2_b])
        if norm is None:
            S.op("pool", lambda e: e.tensor_tensor(out=o_sb[:nrows, :T], in0=u1_sb[:nrows, :T], in1=u2_sb[:nrows, :T], op=ALU.add), reads=[u1_b, u2_b], writes=[o_b])
        else:
            n_sb, n_b = norm
            S.op("pool", lambda e: e.tensor_tensor(out=u1_sb[:nrows, :T], in0=u1_sb[:nrows, :T], in1=u2_sb[:nrows, :T], op=ALU.add), reads=[u1_b, u2_b], writes=[u1_b])
            S.op("dve", lambda e: e.tensor_tensor(out=o_sb[:nrows, :T], in0=u1_sb[:nrows, :T], in1=n_sb[:nrows, :T], op=ALU.mult), reads=[u1_b, n_b], writes=[o_b])
        S.dma("sp", fm_out[row0:row0 + nrows, t0:t0 + T], o_sb[:nrows, :T], reads=[o_b])

    def rsqrt_from_ps(ps, ps_b, out_sb, out_b, T, scale):
        S.op("act", lambda e: e.activation(out=out_sb[:, :T], in_=ps[:, :T], func=AF.Sqrt, scale=scale, bias=eps_sb[:, 0:1]), reads=[ps_b, eps_b], writes=[out_b])
        S.op("dve", lambda e: e.reciprocal(out=out_sb[:, :T], in_=out_sb[:, :T]), reads=[out_b], writes=[out_b])

    for (t0, T, is_ctx) in token_tiles():
        c = 1 if is_ctx else 0
        S.dma("sp", x_sb[:, :, :T], xT[:, t0:t0 + T].rearrange("(kc p) t -> p kc t", p=128), writes=[x_b])
        S.dma("sp", rc_sb[:, :T], ropeC[:, t0:t0 + T], writes=[rc_b])
        S.dma("sp", rs_sb[:, :T], ropeS[:, t0:t0 + T], writes=[rs_b])
        emit_norm_tile(k, S, x_sb, x_b, T, onesb, ones_b, sq_sb, sq_b, ss_ps, ss_b, rstd_sb, rstd_b, eps_sb, eps_b)
        for kc in range(8):
            S.op("dve", lambda e, kc=kc: e.scalar_tensor_tensor(out=t_sb[:, :T], in0=x_sb[:, kc, :T], scalar=A_sb[:, kc, c:c + 1], in1=rstd_sb[:, :T],
                                                             op0=ALU.mult, op1=ALU.mult), reads=[x_b, A_b, rstd_b], writes=[t_b])
            S.op("act", lambda e, kc=kc: e.activation(out=h_sb[:, kc, :T], in_=t_sb[:, :T], func=AF.Identity, bias=mv_sb[:, 0, kc, c:c + 1], scale=1.0),
                 reads=[t_b, mv_b], writes=[h_b])
        if not odd:
            for j in range(5):
                col = j * 128
                swc = 2304 + j * 128
                pa, pa_b = pring.next()
                pb, pb_b = pring.next()
                mm_fm(pa, pa_b, col, 128, T)
                mm_fm(pb, pb_b, swc, 128, T)
                rope_store(pa, pa_b, pb, pb_b, 128, j * 128, t0, T, rc_sb, rc_b, rs_sb, rs_b)
            for j in range(8):
                col = 768 + j * 128
                pa, pa_b = pring.next()
                mm_fm(pa, pa_b, col, 128, T)
                store_fm(lambda pa=pa: pa[:, :T], [pa_b], 640 + j * 128, 128, t0, T)
            tmcols = [(640, 128, 0), (1792, 512, 128)]
        else:
            S.dma("sp", rc2_sb[:, :T], ropeC2[:, t0:t0 + T], writes=[rc2_b])
            S.dma("sp", rs2_sb[:, :T], ropeS2[:, t0:t0 + T], writes=[rs2_b])
            S.dma("sp", rc3_sb[:, :T], ropeC3[:, t0:t0 + T], writes=[rc3_b])
            S.dma("sp", rs3_sb[:, :T], ropeS3[:, t0:t0 + T], writes=[rs3_b])
            for j in range(8):
                pa, pa_b = pring.next()
                mm_fm(pa, pa_b, j * 128, 128, T)
                S.op("act", lambda e, j=j, pa=pa: e.copy(out=cq_sb[:, j, :T], in_=pa[:, :T]), reads=[pa_b], writes=[cq_b])
            S.op("act", lambda e: e.activation(out=sq_sb[:, :, :T], in_=cq_sb[:, :, :T], func=AF.Square), reads=[cq_b], writes=[sq_b])
            pq, pq_b = pring.next()
            for j in range(6):
                S.op("pe", lambda e, j=j: e.matmul(pq[:, :T], lhsT=onesb[:, :], rhs=sq_sb[:, j, :T], start=(j == 0), stop=(j == 5)), reads=[sq_b, ones_b], writes=[pq_b])
            rsqrt_from_ps(pq, pq_b, rq_sb, rq_b, T, 1.0 / 768)
            pk, pk_b = pring.next()
            for j in range(2):
                S.op("pe", lambda e, j=j: e.matmul(pk[:, :T], lhsT=onesb[:, :], rhs=sq_sb[:, 6 + j, :T], start=(j == 0), stop=(j == 1)), reads=[sq_b, ones_b], writes=[pk_b])
            rsqrt_from_ps(pk, pk_b, rkv_sb, rkv_b, T, 1.0 / 256)
            for j in range(8):
                r_sb, r_b = (rq_sb, rq_b) if j < 6 else (rkv_sb, rkv_b)
                S.op("dve", lambda e, j=j, r_sb=r_sb: e.scalar_tensor_tensor(out=cn_sb[:, j, :T], in0=cq_sb[:, j, :T], scalar=lg_sb[:, j:j + 1], in1=r_sb[:, :T], op0=ALU.mult, op1=ALU.mult),
                     reads=[cq_b, r_b, lg_b], writes=[cn_b])
            for hd in range(8):
                pa, pa_b = pring.next()
                pb, pb_b = pring.next()
                mm_fm(pa, pa_b, hd * 96, 96, T, cn_sb, cn_b, wqb_sb, wqb_b, nk=6)
                mm_fm(pb, pb_b, 768 + hd * 96, 96, T, cn_sb, cn_b, wqb_sb, wqb_b, nk=6)
                rope_store(pa, pa_b, pb, pb_b, 96, hd * 96, t0, T, rc2_sb, rc2_b, rs2_sb, rs2_b)
            for j in range(4):
                pa, pa_b = pring.next()
                for kc in range(2):
                    S.op("pe", lambda e, kc=kc, j=j, pa=pa: e.matmul(pa[:, :T], lhsT=wkvb_sb[:, kc, j * 128:(j + 1) * 128], rhs=cn_sb[:, 6 + kc, :T],
                                                                      start=(kc == 0), stop=(kc == 1)), reads=[wkvb_b, cn_b], writes=[pa_b])
                store_fm(lambda pa=pa: pa[:, :T], [pa_b], 768 + j * 128, 128, t0, T)
            pa, pa_b = pring.next()
            pb, pb_b = pring.next()
            mm_fm(pa, pa_b, 1024, 32, T)
            mm_fm(pb, pb_b, 1824, 32, T)
            rope_store(pa, pa_b, pb, pb_b, 32, 768 + 512, t0, T, rc3_sb, rc3_b, rs3_sb, rs3_b)
            for j in range(5):
                col = 1056 + j * 128
                swc = 1856 + j * 128
                pa, pa_b = pring.next()
                pb, pb_b = pring.next()
                mm_fm(pa, pa_b, col, 128, T)
                mm_fm(pb, pb_b, swc, 128, T)
                S.op("act", lambda e, pa=pa: e.activation(out=sq_sb[:, 0, :T], in_=pa[:, :T], func=AF.Square), reads=[pa_b], writes=[sq_b])
                pn, pn_b = pring.next()
                S.op("pe", lambda e, pn=pn: e.matmul(pn[:, :T], lhsT=blk_sb[:, :], rhs=sq_sb[:, 0, :T], start=True, stop=True), reads=[sq_b, blk_b], writes=[pn_b])
                rsqrt_from_ps(pn, pn_b, nrm_sb, nrm_b, T, 1.0 / 64)
                gc = 0 if j < 4 else 2
                o_sb, o_b = oring.next()
                S.op("dve", lambda e, pa=pa, gc=gc: e.scalar_tensor_tensor(out=u1_sb[:, :T], in0=pa[:, :T], scalar=dg_sb[:, gc:gc + 1], in1=rc_sb[:, :T], op0=ALU.mult, op1=ALU.mult),
                     reads=[pa_b, dg_b, rc_b], writes=[u1_b])
                S.op("dve", lambda e, pb=pb, gc=gc: e.scalar_tensor_tensor(out=u2_sb[:, :T], in0=pb[:, :T], scalar=dg_sb[:, gc + 1:gc + 2], in1=rs_sb[:, :T], op0=ALU.mult, op1=ALU.mult),
                     reads=[pb_b, dg_b, rs_b], writes=[u2_b])
                S.op("pool", lambda e: e.tensor_tensor(out=u1_sb[:, :T], in0=u1_sb[:, :T], in1=u2_sb[:, :T], op=ALU.add), reads=[u1_b, u2_b], writes=[u1_b])
                S.op("dve", lambda e, o_sb=o_sb: e.tensor_tensor(out=o_sb[:, :T], in0=u1_sb[:, :T], in1=nrm_sb[:, :T], op=ALU.mult), reads=[u1_b, nrm_b], writes=[o_b])
                S.dma("sp", fm_out[1312 + j * 128:1312 + (j + 1) * 128, t0:t0 + T], o_sb[:, :T], reads=[o_b])
            tmcols = [(1696, 128, 512)]
        for s in range(T // 128):
            vo_sb, vo_b = vo_ring.next()
            for (wc, n, oc) in tmcols:
                pa, pa_b = pring.next()
                for kc in range(8):
                    S.op("pe", lambda e, kc=kc, pa=pa, wc=wc, n=n, s=s: e.matmul(pa[:, :n], lhsT=h_sb[:, kc, s * 128:(s + 1) * 128], rhs=w_sb[:, kc, wc:wc + n],
                                                                               start=(kc == 0), stop=(kc == 7)), reads=[h_b, w_b], writes=[pa_b])
                S.op("act", lambda e, pa=pa, n=n, oc=oc, vo_sb=vo_sb: e.copy(out=vo_sb[:, oc:oc + n], in_=pa[:, :n]), reads=[pa_b], writes=[vo_b])
            if odd:
                pa, pa_b = pring.next()
                for kc in range(2):
                    S.op("pe", lambda e, kc=kc, pa=pa, s=s: e.matmul(pa[:, :512], lhsT=cn_sb[:, 6 + kc, s * 128:(s + 1) * 128], rhs=wkvb_sb[:, kc, 512:1024],
                                                                      start=(kc == 0), stop=(kc == 1)), reads=[cn_b, wkvb_b], writes=[pa_b])
                S.op("act", lambda e, pa=pa, vo_sb=vo_sb: e.copy(out=vo_sb[:, 0:512], in_=pa[:, :512]), reads=[pa_b], writes=[vo_b])
            S.dma("sp", tm_out[t0 + s * 128:t0 + (s + 1) * 128, :], vo_sb[:, :], reads=[vo_b], final=True)
    if own:
        return k.finish()
    k.end_phase()


def _maskA_np():
    m = np.zeros((128, 6, 512), np.float32)
    kl = np.arange(128)[:, None]
    ql = np.arange(128)[None, :]
    for kbrel in range(6):
        for qb in range(4):
            rel = (kbrel - 1) - qb
            if rel == -1:
                m[:, kbrel, qb * 128:(qb + 1) * 128] = (kl >= ql)
            elif rel == 0:
                m[:, kbrel, qb * 128:(qb + 1) * 128] = 1.0
            elif rel == 1:
                m[:, kbrel, qb * 128:(qb + 1) * 128] = (kl <= ql)
    return m


def _nbr_index():
    rows = SEQ // GRID_W
    out = []
    for v, tile in enumerate((0, 1, rows // 8 - 1)):
        r0 = tile * 8
        kp = np.arange(128)
        kr2, kc = kp // 64, kp % 64
        q = np.arange(512)
        qr, c = r0 + q // 64, q % 64
        rs = np.clip(qr - 4, 0, rows - 8)
        cs = np.clip(c - 8, 0, GRID_W - 16)
        valid = np.zeros((128, 8, 512), bool)
        dr = np.zeros((128, 8, 512), np.int64)
        dc = np.zeros((128, 8, 512), np.int64)
        for kbrel in range(8):
            krow = r0 - 4 + 2 * kbrel + kr2
            okr = (krow[:, None] >= rs[None, :]) & (krow[:, None] < rs[None, :] + 8) & (krow[:, None] >= 0) & (krow[:, None] < rows)
            okc = (kc[:, None] >= cs[None, :]) & (kc[:, None] < cs[None, :] + 16)
            valid[:, kbrel, :] = okr & okc
            dr[:, kbrel, :] = krow[:, None] - qr[None, :] + 7
            dc[:, kbrel, :] = kc[:, None] - c[None, :] + 15
        out.append((valid, np.clip(dr, 0, 14), np.clip(dc, 0, 30)))
    return out


_NBR = None


def nbr_index():
    global _NBR
    if _NBR is None:
        _NBR = _nbr_index()
    return _NBR


def build_p2(odd):
    k = K("p2o" if odd else "p2e")
    S = k.S
    dk2 = 96 if odd else 64
    q1T = k.dram("q1T", [2, 64, NTOK], BF16, "ExternalInput")
    k1T = k.dram("k1T", [64, NTOK], BF16, "ExternalInput")
    v1 = k.dram("v1", [NTOK, 64], BF16, "ExternalInput")
    q2T = k.dram("q2T", [2, dk2, NTOK], BF16, "ExternalInput")
    k2T = k.dram("k2T", [2, dk2, NTOK], BF16, "ExternalInput")
    v2 = k.dram("v2", [NTOK, 128], BF16, "ExternalInput")
    ident_d = k.dram("ident", [128, 128], F32, "ExternalInput")
    yT = k.dram("yT", [2, 128, NTOK], BF16, "ExternalOutput")
    identb, identb_b, onesb, ones_b, eps_sb, eps_b = load_consts(k, S, ident_d)
    if not odd:
        maskA_d = k.dram("maskA", [128, 6, 512], F32, "ExternalInput")
        sink_d = k.dram("sink", [128, 2], F32, "ExternalInput")
        rpbx_d = k.dram("rpbx", [2, 3, 128, 8, 512], F32, "ExternalInput")
        maskA, maskA_b = k.sb([128, 6, 512], BF16, "maskA_sb")
        S.dma("pool", maskA[:], maskA_d[:, :, :], writes=[maskA_b])
        esink, esink_b = k.sb([128, 2], F32, "esink_sb")
        S.dma("sp", esink[:], sink_d[:, :], writes=[esink_b])
        S.op("act", lambda e: e.activation(out=esink[:], in_=esink[:], func=AF.Exp), reads=[esink_b], writes=[esink_b])
        MB = {}
        stg, stg_b = k.sb([128, 512], F32, "stg")
        for h in range(2):
            for v in range(3):
                m_sb, m_b = k.sb([128, 8, 512], BF16, f"MB{h}{v}")
                for kb in range(8):
                    S.dma("sp", stg[:], rpbx_d[h, v, :, kb, :], writes=[stg_b])
                    S.op("act", lambda e: e.activation(out=m_sb[:, kb, :], in_=stg[:], func=AF.Exp), reads=[stg_b], writes=[m_b])
                MB[(h, v)] = (m_sb, m_b)
        nbr = nbr_index()
        needB = [[[bool(nbr[v][0][:, kb, qb * 128:(qb + 1) * 128].any()) for qb in range(4)] for kb in range(8)] for v in range(3)]
        mA = _maskA_np()
        needA = [[bool(mA[:, kb, qb * 128:(qb + 1) * 128].any()) for qb in range(4)] for kb in range(6)]

    qT_sb, qT_b = k.sb([dk2, NTOK], BF16, "qT_sb")
    kT_sb, kT_b = k.sb([dk2, NTOK], BF16, "kT_sb")
    va_sb, va_b = k.sb([128, NKB, 65], BF16, "va_sb")
    S.op("dve", lambda e: e.memset(va_sb[:, :, 64:65], 1.0), writes=[va_b])
    sring = Ring([k.ps([128, 512], F32, f"s{i}") for i in range(2)])
    O = [k.ps([128, 512], F32, f"o{i}") for i in range(4)]
    pt_ps, pt_b = k.ps([128, 512], BF16, "ptp")
    pring = Ring([k.sb([128, 512], BF16, f"pT{i}") for i in range(3)])
    den_sb, den_b = k.sb([128, 4], F32, "den")
    y_sb, y_b = k.sb([128, 4, 64], BF16, "y_sb")
    yTring = Ring([k.sb([64, 512], BF16, f"yT{i}") for i in range(2)])

    jobs = [(0, 0), (0, 1), (1, 0), (1, 1)]
    for (mx, h) in jobs:
        dk = 64 if mx == 0 else dk2
        scale = float(dk) ** -0.5
        qsrc = q1T[h] if mx == 0 else q2T[h]
        ksrc = k1T if mx == 0 else k2T[h]
        for c0 in range(0, NTOK, 4160):
            S.dma("sp", qT_sb[:dk, c0:c0 + 4160], qsrc[:, c0:c0 + 4160], writes=[qT_b])
            S.dma("sp", kT_sb[:dk, c0:c0 + 4160], ksrc[:, c0:c0 + 4160], writes=[kT_b])
        if mx == 0:
            if h == 0:
                S.dma("sp", va_sb[:, :, 0:64], v1.rearrange("(kb p) d -> p kb d", p=128), writes=[va_b])
        else:
            S.dma("sp", va_sb[:, :, 0:64], v2[:, h * 64:(h + 1) * 64].rearrange("(kb p) d -> p kb d", p=128), writes=[va_b])
        tiles = [(0, 256, None)] + [(CTX + 512 * i, 512, i) for i in range(SEQ // 512)]
        for (q0, nq, ti) in tiles:
            nqb = nq // 128
            kbl = []
            if ti is None:
                kbl = [(0, None, [True] * nqb), (1, None, [True] * nqb)]
            elif odd:
                kbl = [(kb, None, [True] * 4) for kb in range(NKB)]
            elif mx == 0:
                for kbrel in range(6):
                    lb = 4 * ti + kbrel - 1
                    if 0 <= lb < SEQ // 128:
                        kbl.append((2 + lb, (maskA, maskA_b, kbrel), needA[kbrel]))
                kbl += [(0, None, [True] * 4), (1, None, [True] * 4)]
            else:
                v = 0 if ti == 0 else (2 if ti == SEQ // 512 - 1 else 1)
                for kbrel in range(8):
                    lb = 4 * ti - 2 + kbrel
                    if 0 <= lb < SEQ // 128 and any(needB[v][kbrel]):
                        m_sb, m_b = MB[(h, v)]
                        kbl.append((2 + lb, (m_sb, m_b, kbrel), needB[v][kbrel]))
                kbl += [(0, None, [True] * 4), (1, None, [True] * 4)]
            first = [min(i for i, (_, _, nd) in enumerate(kbl) if nd[qb]) for qb in range(nqb)]
            last = [max(i for i, (_, _, nd) in enumerate(kbl) if nd[qb]) for qb in range(nqb)]
            for i, (kb, msk, nd) in enumerate(kbl):
                s_ps, s_b = sring.next()
                S.op("pe", lambda e: e.matmul(s_ps[:, :nq], lhsT=kT_sb[:dk, kb * 128:(kb + 1) * 128], rhs=qT_sb[:dk, q0:q0 + nq], start=True, stop=True),
                     reads=[kT_b, qT_b], writes=[s_b])
                p_sb, p_b = pring.next()
                S.op("act", lambda e: e.activation(out=p_sb[:, :nq], in_=s_ps[:, :nq], func=AF.Exp, scale=scale), reads=[s_b], writes=[p_b])
                if msk is not None:
                    m_sb, m_b, kbrel = msk
                    S.op("dve" if i % 2 == 0 else "pool", lambda e: e.tensor_tensor(out=p_sb[:, :nq], in0=p_sb[:, :nq], in1=m_sb[:, kbrel, :nq], op=ALU.mult),
                         reads=[p_b, m_b], writes=[p_b])
                for qb in range(nqb):
                    if nd[qb]:
                        o_ps, o_b = O[qb]
                        S.op("pe", lambda e: e.matmul(o_ps[:, 0:65], lhsT=p_sb[:, qb * 128:(qb + 1) * 128], rhs=va_sb[:, kb, :],
                                                      start=(i == first[qb]), stop=(i == last[qb])), reads=[p_b, va_b], writes=[o_b])
            yt_sb, yt_b = yTring.next()
            for qb in range(nqb):
                o_ps, o_b = O[qb]
                if (not odd) and mx == 0:
                    S.op("dve", lambda e: e.tensor_tensor(out=den_sb[:, qb:qb + 1], in0=o_ps[:, 64:65], in1=esink[:, h:h + 1], op=ALU.add), reads=[o_b, esink_b], writes=[den_b])
                    S.op("dve", lambda e: e.reciprocal(out=den_sb[:, qb:qb + 1], in_=den_sb[:, qb:qb + 1]), reads=[den_b], writes=[den_b])
                else:
                    S.op("dve", lambda e: e.reciprocal(out=den_sb[:, qb:qb + 1], in_=o_ps[:, 64:65]), reads=[o_b], writes=[den_b])
                S.op("dve", lambda e: e.tensor_scalar(out=y_sb[:, qb, :], in0=o_ps[:, 0:64], scalar1=den_sb[:, qb:qb + 1], scalar2=None, op0=ALU.mult),
                     reads=[o_b, den_b], writes=[y_b])
                S.op("pe", lambda e: e.transpose(out=pt_ps[:64, qb * 128:(qb + 1) * 128], in_=y_sb[:, qb, :], identity=identb[:, :]), reads=[y_b, identb_b], writes=[pt_b])
            S.op("dve", lambda e: e.tensor_copy(out=yt_sb[:, :nq], in_=pt_ps[:64, :nq]), reads=[pt_b], writes=[yt_b])
            S.dma("sp", yT[mx, h * 64:(h + 1) * 64, q0:q0 + nq], yt_sb[:, :nq], reads=[yt_b], final=True)
    return k.finish()


PASSES = [[0, 1, 2], [3, 4, 5], [6, 7, 8]]


def build_p3(k=None, io=None, do_final=True):
    own = k is None
    if own:
        k = K("p3")
    else:
        k.begin_phase(io)
    S = k.S
    nc = k.nc
    xT = k.dram("xT", [DM, NT], F32, "ExternalInput")
    yT = k.dram("yT", [8, 128, NT], BF16, "ExternalInput")
    cond = k.dram("cond", [128, 8, 2], F32, "ExternalInput")
    modw = k.dram("modw", [DM, 4096], F32, "ExternalInput")
    modb = k.dram("modb", [128, 32], F32, "ExternalInput")
    gain = k.dram("gain", [128, 8], F32, "ExternalInput")
    fgain = k.dram("fgain", [128, 8], F32, "ExternalInput")
    wout = k.dram("wout", [DM, DM], F32, "ExternalInput")
    rw = k.dram("rw", [DM, NEXP], F32, "ExternalInput")
    rb = k.dram("rb", [1, NEXP], F32, "ExternalInput")
    ewin = k.dram("ewin", [NEXP, DM, 2048], F32, "ExternalInput")
    ebin = k.dram("ebin", [128, NEXP, 16], F32, "ExternalInput")
    ewout = k.dram("ewout", [NEXP, DM, DM], F32, "ExternalInput")
    ebout = k.dram("ebout", [NEXP, DM], F32, "ExternalInput")
    ident_d = k.dram("ident", [128, 128], F32, "ExternalInput")
    x2T = k.dram("x2T", [DM, NT], F32, "ExternalOutput")
    xfT = k.dram("xfT", [DM, NT], F32, "ExternalOutput") if do_final else None
    x1T = k.dram("x1T", [DM, NT], F32, "Internal")
    h2T = k.dram("h2T", [DM, NT], BF16, "Internal")
    gT_h = nc.dram_tensor(f"gTd_ph{k.phase}", [NEXP, NT], F32, kind="Internal")
    gT = gT_h.ap()
    x1T_b, h2T_b, gT_b = Buf("x1T"), Buf("h2T"), Buf("gT")

    identb, identb_b, onesb, ones_b, eps_sb, eps_b = load_consts(k, S, ident_d)
    identf, identf_b = k.sb([128, 128], F32, "identf")
    S.dma("sp", identf[:], ident_d[:, :], writes=[identf_b])
    onesf, onesf_b = k.sb([1, 128], F32, "onesf")
    S.op("dve", lambda e: e.memset(onesf[:], 1.0), writes=[onesf_b])
    cond_sb, cond_b = k.sb([128, 8, 2], F32, "cond_sb")
    S.dma("sp", cond_sb[:], cond[:, :, :], writes=[cond_b])
    S.op("act", lambda e: e.activation(out=cond_sb[:], in_=cond_sb[:], func=AF.Silu), reads=[cond_b], writes=[cond_b])
    modb_sb, modb_b = k.sb([128, 32], F32, "modb_sb")
    S.dma("sp", modb_sb[:], modb[:, :], writes=[modb_b])
    gain_sb, gain_b = k.sb([128, 8], F32, "gain_sb")
    S.dma("sp", gain_sb[:], gain[:, :], writes=[gain_b])
    fg_sb, fg_b = k.sb([128, 8], F32, "fg_sb")
    S.dma("sp", fg_sb[:], fgain[:, :], writes=[fg_b])
    mv_sb, mv_b = k.sb([128, 4, 8, 2], F32, "mv_sb")
    A_sb, A_b = k.sb([128, 8, 2], F32, "A_sb")
    ebin_sb, ebin_b = k.sb([128, NEXP, 16], F32, "ebin_sb")
    S.dma("sp", ebin_sb[:], ebin[:, :, :], writes=[ebin_b])
    ebout_sb, ebout_b = k.sb([NEXP, DM], F32, "ebout_sb")
    S.dma("sp", ebout_sb[:], ebout[:, :], writes=[ebout_b])
    rstd_sb, rstd_b = k.sb([128, 512], F32, "rstd_sb")
    sq_sb, sq_b = k.sb([128, 8, 512], BF16, "sq_sb")
    x_sb, x_b = k.sb([128, 8, 512], F32, "x_sb")
    t_sb, t_b = k.sb([128, 512], F32, "t_sb")
    ss_ps, ss_b = k.ps([128, 512], F32, "ss_ps")
    pring = Ring([k.ps([128, 512], F32, f"pp{i}") for i in range(6)])
    tiles = token_tiles()

    stA = contextlib.ExitStack()
    main_stack = k.stack
    k.stack = stA
    wm, wm_b = k.sb([128, 8, 512], F32, "modw_sb")
    emit_mod_vectors(k, S, modw, modb_sb, modb_b, cond_sb, cond_b, 4, mv_sb, mv_b, Ring([(wm, wm_b)]))
    for c in range(2):
        S.op("dve", lambda e: e.scalar_tensor_tensor(out=A_sb[:, :, c], in0=mv_sb[:, 2, :, c], scalar=1.0, in1=gain_sb[:, :], op0=ALU.add, op1=ALU.mult),
             reads=[mv_b, gain_b], writes=[A_b])
    wo_sb, wo_b = k.sb([128, 8, DM], BF16, "wo_sb")
    for kc in range(8):
        S.dma("pool", wo_sb[:, kc, :], wout[kc * 128:(kc + 1) * 128, :], writes=[wo_b])
    rw_sb, rw_b = k.sb([128, 8, NEXP], F32, "rw_sb")
    S.dma("sp", rw_sb[:], rw.rearrange("(kc p) e -> p kc e", p=128), writes=[rw_b])
    rb_sb, rb_b = k.sb([1, NEXP], F32, "rb_sb")
    S.dma("sp", rb_sb[:], rb[:, :], writes=[rb_b])
    y_sb, y_b = k.sb([128, 8, 512], BF16, "y_sb")
    hf_sb, hf_b = k.sb([128, 8, 512], F32, "hf_sb")
    hb_sb, hb_b = k.sb([128, 8, 512], BF16, "hb_sb")
    lg_sb, lg_b = k.sb([128, NEXP], F32, "lg_sb")
    m8_sb, m8_b = k.sb([128, 8], F32, "m8_sb")
    mk_sb, mk_b = k.sb([128, NEXP], F32, "mk_sb")
    ex_sb, ex_b = k.sb([128, NEXP], F32, "ex_sb")
    sm_sb, sm_b = k.sb([128, 2], F32, "sm_sb")
    gt_sb, gt_b = k.sb([NEXP, 512], F32, "gt_sb")
    for (t0, T, is_ctx) in tiles:
        c = 1 if is_ctx else 0
        S.dma("sp", x_sb[:, :, :T], xT[:, t0:t0 + T].rearrange("(kc p) t -> p kc t", p=128), writes=[x_b])
        S.dma("sp", y_sb[:, :, :T], yT[:, :, t0:t0 + T].rearrange("kc p t -> p kc t"), writes=[y_b])
        for o in range(8):
            pa, pa_b = pring.next()
            for kc in range(8):
                S.op("pe", lambda e: e.matmul(pa[:, :T], lhsT=wo_sb[:, kc, o * 128:(o + 1) * 128], rhs=y_sb[:, kc, :T], start=(kc == 0), stop=(kc == 7)),
                     reads=[wo_b, y_b], writes=[pa_b])
            S.op("dve", lambda e: e.scalar_tensor_tensor(out=x_sb[:, o, :T], in0=pa[:, :T], scalar=mv_sb[:, 0, o, c:c + 1], in1=x_sb[:, o, :T], op0=ALU.mult, op1=ALU.add),
                 reads=[pa_b, mv_b, x_b], writes=[x_b])
        S.dma("sp", x1T[:, t0:t0 + T].rearrange("(kc p) t -> p kc t", p=128), x_sb[:, :, :T], reads=[x_b], writes=[x1T_b])
        emit_norm_tile(k, S, x_sb, x_b, T, onesb, ones_b, sq_sb, sq_b, ss_ps, ss_b, rstd_sb, rstd_b, eps_sb, eps_b)
        for kc in range(8):
            S.op("dve", lambda e: e.scalar_tensor_tensor(out=t_sb[:, :T], in0=x_sb[:, kc, :T], scalar=A_sb[:, kc, c:c + 1], in1=rstd_sb[:, :T], op0=ALU.mult, op1=ALU.mult),
                 reads=[x_b, A_b, rstd_b], writes=[t_b])
            S.op("act", lambda e: e.activation(out=hf_sb[:, kc, :T], in_=t_sb[:, :T], func=AF.Identity, bias=mv_sb[:, 1, kc, c:c + 1], scale=1.0),
                 reads=[t_b, mv_b], writes=[hf_b])
            S.op("pool", lambda e: e.tensor_copy(out=hb_sb[:, kc, :T], in_=hf_sb[:, kc, :T]), reads=[hf_b], writes=[hb_b])
        S.dma("sp", h2T[:, t0:t0 + T].rearrange("(kc p) t -> p kc t", p=128), hb_sb[:, :, :T], reads=[hb_b], writes=[h2T_b])
        for s in range(T // 128):
            pr, pr_b = pring.next()
            for kc in range(8):
                S.op("pe", lambda e: e.matmul(pr[:, :NEXP], lhsT=hf_sb[:, kc, s * 128:(s + 1) * 128], rhs=rw_sb[:, kc, :], start=(kc == 0), stop=False),
                     reads=[hf_b, rw_b], writes=[pr_b])
            S.op("pe", lambda e: e.matmul(pr[:, :NEXP], lhsT=onesf[0:1, :], rhs=rb_sb[0:1, :], start=False, stop=True), reads=[onesf_b, rb_b], writes=[pr_b])
            S.op("dve", lambda e: e.tensor_copy(out=lg_sb[:, :], in_=pr[:, :NEXP]), reads=[pr_b], writes=[lg_b])
            S.op("dve", lambda e: e.max(out=m8_sb[:, :], in_=lg_sb[:, :]), reads=[lg_b], writes=[m8_b])
            S.op("dve", lambda e: e.tensor_scalar(out=mk_sb[:, :], in0=lg_sb[:, :], scalar1=m8_sb[:, 3:4], scalar2=None, op0=ALU.is_ge), reads=[lg_b, m8_b], writes=[mk_b])
            S.op("dve", lambda e: e.tensor_scalar(out=sm_sb[:, 0:1], in0=m8_sb[:, 0:1], scalar1=-1.0, scalar2=None, op0=ALU.mult), reads=[m8_b], writes=[sm_b])
            S.op("act", lambda e: e.activation(out=ex_sb[:, :], in_=lg_sb[:, :], func=AF.Exp, bias=sm_sb[:, 0:1], scale=1.0), reads=[lg_b, sm_b], writes=[ex_b])
            S.op("dve", lambda e: e.tensor_tensor(out=ex_sb[:, :], in0=ex_sb[:, :], in1=mk_sb[:, :], op=ALU.mult), reads=[ex_b, mk_b], writes=[ex_b])
            S.op("dve", lambda e: e.reduce_sum(out=sm_sb[:, 1:2], in_=ex_sb[:, :], axis=AX.X), reads=[ex_b], writes=[sm_b])
            S.op("dve", lambda e: e.reciprocal(out=sm_sb[:, 1:2], in_=sm_sb[:, 1:2]), reads=[sm_b], writes=[sm_b])
            S.op("dve", lambda e: e.tensor_scalar(out=ex_sb[:, :], in0=ex_sb[:, :], scalar1=sm_sb[:, 1:2], scalar2=None, op0=ALU.mult), reads=[ex_b, sm_b], writes=[ex_b])
            pt, pt_b = pring.next()
            S.op("pe", lambda e: e.transpose(out=pt[:NEXP, :128], in_=ex_sb[:, :], identity=identf[:, :]), reads=[ex_b, identf_b], writes=[pt_b])
            S.op("act", lambda e: e.copy(out=gt_sb[:, s * 128:(s + 1) * 128], in_=pt[:NEXP, :128]), reads=[pt_b], writes=[gt_b])
        S.dma("sp", gT[:, t0:t0 + T], gt_sb[:, :T], reads=[gt_b], writes=[gT_b])
    k.stack = main_stack
    S.barrier()
    stA.close()

    for tl in PASSES:
        c0 = tiles[tl[0]][0]
        Np = sum(tiles[i][1] for i in tl)
        stB = contextlib.ExitStack()
        k.stack = stB
        hp_sb, hp_b = k.sb([128, 8, Np], BF16, "hp_sb")
        acc_sb, acc_b = k.sb([128, 8, Np], F32, "acc_sb")
        S.dma("sp", hp_sb[:], h2T[:, c0:c0 + Np].rearrange("(kc p) t -> p kc t", p=128), reads=[h2T_b], writes=[hp_b])
        gq_sb, gq_b = k.sb([NEXP, 512], F32, "gq_sb")
        for i in tl:
            t0, T, _ = tiles[i]
            S.dma("sp", gq_sb[:, :T], gT[:, t0:t0 + T], reads=[gT_b], writes=[gq_b])
            for o in range(8):
                pa, pa_b = pring.next()
                S.op("pe", lambda e: e.matmul(pa[:, :T], lhsT=ebout_sb[:, o * 128:(o + 1) * 128], rhs=gq_sb[:, :T], start=True, stop=True), reads=[ebout_b, gq_b], writes=[pa_b])
                S.op("act", lambda e: e.copy(out=acc_sb[:, o, t0 - c0:t0 - c0 + T], in_=pa[:, :T]), reads=[pa_b], writes=[acc_b])
        stE = contextlib.ExitStack()
        k.stack = stE
        wi_ring = Ring([k.sb([128, 8, 1024], BF16, f"wi{i}") for i in range(2)])
        wo_ring = Ring([k.sb([128, 4, 1024], BF16, f"wo{i}") for i in range(2)])
        act_ring = Ring([k.sb([128, 4, 512], BF16, f"ac{i}") for i in range(2)])
        g1r = Ring([k.sb([128, 512], F32, f"g1{i}") for i in range(2)])
        sgr = Ring([k.sb([128, 512], F32, f"sg{i}") for i in range(2)])
        l1r = Ring([k.sb([128, 512], F32, f"l1{i}") for i in range(2)])
        gbr = Ring([k.sb([128, 512], F32, f"gb{i}") for i in range(2)])

        def load_w(he):
            e_, hf = he // 2, he % 2
            wi, wi_b = wi_ring.next()
            wo, wo_b = wo_ring.next()
            for kc in range(8):
                S.dma("pool", wi[:, kc, 0:512], ewin[e_, kc * 128:(kc + 1) * 128, hf * 512:(hf + 1) * 512], writes=[wi_b])
                S.dma("pool", wi[:, kc, 512:1024], ewin[e_, kc * 128:(kc + 1) * 128, 1024 + hf * 512:1024 + (hf + 1) * 512], writes=[wi_b])
            for kc in range(4):
                r0 = hf * 512 + kc * 128
                S.dma("pool", wo[:, kc, :], ewout[e_, r0:r0 + 128, :], writes=[wo_b])
            return wi, wi_b, wo, wo_b

        nxt = load_w(0)
        for he in range(2 * NEXP):
            e_, hf = he // 2, he % 2
            wi, wi_b, wo, wo_b = nxt
            if he + 1 < 2 * NEXP:
                nxt = load_w(he + 1)
            for i in tl:
                t0, T, _ = tiles[i]
                lo = t0 - c0
                gb, gb_b = gbr.next()
                S.dma("sp", gb[:, :T], bass.AP(gT_h, e_ * NT + t0, [[0, 128], [1, T]]), reads=[gT_b], writes=[gb_b])
                ac, ac_b = act_ring.next()
                for dc in range(4):
                    pg, pg_b = pring.next()
                    pl, pl_b = pring.next()
                    for kc in range(8):
                        S.op("pe", lambda e: e.matmul(pg[:, :T], lhsT=wi[:, kc, dc * 128:(dc + 1) * 128], rhs=hp_sb[:, kc, lo:lo + T], start=(kc == 0), stop=(kc == 7)),
                             reads=[wi_b, hp_b], writes=[pg_b])
                    for kc in range(8):
                        S.op("pe", lambda e: e.matmul(pl[:, :T], lhsT=wi[:, kc, 512 + dc * 128:512 + (dc + 1) * 128], rhs=hp_sb[:, kc, lo:lo + T], start=(kc == 0), stop=(kc == 7)),
                             reads=[wi_b, hp_b], writes=[pl_b])
                    gi = hf * 4 + dc
                    li = 8 + hf * 4 + dc
                    g1, g1_b = g1r.next()
                    sg, sg_b = sgr.next()
                    l1, l1_b = l1r.next()
                    S.op("dve", lambda e: e.tensor_scalar(out=g1[:, :T], in0=pg[:, :T], scalar1=ebin_sb[:, e_, gi:gi + 1], scalar2=7.0, op0=ALU.add, op1=ALU.min), reads=[pg_b, ebin_b], writes=[g1_b])
                    S.op("act", lambda e: e.activation(out=sg[:, :T], in_=g1[:, :T], func=AF.Sigmoid, scale=1.702), reads=[g1_b], writes=[sg_b])
                    S.op("dve", lambda e: e.tensor_scalar(out=l1[:, :T], in0=pl[:, :T], scalar1=ebin_sb[:, e_, li:li + 1], scalar2=7.0, op0=ALU.add, op1=ALU.min), reads=[pl_b, ebin_b], writes=[l1_b])
                    S.op("pool", lambda e: e.tensor_scalar(out=l1[:, :T], in0=l1[:, :T], scalar1=-7.0, scalar2=1.0, op0=ALU.max, op1=ALU.add), reads=[l1_b], writes=[l1_b])
                    S.op("pool", lambda e: e.tensor_tensor(out=g1[:, :T], in0=g1[:, :T], in1=sg[:, :T], op=ALU.mult), reads=[g1_b, sg_b], writes=[g1_b])
                    S.op("dve", lambda e: e.tensor_tensor(out=g1[:, :T], in0=g1[:, :T], in1=l1[:, :T], op=ALU.mult), reads=[g1_b, l1_b], writes=[g1_b])
                    S.op("dve", lambda e: e.tensor_tensor(out=ac[:, dc, :T], in0=g1[:, :T], in1=gb[:, :T], op=ALU.mult), reads=[g1_b, gb_b], writes=[ac_b])
                for o in range(8):
                    py, py_b = pring.next()
                    for kc in range(4):
                        S.op("pe", lambda e: e.matmul(py[:, :T], lhsT=wo[:, kc, o * 128:(o + 1) * 128], rhs=ac[:, kc, :T], start=(kc == 0), stop=(kc == 3)),
                             reads=[wo_b, ac_b], writes=[py_b])
                    S.op("dve", lambda e: e.tensor_tensor(out=acc_sb[:, o, lo:lo + T], in0=py[:, :T], in1=acc_sb[:, o, lo:lo + T], op=ALU.add), reads=[py_b, acc_b], writes=[acc_b])
        k.stack = stB
        S.barrier()
        stE.close()
        ob_ring = Ring([k.sb([128, 8, 512], F32, f"ob{i}") for i in range(2)])
        for i in tl:
            t0, T, is_ctx = tiles[i]
            c = 1 if is_ctx else 0
            lo = t0 - c0
            S.dma("sp", x_sb[:, :, :T], x1T[:, t0:t0 + T].rearrange("(kc p) t -> p kc t", p=128), reads=[x1T_b], writes=[x_b])
            for o in range(8):
                S.op("dve", lambda e: e.scalar_tensor_tensor(out=x_sb[:, o, :T], in0=acc_sb[:, o, lo:lo + T], scalar=mv_sb[:, 3, o, c:c + 1], in1=x_sb[:, o, :T], op0=ALU.mult, op1=ALU.add),
                     reads=[acc_b, mv_b, x_b], writes=[x_b])
            S.dma("sp", x2T[:, t0:t0 + T].rearrange("(kc p) t -> p kc t", p=128), x_sb[:, :, :T], reads=[x_b], final=True)
            if not do_final:
                continue
            emit_norm_tile(k, S, x_sb, x_b, T, onesb, ones_b, sq_sb, sq_b, ss_ps, ss_b, rstd_sb, rstd_b, eps_sb, eps_b)
            ob, ob_b = ob_ring.next()
            for o in range(8):
                S.op("dve", lambda e: e.scalar_tensor_tensor(out=ob[:, o, :T], in0=x_sb[:, o, :T], scalar=fg_sb[:, o:o + 1], in1=rstd_sb[:, :T], op0=ALU.mult, op1=ALU.mult),
                     reads=[x_b, fg_b, rstd_b], writes=[ob_b])
            S.dma("sp", xfT[:, t0:t0 + T].rearrange("(kc p) t -> p kc t", p=128), ob[:, :, :T], reads=[ob_b], final=True)
        k.stack = main_stack
        S.barrier()
        stB.close()
    if own:
        return k.finish()
    k.end_phase()


def emit_p2f(k, io, odd):
    k.begin_phase(io)
    S = k.S
    FR = 1952 if odd else 1664
    fm = io["fm"]
    GK = io["GK"]
    GV = io["GV"]
    tm = io["tm"]
    yT = io["yT"]
    ident_d = io["ident"]
    identb, identb_b, onesb, ones_b, eps_sb, eps_b = load_consts(k, S, ident_d)
    NB = 130 if odd else 50
    NCOL = NB * 128
    dkmax = 96 if odd else 64
    qT_sb, qT_b = k.sb([dkmax, NT], BF16, "qT_sb")
    kT_sb, kT_b = k.sb([dkmax, NCOL], BF16, "kT_sb")
    va_sb, va_b = k.sb([128, NB, 65], BF16, "va_sb")
    S.op("dve", lambda e: e.memset(va_sb[:, :, 64:65], 1.0), writes=[va_b])
    sring = Ring([k.ps([128, 512], F32, "s") for i in range(2)])
    O = [k.ps([128, 512], F32, "o") for i in range(4)]
    pt_ps, pt_b = k.ps([128, 512], BF16, "ptp")
    pring = Ring([k.sb([128, 512], BF16, "pT") for i in range(3)])
    den_sb, den_b = k.sb([128, 4], F32, "den")
    y_sb, y_b = k.sb([128, 4, 64], BF16, "y_sb")
    yTring = Ring([k.sb([64, 512], BF16, "yT") for i in range(2)])
    if not odd:
        maskA, maskA_b = k.sb([128, 6, 512], BF16, "maskA_sb")
        S.dma("pool", maskA[:], io["maskA"][:, :, :], writes=[maskA_b])
        candA, candA_b = k.sb([128, 8, 512], BF16, "candA_sb")
        S.dma("pool", candA[:], io["candA"][:, :, :], writes=[candA_b])
        selB, selB_b = k.sb([128, 8], F32, "selB_sb")
        S.dma("sp", selB[:], io["selB"][:, :], writes=[selB_b])
        wvar, wvar_b = k.sb([128, 4], F32, "wvar_sb")
        S.dma("sp", wvar[:], io["wvar"][:, :], writes=[wvar_b])
        esink, esink_b = k.sb([128, 8], F32, "esink_sb")
        S.dma("sp", esink[:], io["sink"][:, :], writes=[esink_b])
        S.op("act", lambda e: e.activation(out=esink[:], in_=esink[:], func=AF.Exp), reads=[esink_b], writes=[esink_b])
        stg_ring = Ring([k.sb([128, 512], F32, "stg") for i in range(2)])
        Mv = [k.sb([128, 8, 512], BF16, f"Mv{v}") for v in range(3)]
        MT0, MT0_b = k.sb([128, 8, 512], BF16, "MT0")
        MT7, MT7_b = k.sb([128, 8, 512], BF16, "MT7")
        tmpM, tmpM_b = k.sb([128, 8, 512], BF16, "tmpM")
        nbr = nbr_index()
        needB = [[any(bool(nbr[v][0][:, kb, qb * 128:(qb + 1) * 128].any()) for v in range(3)) for qb in range(4)] for kb in range(8)]
        mA = _maskA_np()
        needA = [[bool(mA[:, kb, qb * 128:(qb + 1) * 128].any()) for qb in range(4)] for kb in range(6)]

    def hb(r, which, b):
        return 34 + r * 4 + which * 2 + b

    def gv_rows(r, t0, n):
        out = []
        for (c0, cn, ap) in GV:
            a, b = max(t0, c0), min(t0 + n, c0 + cn)
            if a < b:
                out.append((ap[r * cn + (a - c0):r * cn + (b - c0), :], a, b - a))
        return out

    def load_v(dst_blk0, r, t0, n, vcol):
        for (ap, a, m) in gv_rows(r, t0, n):
            b0 = dst_blk0 + (a - t0) // 128
            S.dma("sp", va_sb[:, b0:b0 + m // 128, 0:64], ap[:, vcol:vcol + 64].rearrange("(kb p) d -> p kb d", p=128), writes=[va_b])

    def load_kv(krows, dk_parts, vcol):
        for (row0, nr, p0) in krows:
            gap, gn = GK[row0]
            assert gn == nr
            S.dma("sp", kT_sb[p0:p0 + nr, 0:CTX], fm[row0:row0 + nr, 0:CTX], writes=[kT_b])
            if odd:
                for r in range(4):
                    S.dma("sp", kT_sb[p0:p0 + nr, CTX + r * LAT_PC:CTX + (r + 1) * LAT_PC], gap[r * nr:(r + 1) * nr, CTX:NT], writes=[kT_b])
            else:
                S.dma("sp", kT_sb[p0:p0 + nr, CTX:NT], fm[row0:row0 + nr, CTX:NT], writes=[kT_b])
                for r in range(4):
                    S.dma("sp", kT_sb[p0:p0 + nr, NT + r * 512:NT + r * 512 + 256], gap[r * nr:(r + 1) * nr, NT - 256:NT], writes=[kT_b])
                    S.dma("sp", kT_sb[p0:p0 + nr, NT + r * 512 + 256:NT + r * 512 + 512], gap[r * nr:(r + 1) * nr, CTX:CTX + 256], writes=[kT_b])
        S.dma("sp", va_sb[:, 0:2, 0:64], tm[0:CTX, vcol:vcol + 64].rearrange("(kb p) d -> p kb d", p=128), writes=[va_b])
        if odd:
            for r in range(4):
                load_v(2 + r * 32, r, CTX, LAT_PC, vcol)
        else:
            S.dma("sp", va_sb[:, 2:34, 0:64], tm[CTX:NT, vcol:vcol + 64].rearrange("(kb p) d -> p kb d", p=128), writes=[va_b])
            for r in range(4):
                load_v(34 + r * 4, r, NT - 256, 256, vcol)
                load_v(36 + r * 4, r, CTX, 256, vcol)

    if odd:
        jobs = [("C", h) for h in range(8)] + [("D", h) for h in range(8)]
    else:
        jobs = [("A", h) for h in range(8)] + [("B", h) for h in range(8)]
    for (kind, h) in jobs:
        g = h // 4
        if kind == "A":
            dk, qrow, ychunk = 64, h * 64, h // 2
            if h % 4 == 0:
                load_kv([(512 + g * 64, 64, 0)], 64, g * 64)
        elif kind == "B":
            dk, qrow, ychunk = 64, 640 + h * 64, 4 + h // 2
            load_kv([(1152 + h * 64, 64, 0)], 64, 128 + h * 64)
            for v in range(3):
                for kb in range(8):
                    stg, stg_b = stg_ring.next()
                    S.dma("sp", stg[:], io["rpbx"][h, v, :, kb, :], writes=[stg_b])
                    S.op("act", lambda e: e.activation(out=Mv[v][0][:, kb, :], in_=stg[:], func=AF.Exp), reads=[stg_b], writes=[Mv[v][1]])
            S.op("dve", lambda e: e.tensor_scalar(out=tmpM[:], in0=Mv[1][0][:], scalar1=wvar[:, 1:2], scalar2=None, op0=ALU.mult), reads=[Mv[1][1], wvar_b], writes=[tmpM_b])
            S.op("dve", lambda e: e.scalar_tensor_tensor(out=MT0[:], in0=Mv[0][0][:], scalar=wvar[:, 0:1], in1=tmpM[:], op0=ALU.mult, op1=ALU.add), reads=[Mv[0][1], wvar_b, tmpM_b], writes=[MT0_b])
            S.op("dve", lambda e: e.tensor_scalar(out=tmpM[:], in0=Mv[1][0][:], scalar1=wvar[:, 3:4], scalar2=None, op0=ALU.mult), reads=[Mv[1][1], wvar_b], writes=[tmpM_b])
            S.op("dve", lambda e: e.scalar_tensor_tensor(out=MT7[:], in0=Mv[2][0][:], scalar=wvar[:, 2:3], in1=tmpM[:], op0=ALU.mult, op1=ALU.add), reads=[Mv[2][1], wvar_b, tmpM_b], writes=[MT7_b])
        elif kind == "C":
            dk, qrow, ychunk = 96, h * 96, h // 2
            load_kv([(768 + h * 64, 64, 0), (1280, 32, 64)], 96, h * 64)
        else:
            dk, qrow, ychunk = 64, 1312 + h * 64, 4 + h // 2
            if h % 4 == 0:
                load_kv([(1824 + g * 64, 64, 0)], 64, 512 + g * 64)
        scale = float(dk) ** -0.5
        S.dma("sp", qT_sb[:dk, :], fm[qrow:qrow + dk, :], writes=[qT_b])
        tiles = [(0, 256, None)] + [(CTX + 512 * i, 512, i) for i in range(LAT_PC // 512)]
        for (q0, nq, ti) in tiles:
            nqb = nq // 128
            kbl = []
            allq = [True] * nqb
            if ti is None:
                kbl = [(0, None, None, None, allq), (1, None, None, None, allq)]
            elif odd:
                kbl = [(kb, None, None, None, allq) for kb in range(NB)]
            elif kind == "A":
                for kbrel in range(6):
                    lb = 4 * ti + kbrel - 1
                    if 0 <= lb < 32:
                        kbl.append((2 + lb, maskA[:, kbrel, :], maskA_b, None, needA[kbrel]))
                    elif lb < 0:
                        for r in range(4):
                            kbl.append((hb(r, 0, 1), candA[:, r, :], candA_b, None, needA[kbrel]))
                    else:
                        for r in range(4):
                            kbl.append((hb(r, 1, 0), candA[:, 4 + r, :], candA_b, None, needA[kbrel]))
                kbl += [(0, None, None, None, allq), (1, None, None, None, allq)]
            else:
                M, M_b = (MT0, MT0_b) if ti == 0 else ((MT7, MT7_b) if ti == 7 else Mv[1])
                for kbrel in range(8):
                    lb = 4 * ti - 2 + kbrel
                    if not any(needB[kbrel]):
                        continue
                    if 0 <= lb < 32:
                        kbl.append((2 + lb, M[:, kbrel, :], M_b, None, needB[kbrel]))
                    elif lb < 0:
                        for r in range(4):
                            kbl.append((hb(r, 0, lb + 2), M[:, kbrel, :], M_b, selB[:, r:r + 1], needB[kbrel]))
                    else:
                        for r in range(4):
                            kbl.append((hb(r, 1, lb - 32), M[:, kbrel, :], M_b, selB[:, 4 + r:5 + r], needB[kbrel]))
                kbl += [(0, None, None, None, allq), (1, None, None, None, allq)]
            first = [min(i for i, ent in enumerate(kbl) if ent[4][qb]) for qb in range(nqb)]
            last = [max(i for i, ent in enumerate(kbl) if ent[4][qb]) for qb in range(nqb)]
            for i, (kb, mask_ap, mask_b, scal, nd) in enumerate(kbl):
                s_ps, s_b = sring.next()
                S.op("pe", lambda e: e.matmul(s_ps[:, :nq], lhsT=kT_sb[:dk, kb * 128:(kb + 1) * 128], rhs=qT_sb[:dk, q0:q0 + nq], start=True, stop=True),
                     reads=[kT_b, qT_b], writes=[s_b])
                p_sb, p_b = pring.next()
                S.op("act", lambda e: e.activation(out=p_sb[:, :nq], in_=s_ps[:, :nq], func=AF.Exp, scale=scale), reads=[s_b], writes=[p_b])
                if mask_ap is not None:
                    if scal is None:
                        S.op("dve" if i % 2 == 0 else "pool", lambda e: e.tensor_tensor(out=p_sb[:, :nq], in0=p_sb[:, :nq], in1=mask_ap, op=ALU.mult),
                             reads=[p_b, mask_b], writes=[p_b])
                    else:
                        S.op("dve", lambda e: e.scalar_tensor_tensor(out=p_sb[:, :nq], in0=p_sb[:, :nq], scalar=scal, in1=mask_ap, op0=ALU.mult, op1=ALU.mult),
                             reads=[p_b, mask_b, selB_b], writes=[p_b])
                for qb in range(nqb):
                    if nd[qb]:
                        o_ps, o_b = O[qb]
                        S.op("pe", lambda e: e.matmul(o_ps[:, 0:65], lhsT=p_sb[:, qb * 128:(qb + 1) * 128], rhs=va_sb[:, kb, :],
                                                      start=(i == first[qb]), stop=(i == last[qb])), reads=[p_b, va_b], writes=[o_b])
            yt_sb, yt_b = yTring.next()
            for qb in range(nqb):
                o_ps, o_b = O[qb]
                if kind == "A":
                    S.op("dve", lambda e: e.tensor_tensor(out=den_sb[:, qb:qb + 1], in0=o_ps[:, 64:65], in1=esink[:, h:h + 1], op=ALU.add), reads=[o_b, esink_b], writes=[den_b])
                    S.op("dve", lambda e: e.reciprocal(out=den_sb[:, qb:qb + 1], in_=den_sb[:, qb:qb + 1]), reads=[den_b], writes=[den_b])
                else:
                    S.op("dve", lambda e: e.reciprocal(out=den_sb[:, qb:qb + 1], in_=o_ps[:, 64:65]), reads=[o_b], writes=[den_b])
                S.op("dve", lambda e: e.tensor_scalar(out=y_sb[:, qb, :], in0=o_ps[:, 0:64], scalar1=den_sb[:, qb:qb + 1], scalar2=None, op0=ALU.mult),
                     reads=[o_b, den_b], writes=[y_b])
                S.op("pe", lambda e: e.transpose(out=pt_ps[:64, qb * 128:(qb + 1) * 128], in_=y_sb[:, qb, :], identity=identb[:, :]), reads=[y_b, identb_b], writes=[pt_b])
            S.op("dve", lambda e: e.tensor_copy(out=yt_sb[:, :nq], in_=pt_ps[:64, :nq]), reads=[pt_b], writes=[yt_b])
            S.dma("sp", yT[ychunk, (h % 2) * 64:(h % 2) * 64 + 64, q0:q0 + nq], yt_sb[:, :nq], reads=[yt_b])
    k.end_phase()


def build_fused(depth=DEPTH):
    k = K("fused")
    k.fused = True
    S = k.S
    nc = k.nc
    EI = "ExternalInput"
    g = {}
    g["xT0"] = k.dram("xT0", [DM, NT], F32, EI)
    for nm, shp in (("cond", [128, 8, 2]), ("ident", [128, 128]), ("blk", [128, 128]), ("ropeC", [128, NT]), ("ropeS", [128, NT]),
                    ("ropeC2", [96, NT]), ("ropeS2", [96, NT]), ("ropeC3", [32, NT]), ("ropeS3", [32, NT]), ("fgain", [128, 8]),
                    ("maskA", [128, 6, 512]), ("candA", [128, 8, 512]), ("selB", [128, 8]), ("wvar", [128, 4])):
        g[nm] = k.dram(nm, shp, F32, EI)
    L = []
    for l in range(depth):
        odd = l % 2 == 1
        d = {}
        d["modw"] = k.dram(f"modw{l}", [DM, 6144], F32, EI)
        d["modb"] = k.dram(f"modb{l}", [128, 48], F32, EI)
        d["gmix"] = k.dram(f"gmix{l}", [128, 8], F32, EI)
        d["gffn"] = k.dram(f"gffn{l}", [128, 8], F32, EI)
        d["w"] = k.dram(f"w{l}", [DM, (1824 + 32 + 640) if odd else (2304 + 640)], F32, EI)
        if odd:
            d["wqb"] = k.dram(f"wqb{l}", [768, 1536], F32, EI)
            d["wkvb"] = k.dram(f"wkvb{l}", [256, 1024], F32, EI)
            d["qn"] = k.dram(f"qn{l}", [128, 6], F32, EI)
            d["kvn"] = k.dram(f"kvn{l}", [128, 2], F32, EI)
            d["dgq"] = k.dram(f"dgq{l}", [128, 4], F32, EI)
        else:
            d["sink"] = k.dram(f"sink{l}", [128, 8], F32, EI)
            d["rpbx"] = k.dram(f"rpbx{l}", [8, 3, 128, 8, 512], F32, EI)
        d["wout"] = k.dram(f"wout{l}", [DM, DM], F32, EI)
        d["rw"] = k.dram(f"rw{l}", [DM, NEXP], F32, EI)
        d["rb"] = k.dram(f"rb{l}", [1, NEXP], F32, EI)
        d["ewin"] = k.dram(f"ewin{l}", [NEXP, DM, 2048], F32, EI)
        d["ebin"] = k.dram(f"ebin{l}", [128, NEXP, 16], F32, EI)
        d["ewout"] = k.dram(f"ewout{l}", [NEXP, DM, DM], F32, EI)
        d["ebout"] = k.dram(f"ebout{l}", [NEXP, DM], F32, EI)
        L.append(d)
    out = k.dram("out", [DM, NT], F32, "ExternalOutput")
    XA = k.dram("XA", [DM, NT], F32, "Internal")
    XB = k.dram("XB", [DM, NT], F32, "Internal")
    fmE = k.dram("fmE", [1664, NT], BF16, "Internal")
    fmO = k.dram("fmO", [1952, NT], BF16, "Internal")
    tmE = k.dram("tmE", [NT, 640], BF16, "Internal")
    tmO = k.dram("tmO", [NT, 640], BF16, "Internal")
    def kpieces(odd):
        rows = [(768 + 64 * i, 64) for i in range(8)] + [(1280, 32)] + [(1824, 64), (1888, 64)] if odd else \
               [(512, 64), (576, 64)] + [(1152 + 64 * i, 64) for i in range(8)]
        return rows
    vpieces = [(c * 512, min(512, NT - c * 512)) for c in range((NT + 511) // 512)]
    GKs, GVs = {}, {}
    for par, tag in ((False, "E"), (True, "O")):
        GKs[par] = {r0: (k.dram(f"gk{tag}{r0}", [4 * n, NT], BF16, "Internal"), n) for (r0, n) in kpieces(par)}
        GVs[par] = [(t0, n, k.dram(f"gv{tag}{t0}", [4 * n, 640], BF16, "Internal")) for (t0, n) in vpieces]
    yTd = k.dram("yTd", [8, 128, NT], BF16, "Internal")
    groups = [[0, 1, 2, 3], [4, 5, 6, 7]]
    ccscr, _ = k.sb([1, 4], F32, "ccscr")
    xs = [g["xT0"], XA, XB]
    for l in range(depth):
        odd = l % 2 == 1
        d = L[l]
        xin = xs[0] if l == 0 else xs[1 + (l - 1) % 2]
        xout = xs[1 + l % 2]
        fm_, tm_ = (fmO, tmO) if odd else (fmE, tmE)
        io = {"xT": xin, "cond": g["cond"], "modw": d["modw"][:, 0:2048], "modb": d["modb"][:, 0:16], "gain": d["gmix"],
              "ropeC": g["ropeC"], "ropeS": g["ropeS"], "w": d["w"], "fm": fm_, "tm": tm_, "ident": g["ident"]}
        if odd:
            io.update({"wqb": d["wqb"], "wkvb": d["wkvb"], "qn": d["qn"], "kvn": d["kvn"], "dgq": d["dgq"], "ropeC2": g["ropeC2"],
                       "ropeS2": g["ropeS2"], "ropeC3": g["ropeC3"], "ropeS3": g["ropeS3"], "blk": g["blk"]})
        build_p1(odd, k, io)
        for r0, (gap, n) in GKs[odd].items():
            S.cc(k.stack, lambda e: e.collective_compute("AllGather", ALU.bypass, replica_groups=groups, ins=[fm_[r0:r0 + n, :]], outs=[gap]), ccscr[0:1, 0:4])
        for (t0, n, gap) in GVs[odd]:
            S.cc(k.stack, lambda e: e.collective_compute("AllGather", ALU.bypass, replica_groups=groups, ins=[tm_[t0:t0 + n, :]], outs=[gap]), ccscr[0:1, 0:4])
        S.barrier()
        io2 = {"fm": fm_, "tm": tm_, "GK": GKs[odd], "GV": GVs[odd], "yT": yTd, "ident": g["ident"]}
        if not odd:
            io2.update({"maskA": g["maskA"], "candA": g["candA"], "selB": g["selB"], "wvar": g["wvar"], "sink": d["sink"], "rpbx": d["rpbx"]})
        emit_p2f(k, io2, odd)
        last = l == depth - 1
        io3 = {"xT": xin, "yT": yTd, "cond": g["cond"], "modw": d["modw"][:, 2048:6144], "modb": d["modb"][:, 16:48], "gain": d["gffn"],
               "fgain": g["fgain"], "wout": d["wout"], "rw": d["rw"], "rb": d["rb"], "ewin": d["ewin"], "ebin": d["ebin"],
               "ewout": d["ewout"], "ebout": d["ebout"], "ident": g["ident"], "x2T": xout, "xfT": out}
        build_p3(k, io3, do_final=last)
    return k.finish()


def kernel(x, c, ctx, c_ctx, mod_w, mod_b, norm_mix, norm_ffn, ab_w_in, ab_w_out, a_sink, b_rpb,
                 cd_w_in, c_q_norm, c_w_q_b, c_kv_norm, c_w_kv_b, d_q_norm, d_k_norm, cd_w_out,
                 router_w, router_b, exp_w_in, exp_b_in, exp_w_out, exp_b_out, final_norm, _depth=DEPTH):
    f32 = lambda a: np.ascontiguousarray(np.asarray(a, np.float32))
    x, c, ctx, c_ctx = f32(x), f32(c), f32(ctx), f32(c_ctx)
    if ("fused", _depth) not in _PROGS:
        _PROGS[("fused", _depth)] = build_fused(_depth)
    nc = _PROGS[("fused", _depth)]
    Ch, Sh = _rope_tables(64, [d for _ in range(2) for d in range(64)])
    Cm, Sm = _rope_tables(32, [-1] * 64 + list(range(32)))
    C3, S3 = _rope_tables(32, list(range(32)))
    mA = _maskA_np()
    shared = {"ident": np.eye(128, dtype=np.float32), "blk": np.kron(np.eye(2, dtype=np.float32), np.ones((64, 64), np.float32)),
              "fgain": fm(final_norm, 8), "maskA": mA}
    for l in range(_depth):
        i = l // 2
        odd = l % 2 == 1
        shared[f"modw{l}"] = f32(mod_w[l])
        shared[f"modb{l}"] = fm(f32(mod_b[l]), 48)
        shared[f"gmix{l}"] = fm(norm_mix[l], 8)
        shared[f"gffn{l}"] = fm(norm_ffn[l], 8)
        if not odd:
            w = f32(ab_w_in[i])
            shared[f"w{l}"] = np.ascontiguousarray(np.concatenate([w, _swap_cols(w, 0, 10, 64, 0, 64)], axis=1))
            shared[f"sink{l}"] = np.ascontiguousarray(np.tile(f32(a_sink[i])[None, :], (128, 1)))
            rp = f32(b_rpb[i])
            rx = np.empty((8, 3, 128, 8, 512), np.float32)
            for hh in range(8):
                for v, (valid, dr, dc) in enumerate(nbr_index()):
                    rx[hh, v] = np.where(valid, rp[hh][dr, dc], np.float32(-30000.0))
            shared[f"rpbx{l}"] = rx
            shared[f"wout{l}"] = f32(ab_w_out[i])
        else:
            w = f32(cd_w_in[i])
            shared[f"w{l}"] = np.ascontiguousarray(np.concatenate([w, _swap_cols(w, 1024, 1, 32, 0, 32), _swap_cols(w, 1056, 10, 64, 0, 64)], axis=1))
            wq = f32(c_w_q_b[i])
            shared[f"wqb{l}"] = np.ascontiguousarray(np.concatenate([wq, _swap_cols(wq, 0, 8, 96, 64, 32)], axis=1))
            wkv = f32(c_w_kv_b[i]).reshape(256, 8, 128)
            shared[f"wkvb{l}"] = np.ascontiguousarray(np.concatenate([wkv[:, :, :64].reshape(256, 512), wkv[:, :, 64:].reshape(256, 512)], axis=1))
            shared[f"qn{l}"] = fm(c_q_norm[i], 6)
            shared[f"kvn{l}"] = fm(c_kv_norm[i], 2)
            gq, gk = f32(d_q_norm[i]), f32(d_k_norm[i])
            sw = (np.arange(64) + 32) % 64
            shared[f"dgq{l}"] = np.ascontiguousarray(np.stack([np.tile(gq, 2), np.tile(gq[sw], 2), np.tile(gk, 2), np.tile(gk[sw], 2)], axis=1))
            shared[f"wout{l}"] = f32(cd_w_out[i])
        shared[f"rw{l}"] = f32(router_w[l])
        shared[f"rb{l}"] = f32(router_b[l])[None, :]
        shared[f"ewin{l}"] = f32(exp_w_in[l])
        shared[f"ebin{l}"] = np.ascontiguousarray(f32(exp_b_in[l]).reshape(NEXP, 16, 128).transpose(2, 0, 1))
        shared[f"ewout{l}"] = f32(exp_w_out[l])
        shared[f"ebout{l}"] = f32(exp_b_out[l])
    ins = []
    for core in range(NCORES):
        b, r = core // 4, core % 4
        d = dict(shared)
        tok = np.concatenate([ctx[b], x[b, r * LAT_PC:(r + 1) * LAT_PC]], axis=0)
        d["xT0"] = np.ascontiguousarray(tok.T)
        d["cond"] = np.ascontiguousarray(np.stack([fm(c[b], 8), fm(c_ctx, 8)], axis=-1))
        d["ropeC"], d["ropeS"] = _tabs_for_core(Ch, Sh, r)
        d["ropeC2"], d["ropeS2"] = _tabs_for_core(Cm, Sm, r)
        d["ropeC3"], d["ropeS3"] = _tabs_for_core(C3, S3, r)
        cand = np.zeros((128, 8, 512), np.float32)
        sel = np.zeros((128, 8), np.float32)
        for rr in range(4):
            if rr == r - 1:
                cand[:, rr, :] = mA[:, 0, :]
                sel[:, rr] = 1.0
            if rr == r + 1:
                cand[:, 4 + rr, :] = mA[:, 5, :]
                sel[:, 4 + rr] = 1.0
        d["candA"], d["selB"] = cand, sel
        wv = np.zeros((128, 4), np.float32)
        wv[:, 0] = 1.0 if r == 0 else 0.0
        wv[:, 1] = 0.0 if r == 0 else 1.0
        wv[:, 2] = 1.0 if r == 3 else 0.0
        wv[:, 3] = 0.0 if r == 3 else 1.0
        d["wvar"] = wv
        ins.append(d)
    res = run_bass_kernel_spmd(nc, ins, core_ids=list(range(NCORES))).results
    out = np.empty((BATCH, SEQ, DM), np.float32)
    for core in range(NCORES):
        b, r = core // 4, core % 4
        out[b, r * LAT_PC:(r + 1) * LAT_PC] = res[core]["out"][:, CTX:].T
    return out


_PROGS = {}
BF = ml_dtypes.bfloat16


def _prog(name):
    if name not in _PROGS:
        if name == "p1e":
            _PROGS[name] = build_p1(False)
        elif name == "p1o":
            _PROGS[name] = build_p1(True)
        elif name == "p2e":
            _PROGS[name] = build_p2(False)
        elif name == "p2o":
            _PROGS[name] = build_p2(True)
        else:
            _PROGS[name] = build_p3()
    return _PROGS[name]


def _run(name, in_maps):
    res = run_bass_kernel_spmd(_prog(name), in_maps, core_ids=list(range(NCORES)))
    return res.results


def fm(v, n):
    return np.ascontiguousarray(np.asarray(v, np.float32).reshape(n, 128).T)


def _rope_tables(dim, rows_pattern):
    t = np.arange(SEQ, dtype=np.int32)
    row = (t // GRID_W).astype(np.float32)
    col = (t % GRID_W).astype(np.float32)
    quarter = dim // 4
    inv = (np.float32(10000.0) ** (-np.arange(quarter, dtype=np.float32) / np.float32(quarter))).astype(np.float32)
    ang = np.concatenate([row[:, None] * inv, col[:, None] * inv], axis=-1).astype(np.float32)
    cos, sin = np.cos(ang).astype(np.float32), np.sin(ang).astype(np.float32)
    half = dim // 2
    C = np.ones((len(rows_pattern), SEQ), np.float32)
    Sn = np.zeros((len(rows_pattern), SEQ), np.float32)
    for r, d in enumerate(rows_pattern):
        if d < 0:
            continue
        C[r] = cos[:, d % half]
        Sn[r] = -sin[:, d % half] if d < half else sin[:, d % half]
    return C, Sn


def _core_table(tab, r):
    out = np.empty((tab.shape[0], NT), np.float32)
    out[:, :CTX] = tab[:, :1] * 0 + (1.0 if tab is None else 0.0)
    return out


def _tabs_for_core(C, Sn, r):
    Cc = np.ones((C.shape[0], NT), np.float32)
    Sc = np.zeros((C.shape[0], NT), np.float32)
    Cc[:, CTX:] = C[:, r * LAT_PC:(r + 1) * LAT_PC]
    Sc[:, CTX:] = Sn[:, r * LAT_PC:(r + 1) * LAT_PC]
    return Cc, Sc


def _swap_cols(w, c0, nheads, hd, rot0, rotd):
    blk = w[:, c0:c0 + nheads * hd].copy()
    idx = np.arange(nheads * hd)
    h, d = idx // hd, idx % hd
    src = idx.copy()
    inrot = (d >= rot0) & (d < rot0 + rotd)
    src[inrot] = h[inrot] * hd + rot0 + ((d[inrot] - rot0 + rotd // 2) % rotd)
    return blk[:, src]


def kernel_unfused(x, c, ctx, c_ctx, mod_w, mod_b, norm_mix, norm_ffn, ab_w_in, ab_w_out, a_sink, b_rpb,
           cd_w_in, c_q_norm, c_w_q_b, c_kv_norm, c_w_kv_b, d_q_norm, d_k_norm, cd_w_out,
           router_w, router_b, exp_w_in, exp_b_in, exp_w_out, exp_b_out, final_norm, _depth=DEPTH):
    f32 = lambda a: np.asarray(a, np.float32)
    x, c, ctx, c_ctx = f32(x), f32(c), f32(ctx), f32(c_ctx)
    ident = np.eye(128, dtype=np.float32)
    xT = []
    for core in range(NCORES):
        b, r = core // 4, core % 4
        tok = np.concatenate([ctx[b], x[b, r * LAT_PC:(r + 1) * LAT_PC]], axis=0)
        xT.append(np.ascontiguousarray(tok.T))
    conds = [np.ascontiguousarray(np.stack([fm(c[core // 4], 8), fm(c_ctx, 8)], axis=-1)) for core in range(NCORES)]
    Ch, Sh = _rope_tables(64, [d for _ in range(2) for d in range(64)])
    Cm, Sm = _rope_tables(32, [-1] * 64 + list(range(32)))
    C3, S3 = _rope_tables(32, list(range(32)))
    blk = np.kron(np.eye(2, dtype=np.float32), np.ones((64, 64), np.float32))
    maskA = _maskA_np()
    xf = None
    for l in range(_depth):
        i = l // 2
        odd = l % 2 == 1
        mw, mb = f32(mod_w[l]), f32(mod_b[l])
        ins = []
        for core in range(NCORES):
            r = core % 4
            Cc, Sc = _tabs_for_core(Ch, Sh, r)
            d = {"xT": xT[core], "cond": conds[core], "modw": np.ascontiguousarray(mw[:, :2048]), "modb": fm(mb[:2048], 16),
                 "gain": fm(norm_mix[l], 8), "ropeC": Cc, "ropeS": Sc, "ident": ident}
            if not odd:
                w = f32(ab_w_in[i])
                d["w"] = np.ascontiguousarray(np.concatenate([w, _swap_cols(w, 0, 10, 64, 0, 64)], axis=1))
            else:
                w = f32(cd_w_in[i])
                d["w"] = np.ascontiguousarray(np.concatenate([w, _swap_cols(w, 1024, 1, 32, 0, 32), _swap_cols(w, 1056, 10, 64, 0, 64)], axis=1))
                wq = f32(c_w_q_b[i])
                d["wqb"] = np.ascontiguousarray(np.concatenate([wq, _swap_cols(wq, 0, 8, 96, 64, 32)], axis=1))
                wkv = f32(c_w_kv_b[i]).reshape(256, 8, 128)
                d["wkvb"] = np.ascontiguousarray(np.concatenate([wkv[:, :, :64].reshape(256, 512), wkv[:, :, 64:].reshape(256, 512)], axis=1))
                d["qn"] = fm(c_q_norm[i], 6)
                d["kvn"] = fm(c_kv_norm[i], 2)
                gq, gk = f32(d_q_norm[i]), f32(d_k_norm[i])
                sw = (np.arange(64) + 32) % 64
                d["dgq"] = np.ascontiguousarray(np.stack([np.tile(gq, 2), np.tile(gq[sw], 2), np.tile(gk, 2), np.tile(gk[sw], 2)], axis=1))
                d["ropeC2"], d["ropeS2"] = _tabs_for_core(Cm, Sm, r)
                d["ropeC3"], d["ropeS3"] = _tabs_for_core(C3, S3, r)
                d["blk"] = blk
            ins.append(d)
        res = _run("p1o" if odd else "p1e", ins)
        ins2 = []
        for b in range(BATCH):
            FM = np.concatenate([res[4 * b]["fm"][:, :CTX]] + [res[4 * b + r]["fm"][:, CTX:] for r in range(4)], axis=1)
            TM = np.concatenate([res[4 * b]["tm"][:CTX]] + [res[4 * b + r]["tm"][CTX:] for r in range(4)], axis=0)
            for j in range(4):
                g = j // 2
                if not odd:
                    d = {"q1T": np.stack([FM[(2 * j + hh) * 64:(2 * j + hh + 1) * 64] for hh in range(2)]),
                         "k1T": FM[512 + g * 64:512 + (g + 1) * 64], "v1": TM[:, g * 64:(g + 1) * 64],
                         "q2T": np.stack([FM[640 + (2 * j + hh) * 64:640 + (2 * j + hh + 1) * 64] for hh in range(2)]),
                         "k2T": np.stack([FM[1152 + (2 * j + hh) * 64:1152 + (2 * j + hh + 1) * 64] for hh in range(2)]),
                         "v2": TM[:, 128 + 2 * j * 64:128 + (2 * j + 2) * 64], "maskA": maskA}
                    d["sink"] = np.ascontiguousarray(np.tile(f32(a_sink[i])[None, 2 * j:2 * j + 2], (128, 1)))
                    rp = f32(b_rpb[i])
                    rx = np.empty((2, 3, 128, 8, 512), np.float32)
                    for hh in range(2):
                        for v, (valid, dr, dc) in enumerate(nbr_index()):
                            rx[hh, v] = np.where(valid, rp[2 * j + hh][dr, dc], np.float32(-30000.0))
                    d["rpbx"] = rx
                else:
                    d = {"q1T": np.stack([FM[1312 + (2 * j + hh) * 64:1312 + (2 * j + hh + 1) * 64] for hh in range(2)]),
                         "k1T": FM[1824 + g * 64:1824 + (g + 1) * 64], "v1": TM[:, 512 + g * 64:512 + (g + 1) * 64],
                         "q2T": np.stack([FM[(2 * j + hh) * 96:(2 * j + hh + 1) * 96] for hh in range(2)]),
                         "k2T": np.stack([np.concatenate([FM[768 + (2 * j + hh) * 64:768 + (2 * j + hh + 1) * 64], FM[1280:1312]], axis=0) for hh in range(2)]),
                         "v2": TM[:, 2 * j * 64:(2 * j + 2) * 64]}
                d = {kk: np.ascontiguousarray(vv) for kk, vv in d.items()}
                d["ident"] = ident
                ins2.append(d)
        del res
        res2 = _run("p2o" if odd else "p2e", ins2)
        ins3 = []
        for core in range(NCORES):
            b, r = core // 4, core % 4
            yin = np.empty((8, 128, NT), BF)
            for j in range(4):
                y = res2[4 * b + j]["yT"]
                for mx in range(2):
                    ci = (mx * 4 + j) if not odd else ((1 - mx) * 4 + j)
                    yin[ci, :, :CTX] = y[mx][:, :CTX]
                    yin[ci, :, CTX:] = y[mx][:, CTX + r * LAT_PC:CTX + (r + 1) * LAT_PC]
            d = {"xT": xT[core], "yT": yin, "cond": conds[core], "modw": np.ascontiguousarray(mw[:, 2048:]), "modb": fm(mb[2048:], 32),
                 "gain": fm(norm_ffn[l], 8), "fgain": fm(final_norm, 8), "wout": f32(cd_w_out[i] if odd else ab_w_out[i]),
                 "rw": f32(router_w[l]), "rb": f32(router_b[l])[None, :], "ewin": f32(exp_w_in[l]),
                 "ebin": np.ascontiguousarray(f32(exp_b_in[l]).reshape(NEXP, 16, 128).transpose(2, 0, 1)),
                 "ewout": f32(exp_w_out[l]), "ebout": f32(exp_b_out[l]), "ident": ident}
            ins3.append(d)
        del res2
        res3 = _run("p3", ins3)
        xT = [res3[core]["x2T"] for core in range(NCORES)]
        xf = [res3[core]["xfT"] for core in range(NCORES)]
        del res3
    out = np.empty((BATCH, SEQ, DM), np.float32)
    for core in range(NCORES):
        b, r = core // 4, core % 4
        out[b, r * LAT_PC:(r + 1) * LAT_PC] = xf[core][:, CTX:].T
    return out
```

```python
import contextlib
import numpy as np
import ml_dtypes
import concourse.bass as bass
import concourse.mybir as mybir
from concourse.bass_utils import run_bass_kernel_spmd

F32 = mybir.dt.float32
BF16 = mybir.dt.bfloat16
AF = mybir.ActivationFunctionType
ALU = mybir.AluOpType
AX = mybir.AxisListType

NCORES = 8
DM = 1024
BATCH = 2
SEQ = 16384
DEPTH = 4
GRID_W = 64
CTX = 256
LAT_PC = SEQ // 4
NT = CTX + LAT_PC
NTOK = CTX + SEQ
NKB = NTOK // 128
EPS = 1e-6
NEXP = 32
SAME_ENGINE_SYNC = True


class Buf:
    __slots__ = ("name", "writers", "readers")

    def __init__(self, name):
        self.name = name
        self.writers = {}
        self.readers = {}


class _Rec:
    def __init__(self):
        self.call = None

    def __getattr__(self, m):
        def f(*a, **kw):
            self.call = (m, a, kw)
            return self
        return f


class Sched:
    ENG = ("pe", "act", "dve", "pool", "sp")

    def __init__(self, nc, stack, ndma_sems=12):
        self.nc = nc
        self.prog = {e: [] for e in self.ENG}
        self.sem = {e: stack.enter_context(nc.semaphore("s_" + e)) for e in self.ENG}
        self.cnt = {e: 0 for e in self.ENG}
        self.waited = {e: {} for e in self.ENG}
        self.dq = {}
        for q in ("sp", "pool"):
            sems = [stack.enter_context(nc.semaphore(f"d_{q}{i}")) for i in range(ndma_sems)]
            self.dq[q] = {"sems": sems, "n": 0}
        self.semkey = {}
        self.ccs = []
        self.ccsem = None
        self.final = []
        self.ninst = 0

    def _key(self, sem):
        k = id(sem)
        self.semkey[k] = sem
        return k

    def _wait(self, eng, deps):
        w = self.waited[eng]
        for k, v in deps.items():
            if w.get(k, 0) >= v:
                continue
            w[k] = v
            sem = self.semkey[k]
            self.prog[eng].append(lambda e, sem=sem, v=v: e.wait_ge(sem, v))

    def _deps(self, eng, reads, writes):
        deps = {}
        own = self._key(self.sem[eng])

        def add(d):
            for k, v in d.items():
                if k == own and (eng == "pe" or not SAME_ENGINE_SYNC):
                    continue
                if deps.get(k, 0) < v:
                    deps[k] = v
        for b in reads:
            add(b.writers)
        for b in writes:
            add(b.writers)
            for k, v in b.readers.items():
                if k != own and deps.get(k, 0) < v:
                    deps[k] = v
        return deps

    def _mark(self, tok, reads, writes):
        k, v = tok
        for b in reads:
            if b.readers.get(k, 0) < v:
                b.readers[k] = v
        for b in writes:
            if b.readers:
                b.readers = {}
                b.writers = {}
            b.writers[k] = v

    def op(self, eng, fn, reads=(), writes=()):
        deps = self._deps(eng, reads, writes)
        self._wait(eng, deps)
        self.cnt[eng] += 1
        n = self.cnt[eng]
        sem = self.sem[eng]
        r = _Rec()
        fn(r)
        m, a, kw = r.call
        self.prog[eng].append(lambda e, m=m, a=a, kw=kw, sem=sem: getattr(e, m)(*a, **kw).then_inc(sem, 1))
        self._mark((self._key(sem), n), reads, writes)
        self.ninst += 1

    def cc(self, stack, fn, scratch, reads=(), writes=()):
        deps = self._deps("pool", reads, writes)
        self._wait("pool", deps)
        if self.ccsem is None:
            self.ccsem = stack.enter_context(self.nc.semaphore("ccsem"))
        sem = self.ccsem
        r = _Rec()
        fn(r)
        m, a, kw = r.call
        self.prog["pool"].append(lambda e, m=m, a=a, kw=kw, sem=sem: getattr(e, m)(*a, **kw).then_inc(sem, 1))
        self.ccs.append(sem)
        n = len(self.ccs)
        self.prog["pool"].append(lambda e, sem=sem, n=n: e.wait_ge(sem, n))
        self.op("pool", lambda e: e.memset(scratch, 0.0), reads=reads, writes=writes)

    def dma(self, q, out, in_, reads=(), writes=(), final=False):
        deps = self._deps(q, reads, writes)
        d = self.dq[q]
        i = d["n"]
        d["n"] += 1
        sems = d["sems"]
        sem = sems[i % len(sems)]
        rnd = i // len(sems)
        k = self._key(sem)
        if rnd > 0:
            deps[k] = max(deps.get(k, 0), 16 * rnd)
        self._wait(q, deps)
        self.prog[q].append(lambda e, o=out, a=in_, sem=sem: e.dma_start(out=o, in_=a).then_inc(sem, 16))
        tok = (k, 16 * (rnd + 1))
        self._mark(tok, reads, writes)
        if final:
            self.final.append(tok)
        self.ninst += 1

    def barrier(self):
        deps = {}
        for e in self.ENG:
            if self.cnt[e]:
                deps[self._key(self.sem[e])] = self.cnt[e]
        for q in self.dq.values():
            n = q["n"]
            L = len(q["sems"])
            for j, sem in enumerate(q["sems"]):
                uses = (n - j + L - 1) // L if n > j else 0
                if uses:
                    deps[self._key(sem)] = 16 * uses
        for e in self.ENG:
            own = self._key(self.sem[e])
            self._wait(e, {kk: v for kk, v in deps.items() if kk != own})

    def emit(self):
        nc = self.nc
        fin = {}
        for k, v in self.final:
            fin[k] = max(fin.get(k, 0), v)
        for q in self.dq.values():
            n = q["n"]
            for j, sem in enumerate(q["sems"]):
                uses = (n - j + len(q["sems"]) - 1) // len(q["sems"]) if n > j else 0
                if uses:
                    fin[self._key(sem)] = max(fin.get(self._key(sem), 0), 16 * uses)
        self._wait("sp", fin)
        prog = self.prog
        with nc.Block() as block:
            @block.tensor
            def _(e):
                for f in prog["pe"]:
                    f(e)

            @block.scalar
            def _(e):
                for f in prog["act"]:
                    f(e)

            @block.vector
            def _(e):
                for f in prog["dve"]:
                    f(e)

            @block.gpsimd
            def _(e):
                for f in prog["pool"]:
                    f(e)

            @block.sync
            def _(e):
                for f in prog["sp"]:
                    f(e)


class K:
    def __init__(self, name):
        self.nc = bass.Bass("TRN2", target_bir_lowering=False, name=name)
        self.stack = contextlib.ExitStack()
        self.S = Sched(self.nc, self.stack)
        self.nbuf = 0
        self.io = {}
        self.fused = False
        self.phase = 0

    def dram(self, name, shape, dt, kind):
        if name in self.io:
            return self.io[name]
        if self.fused and self.phase > 0:
            name = f"{name}_ph{self.phase}"
        return self.nc.dram_tensor(name, list(shape), dt, kind=kind).ap()

    def begin_phase(self, io):
        self.phase += 1
        self.io = io
        self.saved = self.stack
        self.stack = contextlib.ExitStack()

    def end_phase(self):
        self.S.barrier()
        self.stack.close()
        self.stack = self.saved
        self.io = {}

    def sb(self, shape, dt, name=None):
        self.nbuf += 1
        name = f"{name or 't'}_{self.nbuf}"
        t = self.stack.enter_context(self.nc.sbuf_tensor(name, list(shape), dt))
        return t, Buf(name)

    def ps(self, shape, dt, name=None):
        self.nbuf += 1
        name = f"{name or 'p'}_{self.nbuf}"
        t = self.stack.enter_context(self.nc.psum_tensor(name, list(shape), dt))
        return t, Buf(name)

    def finish(self):
        self.S.emit()
        self.stack.close()
        return self.nc


class Ring:
    def __init__(self, items):
        self.items = items
        self.i = 0

    def next(self):
        it = self.items[self.i % len(self.items)]
        self.i += 1
        return it


def token_tiles():
    tiles = [(0, CTX, True)]
    for i in range(LAT_PC // 512):
        tiles.append((CTX + 512 * i, 512, False))
    return tiles


def emit_mod_vectors(k, S, modw_d, modb_sb, modb_b, cond_sb, cond_b, nvec, out_sb, out_b, wring):
    ps_t, ps_b = k.ps([128, 4, 2], F32, "modps")
    for v in range(nvec):
        for hf in range(2):
            w_sb, w_b = wring.next()
            c0 = v * 1024 + hf * 512
            S.dma("sp", w_sb[:], modw_d[:, c0:c0 + 512].rearrange("(kc p) f -> p kc f", p=128), writes=[w_b])
            for j in range(4):
                for kc in range(8):
                    S.op("pe", lambda e, j=j, kc=kc, w_sb=w_sb: e.matmul(ps_t[:, j, :], lhsT=w_sb[:, kc, j * 128:(j + 1) * 128],
                                                                      rhs=cond_sb[:, kc, :], start=(kc == 0), stop=(kc == 7)),
                         reads=[w_b, cond_b], writes=[ps_b])
            for c in range(2):
                S.op("dve", lambda e, v=v, c=c, hf=hf: e.tensor_tensor(out=out_sb[:, v, hf * 4:hf * 4 + 4, c], in0=ps_t[:, :, c],
                                                                     in1=modb_sb[:, v * 8 + hf * 4:v * 8 + hf * 4 + 4], op=ALU.add),
                     reads=[ps_b, modb_b], writes=[out_b])


def emit_norm_tile(k, S, x_sb, x_b, T, onesb, ones_b, sq_sb, sq_b, ss_ps, ss_b, rstd_sb, rstd_b, eps_sb, eps_b):
    S.op("act", lambda e: e.activation(out=sq_sb[:, :, :T], in_=x_sb[:, :, :T], func=AF.Square), reads=[x_b], writes=[sq_b])
    for kc in range(8):
        S.op("pe", lambda e, kc=kc: e.matmul(ss_ps[:, :T], lhsT=onesb[:, :], rhs=sq_sb[:, kc, :T], start=(kc == 0), stop=(kc == 7)),
             reads=[sq_b, ones_b], writes=[ss_b])
    S.op("act", lambda e: e.activation(out=rstd_sb[:, :T], in_=ss_ps[:, :T], func=AF.Sqrt, scale=1.0 / DM, bias=eps_sb[:, 0:1]),
         reads=[ss_b, eps_b], writes=[rstd_b])
    S.op("dve", lambda e: e.reciprocal(out=rstd_sb[:, :T], in_=rstd_sb[:, :T]), reads=[rstd_b], writes=[rstd_b])


def load_consts(k, S, ident_d, need_f32_ident=False):
    identb, identb_b = k.sb([128, 128], BF16, "identb")
    S.dma("pool", identb[:], ident_d[:, :], writes=[identb_b])
    onesb, ones_b = k.sb([128, 128], BF16, "onesb")
    S.op("dve", lambda e: e.memset(onesb[:], 1.0), writes=[ones_b])
    eps_sb, eps_b = k.sb([128, 1], F32, "eps")
    S.op("dve", lambda e: e.memset(eps_sb[:], EPS), writes=[eps_b])
    return identb, identb_b, onesb, ones_b, eps_sb, eps_b


def build_p1(odd, k=None, io=None):
    own = k is None
    if own:
        k = K("p1o" if odd else "p1e")
    else:
        k.begin_phase(io)
    S = k.S
    xT = k.dram("xT", [DM, NT], F32, "ExternalInput")
    cond = k.dram("cond", [128, 8, 2], F32, "ExternalInput")
    modw = k.dram("modw", [DM, 2048], F32, "ExternalInput")
    modb = k.dram("modb", [128, 16], F32, "ExternalInput")
    gain = k.dram("gain", [128, 8], F32, "ExternalInput")
    ropeC = k.dram("ropeC", [128, NT], F32, "ExternalInput")
    ropeS = k.dram("ropeS", [128, NT], F32, "ExternalInput")
    if not odd:
        NW = 2304 + 640
        w_d = k.dram("w", [DM, NW], F32, "ExternalInput")
        fm_out = k.dram("fm", [1664, NT], BF16, "ExternalOutput")
        tm_out = k.dram("tm", [NT, 640], BF16, "ExternalOutput")
    else:
        NW = 1824 + 32 + 640
        w_d = k.dram("w", [DM, NW], F32, "ExternalInput")
        wqb_d = k.dram("wqb", [768, 1536], F32, "ExternalInput")
        wkvb_d = k.dram("wkvb", [256, 1024], F32, "ExternalInput")
        qn_d = k.dram("qn", [128, 6], F32, "ExternalInput")
        kvn_d = k.dram("kvn", [128, 2], F32, "ExternalInput")
        dgC = k.dram("dgq", [128, 4], F32, "ExternalInput")
        ropeC2 = k.dram("ropeC2", [96, NT], F32, "ExternalInput")
        ropeS2 = k.dram("ropeS2", [96, NT], F32, "ExternalInput")
        blk_d = k.dram("blk", [128, 128], F32, "ExternalInput")
        ropeC3 = k.dram("ropeC3", [32, NT], F32, "ExternalInput")
        ropeS3 = k.dram("ropeS3", [32, NT], F32, "ExternalInput")
        fm_out = k.dram("fm", [768 + 512 + 32 + 512 + 128, NT], BF16, "ExternalOutput")
        tm_out = k.dram("tm", [NT, 640], BF16, "ExternalOutput")
    ident_d = k.dram("ident", [128, 128], F32, "ExternalInput")

    identb, identb_b, onesb, ones_b, eps_sb, eps_b = load_consts(k, S, ident_d)

    w_sb, w_b = k.sb([128, 8, NW], BF16, "w_sb")
    for kc in range(8):
        for c0 in range(0, NW, 1024):
            c1 = min(NW, c0 + 1024)
            S.dma("pool", w_sb[:, kc, c0:c1], w_d[kc * 128:(kc + 1) * 128, c0:c1], writes=[w_b])
    if odd:
        wqb_sb, wqb_b = k.sb([128, 6, 1536], BF16, "wqb_sb")
        for kc in range(6):
            for c0 in (0, 768):
                S.dma("pool", wqb_sb[:, kc, c0:c0 + 768], wqb_d[kc * 128:(kc + 1) * 128, c0:c0 + 768], writes=[wqb_b])
        wkvb_sb, wkvb_b = k.sb([128, 2, 1024], BF16, "wkvb_sb")
        for kc in range(2):
            S.dma("pool", wkvb_sb[:, kc, :], wkvb_d[kc * 128:(kc + 1) * 128, :], writes=[wkvb_b])
        lg_sb, lg_b = k.sb([128, 8], F32, "lg_sb")
        S.dma("sp", lg_sb[:, 0:6], qn_d[:, :], writes=[lg_b])
        S.dma("sp", lg_sb[:, 6:8], kvn_d[:, :], writes=[lg_b])
        dg_sb, dg_b = k.sb([128, 4], F32, "dg_sb")
        S.dma("sp", dg_sb[:], dgC[:, :], writes=[dg_b])
        blk_sb, blk_b = k.sb([128, 128], BF16, "blk_sb")
        S.dma("pool", blk_sb[:], blk_d[:, :], writes=[blk_b])

    cond_sb, cond_b = k.sb([128, 8, 2], F32, "cond_sb")
    S.dma("sp", cond_sb[:], cond[:, :, :], writes=[cond_b])
    S.op("act", lambda e: e.activation(out=cond_sb[:], in_=cond_sb[:], func=AF.Silu), reads=[cond_b], writes=[cond_b])
    modb_sb, modb_b = k.sb([128, 16], F32, "modb_sb")
    S.dma("sp", modb_sb[:], modb[:, :], writes=[modb_b])
    gain_sb, gain_b = k.sb([128, 8], F32, "gain_sb")
    S.dma("sp", gain_sb[:], gain[:, :], writes=[gain_b])
    mv_sb, mv_b = k.sb([128, 2, 8, 2], F32, "mv_sb")
    wm, wm_b = k.sb([128, 8, 512], F32, "modw_sb")
    emit_mod_vectors(k, S, modw, modb_sb, modb_b, cond_sb, cond_b, 2, mv_sb, mv_b, Ring([(wm, wm_b)]))
    A_sb, A_b = k.sb([128, 8, 2], F32, "A_sb")
    for c in range(2):
        S.op("dve", lambda e, c=c: e.scalar_tensor_tensor(out=A_sb[:, :, c], in0=mv_sb[:, 1, :, c], scalar=1.0, in1=gain_sb[:, :], op0=ALU.add, op1=ALU.mult),
             reads=[mv_b, gain_b], writes=[A_b])

    x_sb, x_b = k.sb([128, 8, 512], F32, "x_sb")
    sq_sb, sq_b = k.sb([128, 8, 512], BF16, "sq_sb")
    rstd_sb, rstd_b = k.sb([128, 512], F32, "rstd_sb")
    t_sb, t_b = k.sb([128, 512], F32, "t_sb")
    h_sb, h_b = k.sb([128, 8, 512], BF16, "h_sb")
    rc_sb, rc_b = k.sb([128, 512], F32, "rc_sb")
    rs_sb, rs_b = k.sb([128, 512], F32, "rs_sb")
    ss_ps, ss_b = k.ps([128, 512], F32, "ss_ps")
    pring = Ring([k.ps([128, 512], F32, f"pp{i}") for i in range(5)])
    oring = Ring([k.sb([128, 512], BF16, f"ob{i}") for i in range(3)])
    u1_sb, u1_b = k.sb([128, 512], F32, "u1")
    u2_sb, u2_b = k.sb([128, 512], F32, "u2")
    vo_ring = Ring([k.sb([128, 640], BF16, f"vo{i}") for i in range(2)])
    if odd:
        rc2_sb, rc2_b = k.sb([96, 512], F32, "rc2_sb")
        rs2_sb, rs2_b = k.sb([96, 512], F32, "rs2_sb")
        rc3_sb, rc3_b = k.sb([32, 512], F32, "rc3_sb")
        rs3_sb, rs3_b = k.sb([32, 512], F32, "rs3_sb")
        cq_sb, cq_b = k.sb([128, 8, 512], F32, "cq_sb")
        cn_sb, cn_b = k.sb([128, 8, 512], BF16, "cn_sb")
        rq_sb, rq_b = k.sb([128, 512], F32, "rq_sb")
        rkv_sb, rkv_b = k.sb([128, 512], F32, "rkv_sb")
        nrm_sb, nrm_b = k.sb([128, 512], F32, "nrm_sb")

    def mm_fm(ps, ps_b, col0, ncols, T, rhs_sb=None, rhs_b=None, wsb=None, wb=None, nk=8):
        rhs_sb = h_sb if rhs_sb is None else rhs_sb
        rhs_b = h_b if rhs_b is None else rhs_b
        wsb = w_sb if wsb is None else wsb
        wb = w_b if wb is None else wb
        for kc in range(nk):
            S.op("pe", lambda e, kc=kc: e.matmul(ps[:ncols, :T], lhsT=wsb[:, kc, col0:col0 + ncols], rhs=rhs_sb[:, kc, :T],
                                                start=(kc == 0), stop=(kc == nk - 1)), reads=[wb, rhs_b], writes=[ps_b])

    def store_fm(src_fn, src_bufs, row0, nrows, t0, T, eng="act"):
        o_sb, o_b = oring.next()
        if eng == "act":
            S.op("act", lambda e: e.copy(out=o_sb[:nrows, :T], in_=src_fn()), reads=src_bufs, writes=[o_b])
        S.dma("sp", fm_out[row0:row0 + nrows, t0:t0 + T], o_sb[:nrows, :T], reads=[o_b])

    def rope_store(psA, psA_b, psB, psB_b, nrows, row0, t0, T, C, C_b, Sn, Sn_b, norm=None):
        o_sb, o_b = oring.next()
        S.op("dve", lambda e: e.tensor_tensor(out=u1_sb[:nrows, :T], in0=psA[:nrows, :T], in1=C[:nrows, :T], op=ALU.mult), reads=[psA_b, C_b], writes=[u1_b])
        S.op("dve", lambda e: e.tensor_tensor(out=u2_sb[:nrows, :T], in0=psB[:nrows, :T], in1=Sn[:nrows, :T], op=ALU.mult), reads=[psB_b, Sn_b], writes=[u2_b])
        if norm is None:
            S.op("pool", lambda e: e.tensor_tensor(out=o_sb[:nrows, :T], in0=u1_sb[:nrows, :T], in1=u2_sb[:nrows, :T], op=ALU.add), reads=[u1_b, u2_b], writes=[o_b])
        else:
            n_sb, n_b = norm
            S.op("pool", lambda e: e.tensor_tensor(out=u1_sb[:nrows, :T], in0=u1_sb[:nrows, :T], in1=u2_sb[:nrows, :T], op=ALU.add), reads=[u1_b, u2_b], writes=[u1_b])
            S.op("dve", lambda e: e.tensor_tensor(out=o_sb[:nrows, :T], in0=u1_sb[:nrows, :T], in1=n_sb[:nrows, :T], op=ALU.mult), reads=[u1_b, n_b], writes=[o_b])
        S.dma("sp", fm_out[row0:row0 + nrows, t0:t0 + T], o_sb[:nrows, :T], reads=[o_b])

    def rsqrt_from_ps(ps, ps_b, out_sb, out_b, T, scale):
        S.op("act", lambda e: e.activation(out=out_sb[:, :T], in_=ps[:, :T], func=AF.Sqrt, scale=scale, bias=eps_sb[:, 0:1]), reads=[ps_b, eps_b], writes=[out_b])
        S.op("dve", lambda e: e.reciprocal(out=out_sb[:, :T], in_=out_sb[:, :T]), reads=[out_b], writes=[out_b])

    for (t0, T, is_ctx) in token_tiles():
        c = 1 if is_ctx else 0
        S.dma("sp", x_sb[:, :, :T], xT[:, t0:t0 + T].rearrange("(kc p) t -> p kc t", p=128), writes=[x_b])
        S.dma("sp", rc_sb[:, :T], ropeC[:, t0:t0 + T], writes=[rc_b])
        S.dma("sp", rs_sb[:, :T], ropeS[:, t0:t0 + T], writes=[rs_b])
        emit_norm_tile(k, S, x_sb, x_b, T, onesb, ones_b, sq_sb, sq_b, ss_ps, ss_b, rstd_sb, rstd_b, eps_sb, eps_b)
        for kc in range(8):
            S.op("dve", lambda e, kc=kc: e.scalar_tensor_tensor(out=t_sb[:, :T], in0=x_sb[:, kc, :T], scalar=A_sb[:, kc, c:c + 1], in1=rstd_sb[:, :T],
                                                             op0=ALU.mult, op1=ALU.mult), reads=[x_b, A_b, rstd_b], writes=[t_b])
            S.op("act", lambda e, kc=kc: e.activation(out=h_sb[:, kc, :T], in_=t_sb[:, :T], func=AF.Identity, bias=mv_sb[:, 0, kc, c:c + 1], scale=1.0),
                 reads=[t_b, mv_b], writes=[h_b])
        if not odd:
            for j in range(5):
                col = j * 128
                swc = 2304 + j * 128
                pa, pa_b = pring.next()
                pb, pb_b = pring.next()
                mm_fm(pa, pa_b, col, 128, T)
                mm_fm(pb, pb_b, swc, 128, T)
                rope_store(pa, pa_b, pb, pb_b, 128, j * 128, t0, T, rc_sb, rc_b, rs_sb, rs_b)
            for j in range(8):
                col = 768 + j * 128
                pa, pa_b = pring.next()
                mm_fm(pa, pa_b, col, 128, T)
                store_fm(lambda pa=pa: pa[:, :T], [pa_b], 640 + j * 128, 128, t0, T)
            tmcols = [(640, 128, 0), (1792, 512, 128)]
        else:
            S.dma("sp", rc2_sb[:, :T], ropeC2[:, t0:t0 + T], writes=[rc2_b])
            S.dma("sp", rs2_sb[:, :T], ropeS2[:, t0:t0 + T], writes=[rs2_b])
            S.dma("sp", rc3_sb[:, :T], ropeC3[:, t0:t0 + T], writes=[rc3_b])
            S.dma("sp", rs3_sb[:, :T], ropeS3[:, t0:t0 + T], writes=[rs3_b])
            for j in range(8):
                pa, pa_b = pring.next()
                mm_fm(pa, pa_b, j * 128, 128, T)
                S.op("act", lambda e, j=j, pa=pa: e.copy(out=cq_sb[:, j, :T], in_=pa[:, :T]), reads=[pa_b], writes=[cq_b])
            S.op("act", lambda e: e.activation(out=sq_sb[:, :, :T], in_=cq_sb[:, :, :T], func=AF.Square), reads=[cq_b], writes=[sq_b])
            pq, pq_b = pring.next()
            for j in range(6):
                S.op("pe", lambda e, j=j: e.matmul(pq[:, :T], lhsT=onesb[:, :], rhs=sq_sb[:, j, :T], start=(j == 0), stop=(j == 5)), reads=[sq_b, ones_b], writes=[pq_b])
            rsqrt_from_ps(pq, pq_b, rq_sb, rq_b, T, 1.0 / 768)
            pk, pk_b = pring.next()
            for j in range(2):
                S.op("pe", lambda e, j=j: e.matmul(pk[:, :T], lhsT=onesb[:, :], rhs=sq_sb[:, 6 + j, :T], start=(j == 0), stop=(j == 1)), reads=[sq_b, ones_b], writes=[pk_b])
            rsqrt_from_ps(pk, pk_b, rkv_sb, rkv_b, T, 1.0 / 256)
            for j in range(8):
                r_sb, r_b = (rq_sb, rq_b) if j < 6 else (rkv_sb, rkv_b)
                S.op("dve", lambda e, j=j, r_sb=r_sb: e.scalar_tensor_tensor(out=cn_sb[:, j, :T], in0=cq_sb[:, j, :T], scalar=lg_sb[:, j:j + 1], in1=r_sb[:, :T], op0=ALU.mult, op1=ALU.mult),
                     reads=[cq_b, r_b, lg_b], writes=[cn_b])
            for hd in range(8):
                pa, pa_b = pring.next()
                pb, pb_b = pring.next()
                mm_fm(pa, pa_b, hd * 96, 96, T, cn_sb, cn_b, wqb_sb, wqb_b, nk=6)
                mm_fm(pb, pb_b, 768 + hd * 96, 96, T, cn_sb, cn_b, wqb_sb, wqb_b, nk=6)
                rope_store(pa, pa_b, pb, pb_b, 96, hd * 96, t0, T, rc2_sb, rc2_b, rs2_sb, rs2_b)
            for j in range(4):
                pa, pa_b = pring.next()
                for kc in range(2):
                    S.op("pe", lambda e, kc=kc, j=j, pa=pa: e.matmul(pa[:, :T], lhsT=wkvb_sb[:, kc, j * 128:(j + 1) * 128], rhs=cn_sb[:, 6 + kc, :T],
                                                                      start=(kc == 0), stop=(kc == 1)), reads=[wkvb_b, cn_b], writes=[pa_b])
                store_fm(lambda pa=pa: pa[:, :T], [pa_b], 768 + j * 128, 128, t0, T)
            pa, pa_b = pring.next()
            pb, pb_b = pring.next()
            mm_fm(pa, pa_b, 1024, 32, T)
            mm_fm(pb, pb_b, 1824, 32, T)
            rope_store(pa, pa_b, pb, pb_b, 32, 768 + 512, t0, T, rc3_sb, rc3_b, rs3_sb, rs3_b)
            for j in range(5):
                col = 1056 + j * 128
                swc = 1856 + j * 128
                pa, pa_b = pring.next()
                pb, pb_b = pring.next()
                mm_fm(pa, pa_b, col, 128, T)
                mm_fm(pb, pb_b, swc, 128, T)
                S.op("act", lambda e, pa=pa: e.activation(out=sq_sb[:, 0, :T], in_=pa[:, :T], func=AF.Square), reads=[pa_b], writes=[sq_b])
                pn, pn_b = pring.next()
                S.op("pe", lambda e, pn=pn: e.matmul(pn[:, :T], lhsT=blk_sb[:, :], rhs=sq_sb[:, 0, :T], start=True, stop=True), reads=[sq_b, blk_b], writes=[pn_b])
                rsqrt_from_ps(pn, pn_b, nrm_sb, nrm_b, T, 1.0 / 64)
                gc = 0 if j < 4 else 2
                o_sb, o_b = oring.next()
                S.op("dve", lambda e, pa=pa, gc=gc: e.scalar_tensor_tensor(out=u1_sb[:, :T], in0=pa[:, :T], scalar=dg_sb[:, gc:gc + 1], in1=rc_sb[:, :T], op0=ALU.mult, op1=ALU.mult),
                     reads=[pa_b, dg_b, rc_b], writes=[u1_b])
                S.op("dve", lambda e, pb=pb, gc=gc: e.scalar_tensor_tensor(out=u2_sb[:, :T], in0=pb[:, :T], scalar=dg_sb[:, gc + 1:gc + 2], in1=rs_sb[:, :T], op0=ALU.mult, op1=ALU.mult),
                     reads=[pb_b, dg_b, rs_b], writes=[u2_b])
                S.op("pool", lambda e: e.tensor_tensor(out=u1_sb[:, :T], in0=u1_sb[:, :T], in1=u2_sb[:, :T], op=ALU.add), reads=[u1_b, u2_b], writes=[u1_b])
                S.op("dve", lambda e, o_sb=o_sb: e.tensor_tensor(out=o_sb[:, :T], in0=u1_sb[:, :T], in1=nrm_sb[:, :T], op=ALU.mult), reads=[u1_b, nrm_b], writes=[o_b])
                S.dma("sp", fm_out[1312 + j * 128:1312 + (j + 1) * 128, t0:t0 + T], o_sb[:, :T], reads=[o_b])
            tmcols = [(1696, 128, 512)]
        for s in range(T // 128):
            vo_sb, vo_b = vo_ring.next()
            for (wc, n, oc) in tmcols:
                pa, pa_b = pring.next()
                for kc in range(8):
                    S.op("pe", lambda e, kc=kc, pa=pa, wc=wc, n=n, s=s: e.matmul(pa[:, :n], lhsT=h_sb[:, kc, s * 128:(s + 1) * 128], rhs=w_sb[:, kc, wc:wc + n],
                                                                               start=(kc == 0), stop=(kc == 7)), reads=[h_b, w_b], writes=[pa_b])
                S.op("act", lambda e, pa=pa, n=n, oc=oc, vo_sb=vo_sb: e.copy(out=vo_sb[:, oc:oc + n], in_=pa[:, :n]), reads=[pa_b], writes=[vo_b])
            if odd:
                pa, pa_b = pring.next()
                for kc in range(2):
                    S.op("pe", lambda e, kc=kc, pa=pa, s=s: e.matmul(pa[:, :512], lhsT=cn_sb[:, 6 + kc, s * 128:(s + 1) * 128], rhs=wkvb_sb[:, kc, 512:1024],
                                                                      start=(kc == 0), stop=(kc == 1)), reads=[cn_b, wkvb_b], writes=[pa_b])
                S.op("act", lambda e, pa=pa, vo_sb=vo_sb: e.copy(out=vo_sb[:, 0:512], in_=pa[:, :512]), reads=[pa_b], writes=[vo_b])
            S.dma("sp", tm_out[t0 + s * 128:t0 + (s + 1) * 128, :], vo_sb[:, :], reads=[vo_b], final=True)
    if own:
        return k.finish()
    k.end_phase()


def _maskA_np():
    m = np.zeros((128, 6, 512), np.float32)
    kl = np.arange(128)[:, None]
    ql = np.arange(128)[None, :]
    for kbrel in range(6):
        for qb in range(4):
            rel = (kbrel - 1) - qb
            if rel == -1:
                m[:, kbrel, qb * 128:(qb + 1) * 128] = (kl >= ql)
            elif rel == 0:
                m[:, kbrel, qb * 128:(qb + 1) * 128] = 1.0
            elif rel == 1:
                m[:, kbrel, qb * 128:(qb + 1) * 128] = (kl <= ql)
    return m


def _nbr_index():
    rows = SEQ // GRID_W
    out = []
    for v, tile in enumerate((0, 1, rows // 8 - 1)):
        r0 = tile * 8
        kp = np.arange(128)
        kr2, kc = kp // 64, kp % 64
        q = np.arange(512)
        qr, c = r0 + q // 64, q % 64
        rs = np.clip(qr - 4, 0, rows - 8)
        cs = np.clip(c - 8, 0, GRID_W - 16)
        valid = np.zeros((128, 8, 512), bool)
        dr = np.zeros((128, 8, 512), np.int64)
        dc = np.zeros((128, 8, 512), np.int64)
        for kbrel in range(8):
            krow = r0 - 4 + 2 * kbrel + kr2
            okr = (krow[:, None] >= rs[None, :]) & (krow[:, None] < rs[None, :] + 8) & (krow[:, None] >= 0) & (krow[:, None] < rows)
            okc = (kc[:, None] >= cs[None, :]) & (kc[:, None] < cs[None, :] + 16)
            valid[:, kbrel, :] = okr & okc
            dr[:, kbrel, :] = krow[:, None] - qr[None, :] + 7
            dc[:, kbrel, :] = kc[:, None] - c[None, :] + 15
        out.append((valid, np.clip(dr, 0, 14), np.clip(dc, 0, 30)))
    return out


_NBR = None


def nbr_index():
    global _NBR
    if _NBR is None:
        _NBR = _nbr_index()
    return _NBR


def build_p2(odd):
    k = K("p2o" if odd else "p2e")
    S = k.S
    dk2 = 96 if odd else 64
    q1T = k.dram("q1T", [2, 64, NTOK], BF16, "ExternalInput")
    k1T = k.dram("k1T", [64, NTOK], BF16, "ExternalInput")
    v1 = k.dram("v1", [NTOK, 64], BF16, "ExternalInput")
    q2T = k.dram("q2T", [2, dk2, NTOK], BF16, "ExternalInput")
    k2T = k.dram("k2T", [2, dk2, NTOK], BF16, "ExternalInput")
    v2 = k.dram("v2", [NTOK, 128], BF16, "ExternalInput")
    ident_d = k.dram("ident", [128, 128], F32, "ExternalInput")
    yT = k.dram("yT", [2, 128, NTOK], BF16, "ExternalOutput")
    identb, identb_b, onesb, ones_b, eps_sb, eps_b = load_consts(k, S, ident_d)
    if not odd:
        maskA_d = k.dram("maskA", [128, 6, 512], F32, "ExternalInput")
        sink_d = k.dram("sink", [128, 2], F32, "ExternalInput")
        rpbx_d = k.dram("rpbx", [2, 3, 128, 8, 512], F32, "ExternalInput")
        maskA, maskA_b = k.sb([128, 6, 512], BF16, "maskA_sb")
        S.dma("pool", maskA[:], maskA_d[:, :, :], writes=[maskA_b])
        esink, esink_b = k.sb([128, 2], F32, "esink_sb")
        S.dma("sp", esink[:], sink_d[:, :], writes=[esink_b])
        S.op("act", lambda e: e.activation(out=esink[:], in_=esink[:], func=AF.Exp), reads=[esink_b], writes=[esink_b])
        MB = {}
        stg, stg_b = k.sb([128, 512], F32, "stg")
        for h in range(2):
            for v in range(3):
                m_sb, m_b = k.sb([128, 8, 512], BF16, f"MB{h}{v}")
                for kb in range(8):
                    S.dma("sp", stg[:], rpbx_d[h, v, :, kb, :], writes=[stg_b])
                    S.op("act", lambda e: e.activation(out=m_sb[:, kb, :], in_=stg[:], func=AF.Exp), reads=[stg_b], writes=[m_b])
                MB[(h, v)] = (m_sb, m_b)
        nbr = nbr_index()
        needB = [[[bool(nbr[v][0][:, kb, qb * 128:(qb + 1) * 128].any()) for qb in range(4)] for kb in range(8)] for v in range(3)]
        mA = _maskA_np()
        needA = [[bool(mA[:, kb, qb * 128:(qb + 1) * 128].any()) for qb in range(4)] for kb in range(6)]

    qT_sb, qT_b = k.sb([dk2, NTOK], BF16, "qT_sb")
    kT_sb, kT_b = k.sb([dk2, NTOK], BF16, "kT_sb")
    va_sb, va_b = k.sb([128, NKB, 65], BF16, "va_sb")
    S.op("dve", lambda e: e.memset(va_sb[:, :, 64:65], 1.0), writes=[va_b])
    sring = Ring([k.ps([128, 512], F32, f"s{i}") for i in range(2)])
    O = [k.ps([128, 512], F32, f"o{i}") for i in range(4)]
    pt_ps, pt_b = k.ps([128, 512], BF16, "ptp")
    pring = Ring([k.sb([128, 512], BF16, f"pT{i}") for i in range(3)])
    den_sb, den_b = k.sb([128, 4], F32, "den")
    y_sb, y_b = k.sb([128, 4, 64], BF16, "y_sb")
    yTring = Ring([k.sb([64, 512], BF16, f"yT{i}") for i in range(2)])

    jobs = [(0, 0), (0, 1), (1, 0), (1, 1)]
    for (mx, h) in jobs:
        dk = 64 if mx == 0 else dk2
        scale = float(dk) ** -0.5
        qsrc = q1T[h] if mx == 0 else q2T[h]
        ksrc = k1T if mx == 0 else k2T[h]
        for c0 in range(0, NTOK, 4160):
            S.dma("sp", qT_sb[:dk, c0:c0 + 4160], qsrc[:, c0:c0 + 4160], writes=[qT_b])
            S.dma("sp", kT_sb[:dk, c0:c0 + 4160], ksrc[:, c0:c0 + 4160], writes=[kT_b])
        if mx == 0:
            if h == 0:
                S.dma("sp", va_sb[:, :, 0:64], v1.rearrange("(kb p) d -> p kb d", p=128), writes=[va_b])
        else:
            S.dma("sp", va_sb[:, :, 0:64], v2[:, h * 64:(h + 1) * 64].rearrange("(kb p) d -> p kb d", p=128), writes=[va_b])
        tiles = [(0, 256, None)] + [(CTX + 512 * i, 512, i) for i in range(SEQ // 512)]
        for (q0, nq, ti) in tiles:
            nqb = nq // 128
            kbl = []
            if ti is None:
                kbl = [(0, None, [True] * nqb), (1, None, [True] * nqb)]
            elif odd:
                kbl = [(kb, None, [True] * 4) for kb in range(NKB)]
            elif mx == 0:
                for kbrel in range(6):
                    lb = 4 * ti + kbrel - 1
                    if 0 <= lb < SEQ // 128:
                        kbl.append((2 + lb, (maskA, maskA_b, kbrel), needA[kbrel]))
                kbl += [(0, None, [True] * 4), (1, None, [True] * 4)]
            else:
                v = 0 if ti == 0 else (2 if ti == SEQ // 512 - 1 else 1)
                for kbrel in range(8):
                    lb = 4 * ti - 2 + kbrel
                    if 0 <= lb < SEQ // 128 and any(needB[v][kbrel]):
                        m_sb, m_b = MB[(h, v)]
                        kbl.append((2 + lb, (m_sb, m_b, kbrel), needB[v][kbrel]))
                kbl += [(0, None, [True] * 4), (1, None, [True] * 4)]
            first = [min(i for i, (_, _, nd) in enumerate(kbl) if nd[qb]) for qb in range(nqb)]
            last = [max(i for i, (_, _, nd) in enumerate(kbl) if nd[qb]) for qb in range(nqb)]
            for i, (kb, msk, nd) in enumerate(kbl):
                s_ps, s_b = sring.next()
                S.op("pe", lambda e: e.matmul(s_ps[:, :nq], lhsT=kT_sb[:dk, kb * 128:(kb + 1) * 128], rhs=qT_sb[:dk, q0:q0 + nq], start=True, stop=True),
                     reads=[kT_b, qT_b], writes=[s_b])
                p_sb, p_b = pring.next()
                S.op("act", lambda e: e.activation(out=p_sb[:, :nq], in_=s_ps[:, :nq], func=AF.Exp, scale=scale), reads=[s_b], writes=[p_b])
                if msk is not None:
                    m_sb, m_b, kbrel = msk
                    S.op("dve" if i % 2 == 0 else "pool", lambda e: e.tensor_tensor(out=p_sb[:, :nq], in0=p_sb[:, :nq], in1=m_sb[:, kbrel, :nq], op=ALU.mult),
                         reads=[p_b, m_b], writes=[p_b])
                for qb in range(nqb):
                    if nd[qb]:
                        o_ps, o_b = O[qb]
                        S.op("pe", lambda e: e.matmul(o_ps[:, 0:65], lhsT=p_sb[:, qb * 128:(qb + 1) * 128], rhs=va_sb[:, kb, :],
                                                      start=(i == first[qb]), stop=(i == last[qb])), reads=[p_b, va_b], writes=[o_b])
            yt_sb, yt_b = yTring.next()
            for qb in range(nqb):
                o_ps, o_b = O[qb]
                if (not odd) and mx == 0:
                    S.op("dve", lambda e: e.tensor_tensor(out=den_sb[:, qb:qb + 1], in0=o_ps[:, 64:65], in1=esink[:, h:h + 1], op=ALU.add), reads=[o_b, esink_b], writes=[den_b])
                    S.op("dve", lambda e: e.reciprocal(out=den_sb[:, qb:qb + 1], in_=den_sb[:, qb:qb + 1]), reads=[den_b], writes=[den_b])
                else:
                    S.op("dve", lambda e: e.reciprocal(out=den_sb[:, qb:qb + 1], in_=o_ps[:, 64:65]), reads=[o_b], writes=[den_b])
                S.op("dve", lambda e: e.tensor_scalar(out=y_sb[:, qb, :], in0=o_ps[:, 0:64], scalar1=den_sb[:, qb:qb + 1], scalar2=None, op0=ALU.mult),
                     reads=[o_b, den_b], writes=[y_b])
                S.op("pe", lambda e: e.transpose(out=pt_ps[:64, qb * 128:(qb + 1) * 128], in_=y_sb[:, qb, :], identity=identb[:, :]), reads=[y_b, identb_b], writes=[pt_b])
            S.op("dve", lambda e: e.tensor_copy(out=yt_sb[:, :nq], in_=pt_ps[:64, :nq]), reads=[pt_b], writes=[yt_b])
            S.dma("sp", yT[mx, h * 64:(h + 1) * 64, q0:q0 + nq], yt_sb[:, :nq], reads=[yt_b], final=True)
    return k.finish()


PASSES = [[0, 1, 2], [3, 4, 5], [6, 7, 8]]


def build_p3(k=None, io=None, do_final=True):
    own = k is None
    if own:
        k = K("p3")
    else:
        k.begin_phase(io)
    S = k.S
    nc = k.nc
    xT = k.dram("xT", [DM, NT], F32, "ExternalInput")
    yT = k.dram("yT", [8, 128, NT], BF16, "ExternalInput")
    cond = k.dram("cond", [128, 8, 2], F32, "ExternalInput")
    modw = k.dram("modw", [DM, 4096], F32, "ExternalInput")
    modb = k.dram("modb", [128, 32], F32, "ExternalInput")
    gain = k.dram("gain", [128, 8], F32, "ExternalInput")
    fgain = k.dram("fgain", [128, 8], F32, "ExternalInput")
    wout = k.dram("wout", [DM, DM], F32, "ExternalInput")
    rw = k.dram("rw", [DM, NEXP], F32, "ExternalInput")
    rb = k.dram("rb", [1, NEXP], F32, "ExternalInput")
    ewin = k.dram("ewin", [NEXP, DM, 2048], F32, "ExternalInput")
    ebin = k.dram("ebin", [128, NEXP, 16], F32, "ExternalInput")
    ewout = k.dram("ewout", [NEXP, DM, DM], F32, "ExternalInput")
    ebout = k.dram("ebout", [NEXP, DM], F32, "ExternalInput")
    ident_d = k.dram("ident", [128, 128], F32, "ExternalInput")
    x2T = k.dram("x2T", [DM, NT], F32, "ExternalOutput")
    xfT = k.dram("xfT", [DM, NT], F32, "ExternalOutput") if do_final else None
    x1T = k.dram("x1T", [DM, NT], F32, "Internal")
    h2T = k.dram("h2T", [DM, NT], BF16, "Internal")
    gT_h = nc.dram_tensor(f"gTd_ph{k.phase}", [NEXP, NT], F32, kind="Internal")
    gT = gT_h.ap()
    x1T_b, h2T_b, gT_b = Buf("x1T"), Buf("h2T"), Buf("gT")

    identb, identb_b, onesb, ones_b, eps_sb, eps_b = load_consts(k, S, ident_d)
    identf, identf_b = k.sb([128, 128], F32, "identf")
    S.dma("sp", identf[:], ident_d[:, :], writes=[identf_b])
    onesf, onesf_b = k.sb([1, 128], F32, "onesf")
    S.op("dve", lambda e: e.memset(onesf[:], 1.0), writes=[onesf_b])
    cond_sb, cond_b = k.sb([128, 8, 2], F32, "cond_sb")
    S.dma("sp", cond_sb[:], cond[:, :, :], writes=[cond_b])
    S.op("act", lambda e: e.activation(out=cond_sb[:], in_=cond_sb[:], func=AF.Silu), reads=[cond_b], writes=[cond_b])
    modb_sb, modb_b = k.sb([128, 32], F32, "modb_sb")
    S.dma("sp", modb_sb[:], modb[:, :], writes=[modb_b])
    gain_sb, gain_b = k.sb([128, 8], F32, "gain_sb")
    S.dma("sp", gain_sb[:], gain[:, :], writes=[gain_b])
    fg_sb, fg_b = k.sb([128, 8], F32, "fg_sb")
    S.dma("sp", fg_sb[:], fgain[:, :], writes=[fg_b])
    mv_sb, mv_b = k.sb([128, 4, 8, 2], F32, "mv_sb")
    A_sb, A_b = k.sb([128, 8, 2], F32, "A_sb")
    ebin_sb, ebin_b = k.sb([128, NEXP, 16], F32, "ebin_sb")
    S.dma("sp", ebin_sb[:], ebin[:, :, :], writes=[ebin_b])
    ebout_sb, ebout_b = k.sb([NEXP, DM], F32, "ebout_sb")
    S.dma("sp", ebout_sb[:], ebout[:, :], writes=[ebout_b])
    rstd_sb, rstd_b = k.sb([128, 512], F32, "rstd_sb")
    sq_sb, sq_b = k.sb([128, 8, 512], BF16, "sq_sb")
    x_sb, x_b = k.sb([128, 8, 512], F32, "x_sb")
    t_sb, t_b = k.sb([128, 512], F32, "t_sb")
    ss_ps, ss_b = k.ps([128, 512], F32, "ss_ps")
    pring = Ring([k.ps([128, 512], F32, f"pp{i}") for i in range(6)])
    tiles = token_tiles()

    stA = contextlib.ExitStack()
    main_stack = k.stack
    k.stack = stA
    wm, wm_b = k.sb([128, 8, 512], F32, "modw_sb")
    emit_mod_vectors(k, S, modw, modb_sb, modb_b, cond_sb, cond_b, 4, mv_sb, mv_b, Ring([(wm, wm_b)]))
    for c in range(2):
        S.op("dve", lambda e: e.scalar_tensor_tensor(out=A_sb[:, :, c], in0=mv_sb[:, 2, :, c], scalar=1.0, in1=gain_sb[:, :], op0=ALU.add, op1=ALU.mult),
             reads=[mv_b, gain_b], writes=[A_b])
    wo_sb, wo_b = k.sb([128, 8, DM], BF16, "wo_sb")
    for kc in range(8):
        S.dma("pool", wo_sb[:, kc, :], wout[kc * 128:(kc + 1) * 128, :], writes=[wo_b])
    rw_sb, rw_b = k.sb([128, 8, NEXP], F32, "rw_sb")
    S.dma("sp", rw_sb[:], rw.rearrange("(kc p) e -> p kc e", p=128), writes=[rw_b])
    rb_sb, rb_b = k.sb([1, NEXP], F32, "rb_sb")
    S.dma("sp", rb_sb[:], rb[:, :], writes=[rb_b])
    y_sb, y_b = k.sb([128, 8, 512], BF16, "y_sb")
    hf_sb, hf_b = k.sb([128, 8, 512], F32, "hf_sb")
    hb_sb, hb_b = k.sb([128, 8, 512], BF16, "hb_sb")
    lg_sb, lg_b = k.sb([128, NEXP], F32, "lg_sb")
    m8_sb, m8_b = k.sb([128, 8], F32, "m8_sb")
    mk_sb, mk_b = k.sb([128, NEXP], F32, "mk_sb")
    ex_sb, ex_b = k.sb([128, NEXP], F32, "ex_sb")
    sm_sb, sm_b = k.sb([128, 2], F32, "sm_sb")
    gt_sb, gt_b = k.sb([NEXP, 512], F32, "gt_sb")
    for (t0, T, is_ctx) in tiles:
        c = 1 if is_ctx else 0
        S.dma("sp", x_sb[:, :, :T], xT[:, t0:t0 + T].rearrange("(kc p) t -> p kc t", p=128), writes=[x_b])
        S.dma("sp", y_sb[:, :, :T], yT[:, :, t0:t0 + T].rearrange("kc p t -> p kc t"), writes=[y_b])
        for o in range(8):
            pa, pa_b = pring.next()
            for kc in range(8):
                S.op("pe", lambda e: e.matmul(pa[:, :T], lhsT=wo_sb[:, kc, o * 128:(o + 1) * 128], rhs=y_sb[:, kc, :T], start=(kc == 0), stop=(kc == 7)),
                     reads=[wo_b, y_b], writes=[pa_b])
            S.op("dve", lambda e: e.scalar_tensor_tensor(out=x_sb[:, o, :T], in0=pa[:, :T], scalar=mv_sb[:, 0, o, c:c + 1], in1=x_sb[:, o, :T], op0=ALU.mult, op1=ALU.add),
                 reads=[pa_b, mv_b, x_b], writes=[x_b])
        S.dma("sp", x1T[:, t0:t0 + T].rearrange("(kc p) t -> p kc t", p=128), x_sb[:, :, :T], reads=[x_b], writes=[x1T_b])
        emit_norm_tile(k, S, x_sb, x_b, T, onesb, ones_b, sq_sb, sq_b, ss_ps, ss_b, rstd_sb, rstd_b, eps_sb, eps_b)
        for kc in range(8):
            S.op("dve", lambda e: e.scalar_tensor_tensor(out=t_sb[:, :T], in0=x_sb[:, kc, :T], scalar=A_sb[:, kc, c:c + 1], in1=rstd_sb[:, :T], op0=ALU.mult, op1=ALU.mult),
                 reads=[x_b, A_b, rstd_b], writes=[t_b])
            S.op("act", lambda e: e.activation(out=hf_sb[:, kc, :T], in_=t_sb[:, :T], func=AF.Identity, bias=mv_sb[:, 1, kc, c:c + 1], scale=1.0),
                 reads=[t_b, mv_b], writes=[hf_b])
            S.op("pool", lambda e: e.tensor_copy(out=hb_sb[:, kc, :T], in_=hf_sb[:, kc, :T]), reads=[hf_b], writes=[hb_b])
        S.dma("sp", h2T[:, t0:t0 + T].rearrange("(kc p) t -> p kc t", p=128), hb_sb[:, :, :T], reads=[hb_b], writes=[h2T_b])
        for s in range(T // 128):
            pr, pr_b = pring.next()
            for kc in range(8):
                S.op("pe", lambda e: e.matmul(pr[:, :NEXP], lhsT=hf_sb[:, kc, s * 128:(s + 1) * 128], rhs=rw_sb[:, kc, :], start=(kc == 0), stop=False),
                     reads=[hf_b, rw_b], writes=[pr_b])
            S.op("pe", lambda e: e.matmul(pr[:, :NEXP], lhsT=onesf[0:1, :], rhs=rb_sb[0:1, :], start=False, stop=True), reads=[onesf_b, rb_b], writes=[pr_b])
            S.op("dve", lambda e: e.tensor_copy(out=lg_sb[:, :], in_=pr[:, :NEXP]), reads=[pr_b], writes=[lg_b])
            S.op("dve", lambda e: e.max(out=m8_sb[:, :], in_=lg_sb[:, :]), reads=[lg_b], writes=[m8_b])
            S.op("dve", lambda e: e.tensor_scalar(out=mk_sb[:, :], in0=lg_sb[:, :], scalar1=m8_sb[:, 3:4], scalar2=None, op0=ALU.is_ge), reads=[lg_b, m8_b], writes=[mk_b])
            S.op("dve", lambda e: e.tensor_scalar(out=sm_sb[:, 0:1], in0=m8_sb[:, 0:1], scalar1=-1.0, scalar2=None, op0=ALU.mult), reads=[m8_b], writes=[sm_b])
            S.op("act", lambda e: e.activation(out=ex_sb[:, :], in_=lg_sb[:, :], func=AF.Exp, bias=sm_sb[:, 0:1], scale=1.0), reads=[lg_b, sm_b], writes=[ex_b])
            S.op("dve", lambda e: e.tensor_tensor(out=ex_sb[:, :], in0=ex_sb[:, :], in1=mk_sb[:, :], op=ALU.mult), reads=[ex_b, mk_b], writes=[ex_b])
            S.op("dve", lambda e: e.reduce_sum(out=sm_sb[:, 1:2], in_=ex_sb[:, :], axis=AX.X), reads=[ex_b], writes=[sm_b])
            S.op("dve", lambda e: e.reciprocal(out=sm_sb[:, 1:2], in_=sm_sb[:, 1:2]), reads=[sm_b], writes=[sm_b])
            S.op("dve", lambda e: e.tensor_scalar(out=ex_sb[:, :], in0=ex_sb[:, :], scalar1=sm_sb[:, 1:2], scalar2=None, op0=ALU.mult), reads=[ex_b, sm_b], writes=[ex_b])
            pt, pt_b = pring.next()
            S.op("pe", lambda e: e.transpose(out=pt[:NEXP, :128], in_=ex_sb[:, :], identity=identf[:, :]), reads=[ex_b, identf_b], writes=[pt_b])
            S.op("act", lambda e: e.copy(out=gt_sb[:, s * 128:(s + 1) * 128], in_=pt[:NEXP, :128]), reads=[pt_b], writes=[gt_b])
        S.dma("sp", gT[:, t0:t0 + T], gt_sb[:, :T], reads=[gt_b], writes=[gT_b])
    k.stack = main_stack
    S.barrier()
    stA.close()

    for tl in PASSES:
        c0 = tiles[tl[0]][0]
        Np = sum(tiles[i][1] for i in tl)
        stB = contextlib.ExitStack()
        k.stack = stB
        hp_sb, hp_b = k.sb([128, 8, Np], BF16, "hp_sb")
        acc_sb, acc_b = k.sb([128, 8, Np], F32, "acc_sb")
        S.dma("sp", hp_sb[:], h2T[:, c0:c0 + Np].rearrange("(kc p) t -> p kc t", p=128), reads=[h2T_b], writes=[hp_b])
        gq_sb, gq_b = k.sb([NEXP, 512], F32, "gq_sb")
        for i in tl:
            t0, T, _ = tiles[i]
            S.dma("sp", gq_sb[:, :T], gT[:, t0:t0 + T], reads=[gT_b], writes=[gq_b])
            for o in range(8):
                pa, pa_b = pring.next()
                S.op("pe", lambda e: e.matmul(pa[:, :T], lhsT=ebout_sb[:, o * 128:(o + 1) * 128], rhs=gq_sb[:, :T], start=True, stop=True), reads=[ebout_b, gq_b], writes=[pa_b])
                S.op("act", lambda e: e.copy(out=acc_sb[:, o, t0 - c0:t0 - c0 + T], in_=pa[:, :T]), reads=[pa_b], writes=[acc_b])
        stE = contextlib.ExitStack()
        k.stack = stE
        wi_ring = Ring([k.sb([128, 8, 1024], BF16, f"wi{i}") for i in range(2)])
        wo_ring = Ring([k.sb([128, 4, 1024], BF16, f"wo{i}") for i in range(2)])
        act_ring = Ring([k.sb([128, 4, 512], BF16, f"ac{i}") for i in range(2)])
        g1r = Ring([k.sb([128, 512], F32, f"g1{i}") for i in range(2)])
        sgr = Ring([k.sb([128, 512], F32, f"sg{i}") for i in range(2)])
        l1r = Ring([k.sb([128, 512], F32, f"l1{i}") for i in range(2)])
        gbr = Ring([k.sb([128, 512], F32, f"gb{i}") for i in range(2)])

        wstg = Ring([k.sb([128, 1024], F32, "wstg") for i in range(4)])

        def load_pieces(he):
            e_, hf = he // 2, he % 2
            wi, wi_b = wi_ring.next()
            wo, wo_b = wo_ring.next()
            pieces = []
            for kc in range(8):
                def p_in(kc=kc):
                    st, st_b = wstg.next()
                    S.dma("sp", st[:, 0:512], ewin[e_, kc * 128:(kc + 1) * 128, hf * 512:(hf + 1) * 512], writes=[st_b])
                    S.dma("sp", st[:, 512:1024], ewin[e_, kc * 128:(kc + 1) * 128, 1024 + hf * 512:1024 + (hf + 1) * 512], writes=[st_b])
                    S.op("pool", lambda e: e.tensor_copy(out=wi[:, kc, :], in_=st[:, :]), reads=[st_b], writes=[wi_b])
                pieces.append(p_in)
            for kc in range(4):
                def p_out(kc=kc):
                    st, st_b = wstg.next()
                    r0 = hf * 512 + kc * 128
                    S.dma("sp", st[:, :], ewout[e_, r0:r0 + 128, :], writes=[st_b])
                    S.op("pool", lambda e: e.tensor_copy(out=wo[:, kc, :], in_=st[:, :]), reads=[st_b], writes=[wo_b])
                pieces.append(p_out)
            return (wi, wi_b, wo, wo_b), pieces

        def load_w(he):
            bufs, pieces = load_pieces(he)
            for p in pieces:
                p()
            return bufs

        nxt = load_w(0)
        for he in range(2 * NEXP):
            e_, hf = he // 2, he % 2
            wi, wi_b, wo, wo_b = nxt
            pend = []
            if he + 1 < 2 * NEXP:
                nxt, pend = load_pieces(he + 1)
            for i in tl:
                t0, T, _ = tiles[i]
                lo = t0 - c0
                gb, gb_b = gbr.next()
                S.dma("sp", gb[:, :T], bass.AP(gT_h, e_ * NT + t0, [[0, 128], [1, T]]), reads=[gT_b], writes=[gb_b])
                ac, ac_b = act_ring.next()
                for dc in range(4):
                    pg, pg_b = pring.next()
                    pl, pl_b = pring.next()
                    for kc in range(8):
                        S.op("pe", lambda e: e.matmul(pg[:, :T], lhsT=wi[:, kc, dc * 128:(dc + 1) * 128], rhs=hp_sb[:, kc, lo:lo + T], start=(kc == 0), stop=(kc == 7)),
                             reads=[wi_b, hp_b], writes=[pg_b])
                    for kc in range(8):
                        S.op("pe", lambda e: e.matmul(pl[:, :T], lhsT=wi[:, kc, 512 + dc * 128:512 + (dc + 1) * 128], rhs=hp_sb[:, kc, lo:lo + T], start=(kc == 0), stop=(kc == 7)),
                             reads=[wi_b, hp_b], writes=[pl_b])
                    gi = hf * 4 + dc
                    li = 8 + hf * 4 + dc
                    g1, g1_b = g1r.next()
                    sg, sg_b = sgr.next()
                    l1, l1_b = l1r.next()
                    S.op("dve", lambda e: e.tensor_scalar(out=g1[:, :T], in0=pg[:, :T], scalar1=ebin_sb[:, e_, gi:gi + 1], scalar2=7.0, op0=ALU.add, op1=ALU.min), reads=[pg_b, ebin_b], writes=[g1_b])
                    S.op("act", lambda e: e.activation(out=sg[:, :T], in_=g1[:, :T], func=AF.Sigmoid, scale=1.702), reads=[g1_b], writes=[sg_b])
                    S.op("dve", lambda e: e.tensor_scalar(out=l1[:, :T], in0=pl[:, :T], scalar1=ebin_sb[:, e_, li:li + 1], scalar2=7.0, op0=ALU.add, op1=ALU.min), reads=[pl_b, ebin_b], writes=[l1_b])
                    S.op("pool", lambda e: e.tensor_scalar(out=l1[:, :T], in0=l1[:, :T], scalar1=-7.0, scalar2=1.0, op0=ALU.max, op1=ALU.add), reads=[l1_b], writes=[l1_b])
                    S.op("pool", lambda e: e.tensor_tensor(out=g1[:, :T], in0=g1[:, :T], in1=sg[:, :T], op=ALU.mult), reads=[g1_b, sg_b], writes=[g1_b])
                    S.op("dve", lambda e: e.tensor_tensor(out=g1[:, :T], in0=g1[:, :T], in1=l1[:, :T], op=ALU.mult), reads=[g1_b, l1_b], writes=[g1_b])
                    S.op("dve", lambda e: e.tensor_tensor(out=ac[:, dc, :T], in0=g1[:, :T], in1=gb[:, :T], op=ALU.mult), reads=[g1_b, gb_b], writes=[ac_b])
                    if pend:
                        pend.pop(0)()
                for o in range(8):
                    py, py_b = pring.next()
                    for kc in range(4):
                        S.op("pe", lambda e: e.matmul(py[:, :T], lhsT=wo[:, kc, o * 128:(o + 1) * 128], rhs=ac[:, kc, :T], start=(kc == 0), stop=(kc == 3)),
                             reads=[wo_b, ac_b], writes=[py_b])
                    S.op("dve", lambda e: e.tensor_tensor(out=acc_sb[:, o, lo:lo + T], in0=py[:, :T], in1=acc_sb[:, o, lo:lo + T], op=ALU.add), reads=[py_b, acc_b], writes=[acc_b])
            while pend:
                pend.pop(0)()
        k.stack = stB
        S.barrier()
        stE.close()
        ob_ring = Ring([k.sb([128, 8, 512], F32, f"ob{i}") for i in range(2)])
        for i in tl:
            t0, T, is_ctx = tiles[i]
            c = 1 if is_ctx else 0
            lo = t0 - c0
            S.dma("sp", x_sb[:, :, :T], x1T[:, t0:t0 + T].rearrange("(kc p) t -> p kc t", p=128), reads=[x1T_b], writes=[x_b])
            for o in range(8):
                S.op("dve", lambda e: e.scalar_tensor_tensor(out=x_sb[:, o, :T], in0=acc_sb[:, o, lo:lo + T], scalar=mv_sb[:, 3, o, c:c + 1], in1=x_sb[:, o, :T], op0=ALU.mult, op1=ALU.add),
                     reads=[acc_b, mv_b, x_b], writes=[x_b])
            S.dma("sp", x2T[:, t0:t0 + T].rearrange("(kc p) t -> p kc t", p=128), x_sb[:, :, :T], reads=[x_b], final=True)
            if not do_final:
                continue
            emit_norm_tile(k, S, x_sb, x_b, T, onesb, ones_b, sq_sb, sq_b, ss_ps, ss_b, rstd_sb, rstd_b, eps_sb, eps_b)
            ob, ob_b = ob_ring.next()
            for o in range(8):
                S.op("dve", lambda e: e.scalar_tensor_tensor(out=ob[:, o, :T], in0=x_sb[:, o, :T], scalar=fg_sb[:, o:o + 1], in1=rstd_sb[:, :T], op0=ALU.mult, op1=ALU.mult),
                     reads=[x_b, fg_b, rstd_b], writes=[ob_b])
            S.dma("sp", xfT[:, t0:t0 + T].rearrange("(kc p) t -> p kc t", p=128), ob[:, :, :T], reads=[ob_b], final=True)
        k.stack = main_stack
        S.barrier()
        stB.close()
    if own:
        return k.finish()
    k.end_phase()


def emit_p2f(k, io, odd):
    k.begin_phase(io)
    S = k.S
    FR = 1952 if odd else 1664
    fm = io["fm"]
    GK = io["GK"]
    GV = io["GV"]
    tm = io["tm"]
    yT = io["yT"]
    ident_d = io["ident"]
    identb, identb_b, onesb, ones_b, eps_sb, eps_b = load_consts(k, S, ident_d)
    NB = 130 if odd else 50
    NCOL = NB * 128
    dkmax = 96 if odd else 64
    qT_sb, qT_b = k.sb([dkmax, NT], BF16, "qT_sb")
    kT_sb, kT_b = k.sb([dkmax, NCOL], BF16, "kT_sb")
    va_sb, va_b = k.sb([128, NB, 65], BF16, "va_sb")
    S.op("dve", lambda e: e.memset(va_sb[:, :, 64:65], 1.0), writes=[va_b])
    sring = Ring([k.ps([128, 512], F32, "s") for i in range(2)])
    O = [k.ps([128, 512], F32, "o") for i in range(4)]
    pt_ps, pt_b = k.ps([128, 512], BF16, "ptp")
    pring = Ring([k.sb([128, 512], BF16, "pT") for i in range(3)])
    den_sb, den_b = k.sb([128, 4], F32, "den")
    y_sb, y_b = k.sb([128, 4, 64], BF16, "y_sb")
    yTring = Ring([k.sb([64, 512], BF16, "yT") for i in range(2)])
    if not odd:
        maskA, maskA_b = k.sb([128, 6, 512], BF16, "maskA_sb")
        S.dma("pool", maskA[:], io["maskA"][:, :, :], writes=[maskA_b])
        candA, candA_b = k.sb([128, 8, 512], BF16, "candA_sb")
        S.dma("pool", candA[:], io["candA"][:, :, :], writes=[candA_b])
        selB, selB_b = k.sb([128, 8], F32, "selB_sb")
        S.dma("sp", selB[:], io["selB"][:, :], writes=[selB_b])
        wvar, wvar_b = k.sb([128, 4], F32, "wvar_sb")
        S.dma("sp", wvar[:], io["wvar"][:, :], writes=[wvar_b])
        esink, esink_b = k.sb([128, 8], F32, "esink_sb")
        S.dma("sp", esink[:], io["sink"][:, :], writes=[esink_b])
        S.op("act", lambda e: e.activation(out=esink[:], in_=esink[:], func=AF.Exp), reads=[esink_b], writes=[esink_b])
        stg_ring = Ring([k.sb([128, 512], F32, "stg") for i in range(2)])
        Mv = [k.sb([128, 8, 512], BF16, f"Mv{v}") for v in range(3)]
        MT0, MT0_b = k.sb([128, 8, 512], BF16, "MT0")
        MT7, MT7_b = k.sb([128, 8, 512], BF16, "MT7")
        tmpM, tmpM_b = k.sb([128, 8, 512], BF16, "tmpM")
        nbr = nbr_index()
        needB = [[any(bool(nbr[v][0][:, kb, qb * 128:(qb + 1) * 128].any()) for v in range(3)) for qb in range(4)] for kb in range(8)]
        mA = _maskA_np()
        needA = [[bool(mA[:, kb, qb * 128:(qb + 1) * 128].any()) for qb in range(4)] for kb in range(6)]

    def hb(r, which, b):
        return 34 + r * 4 + which * 2 + b

    def gv_rows(r, t0, n):
        out = []
        for (c0, cn, ap) in GV:
            a, b = max(t0, c0), min(t0 + n, c0 + cn)
            if a < b:
                out.append((ap[r * cn + (a - c0):r * cn + (b - c0), :], a, b - a))
        return out

    def load_v(dst_blk0, r, t0, n, vcol):
        for (ap, a, m) in gv_rows(r, t0, n):
            b0 = dst_blk0 + (a - t0) // 128
            S.dma("sp", va_sb[:, b0:b0 + m // 128, 0:64], ap[:, vcol:vcol + 64].rearrange("(kb p) d -> p kb d", p=128), writes=[va_b])

    def load_kv(krows, dk_parts, vcol):
        for (row0, nr, p0) in krows:
            gap, gn = GK[row0]
            assert gn == nr
            S.dma("sp", kT_sb[p0:p0 + nr, 0:CTX], fm[row0:row0 + nr, 0:CTX], writes=[kT_b])
            if odd:
                for r in range(4):
                    S.dma("sp", kT_sb[p0:p0 + nr, CTX + r * LAT_PC:CTX + (r + 1) * LAT_PC], gap[r * nr:(r + 1) * nr, CTX:NT], writes=[kT_b])
            else:
                S.dma("sp", kT_sb[p0:p0 + nr, CTX:NT], fm[row0:row0 + nr, CTX:NT], writes=[kT_b])
                for r in range(4):
                    S.dma("sp", kT_sb[p0:p0 + nr, NT + r * 512:NT + r * 512 + 256], gap[r * nr:(r + 1) * nr, NT - 256:NT], writes=[kT_b])
                    S.dma("sp", kT_sb[p0:p0 + nr, NT + r * 512 + 256:NT + r * 512 + 512], gap[r * nr:(r + 1) * nr, CTX:CTX + 256], writes=[kT_b])
        S.dma("sp", va_sb[:, 0:2, 0:64], tm[0:CTX, vcol:vcol + 64].rearrange("(kb p) d -> p kb d", p=128), writes=[va_b])
        if odd:
            for r in range(4):
                load_v(2 + r * 32, r, CTX, LAT_PC, vcol)
        else:
            S.dma("sp", va_sb[:, 2:34, 0:64], tm[CTX:NT, vcol:vcol + 64].rearrange("(kb p) d -> p kb d", p=128), writes=[va_b])
            for r in range(4):
                load_v(34 + r * 4, r, NT - 256, 256, vcol)
                load_v(36 + r * 4, r, CTX, 256, vcol)

    if odd:
        jobs = [("C", h) for h in range(8)] + [("D", h) for h in range(8)]
    else:
        jobs = [("A", h) for h in range(8)] + [("B", h) for h in range(8)]
    for (kind, h) in jobs:
        g = h // 4
        if kind == "A":
            dk, qrow, ychunk = 64, h * 64, h // 2
            if h % 4 == 0:
                load_kv([(512 + g * 64, 64, 0)], 64, g * 64)
        elif kind == "B":
            dk, qrow, ychunk = 64, 640 + h * 64, 4 + h // 2
            load_kv([(1152 + h * 64, 64, 0)], 64, 128 + h * 64)
            for v in range(3):
                for kb in range(8):
                    stg, stg_b = stg_ring.next()
                    S.dma("sp", stg[:], io["rpbx"][h, v, :, kb, :], writes=[stg_b])
                    S.op("act", lambda e: e.activation(out=Mv[v][0][:, kb, :], in_=stg[:], func=AF.Exp), reads=[stg_b], writes=[Mv[v][1]])
            S.op("dve", lambda e: e.tensor_scalar(out=tmpM[:], in0=Mv[1][0][:], scalar1=wvar[:, 1:2], scalar2=None, op0=ALU.mult), reads=[Mv[1][1], wvar_b], writes=[tmpM_b])
            S.op("dve", lambda e: e.scalar_tensor_tensor(out=MT0[:], in0=Mv[0][0][:], scalar=wvar[:, 0:1], in1=tmpM[:], op0=ALU.mult, op1=ALU.add), reads=[Mv[0][1], wvar_b, tmpM_b], writes=[MT0_b])
            S.op("dve", lambda e: e.tensor_scalar(out=tmpM[:], in0=Mv[1][0][:], scalar1=wvar[:, 3:4], scalar2=None, op0=ALU.mult), reads=[Mv[1][1], wvar_b], writes=[tmpM_b])
            S.op("dve", lambda e: e.scalar_tensor_tensor(out=MT7[:], in0=Mv[2][0][:], scalar=wvar[:, 2:3], in1=tmpM[:], op0=ALU.mult, op1=ALU.add), reads=[Mv[2][1], wvar_b, tmpM_b], writes=[MT7_b])
        elif kind == "C":
            dk, qrow, ychunk = 96, h * 96, h // 2
            load_kv([(768 + h * 64, 64, 0), (1280, 32, 64)], 96, h * 64)
        else:
            dk, qrow, ychunk = 64, 1312 + h * 64, 4 + h // 2
            if h % 4 == 0:
                load_kv([(1824 + g * 64, 64, 0)], 64, 512 + g * 64)
        scale = float(dk) ** -0.5
        S.dma("sp", qT_sb[:dk, :], fm[qrow:qrow + dk, :], writes=[qT_b])
        tiles = [(0, 256, None)] + [(CTX + 512 * i, 512, i) for i in range(LAT_PC // 512)]
        for (q0, nq, ti) in tiles:
            nqb = nq // 128
            kbl = []
            allq = [True] * nqb
            if ti is None:
                kbl = [(0, None, None, None, allq), (1, None, None, None, allq)]
            elif odd:
                kbl = [(kb, None, None, None, allq) for kb in range(NB)]
            elif kind == "A":
                for kbrel in range(6):
                    lb = 4 * ti + kbrel - 1
                    if 0 <= lb < 32:
                        kbl.append((2 + lb, maskA[:, kbrel, :], maskA_b, None, needA[kbrel]))
                    elif lb < 0:
                        for r in range(4):
                            kbl.append((hb(r, 0, 1), candA[:, r, :], candA_b, None, needA[kbrel]))
                    else:
                        for r in range(4):
                            kbl.append((hb(r, 1, 0), candA[:, 4 + r, :], candA_b, None, needA[kbrel]))
                kbl += [(0, None, None, None, allq), (1, None, None, None, allq)]
            else:
                M, M_b = (MT0, MT0_b) if ti == 0 else ((MT7, MT7_b) if ti == 7 else Mv[1])
                for kbrel in range(8):
                    lb = 4 * ti - 2 + kbrel
                    if not any(needB[kbrel]):
                        continue
                    if 0 <= lb < 32:
                        kbl.append((2 + lb, M[:, kbrel, :], M_b, None, needB[kbrel]))
                    elif lb < 0:
                        for r in range(4):
                            kbl.append((hb(r, 0, lb + 2), M[:, kbrel, :], M_b, selB[:, r:r + 1], needB[kbrel]))
                    else:
                        for r in range(4):
                            kbl.append((hb(r, 1, lb - 32), M[:, kbrel, :], M_b, selB[:, 4 + r:5 + r], needB[kbrel]))
                kbl += [(0, None, None, None, allq), (1, None, None, None, allq)]
            first = [min(i for i, ent in enumerate(kbl) if ent[4][qb]) for qb in range(nqb)]
            last = [max(i for i, ent in enumerate(kbl) if ent[4][qb]) for qb in range(nqb)]
            for i, (kb, mask_ap, mask_b, scal, nd) in enumerate(kbl):
                s_ps, s_b = sring.next()
                S.op("pe", lambda e: e.matmul(s_ps[:, :nq], lhsT=kT_sb[:dk, kb * 128:(kb + 1) * 128], rhs=qT_sb[:dk, q0:q0 + nq], start=True, stop=True),
                     reads=[kT_b, qT_b], writes=[s_b])
                p_sb, p_b = pring.next()
                S.op("act", lambda e: e.activation(out=p_sb[:, :nq], in_=s_ps[:, :nq], func=AF.Exp, scale=scale), reads=[s_b], writes=[p_b])
                if mask_ap is not None:
                    if scal is None:
                        S.op("dve" if i % 2 == 0 else "pool", lambda e: e.tensor_tensor(out=p_sb[:, :nq], in0=p_sb[:, :nq], in1=mask_ap, op=ALU.mult),
                             reads=[p_b, mask_b], writes=[p_b])
                    else:
                        S.op("dve", lambda e: e.scalar_tensor_tensor(out=p_sb[:, :nq], in0=p_sb[:, :nq], scalar=scal, in1=mask_ap, op0=ALU.mult, op1=ALU.mult),
                             reads=[p_b, mask_b, selB_b], writes=[p_b])
                for qb in range(nqb):
                    if nd[qb]:
                        o_ps, o_b = O[qb]
                        S.op("pe", lambda e: e.matmul(o_ps[:, 0:65], lhsT=p_sb[:, qb * 128:(qb + 1) * 128], rhs=va_sb[:, kb, :],
                                                      start=(i == first[qb]), stop=(i == last[qb])), reads=[p_b, va_b], writes=[o_b])
            yt_sb, yt_b = yTring.next()
            for qb in range(nqb):
                o_ps, o_b = O[qb]
                if kind == "A":
                    S.op("dve", lambda e: e.tensor_tensor(out=den_sb[:, qb:qb + 1], in0=o_ps[:, 64:65], in1=esink[:, h:h + 1], op=ALU.add), reads=[o_b, esink_b], writes=[den_b])
                    S.op("dve", lambda e: e.reciprocal(out=den_sb[:, qb:qb + 1], in_=den_sb[:, qb:qb + 1]), reads=[den_b], writes=[den_b])
                else:
                    S.op("dve", lambda e: e.reciprocal(out=den_sb[:, qb:qb + 1], in_=o_ps[:, 64:65]), reads=[o_b], writes=[den_b])
                S.op("dve", lambda e: e.tensor_scalar(out=y_sb[:, qb, :], in0=o_ps[:, 0:64], scalar1=den_sb[:, qb:qb + 1], scalar2=None, op0=ALU.mult),
                     reads=[o_b, den_b], writes=[y_b])
                S.op("pe", lambda e: e.transpose(out=pt_ps[:64, qb * 128:(qb + 1) * 128], in_=y_sb[:, qb, :], identity=identb[:, :]), reads=[y_b, identb_b], writes=[pt_b])
            S.op("dve", lambda e: e.tensor_copy(out=yt_sb[:, :nq], in_=pt_ps[:64, :nq]), reads=[pt_b], writes=[yt_b])
            S.dma("sp", yT[ychunk, (h % 2) * 64:(h % 2) * 64 + 64, q0:q0 + nq], yt_sb[:, :nq], reads=[yt_b])
    k.end_phase()


def build_fused(depth=DEPTH):
    k = K("fused")
    k.fused = True
    S = k.S
    nc = k.nc
    EI = "ExternalInput"
    g = {}
    g["xT0"] = k.dram("xT0", [DM, NT], F32, EI)
    for nm, shp in (("cond", [128, 8, 2]), ("ident", [128, 128]), ("blk", [128, 128]), ("ropeC", [128, NT]), ("ropeS", [128, NT]),
                    ("ropeC2", [96, NT]), ("ropeS2", [96, NT]), ("ropeC3", [32, NT]), ("ropeS3", [32, NT]), ("fgain", [128, 8]),
                    ("maskA", [128, 6, 512]), ("candA", [128, 8, 512]), ("selB", [128, 8]), ("wvar", [128, 4])):
        g[nm] = k.dram(nm, shp, F32, EI)
    L = []
    for l in range(depth):
        odd = l % 2 == 1
        d = {}
        d["modw"] = k.dram(f"modw{l}", [DM, 6144], F32, EI)
        d["modb"] = k.dram(f"modb{l}", [128, 48], F32, EI)
        d["gmix"] = k.dram(f"gmix{l}", [128, 8], F32, EI)
        d["gffn"] = k.dram(f"gffn{l}", [128, 8], F32, EI)
        d["w"] = k.dram(f"w{l}", [DM, (1824 + 32 + 640) if odd else (2304 + 640)], F32, EI)
        if odd:
            d["wqb"] = k.dram(f"wqb{l}", [768, 1536], F32, EI)
            d["wkvb"] = k.dram(f"wkvb{l}", [256, 1024], F32, EI)
            d["qn"] = k.dram(f"qn{l}", [128, 6], F32, EI)
            d["kvn"] = k.dram(f"kvn{l}", [128, 2], F32, EI)
            d["dgq"] = k.dram(f"dgq{l}", [128, 4], F32, EI)
        else:
            d["sink"] = k.dram(f"sink{l}", [128, 8], F32, EI)
            d["rpbx"] = k.dram(f"rpbx{l}", [8, 3, 128, 8, 512], F32, EI)
        d["wout"] = k.dram(f"wout{l}", [DM, DM], F32, EI)
        d["rw"] = k.dram(f"rw{l}", [DM, NEXP], F32, EI)
        d["rb"] = k.dram(f"rb{l}", [1, NEXP], F32, EI)
        d["ewin"] = k.dram(f"ewin{l}", [NEXP, DM, 2048], F32, EI)
        d["ebin"] = k.dram(f"ebin{l}", [128, NEXP, 16], F32, EI)
        d["ewout"] = k.dram(f"ewout{l}", [NEXP, DM, DM], F32, EI)
        d["ebout"] = k.dram(f"ebout{l}", [NEXP, DM], F32, EI)
        L.append(d)
    out = k.dram("out", [DM, NT], F32, "ExternalOutput")
    XA = k.dram("XA", [DM, NT], F32, "Internal")
    XB = k.dram("XB", [DM, NT], F32, "Internal")
    fmE = k.dram("fmE", [1664, NT], BF16, "Internal")
    fmO = k.dram("fmO", [1952, NT], BF16, "Internal")
    tmE = k.dram("tmE", [NT, 640], BF16, "Internal")
    tmO = k.dram("tmO", [NT, 640], BF16, "Internal")
    def kpieces(odd):
        rows = [(768 + 64 * i, 64) for i in range(8)] + [(1280, 32)] + [(1824, 64), (1888, 64)] if odd else \
               [(512, 64), (576, 64)] + [(1152 + 64 * i, 64) for i in range(8)]
        return rows
    vpieces = [(c * 512, min(512, NT - c * 512)) for c in range((NT + 511) // 512)]
    GKs, GVs = {}, {}
    for par, tag in ((False, "E"), (True, "O")):
        GKs[par] = {r0: (k.dram(f"gk{tag}{r0}", [4 * n, NT], BF16, "Internal"), n) for (r0, n) in kpieces(par)}
        GVs[par] = [(t0, n, k.dram(f"gv{tag}{t0}", [4 * n, 640], BF16, "Internal")) for (t0, n) in vpieces]
    yTd = k.dram("yTd", [8, 128, NT], BF16, "Internal")
    groups = [[0, 1, 2, 3], [4, 5, 6, 7]]
    ccscr, _ = k.sb([1, 4], F32, "ccscr")
    xs = [g["xT0"], XA, XB]
    for l in range(depth):
        odd = l % 2 == 1
        d = L[l]
        xin = xs[0] if l == 0 else xs[1 + (l - 1) % 2]
        xout = xs[1 + l % 2]
        fm_, tm_ = (fmO, tmO) if odd else (fmE, tmE)
        io = {"xT": xin, "cond": g["cond"], "modw": d["modw"][:, 0:2048], "modb": d["modb"][:, 0:16], "gain": d["gmix"],
              "ropeC": g["ropeC"], "ropeS": g["ropeS"], "w": d["w"], "fm": fm_, "tm": tm_, "ident": g["ident"]}
        if odd:
            io.update({"wqb": d["wqb"], "wkvb": d["wkvb"], "qn": d["qn"], "kvn": d["kvn"], "dgq": d["dgq"], "ropeC2": g["ropeC2"],
                       "ropeS2": g["ropeS2"], "ropeC3": g["ropeC3"], "ropeS3": g["ropeS3"], "blk": g["blk"]})
        build_p1(odd, k, io)
        for r0, (gap, n) in GKs[odd].items():
            S.cc(k.stack, lambda e: e.collective_compute("AllGather", ALU.bypass, replica_groups=groups, ins=[fm_[r0:r0 + n, :]], outs=[gap]), ccscr[0:1, 0:4])
        for (t0, n, gap) in GVs[odd]:
            S.cc(k.stack, lambda e: e.collective_compute("AllGather", ALU.bypass, replica_groups=groups, ins=[tm_[t0:t0 + n, :]], outs=[gap]), ccscr[0:1, 0:4])
        S.barrier()
        io2 = {"fm": fm_, "tm": tm_, "GK": GKs[odd], "GV": GVs[odd], "yT": yTd, "ident": g["ident"]}
        if not odd:
            io2.update({"maskA": g["maskA"], "candA": g["candA"], "selB": g["selB"], "wvar": g["wvar"], "sink": d["sink"], "rpbx": d["rpbx"]})
        emit_p2f(k, io2, odd)
        last = l == depth - 1
        io3 = {"xT": xin, "yT": yTd, "cond": g["cond"], "modw": d["modw"][:, 2048:6144], "modb": d["modb"][:, 16:48], "gain": d["gffn"],
               "fgain": g["fgain"], "wout": d["wout"], "rw": d["rw"], "rb": d["rb"], "ewin": d["ewin"], "ebin": d["ebin"],
               "ewout": d["ewout"], "ebout": d["ebout"], "ident": g["ident"], "x2T": xout, "xfT": out}
        build_p3(k, io3, do_final=last)
    return k.finish()


def kernel(x, c, ctx, c_ctx, mod_w, mod_b, norm_mix, norm_ffn, ab_w_in, ab_w_out, a_sink, b_rpb,
                 cd_w_in, c_q_norm, c_w_q_b, c_kv_norm, c_w_kv_b, d_q_norm, d_k_norm, cd_w_out,
                 router_w, router_b, exp_w_in, exp_b_in, exp_w_out, exp_b_out, final_norm, _depth=DEPTH):
    f32 = lambda a: np.ascontiguousarray(np.asarray(a, np.float32))
    x, c, ctx, c_ctx = f32(x), f32(c), f32(ctx), f32(c_ctx)
    if ("fused", _depth) not in _PROGS:
        _PROGS[("fused", _depth)] = build_fused(_depth)
    nc = _PROGS[("fused", _depth)]
    Ch, Sh = _rope_tables(64, [d for _ in range(2) for d in range(64)])
    Cm, Sm = _rope_tables(32, [-1] * 64 + list(range(32)))
    C3, S3 = _rope_tables(32, list(range(32)))
    mA = _maskA_np()
    shared = {"ident": np.eye(128, dtype=np.float32), "blk": np.kron(np.eye(2, dtype=np.float32), np.ones((64, 64), np.float32)),
              "fgain": fm(final_norm, 8), "maskA": mA}
    for l in range(_depth):
        i = l // 2
        odd = l % 2 == 1
        shared[f"modw{l}"] = f32(mod_w[l])
        shared[f"modb{l}"] = fm(f32(mod_b[l]), 48)
        shared[f"gmix{l}"] = fm(norm_mix[l], 8)
        shared[f"gffn{l}"] = fm(norm_ffn[l], 8)
        if not odd:
            w = f32(ab_w_in[i])
            shared[f"w{l}"] = np.ascontiguousarray(np.concatenate([w, _swap_cols(w, 0, 10, 64, 0, 64)], axis=1))
            shared[f"sink{l}"] = np.ascontiguousarray(np.tile(f32(a_sink[i])[None, :], (128, 1)))
            rp = f32(b_rpb[i])
            rx = np.empty((8, 3, 128, 8, 512), np.float32)
            for hh in range(8):
                for v, (valid, dr, dc) in enumerate(nbr_index()):
                    rx[hh, v] = np.where(valid, rp[hh][dr, dc], np.float32(-30000.0))
            shared[f"rpbx{l}"] = rx
            shared[f"wout{l}"] = f32(ab_w_out[i])
        else:
            w = f32(cd_w_in[i])
            shared[f"w{l}"] = np.ascontiguousarray(np.concatenate([w, _swap_cols(w, 1024, 1, 32, 0, 32), _swap_cols(w, 1056, 10, 64, 0, 64)], axis=1))
            wq = f32(c_w_q_b[i])
            shared[f"wqb{l}"] = np.ascontiguousarray(np.concatenate([wq, _swap_cols(wq, 0, 8, 96, 64, 32)], axis=1))
            wkv = f32(c_w_kv_b[i]).reshape(256, 8, 128)
            shared[f"wkvb{l}"] = np.ascontiguousarray(np.concatenate([wkv[:, :, :64].reshape(256, 512), wkv[:, :, 64:].reshape(256, 512)], axis=1))
            shared[f"qn{l}"] = fm(c_q_norm[i], 6)
            shared[f"kvn{l}"] = fm(c_kv_norm[i], 2)
            gq, gk = f32(d_q_norm[i]), f32(d_k_norm[i])
            sw = (np.arange(64) + 32) % 64
            shared[f"dgq{l}"] = np.ascontiguousarray(np.stack([np.tile(gq, 2), np.tile(gq[sw], 2), np.tile(gk, 2), np.tile(gk[sw], 2)], axis=1))
            shared[f"wout{l}"] = f32(cd_w_out[i])
        shared[f"rw{l}"] = f32(router_w[l])
        shared[f"rb{l}"] = f32(router_b[l])[None, :]
        shared[f"ewin{l}"] = f32(exp_w_in[l])
        shared[f"ebin{l}"] = np.ascontiguousarray(f32(exp_b_in[l]).reshape(NEXP, 16, 128).transpose(2, 0, 1))
        shared[f"ewout{l}"] = f32(exp_w_out[l])
        shared[f"ebout{l}"] = f32(exp_b_out[l])
    ins = []
    for core in range(NCORES):
        b, r = core // 4, core % 4
        d = dict(shared)
        tok = np.concatenate([ctx[b], x[b, r * LAT_PC:(r + 1) * LAT_PC]], axis=0)
        d["xT0"] = np.ascontiguousarray(tok.T)
        d["cond"] = np.ascontiguousarray(np.stack([fm(c[b], 8), fm(c_ctx, 8)], axis=-1))
        d["ropeC"], d["ropeS"] = _tabs_for_core(Ch, Sh, r)
        d["ropeC2"], d["ropeS2"] = _tabs_for_core(Cm, Sm, r)
        d["ropeC3"], d["ropeS3"] = _tabs_for_core(C3, S3, r)
        cand = np.zeros((128, 8, 512), np.float32)
        sel = np.zeros((128, 8), np.float32)
        for rr in range(4):
            if rr == r - 1:
                cand[:, rr, :] = mA[:, 0, :]
                sel[:, rr] = 1.0
            if rr == r + 1:
                cand[:, 4 + rr, :] = mA[:, 5, :]
                sel[:, 4 + rr] = 1.0
        d["candA"], d["selB"] = cand, sel
        wv = np.zeros((128, 4), np.float32)
        wv[:, 0] = 1.0 if r == 0 else 0.0
        wv[:, 1] = 0.0 if r == 0 else 1.0
        wv[:, 2] = 1.0 if r == 3 else 0.0
        wv[:, 3] = 0.0 if r == 3 else 1.0
        d["wvar"] = wv
        ins.append(d)
    res = run_bass_kernel_spmd(nc, ins, core_ids=list(range(NCORES))).results
    out = np.empty((BATCH, SEQ, DM), np.float32)
    for core in range(NCORES):
        b, r = core // 4, core % 4
        out[b, r * LAT_PC:(r + 1) * LAT_PC] = res[core]["out"][:, CTX:].T
    return out


_PROGS = {}
BF = ml_dtypes.bfloat16


def _prog(name):
    if name not in _PROGS:
        if name == "p1e":
            _PROGS[name] = build_p1(False)
        elif name == "p1o":
            _PROGS[name] = build_p1(True)
        elif name == "p2e":
            _PROGS[name] = build_p2(False)
        elif name == "p2o":
            _PROGS[name] = build_p2(True)
        else:
            _PROGS[name] = build_p3()
    return _PROGS[name]


def _run(name, in_maps):
    res = run_bass_kernel_spmd(_prog(name), in_maps, core_ids=list(range(NCORES)))
    return res.results


def fm(v, n):
    return np.ascontiguousarray(np.asarray(v, np.float32).reshape(n, 128).T)


def _rope_tables(dim, rows_pattern):
    t = np.arange(SEQ, dtype=np.int32)
    row = (t // GRID_W).astype(np.float32)
    col = (t % GRID_W).astype(np.float32)
    quarter = dim // 4
    inv = (np.float32(10000.0) ** (-np.arange(quarter, dtype=np.float32) / np.float32(quarter))).astype(np.float32)
    ang = np.concatenate([row[:, None] * inv, col[:, None] * inv], axis=-1).astype(np.float32)
    cos, sin = np.cos(ang).astype(np.float32), np.sin(ang).astype(np.float32)
    half = dim // 2
    C = np.ones((len(rows_pattern), SEQ), np.float32)
    Sn = np.zeros((len(rows_pattern), SEQ), np.float32)
    for r, d in enumerate(rows_pattern):
        if d < 0:
            continue
        C[r] = cos[:, d % half]
        Sn[r] = -sin[:, d % half] if d < half else sin[:, d % half]
    return C, Sn


def _core_table(tab, r):
    out = np.empty((tab.shape[0], NT), np.float32)
    out[:, :CTX] = tab[:, :1] * 0 + (1.0 if tab is None else 0.0)
    return out


def _tabs_for_core(C, Sn, r):
    Cc = np.ones((C.shape[0], NT), np.float32)
    Sc = np.zeros((C.shape[0], NT), np.float32)
    Cc[:, CTX:] = C[:, r * LAT_PC:(r + 1) * LAT_PC]
    Sc[:, CTX:] = Sn[:, r * LAT_PC:(r + 1) * LAT_PC]
    return Cc, Sc


def _swap_cols(w, c0, nheads, hd, rot0, rotd):
    blk = w[:, c0:c0 + nheads * hd].copy()
    idx = np.arange(nheads * hd)
    h, d = idx // hd, idx % hd
    src = idx.copy()
    inrot = (d >= rot0) & (d < rot0 + rotd)
    src[inrot] = h[inrot] * hd + rot0 + ((d[inrot] - rot0 + rotd // 2) % rotd)
    return blk[:, src]


def kernel_unfused(x, c, ctx, c_ctx, mod_w, mod_b, norm_mix, norm_ffn, ab_w_in, ab_w_out, a_sink, b_rpb,
           cd_w_in, c_q_norm, c_w_q_b, c_kv_norm, c_w_kv_b, d_q_norm, d_k_norm, cd_w_out,
           router_w, router_b, exp_w_in, exp_b_in, exp_w_out, exp_b_out, final_norm, _depth=DEPTH):
    f32 = lambda a: np.asarray(a, np.float32)
    x, c, ctx, c_ctx = f32(x), f32(c), f32(ctx), f32(c_ctx)
    ident = np.eye(128, dtype=np.float32)
    xT = []
    for core in range(NCORES):
        b, r = core // 4, core % 4
        tok = np.concatenate([ctx[b], x[b, r * LAT_PC:(r + 1) * LAT_PC]], axis=0)
        xT.append(np.ascontiguousarray(tok.T))
    conds = [np.ascontiguousarray(np.stack([fm(c[core // 4], 8), fm(c_ctx, 8)], axis=-1)) for core in range(NCORES)]
    Ch, Sh = _rope_tables(64, [d for _ in range(2) for d in range(64)])
    Cm, Sm = _rope_tables(32, [-1] * 64 + list(range(32)))
    C3, S3 = _rope_tables(32, list(range(32)))
    blk = np.kron(np.eye(2, dtype=np.float32), np.ones((64, 64), np.float32))
    maskA = _maskA_np()
    xf = None
    for l in range(_depth):
        i = l // 2
        odd = l % 2 == 1
        mw, mb = f32(mod_w[l]), f32(mod_b[l])
        ins = []
        for core in range(NCORES):
            r = core % 4
            Cc, Sc = _tabs_for_core(Ch, Sh, r)
            d = {"xT": xT[core], "cond": conds[core], "modw": np.ascontiguousarray(mw[:, :2048]), "modb": fm(mb[:2048], 16),
                 "gain": fm(norm_mix[l], 8), "ropeC": Cc, "ropeS": Sc, "ident": ident}
            if not odd:
                w = f32(ab_w_in[i])
                d["w"] = np.ascontiguousarray(np.concatenate([w, _swap_cols(w, 0, 10, 64, 0, 64)], axis=1))
            else:
                w = f32(cd_w_in[i])
                d["w"] = np.ascontiguousarray(np.concatenate([w, _swap_cols(w, 1024, 1, 32, 0, 32), _swap_cols(w, 1056, 10, 64, 0, 64)], axis=1))
                wq = f32(c_w_q_b[i])
                d["wqb"] = np.ascontiguousarray(np.concatenate([wq, _swap_cols(wq, 0, 8, 96, 64, 32)], axis=1))
                wkv = f32(c_w_kv_b[i]).reshape(256, 8, 128)
                d["wkvb"] = np.ascontiguousarray(np.concatenate([wkv[:, :, :64].reshape(256, 512), wkv[:, :, 64:].reshape(256, 512)], axis=1))
                d["qn"] = fm(c_q_norm[i], 6)
                d["kvn"] = fm(c_kv_norm[i], 2)
                gq, gk = f32(d_q_norm[i]), f32(d_k_norm[i])
                sw = (np.arange(64) + 32) % 64
                d["dgq"] = np.ascontiguousarray(np.stack([np.tile(gq, 2), np.tile(gq[sw], 2), np.tile(gk, 2), np.tile(gk[sw], 2)], axis=1))
                d["ropeC2"], d["ropeS2"] = _tabs_for_core(Cm, Sm, r)
                d["ropeC3"], d["ropeS3"] = _tabs_for_core(C3, S3, r)
                d["blk"] = blk
            ins.append(d)
        res = _run("p1o" if odd else "p1e", ins)
        ins2 = []
        for b in range(BATCH):
            FM = np.concatenate([res[4 * b]["fm"][:, :CTX]] + [res[4 * b + r]["fm"][:, CTX:] for r in range(4)], axis=1)
            TM = np.concatenate([res[4 * b]["tm"][:CTX]] + [res[4 * b + r]["tm"][CTX:] for r in range(4)], axis=0)
            for j in range(4):
                g = j // 2
                if not odd:
                    d = {"q1T": np.stack([FM[(2 * j + hh) * 64:(2 * j + hh + 1) * 64] for hh in range(2)]),
                         "k1T": FM[512 + g * 64:512 + (g + 1) * 64], "v1": TM[:, g * 64:(g + 1) * 64],
                         "q2T": np.stack([FM[640 + (2 * j + hh) * 64:640 + (2 * j + hh + 1) * 64] for hh in range(2)]),
                         "k2T": np.stack([FM[1152 + (2 * j + hh) * 64:1152 + (2 * j + hh + 1) * 64] for hh in range(2)]),
                         "v2": TM[:, 128 + 2 * j * 64:128 + (2 * j + 2) * 64], "maskA": maskA}
                    d["sink"] = np.ascontiguousarray(np.tile(f32(a_sink[i])[None, 2 * j:2 * j + 2], (128, 1)))
                    rp = f32(b_rpb[i])
                    rx = np.empty((2, 3, 128, 8, 512), np.float32)
                    for hh in range(2):
                        for v, (valid, dr, dc) in enumerate(nbr_index()):
                            rx[hh, v] = np.where(valid, rp[2 * j + hh][dr, dc], np.float32(-30000.0))
                    d["rpbx"] = rx
                else:
                    d = {"q1T": np.stack([FM[1312 + (2 * j + hh) * 64:1312 + (2 * j + hh + 1) * 64] for hh in range(2)]),
                         "k1T": FM[1824 + g * 64:1824 + (g + 1) * 64], "v1": TM[:, 512 + g * 64:512 + (g + 1) * 64],
                         "q2T": np.stack([FM[(2 * j + hh) * 96:(2 * j + hh + 1) * 96] for hh in range(2)]),
                         "k2T": np.stack([np.concatenate([FM[768 + (2 * j + hh) * 64:768 + (2 * j + hh + 1) * 64], FM[1280:1312]], axis=0) for hh in range(2)]),
                         "v2": TM[:, 2 * j * 64:(2 * j + 2) * 64]}
                d = {kk: np.ascontiguousarray(vv) for kk, vv in d.items()}
                d["ident"] = ident
                ins2.append(d)
        del res
        res2 = _run("p2o" if odd else "p2e", ins2)
        ins3 = []
        for core in range(NCORES):
            b, r = core // 4, core % 4
            yin = np.empty((8, 128, NT), BF)
            for j in range(4):
                y = res2[4 * b + j]["yT"]
                for mx in range(2):
                    ci = (mx * 4 + j) if not odd else ((1 - mx) * 4 + j)
                    yin[ci, :, :CTX] = y[mx][:, :CTX]
                    yin[ci, :, CTX:] = y[mx][:, CTX + r * LAT_PC:CTX + (r + 1) * LAT_PC]
            d = {"xT": xT[core], "yT": yin, "cond": conds[core], "modw": np.ascontiguousarray(mw[:, 2048:]), "modb": fm(mb[2048:], 32),
                 "gain": fm(norm_ffn[l], 8), "fgain": fm(final_norm, 8), "wout": f32(cd_w_out[i] if odd else ab_w_out[i]),
                 "rw": f32(router_w[l]), "rb": f32(router_b[l])[None, :], "ewin": f32(exp_w_in[l]),
                 "ebin": np.ascontiguousarray(f32(exp_b_in[l]).reshape(NEXP, 16, 128).transpose(2, 0, 1)),
                 "ewout": f32(exp_w_out[l]), "ebout": f32(exp_b_out[l]), "ident": ident}
            ins3.append(d)
        del res2
        res3 = _run("p3", ins3)
        xT = [res3[core]["x2T"] for core in range(NCORES)]
        xf = [res3[core]["xfT"] for core in range(NCORES)]
        del res3
    out = np.empty((BATCH, SEQ, DM), np.float32)
    for core in range(NCORES):
        b, r = core // 4, core % 4
        out[b, r * LAT_PC:(r + 1) * LAT_PC] = xf[core][:, CTX:].T
    return out
```

```python
import contextlib
import numpy as np
import ml_dtypes
import concourse.bass as bass
import concourse.mybir as mybir
from concourse.bass_utils import run_bass_kernel_spmd

F32 = mybir.dt.float32
BF16 = mybir.dt.bfloat16
AF = mybir.ActivationFunctionType
ALU = mybir.AluOpType
AX = mybir.AxisListType

NCORES = 8
DM = 1024
BATCH = 2
SEQ = 16384
DEPTH = 4
GRID_W = 64
CTX = 256
LAT_PC = SEQ // 4
NT = CTX + LAT_PC
NTOK = CTX + SEQ
NKB = NTOK // 128
EPS = 1e-6
NEXP = 32
SAME_ENGINE_SYNC = True


class Buf:
    __slots__ = ("name", "writers", "readers")

    def __init__(self, name):
        self.name = name
        self.writers = {}
        self.readers = {}


class _Rec:
    def __init__(self):
        self.call = None

    def __getattr__(self, m):
        def f(*a, **kw):
            self.call = (m, a, kw)
            return self
        return f


class Sched:
    ENG = ("pe", "act", "dve", "pool", "sp")

    def __init__(self, nc, stack, ndma_sems=12):
        self.nc = nc
        self.prog = {e: [] for e in self.ENG}
        self.sem = {e: stack.enter_context(nc.semaphore("s_" + e)) for e in self.ENG}
        self.cnt = {e: 0 for e in self.ENG}
        self.waited = {e: {} for e in self.ENG}
        self.dq = {}
        for q in ("sp", "pool"):
            sems = [stack.enter_context(nc.semaphore(f"d_{q}{i}")) for i in range(ndma_sems)]
            self.dq[q] = {"sems": sems, "n": 0}
        self.semkey = {}
        self.ccs = []
        self.ccsem = None
        self.final = []
        self.ninst = 0

    def _key(self, sem):
        k = id(sem)
        self.semkey[k] = sem
        return k

    def _wait(self, eng, deps):
        w = self.waited[eng]
        for k, v in deps.items():
            if w.get(k, 0) >= v:
                continue
            w[k] = v
            sem = self.semkey[k]
            self.prog[eng].append(lambda e, sem=sem, v=v: e.wait_ge(sem, v))

    def _deps(self, eng, reads, writes):
        deps = {}
        own = self._key(self.sem[eng])

        def add(d):
            for k, v in d.items():
                if k == own and (eng == "pe" or not SAME_ENGINE_SYNC):
                    continue
                if deps.get(k, 0) < v:
                    deps[k] = v
        for b in reads:
            add(b.writers)
        for b in writes:
            add(b.writers)
            for k, v in b.readers.items():
                if k != own and deps.get(k, 0) < v:
                    deps[k] = v
        return deps

    def _mark(self, tok, reads, writes):
        k, v = tok
        for b in reads:
            if b.readers.get(k, 0) < v:
                b.readers[k] = v
        for b in writes:
            if b.readers:
                b.readers = {}
                b.writers = {}
            b.writers[k] = v

    def op(self, eng, fn, reads=(), writes=()):
        deps = self._deps(eng, reads, writes)
        self._wait(eng, deps)
        self.cnt[eng] += 1
        n = self.cnt[eng]
        sem = self.sem[eng]
        r = _Rec()
        fn(r)
        m, a, kw = r.call
        self.prog[eng].append(lambda e, m=m, a=a, kw=kw, sem=sem: getattr(e, m)(*a, **kw).then_inc(sem, 1))
        self._mark((self._key(sem), n), reads, writes)
        self.ninst += 1

    def cc(self, stack, fn, scratch, reads=(), writes=()):
        deps = self._deps("pool", reads, writes)
        self._wait("pool", deps)
        if self.ccsem is None:
            self.ccsem = stack.enter_context(self.nc.semaphore("ccsem"))
        sem = self.ccsem
        r = _Rec()
        fn(r)
        m, a, kw = r.call
        self.prog["pool"].append(lambda e, m=m, a=a, kw=kw, sem=sem: getattr(e, m)(*a, **kw).then_inc(sem, 1))
        self.ccs.append(sem)
        n = len(self.ccs)
        self.prog["pool"].append(lambda e, sem=sem, n=n: e.wait_ge(sem, n))
        self.op("pool", lambda e: e.memset(scratch, 0.0), reads=reads, writes=writes)

    def dma(self, q, out, in_, reads=(), writes=(), final=False):
        deps = self._deps(q, reads, writes)
        d = self.dq[q]
        i = d["n"]
        d["n"] += 1
        sems = d["sems"]
        sem = sems[i % len(sems)]
        rnd = i // len(sems)
        k = self._key(sem)
        if rnd > 0:
            deps[k] = max(deps.get(k, 0), 16 * rnd)
        self._wait(q, deps)
        self.prog[q].append(lambda e, o=out, a=in_, sem=sem: e.dma_start(out=o, in_=a).then_inc(sem, 16))
        tok = (k, 16 * (rnd + 1))
        self._mark(tok, reads, writes)
        if final:
            self.final.append(tok)
        self.ninst += 1

    def barrier(self):
        deps = {}
        for e in self.ENG:
            if self.cnt[e]:
                deps[self._key(self.sem[e])] = self.cnt[e]
        for q in self.dq.values():
            n = q["n"]
            L = len(q["sems"])
            for j, sem in enumerate(q["sems"]):
                uses = (n - j + L - 1) // L if n > j else 0
                if uses:
                    deps[self._key(sem)] = 16 * uses
        for e in self.ENG:
            own = self._key(self.sem[e])
            self._wait(e, {kk: v for kk, v in deps.items() if kk != own})

    def emit(self):
        nc = self.nc
        fin = {}
        for k, v in self.final:
            fin[k] = max(fin.get(k, 0), v)
        for q in self.dq.values():
            n = q["n"]
            for j, sem in enumerate(q["sems"]):
                uses = (n - j + len(q["sems"]) - 1) // len(q["sems"]) if n > j else 0
                if uses:
                    fin[self._key(sem)] = max(fin.get(self._key(sem), 0), 16 * uses)
        self._wait("sp", fin)
        prog = self.prog
        with nc.Block() as block:
            @block.tensor
            def _(e):
                for f in prog["pe"]:
                    f(e)

            @block.scalar
            def _(e):
                for f in prog["act"]:
                    f(e)

            @block.vector
            def _(e):
                for f in prog["dve"]:
                    f(e)

            @block.gpsimd
            def _(e):
                for f in prog["pool"]:
                    f(e)

            @block.sync
            def _(e):
                for f in prog["sp"]:
                    f(e)


class K:
    def __init__(self, name):
        self.nc = bass.Bass("TRN2", target_bir_lowering=False, name=name)
        self.stack = contextlib.ExitStack()
        self.S = Sched(self.nc, self.stack)
        self.nbuf = 0
        self.io = {}
        self.fused = False
        self.phase = 0

    def dram(self, name, shape, dt, kind):
        if name in self.io:
            return self.io[name]
        if self.fused and self.phase > 0:
            name = f"{name}_ph{self.phase}"
        return self.nc.dram_tensor(name, list(shape), dt, kind=kind).ap()

    def begin_phase(self, io):
        self.phase += 1
        self.io = io
        self.saved = self.stack
        self.stack = contextlib.ExitStack()

    def end_phase(self):
        self.S.barrier()
        self.stack.close()
        self.stack = self.saved
        self.io = {}

    def sb(self, shape, dt, name=None):
        self.nbuf += 1
        name = f"{name or 't'}_{self.nbuf}"
        t = self.stack.enter_context(self.nc.sbuf_tensor(name, list(shape), dt))
        return t, Buf(name)

    def ps(self, shape, dt, name=None):
        self.nbuf += 1
        name = f"{name or 'p'}_{self.nbuf}"
        t = self.stack.enter_context(self.nc.psum_tensor(name, list(shape), dt))
        return t, Buf(name)

    def finish(self):
        self.S.emit()
        self.stack.close()
        return self.nc


class Ring:
    def __init__(self, items):
        self.items = items
        self.i = 0

    def next(self):
        it = self.items[self.i % len(self.items)]
        self.i += 1
        return it


def token_tiles():
    tiles = [(0, CTX, True)]
    for i in range(LAT_PC // 512):
        tiles.append((CTX + 512 * i, 512, False))
    return tiles


def emit_mod_vectors(k, S, modw_d, modb_sb, modb_b, cond_sb, cond_b, nvec, out_sb, out_b, wring):
    ps_t, ps_b = k.ps([128, 4, 2], F32, "modps")
    for v in range(nvec):
        for hf in range(2):
            w_sb, w_b = wring.next()
            c0 = v * 1024 + hf * 512
            S.dma("sp", w_sb[:], modw_d[:, c0:c0 + 512].rearrange("(kc p) f -> p kc f", p=128), writes=[w_b])
            for j in range(4):
                for kc in range(8):
                    S.op("pe", lambda e, j=j, kc=kc, w_sb=w_sb: e.matmul(ps_t[:, j, :], lhsT=w_sb[:, kc, j * 128:(j + 1) * 128],
                                                                      rhs=cond_sb[:, kc, :], start=(kc == 0), stop=(kc == 7)),
                         reads=[w_b, cond_b], writes=[ps_b])
            for c in range(2):
                S.op("dve", lambda e, v=v, c=c, hf=hf: e.tensor_tensor(out=out_sb[:, v, hf * 4:hf * 4 + 4, c], in0=ps_t[:, :, c],
                                                                     in1=modb_sb[:, v * 8 + hf * 4:v * 8 + hf * 4 + 4], op=ALU.add),
                     reads=[ps_b, modb_b], writes=[out_b])


def emit_norm_tile(k, S, x_sb, x_b, T, onesb, ones_b, sq_sb, sq_b, ss_ps, ss_b, rstd_sb, rstd_b, eps_sb, eps_b):
    S.op("act", lambda e: e.activation(out=sq_sb[:, :, :T], in_=x_sb[:, :, :T], func=AF.Square), reads=[x_b], writes=[sq_b])
    for kc in range(8):
        S.op("pe", lambda e, kc=kc: e.matmul(ss_ps[:, :T], lhsT=onesb[:, :], rhs=sq_sb[:, kc, :T], start=(kc == 0), stop=(kc == 7)),
             reads=[sq_b, ones_b], writes=[ss_b])
    S.op("act", lambda e: e.activation(out=rstd_sb[:, :T], in_=ss_ps[:, :T], func=AF.Sqrt, scale=1.0 / DM, bias=eps_sb[:, 0:1]),
         reads=[ss_b, eps_b], writes=[rstd_b])
    S.op("dve", lambda e: e.reciprocal(out=rstd_sb[:, :T], in_=rstd_sb[:, :T]), reads=[rstd_b], writes=[rstd_b])


def load_consts(k, S, ident_d, need_f32_ident=False):
    identb, identb_b = k.sb([128, 128], BF16, "identb")
    S.dma("pool", identb[:], ident_d[:, :], writes=[identb_b])
    onesb, ones_b = k.sb([128, 128], BF16, "onesb")
    S.op("dve", lambda e: e.memset(onesb[:], 1.0), writes=[ones_b])
    eps_sb, eps_b = k.sb([128, 1], F32, "eps")
    S.op("dve", lambda e: e.memset(eps_sb[:], EPS), writes=[eps_b])
    return identb, identb_b, onesb, ones_b, eps_sb, eps_b


def build_p1(odd, k=None, io=None):
    own = k is None
    if own:
        k = K("p1o" if odd else "p1e")
    else:
        k.begin_phase(io)
    S = k.S
    xT = k.dram("xT", [DM, NT], F32, "ExternalInput")
    cond = k.dram("cond", [128, 8, 2], F32, "ExternalInput")
    modw = k.dram("modw", [DM, 2048], F32, "ExternalInput")
    modb = k.dram("modb", [128, 16], F32, "ExternalInput")
    gain = k.dram("gain", [128, 8], F32, "ExternalInput")
    ropeC = k.dram("ropeC", [128, NT], F32, "ExternalInput")
    ropeS = k.dram("ropeS", [128, NT], F32, "ExternalInput")
    if not odd:
        NW = 2304 + 640
        w_d = k.dram("w", [DM, NW], F32, "ExternalInput")
        fm_out = k.dram("fm", [1664, NT], BF16, "ExternalOutput")
        tm_out = k.dram("tm", [NT, 640], BF16, "ExternalOutput")
    else:
        NW = 1824 + 32 + 640
        w_d = k.dram("w", [DM, NW], F32, "ExternalInput")
        wqb_d = k.dram("wqb", [768, 1536], F32, "ExternalInput")
        wkvb_d = k.dram("wkvb", [256, 1024], F32, "ExternalInput")
        qn_d = k.dram("qn", [128, 6], F32, "ExternalInput")
        kvn_d = k.dram("kvn", [128, 2], F32, "ExternalInput")
        dgC = k.dram("dgq", [128, 4], F32, "ExternalInput")
        ropeC2 = k.dram("ropeC2", [96, NT], F32, "ExternalInput")
        ropeS2 = k.dram("ropeS2", [96, NT], F32, "ExternalInput")
        blk_d = k.dram("blk", [128, 128], F32, "ExternalInput")
        ropeC3 = k.dram("ropeC3", [32, NT], F32, "ExternalInput")
        ropeS3 = k.dram("ropeS3", [32, NT], F32, "ExternalInput")
        fm_out = k.dram("fm", [768 + 512 + 32 + 512 + 128, NT], BF16, "ExternalOutput")
        tm_out = k.dram("tm", [NT, 640], BF16, "ExternalOutput")
    ident_d = k.dram("ident", [128, 128], F32, "ExternalInput")

    identb, identb_b, onesb, ones_b, eps_sb, eps_b = load_consts(k, S, ident_d)

    w_sb, w_b = k.sb([128, 8, NW], BF16, "w_sb")
    for kc in range(8):
        for c0 in range(0, NW, 1024):
            c1 = min(NW, c0 + 1024)
            S.dma("pool", w_sb[:, kc, c0:c1], w_d[kc * 128:(kc + 1) * 128, c0:c1], writes=[w_b])
    if odd:
        wqb_sb, wqb_b = k.sb([128, 6, 1536], BF16, "wqb_sb")
        for kc in range(6):
            for c0 in (0, 768):
                S.dma("pool", wqb_sb[:, kc, c0:c0 + 768], wqb_d[kc * 128:(kc + 1) * 128, c0:c0 + 768], writes=[wqb_b])
        wkvb_sb, wkvb_b = k.sb([128, 2, 1024], BF16, "wkvb_sb")
        for kc in range(2):
            S.dma("pool", wkvb_sb[:, kc, :], wkvb_d[kc * 128:(kc + 1) * 128, :], writes=[wkvb_b])
        lg_sb, lg_b = k.sb([128, 8], F32, "lg_sb")
        S.dma("sp", lg_sb[:, 0:6], qn_d[:, :], writes=[lg_b])
        S.dma("sp", lg_sb[:, 6:8], kvn_d[:, :], writes=[lg_b])
        dg_sb, dg_b = k.sb([128, 4], F32, "dg_sb")
        S.dma("sp", dg_sb[:], dgC[:, :], writes=[dg_b])
        blk_sb, blk_b = k.sb([128, 128], BF16, "blk_sb")
        S.dma("pool", blk_sb[:], blk_d[:, :], writes=[blk_b])

    cond_sb, cond_b = k.sb([128, 8, 2], F32, "cond_sb")
    S.dma("sp", cond_sb[:], cond[:, :, :], writes=[cond_b])
    S.op("act", lambda e: e.activation(out=cond_sb[:], in_=cond_sb[:], func=AF.Silu), reads=[cond_b], writes=[cond_b])
    modb_sb, modb_b = k.sb([128, 16], F32, "modb_sb")
    S.dma("sp", modb_sb[:], modb[:, :], writes=[modb_b])
    gain_sb, gain_b = k.sb([128, 8], F32, "gain_sb")
    S.dma("sp", gain_sb[:], gain[:, :], writes=[gain_b])
    mv_sb, mv_b = k.sb([128, 2, 8, 2], F32, "mv_sb")
    wm, wm_b = k.sb([128, 8, 512], F32, "modw_sb")
    emit_mod_vectors(k, S, modw, modb_sb, modb_b, cond_sb, cond_b, 2, mv_sb, mv_b, Ring([(wm, wm_b)]))
    A_sb, A_b = k.sb([128, 8, 2], F32, "A_sb")
    for c in range(2):
        S.op("dve", lambda e, c=c: e.scalar_tensor_tensor(out=A_sb[:, :, c], in0=mv_sb[:, 1, :, c], scalar=1.0, in1=gain_sb[:, :], op0=ALU.add, op1=ALU.mult),
             reads=[mv_b, gain_b], writes=[A_b])

    x_sb, x_b = k.sb([128, 8, 512], F32, "x_sb")
    sq_sb, sq_b = k.sb([128, 8, 512], BF16, "sq_sb")
    rstd_sb, rstd_b = k.sb([128, 512], F32, "rstd_sb")
    t_sb, t_b = k.sb([128, 512], F32, "t_sb")
    h_sb, h_b = k.sb([128, 8, 512], BF16, "h_sb")
    rc_sb, rc_b = k.sb([128, 512], F32, "rc_sb")
    rs_sb, rs_b = k.sb([128, 512], F32, "rs_sb")
    ss_ps, ss_b = k.ps([128, 512], F32, "ss_ps")
    pring = Ring([k.ps([128, 512], F32, f"pp{i}") for i in range(5)])
    oring = Ring([k.sb([128, 512], BF16, f"ob{i}") for i in range(3)])
    u1_sb, u1_b = k.sb([128, 512], F32, "u1")
    u2_sb, u2_b = k.sb([128, 512], F32, "u2")
    vo_ring = Ring([k.sb([128, 640], BF16, f"vo{i}") for i in range(2)])
    if odd:
        rc2_sb, rc2_b = k.sb([96, 512], F32, "rc2_sb")
        rs2_sb, rs2_b = k.sb([96, 512], F32, "rs2_sb")
        rc3_sb, rc3_b = k.sb([32, 512], F32, "rc3_sb")
        rs3_sb, rs3_b = k.sb([32, 512], F32, "rs3_sb")
        cq_sb, cq_b = k.sb([128, 8, 512], F32, "cq_sb")
        cn_sb, cn_b = k.sb([128, 8, 512], BF16, "cn_sb")
        rq_sb, rq_b = k.sb([128, 512], F32, "rq_sb")
        rkv_sb, rkv_b = k.sb([128, 512], F32, "rkv_sb")
        nrm_sb, nrm_b = k.sb([128, 512], F32, "nrm_sb")

    def mm_fm(ps, ps_b, col0, ncols, T, rhs_sb=None, rhs_b=None, wsb=None, wb=None, nk=8):
        rhs_sb = h_sb if rhs_sb is None else rhs_sb
        rhs_b = h_b if rhs_b is None else rhs_b
        wsb = w_sb if wsb is None else wsb
        wb = w_b if wb is None else wb
        for kc in range(nk):
            S.op("pe", lambda e, kc=kc: e.matmul(ps[:ncols, :T], lhsT=wsb[:, kc, col0:col0 + ncols], rhs=rhs_sb[:, kc, :T],
                                                start=(kc == 0), stop=(kc == nk - 1)), reads=[wb, rhs_b], writes=[ps_b])

    def store_fm(src_fn, src_bufs, row0, nrows, t0, T, eng="act"):
        o_sb, o_b = oring.next()
        if eng == "act":
            S.op("act", lambda e: e.copy(out=o_sb[:nrows, :T], in_=src_fn()), reads=src_bufs, writes=[o_b])
        S.dma("sp", fm_out[row0:row0 + nrows, t0:t0 + T], o_sb[:nrows, :T], reads=[o_b])

    def rope_store(psA, psA_b, psB, psB_b, nrows, row0, t0, T, C, C_b, Sn, Sn_b, norm=None):
        o_sb, o_b = oring.next()
        S.op("dve", lambda e: e.tensor_tensor(out=u1_sb[:nrows, :T], in0=psA[:nrows, :T], in1=C[:nrows, :T], op=ALU.mult), reads=[psA_b, C_b], writes=[u1_b])
        S.op("dve", lambda e: e.tensor_tensor(out=u2_sb[:nrows, :T], in0=psB[:nrows, :T], in1=Sn[:nrows, :T], op=ALU.mult), reads=[psB_b, Sn_b], writes=[u2_b])
        if norm is None:
            S.op("pool", lambda e: e.tensor_tensor(out=o_sb[:nrows, :T], in0=u1_sb[:nrows, :T], in1=u2_sb[:nrows, :T], op=ALU.add), reads=[u1_b, u2_b], writes=[o_b])
        else:
            n_sb, n_b = norm
            S.op("pool", lambda e: e.tensor_tensor(out=u1_sb[:nrows, :T], in0=u1_sb[:nrows, :T], in1=u2_sb[:nrows, :T], op=ALU.add), reads=[u1_b, u2_b], writes=[u1_b])
            S.op("dve", lambda e: e.tensor_tensor(out=o_sb[:nrows, :T], in0=u1_sb[:nrows, :T], in1=n_sb[:nrows, :T], op=ALU.mult), reads=[u1_b, n_b], writes=[o_b])
        S.dma("sp", fm_out[row0:row0 + nrows, t0:t0 + T], o_sb[:nrows, :T], reads=[o_b])

    def rsqrt_from_ps(ps, ps_b, out_sb, out_b, T, scale):
        S.op("act", lambda e: e.activation(out=out_sb[:, :T], in_=ps[:, :T], func=AF.Sqrt, scale=scale, bias=eps_sb[:, 0:1]), reads=[ps_b, eps_b], writes=[out_b])
        S.op("dve", lambda e: e.reciprocal(out=out_sb[:, :T], in_=out_sb[:, :T]), reads=[out_b], writes=[out_b])

    for (t0, T, is_ctx) in token_tiles():
        c = 1 if is_ctx else 0
        S.dma("sp", x_sb[:, :, :T], xT[:, t0:t0 + T].rearrange("(kc p) t -> p kc t", p=128), writes=[x_b])
        S.dma("sp", rc_sb[:, :T], ropeC[:, t0:t0 + T], writes=[rc_b])
        S.dma("sp", rs_sb[:, :T], ropeS[:, t0:t0 + T], writes=[rs_b])
        emit_norm_tile(k, S, x_sb, x_b, T, onesb, ones_b, sq_sb, sq_b, ss_ps, ss_b, rstd_sb, rstd_b, eps_sb, eps_b)
        for kc in range(8):
            S.op("dve", lambda e, kc=kc: e.scalar_tensor_tensor(out=t_sb[:, :T], in0=x_sb[:, kc, :T], scalar=A_sb[:, kc, c:c + 1], in1=rstd_sb[:, :T],
                                                             op0=ALU.mult, op1=ALU.mult), reads=[x_b, A_b, rstd_b], writes=[t_b])
            S.op("act", lambda e, kc=kc: e.activation(out=h_sb[:, kc, :T], in_=t_sb[:, :T], func=AF.Identity, bias=mv_sb[:, 0, kc, c:c + 1], scale=1.0),
                 reads=[t_b, mv_b], writes=[h_b])
        if not odd:
            for j in range(5):
                col = j * 128
                swc = 2304 + j * 128
                pa, pa_b = pring.next()
                pb, pb_b = pring.next()
                mm_fm(pa, pa_b, col, 128, T)
                mm_fm(pb, pb_b, swc, 128, T)
                rope_store(pa, pa_b, pb, pb_b, 128, j * 128, t0, T, rc_sb, rc_b, rs_sb, rs_b)
            for j in range(8):
                col = 768 + j * 128
                pa, pa_b = pring.next()
                mm_fm(pa, pa_b, col, 128, T)
                store_fm(lambda pa=pa: pa[:, :T], [pa_b], 640 + j * 128, 128, t0, T)
            tmcols = [(640, 128, 0), (1792, 512, 128)]
        else:
            S.dma("sp", rc2_sb[:, :T], ropeC2[:, t0:t0 + T], writes=[rc2_b])
            S.dma("sp", rs2_sb[:, :T], ropeS2[:, t0:t0 + T], writes=[rs2_b])
            S.dma("sp", rc3_sb[:, :T], ropeC3[:, t0:t0 + T], writes=[rc3_b])
            S.dma("sp", rs3_sb[:, :T], ropeS3[:, t0:t0 + T], writes=[rs3_b])
            for j in range(8):
                pa, pa_b = pring.next()
                mm_fm(pa, pa_b, j * 128, 128, T)
                S.op("act", lambda e, j=j, pa=pa: e.copy(out=cq_sb[:, j, :T], in_=pa[:, :T]), reads=[pa_b], writes=[cq_b])
            S.op("act", lambda e: e.activation(out=sq_sb[:, :, :T], in_=cq_sb[:, :, :T], func=AF.Square), reads=[cq_b], writes=[sq_b])
            pq, pq_b = pring.next()
            for j in range(6):
                S.op("pe", lambda e, j=j: e.matmul(pq[:, :T], lhsT=onesb[:, :], rhs=sq_sb[:, j, :T], start=(j == 0), stop=(j == 5)), reads=[sq_b, ones_b], writes=[pq_b])
            rsqrt_from_ps(pq, pq_b, rq_sb, rq_b, T, 1.0 / 768)
            pk, pk_b = pring.next()
            for j in range(2):
                S.op("pe", lambda e, j=j: e.matmul(pk[:, :T], lhsT=onesb[:, :], rhs=sq_sb[:, 6 + j, :T], start=(j == 0), stop=(j == 1)), reads=[sq_b, ones_b], writes=[pk_b])
            rsqrt_from_ps(pk, pk_b, rkv_sb, rkv_b, T, 1.0 / 256)
            for j in range(8):
                r_sb, r_b = (rq_sb, rq_b) if j < 6 else (rkv_sb, rkv_b)
                S.op("dve", lambda e, j=j, r_sb=r_sb: e.scalar_tensor_tensor(out=cn_sb[:, j, :T], in0=cq_sb[:, j, :T], scalar=lg_sb[:, j:j + 1], in1=r_sb[:, :T], op0=ALU.mult, op1=ALU.mult),
                     reads=[cq_b, r_b, lg_b], writes=[cn_b])
            for hd in range(8):
                pa, pa_b = pring.next()
                pb, pb_b = pring.next()
                mm_fm(pa, pa_b, hd * 96, 96, T, cn_sb, cn_b, wqb_sb, wqb_b, nk=6)
                mm_fm(pb, pb_b, 768 + hd * 96, 96, T, cn_sb, cn_b, wqb_sb, wqb_b, nk=6)
                rope_store(pa, pa_b, pb, pb_b, 96, hd * 96, t0, T, rc2_sb, rc2_b, rs2_sb, rs2_b)
            for j in range(4):
                pa, pa_b = pring.next()
                for kc in range(2):
                    S.op("pe", lambda e, kc=kc, j=j, pa=pa: e.matmul(pa[:, :T], lhsT=wkvb_sb[:, kc, j * 128:(j + 1) * 128], rhs=cn_sb[:, 6 + kc, :T],
                                                                      start=(kc == 0), stop=(kc == 1)), reads=[wkvb_b, cn_b], writes=[pa_b])
                store_fm(lambda pa=pa: pa[:, :T], [pa_b], 768 + j * 128, 128, t0, T)
            pa, pa_b = pring.next()
            pb, pb_b = pring.next()
            mm_fm(pa, pa_b, 1024, 32, T)
            mm_fm(pb, pb_b, 1824, 32, T)
            rope_store(pa, pa_b, pb, pb_b, 32, 768 + 512, t0, T, rc3_sb, rc3_b, rs3_sb, rs3_b)
            for j in range(5):
                col = 1056 + j * 128
                swc = 1856 + j * 128
                pa, pa_b = pring.next()
                pb, pb_b = pring.next()
                mm_fm(pa, pa_b, col, 128, T)
                mm_fm(pb, pb_b, swc, 128, T)
                S.op("act", lambda e, pa=pa: e.activation(out=sq_sb[:, 0, :T], in_=pa[:, :T], func=AF.Square), reads=[pa_b], writes=[sq_b])
                pn, pn_b = pring.next()
                S.op("pe", lambda e, pn=pn: e.matmul(pn[:, :T], lhsT=blk_sb[:, :], rhs=sq_sb[:, 0, :T], start=True, stop=True), reads=[sq_b, blk_b], writes=[pn_b])
                rsqrt_from_ps(pn, pn_b, nrm_sb, nrm_b, T, 1.0 / 64)
                gc = 0 if j < 4 else 2
                o_sb, o_b = oring.next()
                S.op("dve", lambda e, pa=pa, gc=gc: e.scalar_tensor_tensor(out=u1_sb[:, :T], in0=pa[:, :T], scalar=dg_sb[:, gc:gc + 1], in1=rc_sb[:, :T], op0=ALU.mult, op1=ALU.mult),
                     reads=[pa_b, dg_b, rc_b], writes=[u1_b])
                S.op("dve", lambda e, pb=pb, gc=gc: e.scalar_tensor_tensor(out=u2_sb[:, :T], in0=pb[:, :T], scalar=dg_sb[:, gc + 1:gc + 2], in1=rs_sb[:, :T], op0=ALU.mult, op1=ALU.mult),
                     reads=[pb_b, dg_b, rs_b], writes=[u2_b])
                S.op("pool", lambda e: e.tensor_tensor(out=u1_sb[:, :T], in0=u1_sb[:, :T], in1=u2_sb[:, :T], op=ALU.add), reads=[u1_b, u2_b], writes=[u1_b])
                S.op("dve", lambda e, o_sb=o_sb: e.tensor_tensor(out=o_sb[:, :T], in0=u1_sb[:, :T], in1=nrm_sb[:, :T], op=ALU.mult), reads=[u1_b, nrm_b], writes=[o_b])
                S.dma("sp", fm_out[1312 + j * 128:1312 + (j + 1) * 128, t0:t0 + T], o_sb[:, :T], reads=[o_b])
            tmcols = [(1696, 128, 512)]
        for s in range(T // 128):
            vo_sb, vo_b = vo_ring.next()
            for (wc, n, oc) in tmcols:
                pa, pa_b = pring.next()
                for kc in range(8):
                    S.op("pe", lambda e, kc=kc, pa=pa, wc=wc, n=n, s=s: e.matmul(pa[:, :n], lhsT=h_sb[:, kc, s * 128:(s + 1) * 128], rhs=w_sb[:, kc, wc:wc + n],
                                                                               start=(kc == 0), stop=(kc == 7)), reads=[h_b, w_b], writes=[pa_b])
                S.op("act", lambda e, pa=pa, n=n, oc=oc, vo_sb=vo_sb: e.copy(out=vo_sb[:, oc:oc + n], in_=pa[:, :n]), reads=[pa_b], writes=[vo_b])
            if odd:
                pa, pa_b = pring.next()
                for kc in range(2):
                    S.op("pe", lambda e, kc=kc, pa=pa, s=s: e.matmul(pa[:, :512], lhsT=cn_sb[:, 6 + kc, s * 128:(s + 1) * 128], rhs=wkvb_sb[:, kc, 512:1024],
                                                                      start=(kc == 0), stop=(kc == 1)), reads=[cn_b, wkvb_b], writes=[pa_b])
                S.op("act", lambda e, pa=pa, vo_sb=vo_sb: e.copy(out=vo_sb[:, 0:512], in_=pa[:, :512]), reads=[pa_b], writes=[vo_b])
            S.dma("sp", tm_out[t0 + s * 128:t0 + (s + 1) * 128, :], vo_sb[:, :], reads=[vo_b], final=True)
    if own:
        return k.finish()
    k.end_phase()


def _maskA_np():
    m = np.zeros((128, 6, 512), np.float32)
    kl = np.arange(128)[:, None]
    ql = np.arange(128)[None, :]
    for kbrel in range(6):
        for qb in range(4):
            rel = (kbrel - 1) - qb
            if rel == -1:
                m[:, kbrel, qb * 128:(qb + 1) * 128] = (kl >= ql)
            elif rel == 0:
                m[:, kbrel, qb * 128:(qb + 1) * 128] = 1.0
            elif rel == 1:
                m[:, kbrel, qb * 128:(qb + 1) * 128] = (kl <= ql)
    return m


def _nbr_index():
    rows = SEQ // GRID_W
    out = []
    for v, tile in enumerate((0, 1, rows // 8 - 1)):
        r0 = tile * 8
        kp = np.arange(128)
        kr2, kc = kp // 64, kp % 64
        q = np.arange(512)
        qr, c = r0 + q // 64, q % 64
        rs = np.clip(qr - 4, 0, rows - 8)
        cs = np.clip(c - 8, 0, GRID_W - 16)
        valid = np.zeros((128, 8, 512), bool)
        dr = np.zeros((128, 8, 512), np.int64)
        dc = np.zeros((128, 8, 512), np.int64)
        for kbrel in range(8):
            krow = r0 - 4 + 2 * kbrel + kr2
            okr = (krow[:, None] >= rs[None, :]) & (krow[:, None] < rs[None, :] + 8) & (krow[:, None] >= 0) & (krow[:, None] < rows)
            okc = (kc[:, None] >= cs[None, :]) & (kc[:, None] < cs[None, :] + 16)
            valid[:, kbrel, :] = okr & okc
            dr[:, kbrel, :] = krow[:, None] - qr[None, :] + 7
            dc[:, kbrel, :] = kc[:, None] - c[None, :] + 15
        out.append((valid, np.clip(dr, 0, 14), np.clip(dc, 0, 30)))
    return out


_NBR = None


def nbr_index():
    global _NBR
    if _NBR is None:
        _NBR = _nbr_index()
    return _NBR


def build_p2(odd):
    k = K("p2o" if odd else "p2e")
    S = k.S
    dk2 = 96 if odd else 64
    q1T = k.dram("q1T", [2, 64, NTOK], BF16, "ExternalInput")
    k1T = k.dram("k1T", [64, NTOK], BF16, "ExternalInput")
    v1 = k.dram("v1", [NTOK, 64], BF16, "ExternalInput")
    q2T = k.dram("q2T", [2, dk2, NTOK], BF16, "ExternalInput")
    k2T = k.dram("k2T", [2, dk2, NTOK], BF16, "ExternalInput")
    v2 = k.dram("v2", [NTOK, 128], BF16, "ExternalInput")
    ident_d = k.dram("ident", [128, 128], F32, "ExternalInput")
    yT = k.dram("yT", [2, 128, NTOK], BF16, "ExternalOutput")
    identb, identb_b, onesb, ones_b, eps_sb, eps_b = load_consts(k, S, ident_d)
    if not odd:
        maskA_d = k.dram("maskA", [128, 6, 512], F32, "ExternalInput")
        sink_d = k.dram("sink", [128, 2], F32, "ExternalInput")
        rpbx_d = k.dram("rpbx", [2, 3, 128, 8, 512], F32, "ExternalInput")
        maskA, maskA_b = k.sb([128, 6, 512], BF16, "maskA_sb")
        S.dma("pool", maskA[:], maskA_d[:, :, :], writes=[maskA_b])
        esink, esink_b = k.sb([128, 2], F32, "esink_sb")
        S.dma("sp", esink[:], sink_d[:, :], writes=[esink_b])
        S.op("act", lambda e: e.activation(out=esink[:], in_=esink[:], func=AF.Exp), reads=[esink_b], writes=[esink_b])
        MB = {}
        stg, stg_b = k.sb([128, 512], F32, "stg")
        for h in range(2):
            for v in range(3):
                m_sb, m_b = k.sb([128, 8, 512], BF16, f"MB{h}{v}")
                for kb in range(8):
                    S.dma("sp", stg[:], rpbx_d[h, v, :, kb, :], writes=[stg_b])
                    S.op("act", lambda e: e.activation(out=m_sb[:, kb, :], in_=stg[:], func=AF.Exp), reads=[stg_b], writes=[m_b])
                MB[(h, v)] = (m_sb, m_b)
        nbr = nbr_index()
        needB = [[[bool(nbr[v][0][:, kb, qb * 128:(qb + 1) * 128].any()) for qb in range(4)] for kb in range(8)] for v in range(3)]
        mA = _maskA_np()
        needA = [[bool(mA[:, kb, qb * 128:(qb + 1) * 128].any()) for qb in range(4)] for kb in range(6)]

    qT_sb, qT_b = k.sb([dk2, NTOK], BF16, "qT_sb")
    kT_sb, kT_b = k.sb([dk2, NTOK], BF16, "kT_sb")
    va_sb, va_b = k.sb([128, NKB, 65], BF16, "va_sb")
    S.op("dve", lambda e: e.memset(va_sb[:, :, 64:65], 1.0), writes=[va_b])
    sring = Ring([k.ps([128, 512], F32, f"s{i}") for i in range(2)])
    O = [k.ps([128, 512], F32, f"o{i}") for i in range(4)]
    pt_ps, pt_b = k.ps([128, 512], BF16, "ptp")
    pring = Ring([k.sb([128, 512], BF16, f"pT{i}") for i in range(3)])
    den_sb, den_b = k.sb([128, 4], F32, "den")
    y_sb, y_b = k.sb([128, 4, 64], BF16, "y_sb")
    yTring = Ring([k.sb([64, 512], BF16, f"yT{i}") for i in range(2)])

    jobs = [(0, 0), (0, 1), (1, 0), (1, 1)]
    for (mx, h) in jobs:
        dk = 64 if mx == 0 else dk2
        scale = float(dk) ** -0.5
        qsrc = q1T[h] if mx == 0 else q2T[h]
        ksrc = k1T if mx == 0 else k2T[h]
        for c0 in range(0, NTOK, 4160):
            S.dma("sp", qT_sb[:dk, c0:c0 + 4160], qsrc[:, c0:c0 + 4160], writes=[qT_b])
            S.dma("sp", kT_sb[:dk, c0:c0 + 4160], ksrc[:, c0:c0 + 4160], writes=[kT_b])
        if mx == 0:
            if h == 0:
                S.dma("sp", va_sb[:, :, 0:64], v1.rearrange("(kb p) d -> p kb d", p=128), writes=[va_b])
        else:
            S.dma("sp", va_sb[:, :, 0:64], v2[:, h * 64:(h + 1) * 64].rearrange("(kb p) d -> p kb d", p=128), writes=[va_b])
        tiles = [(0, 256, None)] + [(CTX + 512 * i, 512, i) for i in range(SEQ // 512)]
        for (q0, nq, ti) in tiles:
            nqb = nq // 128
            kbl = []
            if ti is None:
                kbl = [(0, None, [True] * nqb), (1, None, [True] * nqb)]
            elif odd:
                kbl = [(kb, None, [True] * 4) for kb in range(NKB)]
            elif mx == 0:
                for kbrel in range(6):
                    lb = 4 * ti + kbrel - 1
                    if 0 <= lb < SEQ // 128:
                        kbl.append((2 + lb, (maskA, maskA_b, kbrel), needA[kbrel]))
                kbl += [(0, None, [True] * 4), (1, None, [True] * 4)]
            else:
                v = 0 if ti == 0 else (2 if ti == SEQ // 512 - 1 else 1)
                for kbrel in range(8):
                    lb = 4 * ti - 2 + kbrel
                    if 0 <= lb < SEQ // 128 and any(needB[v][kbrel]):
                        m_sb, m_b = MB[(h, v)]
                        kbl.append((2 + lb, (m_sb, m_b, kbrel), needB[v][kbrel]))
                kbl += [(0, None, [True] * 4), (1, None, [True] * 4)]
            first = [min(i for i, (_, _, nd) in enumerate(kbl) if nd[qb]) for qb in range(nqb)]
            last = [max(i for i, (_, _, nd) in enumerate(kbl) if nd[qb]) for qb in range(nqb)]
            for i, (kb, msk, nd) in enumerate(kbl):
                s_ps, s_b = sring.next()
                S.op("pe", lambda e: e.matmul(s_ps[:, :nq], lhsT=kT_sb[:dk, kb * 128:(kb + 1) * 128], rhs=qT_sb[:dk, q0:q0 + nq], start=True, stop=True),
                     reads=[kT_b, qT_b], writes=[s_b])
                p_sb, p_b = pring.next()
                S.op("act", lambda e: e.activation(out=p_sb[:, :nq], in_=s_ps[:, :nq], func=AF.Exp, scale=scale), reads=[s_b], writes=[p_b])
                if msk is not None:
                    m_sb, m_b, kbrel = msk
                    S.op("dve" if i % 2 == 0 else "pool", lambda e: e.tensor_tensor(out=p_sb[:, :nq], in0=p_sb[:, :nq], in1=m_sb[:, kbrel, :nq], op=ALU.mult),
                         reads=[p_b, m_b], writes=[p_b])
                for qb in range(nqb):
                    if nd[qb]:
                        o_ps, o_b = O[qb]
                        S.op("pe", lambda e: e.matmul(o_ps[:, 0:65], lhsT=p_sb[:, qb * 128:(qb + 1) * 128], rhs=va_sb[:, kb, :],
                                                      start=(i == first[qb]), stop=(i == last[qb])), reads=[p_b, va_b], writes=[o_b])
            yt_sb, yt_b = yTring.next()
            for qb in range(nqb):
                o_ps, o_b = O[qb]
                if (not odd) and mx == 0:
                    S.op("dve", lambda e: e.tensor_tensor(out=den_sb[:, qb:qb + 1], in0=o_ps[:, 64:65], in1=esink[:, h:h + 1], op=ALU.add), reads=[o_b, esink_b], writes=[den_b])
                    S.op("dve", lambda e: e.reciprocal(out=den_sb[:, qb:qb + 1], in_=den_sb[:, qb:qb + 1]), reads=[den_b], writes=[den_b])
                else:
                    S.op("dve", lambda e: e.reciprocal(out=den_sb[:, qb:qb + 1], in_=o_ps[:, 64:65]), reads=[o_b], writes=[den_b])
                S.op("dve", lambda e: e.tensor_scalar(out=y_sb[:, qb, :], in0=o_ps[:, 0:64], scalar1=den_sb[:, qb:qb + 1], scalar2=None, op0=ALU.mult),
                     reads=[o_b, den_b], writes=[y_b])
                S.op("pe", lambda e: e.transpose(out=pt_ps[:64, qb * 128:(qb + 1) * 128], in_=y_sb[:, qb, :], identity=identb[:, :]), reads=[y_b, identb_b], writes=[pt_b])
            S.op("dve", lambda e: e.tensor_copy(out=yt_sb[:, :nq], in_=pt_ps[:64, :nq]), reads=[pt_b], writes=[yt_b])
            S.dma("sp", yT[mx, h * 64:(h + 1) * 64, q0:q0 + nq], yt_sb[:, :nq], reads=[yt_b], final=True)
    return k.finish()


PASSES = [[0, 1, 2], [3, 4, 5], [6, 7, 8]]


def build_p3(k=None, io=None, do_final=True):
    own = k is None
    if own:
        k = K("p3")
    else:
        k.begin_phase(io)
    S = k.S
    nc = k.nc
    xT = k.dram("xT", [DM, NT], F32, "ExternalInput")
    yT = k.dram("yT", [8, 128, NT], BF16, "ExternalInput")
    cond = k.dram("cond", [128, 8, 2], F32, "ExternalInput")
    modw = k.dram("modw", [DM, 4096], F32, "ExternalInput")
    modb = k.dram("modb", [128, 32], F32, "ExternalInput")
    gain = k.dram("gain", [128, 8], F32, "ExternalInput")
    fgain = k.dram("fgain", [128, 8], F32, "ExternalInput")
    wout = k.dram("wout", [DM, DM], F32, "ExternalInput")
    rw = k.dram("rw", [DM, NEXP], F32, "ExternalInput")
    rb = k.dram("rb", [1, NEXP], F32, "ExternalInput")
    ewin = k.dram("ewin", [NEXP, DM, 2048], F32, "ExternalInput")
    ebin = k.dram("ebin", [128, NEXP, 16], F32, "ExternalInput")
    ewout = k.dram("ewout", [NEXP, DM, DM], F32, "ExternalInput")
    ebout = k.dram("ebout", [NEXP, DM], F32, "ExternalInput")
    ident_d = k.dram("ident", [128, 128], F32, "ExternalInput")
    x2T = k.dram("x2T", [DM, NT], F32, "ExternalOutput")
    xfT = k.dram("xfT", [DM, NT], F32, "ExternalOutput") if do_final else None
    x1T = k.dram("x1T", [DM, NT], F32, "Internal")
    h2T = k.dram("h2T", [DM, NT], BF16, "Internal")
    gT_h = nc.dram_tensor(f"gTd_ph{k.phase}", [NEXP, NT], F32, kind="Internal")
    gT = gT_h.ap()
    x1T_b, h2T_b, gT_b = Buf("x1T"), Buf("h2T"), Buf("gT")

    identb, identb_b, onesb, ones_b, eps_sb, eps_b = load_consts(k, S, ident_d)
    identf, identf_b = k.sb([128, 128], F32, "identf")
    S.dma("sp", identf[:], ident_d[:, :], writes=[identf_b])
    onesf, onesf_b = k.sb([1, 128], F32, "onesf")
    S.op("dve", lambda e: e.memset(onesf[:], 1.0), writes=[onesf_b])
    cond_sb, cond_b = k.sb([128, 8, 2], F32, "cond_sb")
    S.dma("sp", cond_sb[:], cond[:, :, :], writes=[cond_b])
    S.op("act", lambda e: e.activation(out=cond_sb[:], in_=cond_sb[:], func=AF.Silu), reads=[cond_b], writes=[cond_b])
    modb_sb, modb_b = k.sb([128, 32], F32, "modb_sb")
    S.dma("sp", modb_sb[:], modb[:, :], writes=[modb_b])
    gain_sb, gain_b = k.sb([128, 8], F32, "gain_sb")
    S.dma("sp", gain_sb[:], gain[:, :], writes=[gain_b])
    fg_sb, fg_b = k.sb([128, 8], F32, "fg_sb")
    S.dma("sp", fg_sb[:], fgain[:, :], writes=[fg_b])
    mv_sb, mv_b = k.sb([128, 4, 8, 2], F32, "mv_sb")
    A_sb, A_b = k.sb([128, 8, 2], F32, "A_sb")
    ebin_sb, ebin_b = k.sb([128, NEXP, 16], F32, "ebin_sb")
    S.dma("sp", ebin_sb[:], ebin[:, :, :], writes=[ebin_b])
    ebout_sb, ebout_b = k.sb([NEXP, DM], F32, "ebout_sb")
    S.dma("sp", ebout_sb[:], ebout[:, :], writes=[ebout_b])
    rstd_sb, rstd_b = k.sb([128, 512], F32, "rstd_sb")
    sq_sb, sq_b = k.sb([128, 8, 512], BF16, "sq_sb")
    x_sb, x_b = k.sb([128, 8, 512], F32, "x_sb")
    t_sb, t_b = k.sb([128, 512], F32, "t_sb")
    ss_ps, ss_b = k.ps([128, 512], F32, "ss_ps")
    pring = Ring([k.ps([128, 512], F32, f"pp{i}") for i in range(6)])
    tiles = token_tiles()

    stA = contextlib.ExitStack()
    main_stack = k.stack
    k.stack = stA
    wm, wm_b = k.sb([128, 8, 512], F32, "modw_sb")
    emit_mod_vectors(k, S, modw, modb_sb, modb_b, cond_sb, cond_b, 4, mv_sb, mv_b, Ring([(wm, wm_b)]))
    for c in range(2):
        S.op("dve", lambda e: e.scalar_tensor_tensor(out=A_sb[:, :, c], in0=mv_sb[:, 2, :, c], scalar=1.0, in1=gain_sb[:, :], op0=ALU.add, op1=ALU.mult),
             reads=[mv_b, gain_b], writes=[A_b])
    wo_sb, wo_b = k.sb([128, 8, DM], BF16, "wo_sb")
    for kc in range(8):
        S.dma("pool", wo_sb[:, kc, :], wout[kc * 128:(kc + 1) * 128, :], writes=[wo_b])
    rw_sb, rw_b = k.sb([128, 8, NEXP], F32, "rw_sb")
    S.dma("sp", rw_sb[:], rw.rearrange("(kc p) e -> p kc e", p=128), writes=[rw_b])
    rb_sb, rb_b = k.sb([1, NEXP], F32, "rb_sb")
    S.dma("sp", rb_sb[:], rb[:, :], writes=[rb_b])
    y_sb, y_b = k.sb([128, 8, 512], BF16, "y_sb")
    hf_sb, hf_b = k.sb([128, 8, 512], F32, "hf_sb")
    hb_sb, hb_b = k.sb([128, 8, 512], BF16, "hb_sb")
    lg_sb, lg_b = k.sb([128, NEXP], F32, "lg_sb")
    m8_sb, m8_b = k.sb([128, 8], F32, "m8_sb")
    mk_sb, mk_b = k.sb([128, NEXP], F32, "mk_sb")
    ex_sb, ex_b = k.sb([128, NEXP], F32, "ex_sb")
    sm_sb, sm_b = k.sb([128, 2], F32, "sm_sb")
    gt_sb, gt_b = k.sb([NEXP, 512], F32, "gt_sb")
    for (t0, T, is_ctx) in tiles:
        c = 1 if is_ctx else 0
        S.dma("sp", x_sb[:, :, :T], xT[:, t0:t0 + T].rearrange("(kc p) t -> p kc t", p=128), writes=[x_b])
        S.dma("sp", y_sb[:, :, :T], yT[:, :, t0:t0 + T].rearrange("kc p t -> p kc t"), writes=[y_b])
        for o in range(8):
            pa, pa_b = pring.next()
            for kc in range(8):
                S.op("pe", lambda e: e.matmul(pa[:, :T], lhsT=wo_sb[:, kc, o * 128:(o + 1) * 128], rhs=y_sb[:, kc, :T], start=(kc == 0), stop=(kc == 7)),
                     reads=[wo_b, y_b], writes=[pa_b])
            S.op("dve", lambda e: e.scalar_tensor_tensor(out=x_sb[:, o, :T], in0=pa[:, :T], scalar=mv_sb[:, 0, o, c:c + 1], in1=x_sb[:, o, :T], op0=ALU.mult, op1=ALU.add),
                 reads=[pa_b, mv_b, x_b], writes=[x_b])
        S.dma("sp", x1T[:, t0:t0 + T].rearrange("(kc p) t -> p kc t", p=128), x_sb[:, :, :T], reads=[x_b], writes=[x1T_b])
        emit_norm_tile(k, S, x_sb, x_b, T, onesb, ones_b, sq_sb, sq_b, ss_ps, ss_b, rstd_sb, rstd_b, eps_sb, eps_b)
        for kc in range(8):
            S.op("dve", lambda e: e.scalar_tensor_tensor(out=t_sb[:, :T], in0=x_sb[:, kc, :T], scalar=A_sb[:, kc, c:c + 1], in1=rstd_sb[:, :T], op0=ALU.mult, op1=ALU.mult),
                 reads=[x_b, A_b, rstd_b], writes=[t_b])
            S.op("act", lambda e: e.activation(out=hf_sb[:, kc, :T], in_=t_sb[:, :T], func=AF.Identity, bias=mv_sb[:, 1, kc, c:c + 1], scale=1.0),
                 reads=[t_b, mv_b], writes=[hf_b])
            S.op("pool", lambda e: e.tensor_copy(out=hb_sb[:, kc, :T], in_=hf_sb[:, kc, :T]), reads=[hf_b], writes=[hb_b])
        S.dma("sp", h2T[:, t0:t0 + T].rearrange("(kc p) t -> p kc t", p=128), hb_sb[:, :, :T], reads=[hb_b], writes=[h2T_b])
        for s in range(T // 128):
            pr, pr_b = pring.next()
            for kc in range(8):
                S.op("pe", lambda e: e.matmul(pr[:, :NEXP], lhsT=hf_sb[:, kc, s * 128:(s + 1) * 128], rhs=rw_sb[:, kc, :], start=(kc == 0), stop=False),
                     reads=[hf_b, rw_b], writes=[pr_b])
            S.op("pe", lambda e: e.matmul(pr[:, :NEXP], lhsT=onesf[0:1, :], rhs=rb_sb[0:1, :], start=False, stop=True), reads=[onesf_b, rb_b], writes=[pr_b])
            S.op("dve", lambda e: e.tensor_copy(out=lg_sb[:, :], in_=pr[:, :NEXP]), reads=[pr_b], writes=[lg_b])
            S.op("dve", lambda e: e.max(out=m8_sb[:, :], in_=lg_sb[:, :]), reads=[lg_b], writes=[m8_b])
            S.op("dve", lambda e: e.tensor_scalar(out=mk_sb[:, :], in0=lg_sb[:, :], scalar1=m8_sb[:, 3:4], scalar2=None, op0=ALU.is_ge), reads=[lg_b, m8_b], writes=[mk_b])
            S.op("dve", lambda e: e.tensor_scalar(out=sm_sb[:, 0:1], in0=m8_sb[:, 0:1], scalar1=-1.0, scalar2=None, op0=ALU.mult), reads=[m8_b], writes=[sm_b])
            S.op("act", lambda e: e.activation(out=ex_sb[:, :], in_=lg_sb[:, :], func=AF.Exp, bias=sm_sb[:, 0:1], scale=1.0), reads=[lg_b, sm_b], writes=[ex_b])
            S.op("dve", lambda e: e.tensor_tensor(out=ex_sb[:, :], in0=ex_sb[:, :], in1=mk_sb[:, :], op=ALU.mult), reads=[ex_b, mk_b], writes=[ex_b])
            S.op("dve", lambda e: e.reduce_sum(out=sm_sb[:, 1:2], in_=ex_sb[:, :], axis=AX.X), reads=[ex_b], writes=[sm_b])
            S.op("dve", lambda e: e.reciprocal(out=sm_sb[:, 1:2], in_=sm_sb[:, 1:2]), reads=[sm_b], writes=[sm_b])
            S.op("dve", lambda e: e.tensor_scalar(out=ex_sb[:, :], in0=ex_sb[:, :], scalar1=sm_sb[:, 1:2], scalar2=None, op0=ALU.mult), reads=[ex_b, sm_b], writes=[ex_b])
            pt, pt_b = pring.next()
            S.op("pe", lambda e: e.transpose(out=pt[:NEXP, :128], in_=ex_sb[:, :], identity=identf[:, :]), reads=[ex_b, identf_b], writes=[pt_b])
            S.op("act", lambda e: e.copy(out=gt_sb[:, s * 128:(s + 1) * 128], in_=pt[:NEXP, :128]), reads=[pt_b], writes=[gt_b])
        S.dma("sp", gT[:, t0:t0 + T], gt_sb[:, :T], reads=[gt_b], writes=[gT_b])
    k.stack = main_stack
    S.barrier()
    stA.close()

    for tl in PASSES:
        c0 = tiles[tl[0]][0]
        Np = sum(tiles[i][1] for i in tl)
        stB = contextlib.ExitStack()
        k.stack = stB
        hp_sb, hp_b = k.sb([128, 8, Np], BF16, "hp_sb")
        acc_sb, acc_b = k.sb([128, 8, Np], F32, "acc_sb")
        S.dma("sp", hp_sb[:], h2T[:, c0:c0 + Np].rearrange("(kc p) t -> p kc t", p=128), reads=[h2T_b], writes=[hp_b])
        gq_sb, gq_b = k.sb([NEXP, 512], F32, "gq_sb")
        for i in tl:
            t0, T, _ = tiles[i]
            S.dma("sp", gq_sb[:, :T], gT[:, t0:t0 + T], reads=[gT_b], writes=[gq_b])
            for o in range(8):
                pa, pa_b = pring.next()
                S.op("pe", lambda e: e.matmul(pa[:, :T], lhsT=ebout_sb[:, o * 128:(o + 1) * 128], rhs=gq_sb[:, :T], start=True, stop=True), reads=[ebout_b, gq_b], writes=[pa_b])
                S.op("act", lambda e: e.copy(out=acc_sb[:, o, t0 - c0:t0 - c0 + T], in_=pa[:, :T]), reads=[pa_b], writes=[acc_b])
        stE = contextlib.ExitStack()
        k.stack = stE
        wi_ring = Ring([k.sb([128, 8, 1024], BF16, f"wi{i}") for i in range(2)])
        wo_ring = Ring([k.sb([128, 4, 1024], BF16, f"wo{i}") for i in range(2)])
        act_ring = Ring([k.sb([128, 4, 512], BF16, f"ac{i}") for i in range(2)])
        g1r = Ring([k.sb([128, 512], F32, f"g1{i}") for i in range(2)])
        sgr = Ring([k.sb([128, 512], F32, f"sg{i}") for i in range(2)])
        l1r = Ring([k.sb([128, 512], F32, f"l1{i}") for i in range(2)])
        gbr = Ring([k.sb([128, 512], F32, f"gb{i}") for i in range(2)])

        wstg = Ring([k.sb([128, 1024], F32, "wstg") for i in range(4)])

        def load_pieces(he):
            e_, hf = he // 2, he % 2
            wi, wi_b = wi_ring.next()
            wo, wo_b = wo_ring.next()
            pieces = []
            for kc in range(8):
                def p_in(kc=kc):
                    st, st_b = wstg.next()
                    S.dma("sp", st[:, 0:512], ewin[e_, kc * 128:(kc + 1) * 128, hf * 512:(hf + 1) * 512], writes=[st_b])
                    S.dma("sp", st[:, 512:1024], ewin[e_, kc * 128:(kc + 1) * 128, 1024 + hf * 512:1024 + (hf + 1) * 512], writes=[st_b])
                    S.op("pool", lambda e: e.tensor_copy(out=wi[:, kc, :], in_=st[:, :]), reads=[st_b], writes=[wi_b])
                pieces.append(p_in)
            for kc in range(4):
                def p_out(kc=kc):
                    st, st_b = wstg.next()
                    r0 = hf * 512 + kc * 128
                    S.dma("sp", st[:, :], ewout[e_, r0:r0 + 128, :], writes=[st_b])
                    S.op("pool", lambda e: e.tensor_copy(out=wo[:, kc, :], in_=st[:, :]), reads=[st_b], writes=[wo_b])
                pieces.append(p_out)
            return (wi, wi_b, wo, wo_b), pieces

        def load_w(he):
            bufs, pieces = load_pieces(he)
            for p in pieces:
                p()
            return bufs

        nxt = load_w(0)
        for he in range(2 * NEXP):
            e_, hf = he // 2, he % 2
            wi, wi_b, wo, wo_b = nxt
            pend = []
            if he + 1 < 2 * NEXP:
                nxt, pend = load_pieces(he + 1)
            def emit_down(ac, ac_b, lo, T):
                for o in range(8):
                    py, py_b = pring.next()
                    for kc in range(4):
                        S.op("pe", lambda e: e.matmul(py[:, :T], lhsT=wo[:, kc, o * 128:(o + 1) * 128], rhs=ac[:, kc, :T], start=(kc == 0), stop=(kc == 3)),
                             reads=[wo_b, ac_b], writes=[py_b])
                    S.op("dve", lambda e: e.tensor_tensor(out=acc_sb[:, o, lo:lo + T], in0=py[:, :T], in1=acc_sb[:, o, lo:lo + T], op=ALU.add), reads=[py_b, acc_b], writes=[acc_b])
            prev = None
            for i in tl:
                t0, T, _ = tiles[i]
                lo = t0 - c0
                gb, gb_b = gbr.next()
                S.dma("sp", gb[:, :T], bass.AP(gT_h, e_ * NT + t0, [[0, 128], [1, T]]), reads=[gT_b], writes=[gb_b])
                ac, ac_b = act_ring.next()
                for dc in range(4):
                    pg, pg_b = pring.next()
                    pl, pl_b = pring.next()
                    for kc in range(8):
                        S.op("pe", lambda e: e.matmul(pg[:, :T], lhsT=wi[:, kc, dc * 128:(dc + 1) * 128], rhs=hp_sb[:, kc, lo:lo + T], start=(kc == 0), stop=(kc == 7)),
                             reads=[wi_b, hp_b], writes=[pg_b])
                    for kc in range(8):
                        S.op("pe", lambda e: e.matmul(pl[:, :T], lhsT=wi[:, kc, 512 + dc * 128:512 + (dc + 1) * 128], rhs=hp_sb[:, kc, lo:lo + T], start=(kc == 0), stop=(kc == 7)),
                             reads=[wi_b, hp_b], writes=[pl_b])
                    gi = hf * 4 + dc
                    li = 8 + hf * 4 + dc
                    g1, g1_b = g1r.next()
                    sg, sg_b = sgr.next()
                    l1, l1_b = l1r.next()
                    S.op("dve", lambda e: e.tensor_scalar(out=g1[:, :T], in0=pg[:, :T], scalar1=ebin_sb[:, e_, gi:gi + 1], scalar2=7.0, op0=ALU.add, op1=ALU.min), reads=[pg_b, ebin_b], writes=[g1_b])
                    S.op("act", lambda e: e.activation(out=sg[:, :T], in_=g1[:, :T], func=AF.Sigmoid, scale=1.702), reads=[g1_b], writes=[sg_b])
                    S.op("dve", lambda e: e.tensor_scalar(out=l1[:, :T], in0=pl[:, :T], scalar1=ebin_sb[:, e_, li:li + 1], scalar2=None, op0=ALU.add), reads=[pl_b, ebin_b], writes=[l1_b])
                    S.op("pool", lambda e: e.tensor_scalar(out=l1[:, :T], in0=l1[:, :T], scalar1=7.0, scalar2=-7.0, op0=ALU.min, op1=ALU.max), reads=[l1_b], writes=[l1_b])
                    S.op("pool", lambda e: e.tensor_tensor(out=g1[:, :T], in0=g1[:, :T], in1=sg[:, :T], op=ALU.mult), reads=[g1_b, sg_b], writes=[g1_b])
                    S.op("dve", lambda e: e.scalar_tensor_tensor(out=g1[:, :T], in0=l1[:, :T], scalar=1.0, in1=g1[:, :T], op0=ALU.add, op1=ALU.mult), reads=[g1_b, l1_b], writes=[g1_b])
                    S.op("dve", lambda e: e.tensor_tensor(out=ac[:, dc, :T], in0=g1[:, :T], in1=gb[:, :T], op=ALU.mult), reads=[g1_b, gb_b], writes=[ac_b])
                    if pend:
                        pend.pop(0)()
                if prev is not None:
                    emit_down(*prev)
                prev = (ac, ac_b, lo, T)
            emit_down(*prev)
            while pend:
                pend.pop(0)()
        k.stack = stB
        S.barrier()
        stE.close()
        ob_ring = Ring([k.sb([128, 8, 512], F32, f"ob{i}") for i in range(2)])
        for i in tl:
            t0, T, is_ctx = tiles[i]
            c = 1 if is_ctx else 0
            lo = t0 - c0
            S.dma("sp", x_sb[:, :, :T], x1T[:, t0:t0 + T].rearrange("(kc p) t -> p kc t", p=128), reads=[x1T_b], writes=[x_b])
            for o in range(8):
                S.op("dve", lambda e: e.scalar_tensor_tensor(out=x_sb[:, o, :T], in0=acc_sb[:, o, lo:lo + T], scalar=mv_sb[:, 3, o, c:c + 1], in1=x_sb[:, o, :T], op0=ALU.mult, op1=ALU.add),
                     reads=[acc_b, mv_b, x_b], writes=[x_b])
            S.dma("sp", x2T[:, t0:t0 + T].rearrange("(kc p) t -> p kc t", p=128), x_sb[:, :, :T], reads=[x_b], final=True)
            if not do_final:
                continue
            emit_norm_tile(k, S, x_sb, x_b, T, onesb, ones_b, sq_sb, sq_b, ss_ps, ss_b, rstd_sb, rstd_b, eps_sb, eps_b)
            ob, ob_b = ob_ring.next()
            for o in range(8):
                S.op("dve", lambda e: e.scalar_tensor_tensor(out=ob[:, o, :T], in0=x_sb[:, o, :T], scalar=fg_sb[:, o:o + 1], in1=rstd_sb[:, :T], op0=ALU.mult, op1=ALU.mult),
                     reads=[x_b, fg_b, rstd_b], writes=[ob_b])
            S.dma("sp", xfT[:, t0:t0 + T].rearrange("(kc p) t -> p kc t", p=128), ob[:, :, :T], reads=[ob_b], final=True)
        k.stack = main_stack
        S.barrier()
        stB.close()
    if own:
        return k.finish()
    k.end_phase()


def emit_p2f(k, io, odd):
    k.begin_phase(io)
    S = k.S
    FR = 1952 if odd else 1664
    fm = io["fm"]
    GK = io["GK"]
    GV = io["GV"]
    tm = io["tm"]
    yT = io["yT"]
    ident_d = io["ident"]
    identb, identb_b, onesb, ones_b, eps_sb, eps_b = load_consts(k, S, ident_d)
    NB = 130 if odd else 50
    NCOL = NB * 128
    dkmax = 96 if odd else 64
    qT_sb, qT_b = k.sb([dkmax, NT], BF16, "qT_sb")
    kT_sb, kT_b = k.sb([dkmax, NCOL], BF16, "kT_sb")
    va_sb, va_b = k.sb([128, NB, 65], BF16, "va_sb")
    S.op("dve", lambda e: e.memset(va_sb[:, :, 64:65], 1.0), writes=[va_b])
    sring = Ring([k.ps([128, 512], F32, "s") for i in range(2)])
    O = [k.ps([128, 512], F32, "o") for i in range(4)]
    pt_ps, pt_b = k.ps([128, 512], BF16, "ptp")
    pring = Ring([k.sb([128, 512], BF16, "pT") for i in range(3)])
    den_sb, den_b = k.sb([128, 4], F32, "den")
    y_sb, y_b = k.sb([128, 4, 64], BF16, "y_sb")
    yTring = Ring([k.sb([64, 512], BF16, "yT") for i in range(2)])
    if not odd:
        maskA, maskA_b = k.sb([128, 6, 512], BF16, "maskA_sb")
        S.dma("pool", maskA[:], io["maskA"][:, :, :], writes=[maskA_b])
        candA, candA_b = k.sb([128, 8, 512], BF16, "candA_sb")
        S.dma("pool", candA[:], io["candA"][:, :, :], writes=[candA_b])
        selB, selB_b = k.sb([128, 8], F32, "selB_sb")
        S.dma("sp", selB[:], io["selB"][:, :], writes=[selB_b])
        wvar, wvar_b = k.sb([128, 4], F32, "wvar_sb")
        S.dma("sp", wvar[:], io["wvar"][:, :], writes=[wvar_b])
        esink, esink_b = k.sb([128, 8], F32, "esink_sb")
        S.dma("sp", esink[:], io["sink"][:, :], writes=[esink_b])
        S.op("act", lambda e: e.activation(out=esink[:], in_=esink[:], func=AF.Exp), reads=[esink_b], writes=[esink_b])
        stg_ring = Ring([k.sb([128, 512], F32, "stg") for i in range(2)])
        Mv = [k.sb([128, 8, 512], BF16, f"Mv{v}") for v in range(3)]
        MT0, MT0_b = k.sb([128, 8, 512], BF16, "MT0")
        MT7, MT7_b = k.sb([128, 8, 512], BF16, "MT7")
        tmpM, tmpM_b = k.sb([128, 8, 512], BF16, "tmpM")
        nbr = nbr_index()
        needB = [[any(bool(nbr[v][0][:, kb, qb * 128:(qb + 1) * 128].any()) for v in range(3)) for qb in range(4)] for kb in range(8)]
        mA = _maskA_np()
        needA = [[bool(mA[:, kb, qb * 128:(qb + 1) * 128].any()) for qb in range(4)] for kb in range(6)]

    def hb(r, which, b):
        return 34 + r * 4 + which * 2 + b

    def gv_rows(r, t0, n):
        out = []
        for (c0, cn, ap) in GV:
            a, b = max(t0, c0), min(t0 + n, c0 + cn)
            if a < b:
                out.append((ap[r * cn + (a - c0):r * cn + (b - c0), :], a, b - a))
        return out

    def load_v(dst_blk0, r, t0, n, vcol):
        for (ap, a, m) in gv_rows(r, t0, n):
            b0 = dst_blk0 + (a - t0) // 128
            S.dma("sp", va_sb[:, b0:b0 + m // 128, 0:64], ap[:, vcol:vcol + 64].rearrange("(kb p) d -> p kb d", p=128), writes=[va_b])

    def load_kv(krows, dk_parts, vcol):
        for (row0, nr, p0) in krows:
            gap, gn = GK[row0]
            assert gn == nr
            S.dma("sp", kT_sb[p0:p0 + nr, 0:CTX], fm[row0:row0 + nr, 0:CTX], writes=[kT_b])
            if odd:
                for r in range(4):
                    S.dma("sp", kT_sb[p0:p0 + nr, CTX + r * LAT_PC:CTX + (r + 1) * LAT_PC], gap[r * nr:(r + 1) * nr, CTX:NT], writes=[kT_b])
            else:
                S.dma("sp", kT_sb[p0:p0 + nr, CTX:NT], fm[row0:row0 + nr, CTX:NT], writes=[kT_b])
                for r in range(4):
                    S.dma("sp", kT_sb[p0:p0 + nr, NT + r * 512:NT + r * 512 + 256], gap[r * nr:(r + 1) * nr, NT - 256:NT], writes=[kT_b])
                    S.dma("sp", kT_sb[p0:p0 + nr, NT + r * 512 + 256:NT + r * 512 + 512], gap[r * nr:(r + 1) * nr, CTX:CTX + 256], writes=[kT_b])
        S.dma("sp", va_sb[:, 0:2, 0:64], tm[0:CTX, vcol:vcol + 64].rearrange("(kb p) d -> p kb d", p=128), writes=[va_b])
        if odd:
            for r in range(4):
                load_v(2 + r * 32, r, CTX, LAT_PC, vcol)
        else:
            S.dma("sp", va_sb[:, 2:34, 0:64], tm[CTX:NT, vcol:vcol + 64].rearrange("(kb p) d -> p kb d", p=128), writes=[va_b])
            for r in range(4):
                load_v(34 + r * 4, r, NT - 256, 256, vcol)
                load_v(36 + r * 4, r, CTX, 256, vcol)

    if odd:
        jobs = [("C", h) for h in range(8)] + [("D", h) for h in range(8)]
    else:
        jobs = [("A", h) for h in range(8)] + [("B", h) for h in range(8)]
    for (kind, h) in jobs:
        g = h // 4
        if kind == "A":
            dk, qrow, ychunk = 64, h * 64, h // 2
            if h % 4 == 0:
                load_kv([(512 + g * 64, 64, 0)], 64, g * 64)
        elif kind == "B":
            dk, qrow, ychunk = 64, 640 + h * 64, 4 + h // 2
            load_kv([(1152 + h * 64, 64, 0)], 64, 128 + h * 64)
            for v in range(3):
                for kb in range(8):
                    stg, stg_b = stg_ring.next()
                    S.dma("sp", stg[:], io["rpbx"][h, v, :, kb, :], writes=[stg_b])
                    S.op("act", lambda e: e.activation(out=Mv[v][0][:, kb, :], in_=stg[:], func=AF.Exp), reads=[stg_b], writes=[Mv[v][1]])
            S.op("dve", lambda e: e.tensor_scalar(out=tmpM[:], in0=Mv[1][0][:], scalar1=wvar[:, 1:2], scalar2=None, op0=ALU.mult), reads=[Mv[1][1], wvar_b], writes=[tmpM_b])
            S.op("dve", lambda e: e.scalar_tensor_tensor(out=MT0[:], in0=Mv[0][0][:], scalar=wvar[:, 0:1], in1=tmpM[:], op0=ALU.mult, op1=ALU.add), reads=[Mv[0][1], wvar_b, tmpM_b], writes=[MT0_b])
            S.op("dve", lambda e: e.tensor_scalar(out=tmpM[:], in0=Mv[1][0][:], scalar1=wvar[:, 3:4], scalar2=None, op0=ALU.mult), reads=[Mv[1][1], wvar_b], writes=[tmpM_b])
            S.op("dve", lambda e: e.scalar_tensor_tensor(out=MT7[:], in0=Mv[2][0][:], scalar=wvar[:, 2:3], in1=tmpM[:], op0=ALU.mult, op1=ALU.add), reads=[Mv[2][1], wvar_b, tmpM_b], writes=[MT7_b])
        elif kind == "C":
            dk, qrow, ychunk = 96, h * 96, h // 2
            load_kv([(768 + h * 64, 64, 0), (1280, 32, 64)], 96, h * 64)
        else:
            dk, qrow, ychunk = 64, 1312 + h * 64, 4 + h // 2
            if h % 4 == 0:
                load_kv([(1824 + g * 64, 64, 0)], 64, 512 + g * 64)
        scale = float(dk) ** -0.5
        S.dma("sp", qT_sb[:dk, :], fm[qrow:qrow + dk, :], writes=[qT_b])
        tiles = [(0, 256, None)] + [(CTX + 512 * i, 512, i) for i in range(LAT_PC // 512)]
        for (q0, nq, ti) in tiles:
            nqb = nq // 128
            kbl = []
            allq = [True] * nqb
            if ti is None:
                kbl = [(0, None, None, None, allq), (1, None, None, None, allq)]
            elif odd:
                kbl = [(kb, None, None, None, allq) for kb in range(NB)]
            elif kind == "A":
                for kbrel in range(6):
                    lb = 4 * ti + kbrel - 1
                    if 0 <= lb < 32:
                        kbl.append((2 + lb, maskA[:, kbrel, :], maskA_b, None, needA[kbrel]))
                    elif lb < 0:
                        for r in range(4):
                            kbl.append((hb(r, 0, 1), candA[:, r, :], candA_b, None, needA[kbrel]))
                    else:
                        for r in range(4):
                            kbl.append((hb(r, 1, 0), candA[:, 4 + r, :], candA_b, None, needA[kbrel]))
                kbl += [(0, None, None, None, allq), (1, None, None, None, allq)]
            else:
                M, M_b = (MT0, MT0_b) if ti == 0 else ((MT7, MT7_b) if ti == 7 else Mv[1])
                for kbrel in range(8):
                    lb = 4 * ti - 2 + kbrel
                    if not any(needB[kbrel]):
                        continue
                    if 0 <= lb < 32:
                        kbl.append((2 + lb, M[:, kbrel, :], M_b, None, needB[kbrel]))
                    elif lb < 0:
                        for r in range(4):
                            kbl.append((hb(r, 0, lb + 2), M[:, kbrel, :], M_b, selB[:, r:r + 1], needB[kbrel]))
                    else:
                        for r in range(4):
                            kbl.append((hb(r, 1, lb - 32), M[:, kbrel, :], M_b, selB[:, 4 + r:5 + r], needB[kbrel]))
                kbl += [(0, None, None, None, allq), (1, None, None, None, allq)]
            first = [min(i for i, ent in enumerate(kbl) if ent[4][qb]) for qb in range(nqb)]
            last = [max(i for i, ent in enumerate(kbl) if ent[4][qb]) for qb in range(nqb)]
            for i, (kb, mask_ap, mask_b, scal, nd) in enumerate(kbl):
                s_ps, s_b = sring.next()
                S.op("pe", lambda e: e.matmul(s_ps[:, :nq], lhsT=kT_sb[:dk, kb * 128:(kb + 1) * 128], rhs=qT_sb[:dk, q0:q0 + nq], start=True, stop=True),
                     reads=[kT_b, qT_b], writes=[s_b])
                p_sb, p_b = pring.next()
                S.op("act", lambda e: e.activation(out=p_sb[:, :nq], in_=s_ps[:, :nq], func=AF.Exp, scale=scale), reads=[s_b], writes=[p_b])
                if mask_ap is not None:
                    if scal is None:
                        S.op("dve" if i % 2 == 0 else "pool", lambda e: e.tensor_tensor(out=p_sb[:, :nq], in0=p_sb[:, :nq], in1=mask_ap, op=ALU.mult),
                             reads=[p_b, mask_b], writes=[p_b])
                    else:
                        S.op("dve", lambda e: e.scalar_tensor_tensor(out=p_sb[:, :nq], in0=p_sb[:, :nq], scalar=scal, in1=mask_ap, op0=ALU.mult, op1=ALU.mult),
                             reads=[p_b, mask_b, selB_b], writes=[p_b])
                for qb in range(nqb):
                    if nd[qb]:
                        o_ps, o_b = O[qb]
                        S.op("pe", lambda e: e.matmul(o_ps[:, 0:65], lhsT=p_sb[:, qb * 128:(qb + 1) * 128], rhs=va_sb[:, kb, :],
                                                      start=(i == first[qb]), stop=(i == last[qb])), reads=[p_b, va_b], writes=[o_b])
            yt_sb, yt_b = yTring.next()
            for qb in range(nqb):
                o_ps, o_b = O[qb]
                if kind == "A":
                    S.op("dve", lambda e: e.tensor_tensor(out=den_sb[:, qb:qb + 1], in0=o_ps[:, 64:65], in1=esink[:, h:h + 1], op=ALU.add), reads=[o_b, esink_b], writes=[den_b])
                    S.op("dve", lambda e: e.reciprocal(out=den_sb[:, qb:qb + 1], in_=den_sb[:, qb:qb + 1]), reads=[den_b], writes=[den_b])
                else:
                    S.op("dve", lambda e: e.reciprocal(out=den_sb[:, qb:qb + 1], in_=o_ps[:, 64:65]), reads=[o_b], writes=[den_b])
                S.op("dve", lambda e: e.tensor_scalar(out=y_sb[:, qb, :], in0=o_ps[:, 0:64], scalar1=den_sb[:, qb:qb + 1], scalar2=None, op0=ALU.mult),
                     reads=[o_b, den_b], writes=[y_b])
                S.op("pe", lambda e: e.transpose(out=pt_ps[:64, qb * 128:(qb + 1) * 128], in_=y_sb[:, qb, :], identity=identb[:, :]), reads=[y_b, identb_b], writes=[pt_b])
            S.op("dve", lambda e: e.tensor_copy(out=yt_sb[:, :nq], in_=pt_ps[:64, :nq]), reads=[pt_b], writes=[yt_b])
            S.dma("sp", yT[ychunk, (h % 2) * 64:(h % 2) * 64 + 64, q0:q0 + nq], yt_sb[:, :nq], reads=[yt_b])
    k.end_phase()


def build_fused(depth=DEPTH):
    k = K("fused")
    k.fused = True
    S = k.S
    nc = k.nc
    EI = "ExternalInput"
    g = {}
    g["xT0"] = k.dram("xT0", [DM, NT], F32, EI)
    for nm, shp in (("cond", [128, 8, 2]), ("ident", [128, 128]), ("blk", [128, 128]), ("ropeC", [128, NT]), ("ropeS", [128, NT]),
                    ("ropeC2", [96, NT]), ("ropeS2", [96, NT]), ("ropeC3", [32, NT]), ("ropeS3", [32, NT]), ("fgain", [128, 8]),
                    ("maskA", [128, 6, 512]), ("candA", [128, 8, 512]), ("selB", [128, 8]), ("wvar", [128, 4])):
        g[nm] = k.dram(nm, shp, F32, EI)
    L = []
    for l in range(depth):
        odd = l % 2 == 1
        d = {}
        d["modw"] = k.dram(f"modw{l}", [DM, 6144], F32, EI)
        d["modb"] = k.dram(f"modb{l}", [128, 48], F32, EI)
        d["gmix"] = k.dram(f"gmix{l}", [128, 8], F32, EI)
        d["gffn"] = k.dram(f"gffn{l}", [128, 8], F32, EI)
        d["w"] = k.dram(f"w{l}", [DM, (1824 + 32 + 640) if odd else (2304 + 640)], F32, EI)
        if odd:
            d["wqb"] = k.dram(f"wqb{l}", [768, 1536], F32, EI)
            d["wkvb"] = k.dram(f"wkvb{l}", [256, 1024], F32, EI)
            d["qn"] = k.dram(f"qn{l}", [128, 6], F32, EI)
            d["kvn"] = k.dram(f"kvn{l}", [128, 2], F32, EI)
            d["dgq"] = k.dram(f"dgq{l}", [128, 4], F32, EI)
        else:
            d["sink"] = k.dram(f"sink{l}", [128, 8], F32, EI)
            d["rpbx"] = k.dram(f"rpbx{l}", [8, 3, 128, 8, 512], F32, EI)
        d["wout"] = k.dram(f"wout{l}", [DM, DM], F32, EI)
        d["rw"] = k.dram(f"rw{l}", [DM, NEXP], F32, EI)
        d["rb"] = k.dram(f"rb{l}", [1, NEXP], F32, EI)
        d["ewin"] = k.dram(f"ewin{l}", [NEXP, DM, 2048], F32, EI)
        d["ebin"] = k.dram(f"ebin{l}", [128, NEXP, 16], F32, EI)
        d["ewout"] = k.dram(f"ewout{l}", [NEXP, DM, DM], F32, EI)
        d["ebout"] = k.dram(f"ebout{l}", [NEXP, DM], F32, EI)
        L.append(d)
    out = k.dram("out", [DM, NT], F32, "ExternalOutput")
    XA = k.dram("XA", [DM, NT], F32, "Internal")
    XB = k.dram("XB", [DM, NT], F32, "Internal")
    fmE = k.dram("fmE", [1664, NT], BF16, "Internal")
    fmO = k.dram("fmO", [1952, NT], BF16, "Internal")
    tmE = k.dram("tmE", [NT, 640], BF16, "Internal")
    tmO = k.dram("tmO", [NT, 640], BF16, "Internal")
    def kpieces(odd):
        rows = [(768 + 64 * i, 64) for i in range(8)] + [(1280, 32)] + [(1824, 64), (1888, 64)] if odd else \
               [(512, 64), (576, 64)] + [(1152 + 64 * i, 64) for i in range(8)]
        return rows
    vpieces = [(c * 512, min(512, NT - c * 512)) for c in range((NT + 511) // 512)]
    GKs, GVs = {}, {}
    for par, tag in ((False, "E"), (True, "O")):
        GKs[par] = {r0: (k.dram(f"gk{tag}{r0}", [4 * n, NT], BF16, "Internal"), n) for (r0, n) in kpieces(par)}
        GVs[par] = [(t0, n, k.dram(f"gv{tag}{t0}", [4 * n, 640], BF16, "Internal")) for (t0, n) in vpieces]
    yTd = k.dram("yTd", [8, 128, NT], BF16, "Internal")
    groups = [[0, 1, 2, 3], [4, 5, 6, 7]]
    ccscr, _ = k.sb([1, 4], F32, "ccscr")
    xs = [g["xT0"], XA, XB]
    for l in range(depth):
        odd = l % 2 == 1
        d = L[l]
        xin = xs[0] if l == 0 else xs[1 + (l - 1) % 2]
        xout = xs[1 + l % 2]
        fm_, tm_ = (fmO, tmO) if odd else (fmE, tmE)
        io = {"xT": xin, "cond": g["cond"], "modw": d["modw"][:, 0:2048], "modb": d["modb"][:, 0:16], "gain": d["gmix"],
              "ropeC": g["ropeC"], "ropeS": g["ropeS"], "w": d["w"], "fm": fm_, "tm": tm_, "ident": g["ident"]}
        if odd:
            io.update({"wqb": d["wqb"], "wkvb": d["wkvb"], "qn": d["qn"], "kvn": d["kvn"], "dgq": d["dgq"], "ropeC2": g["ropeC2"],
                       "ropeS2": g["ropeS2"], "ropeC3": g["ropeC3"], "ropeS3": g["ropeS3"], "blk": g["blk"]})
        build_p1(odd, k, io)
        for r0, (gap, n) in GKs[odd].items():
            S.cc(k.stack, lambda e: e.collective_compute("AllGather", ALU.bypass, replica_groups=groups, ins=[fm_[r0:r0 + n, :]], outs=[gap]), ccscr[0:1, 0:4])
        for (t0, n, gap) in GVs[odd]:
            S.cc(k.stack, lambda e: e.collective_compute("AllGather", ALU.bypass, replica_groups=groups, ins=[tm_[t0:t0 + n, :]], outs=[gap]), ccscr[0:1, 0:4])
        S.barrier()
        io2 = {"fm": fm_, "tm": tm_, "GK": GKs[odd], "GV": GVs[odd], "yT": yTd, "ident": g["ident"]}
        if not odd:
            io2.update({"maskA": g["maskA"], "candA": g["candA"], "selB": g["selB"], "wvar": g["wvar"], "sink": d["sink"], "rpbx": d["rpbx"]})
        emit_p2f(k, io2, odd)
        last = l == depth - 1
        io3 = {"xT": xin, "yT": yTd, "cond": g["cond"], "modw": d["modw"][:, 2048:6144], "modb": d["modb"][:, 16:48], "gain": d["gffn"],
               "fgain": g["fgain"], "wout": d["wout"], "rw": d["rw"], "rb": d["rb"], "ewin": d["ewin"], "ebin": d["ebin"],
               "ewout": d["ewout"], "ebout": d["ebout"], "ident": g["ident"], "x2T": xout, "xfT": out}
        build_p3(k, io3, do_final=last)
    return k.finish()


def kernel(x, c, ctx, c_ctx, mod_w, mod_b, norm_mix, norm_ffn, ab_w_in, ab_w_out, a_sink, b_rpb,
                 cd_w_in, c_q_norm, c_w_q_b, c_kv_norm, c_w_kv_b, d_q_norm, d_k_norm, cd_w_out,
                 router_w, router_b, exp_w_in, exp_b_in, exp_w_out, exp_b_out, final_norm, _depth=DEPTH):
    f32 = lambda a: np.ascontiguousarray(np.asarray(a, np.float32))
    x, c, ctx, c_ctx = f32(x), f32(c), f32(ctx), f32(c_ctx)
    if ("fused", _depth) not in _PROGS:
        _PROGS[("fused", _depth)] = build_fused(_depth)
    nc = _PROGS[("fused", _depth)]
    Ch, Sh = _rope_tables(64, [d for _ in range(2) for d in range(64)])
    Cm, Sm = _rope_tables(32, [-1] * 64 + list(range(32)))
    C3, S3 = _rope_tables(32, list(range(32)))
    mA = _maskA_np()
    shared = {"ident": np.eye(128, dtype=np.float32), "blk": np.kron(np.eye(2, dtype=np.float32), np.ones((64, 64), np.float32)),
              "fgain": fm(final_norm, 8), "maskA": mA}
    for l in range(_depth):
        i = l // 2
        odd = l % 2 == 1
        shared[f"modw{l}"] = f32(mod_w[l])
        shared[f"modb{l}"] = fm(f32(mod_b[l]), 48)
        shared[f"gmix{l}"] = fm(norm_mix[l], 8)
        shared[f"gffn{l}"] = fm(norm_ffn[l], 8)
        if not odd:
            w = f32(ab_w_in[i])
            shared[f"w{l}"] = np.ascontiguousarray(np.concatenate([w, _swap_cols(w, 0, 10, 64, 0, 64)], axis=1))
            shared[f"sink{l}"] = np.ascontiguousarray(np.tile(f32(a_sink[i])[None, :], (128, 1)))
            rp = f32(b_rpb[i])
            rx = np.empty((8, 3, 128, 8, 512), np.float32)
            for hh in range(8):
                for v, (valid, dr, dc) in enumerate(nbr_index()):
                    rx[hh, v] = np.where(valid, rp[hh][dr, dc], np.float32(-30000.0))
            shared[f"rpbx{l}"] = rx
            shared[f"wout{l}"] = f32(ab_w_out[i])
        else:
            w = f32(cd_w_in[i])
            shared[f"w{l}"] = np.ascontiguousarray(np.concatenate([w, _swap_cols(w, 1024, 1, 32, 0, 32), _swap_cols(w, 1056, 10, 64, 0, 64)], axis=1))
            wq = f32(c_w_q_b[i])
            shared[f"wqb{l}"] = np.ascontiguousarray(np.concatenate([wq, _swap_cols(wq, 0, 8, 96, 64, 32)], axis=1))
            wkv = f32(c_w_kv_b[i]).reshape(256, 8, 128)
            shared[f"wkvb{l}"] = np.ascontiguousarray(np.concatenate([wkv[:, :, :64].reshape(256, 512), wkv[:, :, 64:].reshape(256, 512)], axis=1))
            shared[f"qn{l}"] = fm(c_q_norm[i], 6)
            shared[f"kvn{l}"] = fm(c_kv_norm[i], 2)
            gq, gk = f32(d_q_norm[i]), f32(d_k_norm[i])
            sw = (np.arange(64) + 32) % 64
            shared[f"dgq{l}"] = np.ascontiguousarray(np.stack([np.tile(gq, 2), np.tile(gq[sw], 2), np.tile(gk, 2), np.tile(gk[sw], 2)], axis=1))
            shared[f"wout{l}"] = f32(cd_w_out[i])
        shared[f"rw{l}"] = f32(router_w[l])
        shared[f"rb{l}"] = f32(router_b[l])[None, :]
        shared[f"ewin{l}"] = f32(exp_w_in[l])
        shared[f"ebin{l}"] = np.ascontiguousarray(f32(exp_b_in[l]).reshape(NEXP, 16, 128).transpose(2, 0, 1))
        shared[f"ewout{l}"] = f32(exp_w_out[l])
        shared[f"ebout{l}"] = f32(exp_b_out[l])
    ins = []
    for core in range(NCORES):
        b, r = core // 4, core % 4
        d = dict(shared)
        tok = np.concatenate([ctx[b], x[b, r * LAT_PC:(r + 1) * LAT_PC]], axis=0)
        d["xT0"] = np.ascontiguousarray(tok.T)
        d["cond"] = np.ascontiguousarray(np.stack([fm(c[b], 8), fm(c_ctx, 8)], axis=-1))
        d["ropeC"], d["ropeS"] = _tabs_for_core(Ch, Sh, r)
        d["ropeC2"], d["ropeS2"] = _tabs_for_core(Cm, Sm, r)
        d["ropeC3"], d["ropeS3"] = _tabs_for_core(C3, S3, r)
        cand = np.zeros((128, 8, 512), np.float32)
        sel = np.zeros((128, 8), np.float32)
        for rr in range(4):
            if rr == r - 1:
                cand[:, rr, :] = mA[:, 0, :]
                sel[:, rr] = 1.0
            if rr == r + 1:
                cand[:, 4 + rr, :] = mA[:, 5, :]
                sel[:, 4 + rr] = 1.0
        d["candA"], d["selB"] = cand, sel
        wv = np.zeros((128, 4), np.float32)
        wv[:, 0] = 1.0 if r == 0 else 0.0
        wv[:, 1] = 0.0 if r == 0 else 1.0
        wv[:, 2] = 1.0 if r == 3 else 0.0
        wv[:, 3] = 0.0 if r == 3 else 1.0
        d["wvar"] = wv
        ins.append(d)
    res = run_bass_kernel_spmd(nc, ins, core_ids=list(range(NCORES))).results
    out = np.empty((BATCH, SEQ, DM), np.float32)
    for core in range(NCORES):
        b, r = core // 4, core % 4
        out[b, r * LAT_PC:(r + 1) * LAT_PC] = res[core]["out"][:, CTX:].T
    return out


_PROGS = {}
BF = ml_dtypes.bfloat16


def _prog(name):
    if name not in _PROGS:
        if name == "p1e":
            _PROGS[name] = build_p1(False)
        elif name == "p1o":
            _PROGS[name] = build_p1(True)
        elif name == "p2e":
            _PROGS[name] = build_p2(False)
        elif name == "p2o":
            _PROGS[name] = build_p2(True)
        else:
            _PROGS[name] = build_p3()
    return _PROGS[name]


def _run(name, in_maps):
    res = run_bass_kernel_spmd(_prog(name), in_maps, core_ids=list(range(NCORES)))
    return res.results


def fm(v, n):
    return np.ascontiguousarray(np.asarray(v, np.float32).reshape(n, 128).T)


def _rope_tables(dim, rows_pattern):
    t = np.arange(SEQ, dtype=np.int32)
    row = (t // GRID_W).astype(np.float32)
    col = (t % GRID_W).astype(np.float32)
    quarter = dim // 4
    inv = (np.float32(10000.0) ** (-np.arange(quarter, dtype=np.float32) / np.float32(quarter))).astype(np.float32)
    ang = np.concatenate([row[:, None] * inv, col[:, None] * inv], axis=-1).astype(np.float32)
    cos, sin = np.cos(ang).astype(np.float32), np.sin(ang).astype(np.float32)
    half = dim // 2
    C = np.ones((len(rows_pattern), SEQ), np.float32)
    Sn = np.zeros((len(rows_pattern), SEQ), np.float32)
    for r, d in enumerate(rows_pattern):
        if d < 0:
            continue
        C[r] = cos[:, d % half]
        Sn[r] = -sin[:, d % half] if d < half else sin[:, d % half]
    return C, Sn


def _core_table(tab, r):
    out = np.empty((tab.shape[0], NT), np.float32)
    out[:, :CTX] = tab[:, :1] * 0 + (1.0 if tab is None else 0.0)
    return out


def _tabs_for_core(C, Sn, r):
    Cc = np.ones((C.shape[0], NT), np.float32)
    Sc = np.zeros((C.shape[0], NT), np.float32)
    Cc[:, CTX:] = C[:, r * LAT_PC:(r + 1) * LAT_PC]
    Sc[:, CTX:] = Sn[:, r * LAT_PC:(r + 1) * LAT_PC]
    return Cc, Sc


def _swap_cols(w, c0, nheads, hd, rot0, rotd):
    blk = w[:, c0:c0 + nheads * hd].copy()
    idx = np.arange(nheads * hd)
    h, d = idx // hd, idx % hd
    src = idx.copy()
    inrot = (d >= rot0) & (d < rot0 + rotd)
    src[inrot] = h[inrot] * hd + rot0 + ((d[inrot] - rot0 + rotd // 2) % rotd)
    return blk[:, src]


def kernel_unfused(x, c, ctx, c_ctx, mod_w, mod_b, norm_mix, norm_ffn, ab_w_in, ab_w_out, a_sink, b_rpb,
           cd_w_in, c_q_norm, c_w_q_b, c_kv_norm, c_w_kv_b, d_q_norm, d_k_norm, cd_w_out,
           router_w, router_b, exp_w_in, exp_b_in, exp_w_out, exp_b_out, final_norm, _depth=DEPTH):
    f32 = lambda a: np.asarray(a, np.float32)
    x, c, ctx, c_ctx = f32(x), f32(c), f32(ctx), f32(c_ctx)
    ident = np.eye(128, dtype=np.float32)
    xT = []
    for core in range(NCORES):
        b, r = core // 4, core % 4
        tok = np.concatenate([ctx[b], x[b, r * LAT_PC:(r + 1) * LAT_PC]], axis=0)
        xT.append(np.ascontiguousarray(tok.T))
    conds = [np.ascontiguousarray(np.stack([fm(c[core // 4], 8), fm(c_ctx, 8)], axis=-1)) for core in range(NCORES)]
    Ch, Sh = _rope_tables(64, [d for _ in range(2) for d in range(64)])
    Cm, Sm = _rope_tables(32, [-1] * 64 + list(range(32)))
    C3, S3 = _rope_tables(32, list(range(32)))
    blk = np.kron(np.eye(2, dtype=np.float32), np.ones((64, 64), np.float32))
    maskA = _maskA_np()
    xf = None
    for l in range(_depth):
        i = l // 2
        odd = l % 2 == 1
        mw, mb = f32(mod_w[l]), f32(mod_b[l])
        ins = []
        for core in range(NCORES):
            r = core % 4
            Cc, Sc = _tabs_for_core(Ch, Sh, r)
            d = {"xT": xT[core], "cond": conds[core], "modw": np.ascontiguousarray(mw[:, :2048]), "modb": fm(mb[:2048], 16),
                 "gain": fm(norm_mix[l], 8), "ropeC": Cc, "ropeS": Sc, "ident": ident}
            if not odd:
                w = f32(ab_w_in[i])
                d["w"] = np.ascontiguousarray(np.concatenate([w, _swap_cols(w, 0, 10, 64, 0, 64)], axis=1))
            else:
                w = f32(cd_w_in[i])
                d["w"] = np.ascontiguousarray(np.concatenate([w, _swap_cols(w, 1024, 1, 32, 0, 32), _swap_cols(w, 1056, 10, 64, 0, 64)], axis=1))
                wq = f32(c_w_q_b[i])
                d["wqb"] = np.ascontiguousarray(np.concatenate([wq, _swap_cols(wq, 0, 8, 96, 64, 32)], axis=1))
                wkv = f32(c_w_kv_b[i]).reshape(256, 8, 128)
                d["wkvb"] = np.ascontiguousarray(np.concatenate([wkv[:, :, :64].reshape(256, 512), wkv[:, :, 64:].reshape(256, 512)], axis=1))
                d["qn"] = fm(c_q_norm[i], 6)
                d["kvn"] = fm(c_kv_norm[i], 2)
                gq, gk = f32(d_q_norm[i]), f32(d_k_norm[i])
                sw = (np.arange(64) + 32) % 64
                d["dgq"] = np.ascontiguousarray(np.stack([np.tile(gq, 2), np.tile(gq[sw], 2), np.tile(gk, 2), np.tile(gk[sw], 2)], axis=1))
                d["ropeC2"], d["ropeS2"] = _tabs_for_core(Cm, Sm, r)
                d["ropeC3"], d["ropeS3"] = _tabs_for_core(C3, S3, r)
                d["blk"] = blk
            ins.append(d)
        res = _run("p1o" if odd else "p1e", ins)
        ins2 = []
        for b in range(BATCH):
            FM = np.concatenate([res[4 * b]["fm"][:, :CTX]] + [res[4 * b + r]["fm"][:, CTX:] for r in range(4)], axis=1)
            TM = np.concatenate([res[4 * b]["tm"][:CTX]] + [res[4 * b + r]["tm"][CTX:] for r in range(4)], axis=0)
            for j in range(4):
                g = j // 2
                if not odd:
                    d = {"q1T": np.stack([FM[(2 * j + hh) * 64:(2 * j + hh + 1) * 64] for hh in range(2)]),
                         "k1T": FM[512 + g * 64:512 + (g + 1) * 64], "v1": TM[:, g * 64:(g + 1) * 64],
                         "q2T": np.stack([FM[640 + (2 * j + hh) * 64:640 + (2 * j + hh + 1) * 64] for hh in range(2)]),
                         "k2T": np.stack([FM[1152 + (2 * j + hh) * 64:1152 + (2 * j + hh + 1) * 64] for hh in range(2)]),
                         "v2": TM[:, 128 + 2 * j * 64:128 + (2 * j + 2) * 64], "maskA": maskA}
                    d["sink"] = np.ascontiguousarray(np.tile(f32(a_sink[i])[None, 2 * j:2 * j + 2], (128, 1)))
                    rp = f32(b_rpb[i])
                    rx = np.empty((2, 3, 128, 8, 512), np.float32)
                    for hh in range(2):
                        for v, (valid, dr, dc) in enumerate(nbr_index()):
                            rx[hh, v] = np.where(valid, rp[2 * j + hh][dr, dc], np.float32(-30000.0))
                    d["rpbx"] = rx
                else:
                    d = {"q1T": np.stack([FM[1312 + (2 * j + hh) * 64:1312 + (2 * j + hh + 1) * 64] for hh in range(2)]),
                         "k1T": FM[1824 + g * 64:1824 + (g + 1) * 64], "v1": TM[:, 512 + g * 64:512 + (g + 1) * 64],
                         "q2T": np.stack([FM[(2 * j + hh) * 96:(2 * j + hh + 1) * 96] for hh in range(2)]),
                         "k2T": np.stack([np.concatenate([FM[768 + (2 * j + hh) * 64:768 + (2 * j + hh + 1) * 64], FM[1280:1312]], axis=0) for hh in range(2)]),
                         "v2": TM[:, 2 * j * 64:(2 * j + 2) * 64]}
                d = {kk: np.ascontiguousarray(vv) for kk, vv in d.items()}
                d["ident"] = ident
                ins2.append(d)
        del res
        res2 = _run("p2o" if odd else "p2e", ins2)
        ins3 = []
        for core in range(NCORES):
            b, r = core // 4, core % 4
            yin = np.empty((8, 128, NT), BF)
            for j in range(4):
                y = res2[4 * b + j]["yT"]
                for mx in range(2):
                    ci = (mx * 4 + j) if not odd else ((1 - mx) * 4 + j)
                    yin[ci, :, :CTX] = y[mx][:, :CTX]
                    yin[ci, :, CTX:] = y[mx][:, CTX + r * LAT_PC:CTX + (r + 1) * LAT_PC]
            d = {"xT": xT[core], "yT": yin, "cond": conds[core], "modw": np.ascontiguousarray(mw[:, 2048:]), "modb": fm(mb[2048:], 32),
                 "gain": fm(norm_ffn[l], 8), "fgain": fm(final_norm, 8), "wout": f32(cd_w_out[i] if odd else ab_w_out[i]),
                 "rw": f32(router_w[l]), "rb": f32(router_b[l])[None, :], "ewin": f32(exp_w_in[l]),
                 "ebin": np.ascontiguousarray(f32(exp_b_in[l]).reshape(NEXP, 16, 128).transpose(2, 0, 1)),
                 "ewout": f32(exp_w_out[l]), "ebout": f32(exp_b_out[l]), "ident": ident}
            ins3.append(d)
        del res2
        res3 = _run("p3", ins3)
        xT = [res3[core]["x2T"] for core in range(NCORES)]
        xf = [res3[core]["xfT"] for core in range(NCORES)]
        del res3
    out = np.empty((BATCH, SEQ, DM), np.float32)
    for core in range(NCORES):
        b, r = core // 4, core % 4
        out[b, r * LAT_PC:(r + 1) * LAT_PC] = xf[core][:, CTX:].T
    return out
```

```python
import contextlib
import numpy as np
import ml_dtypes
import concourse.bass as bass
import concourse.mybir as mybir
from concourse.bass_utils import run_bass_kernel_spmd

F32 = mybir.dt.float32
BF16 = mybir.dt.bfloat16
AF = mybir.ActivationFunctionType
ALU = mybir.AluOpType
AX = mybir.AxisListType

NCORES = 8
DM = 1024
BATCH = 2
SEQ = 16384
DEPTH = 4
GRID_W = 64
CTX = 256
LAT_PC = SEQ // 4
NT = CTX + LAT_PC
NTOK = CTX + SEQ
NKB = NTOK // 128
EPS = 1e-6
NEXP = 32
SAME_ENGINE_SYNC = True


class Buf:
    __slots__ = ("name", "writers", "readers")

    def __init__(self, name):
        self.name = name
        self.writers = {}
        self.readers = {}


class _Rec:
    def __init__(self):
        self.call = None

    def __getattr__(self, m):
        def f(*a, **kw):
            self.call = (m, a, kw)
            return self
        return f


class Sched:
    ENG = ("pe", "act", "dve", "pool", "sp")

    def __init__(self, nc, stack, ndma_sems=12):
        self.nc = nc
        self.prog = {e: [] for e in self.ENG}
        self.sem = {e: stack.enter_context(nc.semaphore("s_" + e)) for e in self.ENG}
        self.cnt = {e: 0 for e in self.ENG}
        self.waited = {e: {} for e in self.ENG}
        self.dq = {}
        for q in ("sp", "pool"):
            sems = [stack.enter_context(nc.semaphore(f"d_{q}{i}")) for i in range(ndma_sems)]
            self.dq[q] = {"sems": sems, "n": 0}
        self.semkey = {}
        self.ccs = []
        self.ccsem = None
        self.final = []
        self.ninst = 0

    def _key(self, sem):
        k = id(sem)
        self.semkey[k] = sem
        return k

    def _wait(self, eng, deps):
        w = self.waited[eng]
        for k, v in deps.items():
            if w.get(k, 0) >= v:
                continue
            w[k] = v
            sem = self.semkey[k]
            self.prog[eng].append(lambda e, sem=sem, v=v: e.wait_ge(sem, v))

    def _deps(self, eng, reads, writes):
        deps = {}
        own = self._key(self.sem[eng])

        def add(d):
            for k, v in d.items():
                if k == own and (eng == "pe" or not SAME_ENGINE_SYNC):
                    continue
                if deps.get(k, 0) < v:
                    deps[k] = v
        for b in reads:
            add(b.writers)
        for b in writes:
            add(b.writers)
            for k, v in b.readers.items():
                if k != own and deps.get(k, 0) < v:
                    deps[k] = v
        return deps

    def _mark(self, tok, reads, writes):
        k, v = tok
        for b in reads:
            if b.readers.get(k, 0) < v:
                b.readers[k] = v
        for b in writes:
            if b.readers:
                b.readers = {}
                b.writers = {}
            b.writers[k] = v

    def op(self, eng, fn, reads=(), writes=()):
        deps = self._deps(eng, reads, writes)
        self._wait(eng, deps)
        self.cnt[eng] += 1
        n = self.cnt[eng]
        sem = self.sem[eng]
        r = _Rec()
        fn(r)
        m, a, kw = r.call
        self.prog[eng].append(lambda e, m=m, a=a, kw=kw, sem=sem: getattr(e, m)(*a, **kw).then_inc(sem, 1))
        self._mark((self._key(sem), n), reads, writes)
        self.ninst += 1

    def cc(self, stack, fn, scratch, reads=(), writes=()):
        deps = self._deps("pool", reads, writes)
        self._wait("pool", deps)
        if self.ccsem is None:
            self.ccsem = stack.enter_context(self.nc.semaphore("ccsem"))
        sem = self.ccsem
        r = _Rec()
        fn(r)
        m, a, kw = r.call
        self.prog["pool"].append(lambda e, m=m, a=a, kw=kw, sem=sem: getattr(e, m)(*a, **kw).then_inc(sem, 1))
        self.ccs.append(sem)
        n = len(self.ccs)
        self.prog["pool"].append(lambda e, sem=sem, n=n: e.wait_ge(sem, n))
        self.op("pool", lambda e: e.memset(scratch, 0.0), reads=reads, writes=writes)

    def dma(self, q, out, in_, reads=(), writes=(), final=False):
        deps = self._deps(q, reads, writes)
        d = self.dq[q]
        i = d["n"]
        d["n"] += 1
        sems = d["sems"]
        sem = sems[i % len(sems)]
        rnd = i // len(sems)
        k = self._key(sem)
        if rnd > 0:
            deps[k] = max(deps.get(k, 0), 16 * rnd)
        self._wait(q, deps)
        self.prog[q].append(lambda e, o=out, a=in_, sem=sem: e.dma_start(out=o, in_=a).then_inc(sem, 16))
        tok = (k, 16 * (rnd + 1))
        self._mark(tok, reads, writes)
        if final:
            self.final.append(tok)
        self.ninst += 1

    def barrier(self):
        deps = {}
        for e in self.ENG:
            if self.cnt[e]:
                deps[self._key(self.sem[e])] = self.cnt[e]
        for q in self.dq.values():
            n = q["n"]
            L = len(q["sems"])
            for j, sem in enumerate(q["sems"]):
                uses = (n - j + L - 1) // L if n > j else 0
                if uses:
                    deps[self._key(sem)] = 16 * uses
        for e in self.ENG:
            own = self._key(self.sem[e])
            self._wait(e, {kk: v for kk, v in deps.items() if kk != own})

    def emit(self):
        nc = self.nc
        fin = {}
        for k, v in self.final:
            fin[k] = max(fin.get(k, 0), v)
        for q in self.dq.values():
            n = q["n"]
            for j, sem in enumerate(q["sems"]):
                uses = (n - j + len(q["sems"]) - 1) // len(q["sems"]) if n > j else 0
                if uses:
                    fin[self._key(sem)] = max(fin.get(self._key(sem), 0), 16 * uses)
        self._wait("sp", fin)
        prog = self.prog
        with nc.Block() as block:
            @block.tensor
            def _(e):
                for f in prog["pe"]:
                    f(e)

            @block.scalar
            def _(e):
                for f in prog["act"]:
                    f(e)

            @block.vector
            def _(e):
                for f in prog["dve"]:
                    f(e)

            @block.gpsimd
            def _(e):
                for f in prog["pool"]:
                    f(e)

            @block.sync
            def _(e):
                for f in prog["sp"]:
                    f(e)


class K:
    def __init__(self, name):
        self.nc = bass.Bass("TRN2", target_bir_lowering=False, name=name)
        self.stack = contextlib.ExitStack()
        self.S = Sched(self.nc, self.stack)
        self.nbuf = 0
        self.io = {}
        self.fused = False
        self.phase = 0

    def dram(self, name, shape, dt, kind):
        if name in self.io:
            return self.io[name]
        if self.fused and self.phase > 0:
            name = f"{name}_ph{self.phase}"
        return self.nc.dram_tensor(name, list(shape), dt, kind=kind).ap()

    def begin_phase(self, io):
        self.phase += 1
        self.io = io
        self.saved = self.stack
        self.stack = contextlib.ExitStack()

    def end_phase(self):
        self.S.barrier()
        self.stack.close()
        self.stack = self.saved
        self.io = {}

    def sb(self, shape, dt, name=None):
        self.nbuf += 1
        name = f"{name or 't'}_{self.nbuf}"
        t = self.stack.enter_context(self.nc.sbuf_tensor(name, list(shape), dt))
        return t, Buf(name)

    def ps(self, shape, dt, name=None):
        self.nbuf += 1
        name = f"{name or 'p'}_{self.nbuf}"
        t = self.stack.enter_context(self.nc.psum_tensor(name, list(shape), dt))
        return t, Buf(name)

    def finish(self):
        self.S.emit()
        self.stack.close()
        return self.nc


class Ring:
    def __init__(self, items):
        self.items = items
        self.i = 0

    def next(self):
        it = self.items[self.i % len(self.items)]
        self.i += 1
        return it


def token_tiles():
    tiles = [(0, CTX, True)]
    for i in range(LAT_PC // 512):
        tiles.append((CTX + 512 * i, 512, False))
    return tiles


def emit_mod_vectors(k, S, modw_d, modb_sb, modb_b, cond_sb, cond_b, nvec, out_sb, out_b, wring):
    ps_t, ps_b = k.ps([128, 4, 2], F32, "modps")
    for v in range(nvec):
        for hf in range(2):
            w_sb, w_b = wring.next()
            c0 = v * 1024 + hf * 512
            S.dma("sp", w_sb[:], modw_d[:, c0:c0 + 512].rearrange("(kc p) f -> p kc f", p=128), writes=[w_b])
            for j in range(4):
                for kc in range(8):
                    S.op("pe", lambda e, j=j, kc=kc, w_sb=w_sb: e.matmul(ps_t[:, j, :], lhsT=w_sb[:, kc, j * 128:(j + 1) * 128],
                                                                      rhs=cond_sb[:, kc, :], start=(kc == 0), stop=(kc == 7)),
                         reads=[w_b, cond_b], writes=[ps_b])
            for c in range(2):
                S.op("dve", lambda e, v=v, c=c, hf=hf: e.tensor_tensor(out=out_sb[:, v, hf * 4:hf * 4 + 4, c], in0=ps_t[:, :, c],
                                                                     in1=modb_sb[:, v * 8 + hf * 4:v * 8 + hf * 4 + 4], op=ALU.add),
                     reads=[ps_b, modb_b], writes=[out_b])


def emit_norm_tile(k, S, x_sb, x_b, T, onesb, ones_b, sq_sb, sq_b, ss_ps, ss_b, rstd_sb, rstd_b, eps_sb, eps_b):
    S.op("act", lambda e: e.activation(out=sq_sb[:, :, :T], in_=x_sb[:, :, :T], func=AF.Square), reads=[x_b], writes=[sq_b])
    for kc in range(8):
        S.op("pe", lambda e, kc=kc: e.matmul(ss_ps[:, :T], lhsT=onesb[:, :], rhs=sq_sb[:, kc, :T], start=(kc == 0), stop=(kc == 7)),
             reads=[sq_b, ones_b], writes=[ss_b])
    S.op("act", lambda e: e.activation(out=rstd_sb[:, :T], in_=ss_ps[:, :T], func=AF.Sqrt, scale=1.0 / DM, bias=eps_sb[:, 0:1]),
         reads=[ss_b, eps_b], writes=[rstd_b])
    S.op("dve", lambda e: e.reciprocal(out=rstd_sb[:, :T], in_=rstd_sb[:, :T]), reads=[rstd_b], writes=[rstd_b])


def load_consts(k, S, ident_d, need_f32_ident=False):
    identb, identb_b = k.sb([128, 128], BF16, "identb")
    S.dma("pool", identb[:], ident_d[:, :], writes=[identb_b])
    onesb, ones_b = k.sb([128, 128], BF16, "onesb")
    S.op("dve", lambda e: e.memset(onesb[:], 1.0), writes=[ones_b])
    eps_sb, eps_b = k.sb([128, 1], F32, "eps")
    S.op("dve", lambda e: e.memset(eps_sb[:], EPS), writes=[eps_b])
    return identb, identb_b, onesb, ones_b, eps_sb, eps_b


def build_p1(odd, k=None, io=None):
    own = k is None
    if own:
        k = K("p1o" if odd else "p1e")
    else:
        k.begin_phase(io)
    S = k.S
    xT = k.dram("xT", [DM, NT], F32, "ExternalInput")
    cond = k.dram("cond", [128, 8, 2], F32, "ExternalInput")
    modw = k.dram("modw", [DM, 2048], F32, "ExternalInput")
    modb = k.dram("modb", [128, 16], F32, "ExternalInput")
    gain = k.dram("gain", [128, 8], F32, "ExternalInput")
    ropeC = k.dram("ropeC", [128, NT], F32, "ExternalInput")
    ropeS = k.dram("ropeS", [128, NT], F32, "ExternalInput")
    if not odd:
        NW = 2304 + 640
        w_d = k.dram("w", [DM, NW], F32, "ExternalInput")
        fm_out = k.dram("fm", [1664, NT], BF16, "ExternalOutput")
        tm_out = k.dram("tm", [NT, 640], BF16, "ExternalOutput")
    else:
        NW = 1824 + 32 + 640
        w_d = k.dram("w", [DM, NW], F32, "ExternalInput")
        wqb_d = k.dram("wqb", [768, 1536], F32, "ExternalInput")
        wkvb_d = k.dram("wkvb", [256, 1024], F32, "ExternalInput")
        qn_d = k.dram("qn", [128, 6], F32, "ExternalInput")
        kvn_d = k.dram("kvn", [128, 2], F32, "ExternalInput")
        dgC = k.dram("dgq", [128, 4], F32, "ExternalInput")
        ropeC2 = k.dram("ropeC2", [96, NT], F32, "ExternalInput")
        ropeS2 = k.dram("ropeS2", [96, NT], F32, "ExternalInput")
        blk_d = k.dram("blk", [128, 128], F32, "ExternalInput")
        ropeC3 = k.dram("ropeC3", [32, NT], F32, "ExternalInput")
        ropeS3 = k.dram("ropeS3", [32, NT], F32, "ExternalInput")
        fm_out = k.dram("fm", [768 + 512 + 32 + 512 + 128, NT], BF16, "ExternalOutput")
        tm_out = k.dram("tm", [NT, 640], BF16, "ExternalOutput")
    ident_d = k.dram("ident", [128, 128], F32, "ExternalInput")

    identb, identb_b, onesb, ones_b, eps_sb, eps_b = load_consts(k, S, ident_d)

    w_sb, w_b = k.sb([128, 8, NW], BF16, "w_sb")
    for kc in range(8):
        for c0 in range(0, NW, 1024):
            c1 = min(NW, c0 + 1024)
            S.dma("pool", w_sb[:, kc, c0:c1], w_d[kc * 128:(kc + 1) * 128, c0:c1], writes=[w_b])
    if odd:
        wqb_sb, wqb_b = k.sb([128, 6, 1536], BF16, "wqb_sb")
        for kc in range(6):
            for c0 in (0, 768):
                S.dma("pool", wqb_sb[:, kc, c0:c0 + 768], wqb_d[kc * 128:(kc + 1) * 128, c0:c0 + 768], writes=[wqb_b])
        wkvb_sb, wkvb_b = k.sb([128, 2, 1024], BF16, "wkvb_sb")
        for kc in range(2):
            S.dma("pool", wkvb_sb[:, kc, :], wkvb_d[kc * 128:(kc + 1) * 128, :], writes=[wkvb_b])
        lg_sb, lg_b = k.sb([128, 8], F32, "lg_sb")
        S.dma("sp", lg_sb[:, 0:6], qn_d[:, :], writes=[lg_b])
        S.dma("sp", lg_sb[:, 6:8], kvn_d[:, :], writes=[lg_b])
        dg_sb, dg_b = k.sb([128, 4], F32, "dg_sb")
        S.dma("sp", dg_sb[:], dgC[:, :], writes=[dg_b])
        blk_sb, blk_b = k.sb([128, 128], BF16, "blk_sb")
        S.dma("pool", blk_sb[:], blk_d[:, :], writes=[blk_b])

    cond_sb, cond_b = k.sb([128, 8, 2], F32, "cond_sb")
    S.dma("sp", cond_sb[:], cond[:, :, :], writes=[cond_b])
    S.op("act", lambda e: e.activation(out=cond_sb[:], in_=cond_sb[:], func=AF.Silu), reads=[cond_b], writes=[cond_b])
    modb_sb, modb_b = k.sb([128, 16], F32, "modb_sb")
    S.dma("sp", modb_sb[:], modb[:, :], writes=[modb_b])
    gain_sb, gain_b = k.sb([128, 8], F32, "gain_sb")
    S.dma("sp", gain_sb[:], gain[:, :], writes=[gain_b])
    mv_sb, mv_b = k.sb([128, 2, 8, 2], F32, "mv_sb")
    wm, wm_b = k.sb([128, 8, 512], F32, "modw_sb")
    emit_mod_vectors(k, S, modw, modb_sb, modb_b, cond_sb, cond_b, 2, mv_sb, mv_b, Ring([(wm, wm_b)]))
    A_sb, A_b = k.sb([128, 8, 2], F32, "A_sb")
    for c in range(2):
        S.op("dve", lambda e, c=c: e.scalar_tensor_tensor(out=A_sb[:, :, c], in0=mv_sb[:, 1, :, c], scalar=1.0, in1=gain_sb[:, :], op0=ALU.add, op1=ALU.mult),
             reads=[mv_b, gain_b], writes=[A_b])

    x_sb, x_b = k.sb([128, 8, 512], F32, "x_sb")
    sq_sb, sq_b = k.sb([128, 8, 512], BF16, "sq_sb")
    rstd_sb, rstd_b = k.sb([128, 512], F32, "rstd_sb")
    t_sb, t_b = k.sb([128, 512], F32, "t_sb")
    h_sb, h_b = k.sb([128, 8, 512], BF16, "h_sb")
    rc_sb, rc_b = k.sb([128, 512], F32, "rc_sb")
    rs_sb, rs_b = k.sb([128, 512], F32, "rs_sb")
    ss_ps, ss_b = k.ps([128, 512], F32, "ss_ps")
    pring = Ring([k.ps([128, 512], F32, f"pp{i}") for i in range(5)])
    oring = Ring([k.sb([128, 512], BF16, f"ob{i}") for i in range(3)])
    u1_sb, u1_b = k.sb([128, 512], F32, "u1")
    u2_sb, u2_b = k.sb([128, 512], F32, "u2")
    vo_ring = Ring([k.sb([128, 640], BF16, f"vo{i}") for i in range(2)])
    if odd:
        rc2_sb, rc2_b = k.sb([96, 512], F32, "rc2_sb")
        rs2_sb, rs2_b = k.sb([96, 512], F32, "rs2_sb")
        rc3_sb, rc3_b = k.sb([32, 512], F32, "rc3_sb")
        rs3_sb, rs3_b = k.sb([32, 512], F32, "rs3_sb")
        cq_sb, cq_b = k.sb([128, 8, 512], F32, "cq_sb")
        cn_sb, cn_b = k.sb([128, 8, 512], BF16, "cn_sb")
        rq_sb, rq_b = k.sb([128, 512], F32, "rq_sb")
        rkv_sb, rkv_b = k.sb([128, 512], F32, "rkv_sb")
        nrm_sb, nrm_b = k.sb([128, 512], F32, "nrm_sb")

    def mm_fm(ps, ps_b, col0, ncols, T, rhs_sb=None, rhs_b=None, wsb=None, wb=None, nk=8):
        rhs_sb = h_sb if rhs_sb is None else rhs_sb
        rhs_b = h_b if rhs_b is None else rhs_b
        wsb = w_sb if wsb is None else wsb
        wb = w_b if wb is None else wb
        for kc in range(nk):
            S.op("pe", lambda e, kc=kc: e.matmul(ps[:ncols, :T], lhsT=wsb[:, kc, col0:col0 + ncols], rhs=rhs_sb[:, kc, :T],
                                                start=(kc == 0), stop=(kc == nk - 1)), reads=[wb, rhs_b], writes=[ps_b])

    def store_fm(src_fn, src_bufs, row0, nrows, t0, T, eng="act"):
        o_sb, o_b = oring.next()
        if eng == "act":
            S.op("act", lambda e: e.copy(out=o_sb[:nrows, :T], in_=src_fn()), reads=src_bufs, writes=[o_b])
        S.dma("sp", fm_out[row0:row0 + nrows, t0:t0 + T], o_sb[:nrows, :T], reads=[o_b])

    def rope_store(psA, psA_b, psB, psB_b, nrows, row0, t0, T, C, C_b, Sn, Sn_b, norm=None):
        o_sb, o_b = oring.next()
        S.op("dve", lambda e: e.tensor_tensor(out=u1_sb[:nrows, :T], in0=psA[:nrows, :T], in1=C[:nrows, :T], op=ALU.mult), reads=[psA_b, C_b], writes=[u1_b])
        S.op("dve", lambda e: e.tensor_tensor(out=u2_sb[:nrows, :T], in0=psB[:nrows, :T], in1=Sn[:nrows, :T], op=ALU.mult), reads=[psB_b, Sn_b], writes=[u2_b])
        if norm is None:
            S.op("pool", lambda e: e.tensor_tensor(out=o_sb[:nrows, :T], in0=u1_sb[:nrows, :T], in1=u2_sb[:nrows, :T], op=ALU.add), reads=[u1_b, u2_b], writes=[o_b])
        else:
            n_sb, n_b = norm
            S.op("pool", lambda e: e.tensor_tensor(out=u1_sb[:nrows, :T], in0=u1_sb[:nrows, :T], in1=u2_sb[:nrows, :T], op=ALU.add), reads=[u1_b, u2_b], writes=[u1_b])
            S.op("dve", lambda e: e.tensor_tensor(out=o_sb[:nrows, :T], in0=u1_sb[:nrows, :T], in1=n_sb[:nrows, :T], op=ALU.mult), reads=[u1_b, n_b], writes=[o_b])
        S.dma("sp", fm_out[row0:row0 + nrows, t0:t0 + T], o_sb[:nrows, :T], reads=[o_b])

    def rsqrt_from_ps(ps, ps_b, out_sb, out_b, T, scale):
        S.op("act", lambda e: e.activation(out=out_sb[:, :T], in_=ps[:, :T], func=AF.Sqrt, scale=scale, bias=eps_sb[:, 0:1]), reads=[ps_b, eps_b], writes=[out_b])
        S.op("dve", lambda e: e.reciprocal(out=out_sb[:, :T], in_=out_sb[:, :T]), reads=[out_b], writes=[out_b])

    for (t0, T, is_ctx) in token_tiles():
        c = 1 if is_ctx else 0
        S.dma("sp", x_sb[:, :, :T], xT[:, t0:t0 + T].rearrange("(kc p) t -> p kc t", p=128), writes=[x_b])
        S.dma("sp", rc_sb[:, :T], ropeC[:, t0:t0 + T], writes=[rc_b])
        S.dma("sp", rs_sb[:, :T], ropeS[:, t0:t0 + T], writes=[rs_b])
        emit_norm_tile(k, S, x_sb, x_b, T, onesb, ones_b, sq_sb, sq_b, ss_ps, ss_b, rstd_sb, rstd_b, eps_sb, eps_b)
        for kc in range(8):
            S.op("dve", lambda e, kc=kc: e.scalar_tensor_tensor(out=t_sb[:, :T], in0=x_sb[:, kc, :T], scalar=A_sb[:, kc, c:c + 1], in1=rstd_sb[:, :T],
                                                             op0=ALU.mult, op1=ALU.mult), reads=[x_b, A_b, rstd_b], writes=[t_b])
            S.op("act", lambda e, kc=kc: e.activation(out=h_sb[:, kc, :T], in_=t_sb[:, :T], func=AF.Identity, bias=mv_sb[:, 0, kc, c:c + 1], scale=1.0),
                 reads=[t_b, mv_b], writes=[h_b])
        if not odd:
            for j in range(5):
                col = j * 128
                swc = 2304 + j * 128
                pa, pa_b = pring.next()
                pb, pb_b = pring.next()
                mm_fm(pa, pa_b, col, 128, T)
                mm_fm(pb, pb_b, swc, 128, T)
                rope_store(pa, pa_b, pb, pb_b, 128, j * 128, t0, T, rc_sb, rc_b, rs_sb, rs_b)
            for j in range(8):
                col = 768 + j * 128
                pa, pa_b = pring.next()
                mm_fm(pa, pa_b, col, 128, T)
                store_fm(lambda pa=pa: pa[:, :T], [pa_b], 640 + j * 128, 128, t0, T)
            tmcols = [(640, 128, 0), (1792, 512, 128)]
        else:
            S.dma("sp", rc2_sb[:, :T], ropeC2[:, t0:t0 + T], writes=[rc2_b])
            S.dma("sp", rs2_sb[:, :T], ropeS2[:, t0:t0 + T], writes=[rs2_b])
            S.dma("sp", rc3_sb[:, :T], ropeC3[:, t0:t0 + T], writes=[rc3_b])
            S.dma("sp", rs3_sb[:, :T], ropeS3[:, t0:t0 + T], writes=[rs3_b])
            for j in range(8):
                pa, pa_b = pring.next()
                mm_fm(pa, pa_b, j * 128, 128, T)
                S.op("act", lambda e, j=j, pa=pa: e.copy(out=cq_sb[:, j, :T], in_=pa[:, :T]), reads=[pa_b], writes=[cq_b])
            S.op("act", lambda e: e.activation(out=sq_sb[:, :, :T], in_=cq_sb[:, :, :T], func=AF.Square), reads=[cq_b], writes=[sq_b])
            pq, pq_b = pring.next()
            for j in range(6):
                S.op("pe", lambda e, j=j: e.matmul(pq[:, :T], lhsT=onesb[:, :], rhs=sq_sb[:, j, :T], start=(j == 0), stop=(j == 5)), reads=[sq_b, ones_b], writes=[pq_b])
            rsqrt_from_ps(pq, pq_b, rq_sb, rq_b, T, 1.0 / 768)
            pk, pk_b = pring.next()
            for j in range(2):
                S.op("pe", lambda e, j=j: e.matmul(pk[:, :T], lhsT=onesb[:, :], rhs=sq_sb[:, 6 + j, :T], start=(j == 0), stop=(j == 1)), reads=[sq_b, ones_b], writes=[pk_b])
            rsqrt_from_ps(pk, pk_b, rkv_sb, rkv_b, T, 1.0 / 256)
            for j in range(8):
                r_sb, r_b = (rq_sb, rq_b) if j < 6 else (rkv_sb, rkv_b)
                S.op("dve", lambda e, j=j, r_sb=r_sb: e.scalar_tensor_tensor(out=cn_sb[:, j, :T], in0=cq_sb[:, j, :T], scalar=lg_sb[:, j:j + 1], in1=r_sb[:, :T], op0=ALU.mult, op1=ALU.mult),
                     reads=[cq_b, r_b, lg_b], writes=[cn_b])
            for hd in range(8):
                pa, pa_b = pring.next()
                pb, pb_b = pring.next()
                mm_fm(pa, pa_b, hd * 96, 96, T, cn_sb, cn_b, wqb_sb, wqb_b, nk=6)
                mm_fm(pb, pb_b, 768 + hd * 96, 96, T, cn_sb, cn_b, wqb_sb, wqb_b, nk=6)
                rope_store(pa, pa_b, pb, pb_b, 96, hd * 96, t0, T, rc2_sb, rc2_b, rs2_sb, rs2_b)
            for j in range(4):
                pa, pa_b = pring.next()
                for kc in range(2):
                    S.op("pe", lambda e, kc=kc, j=j, pa=pa: e.matmul(pa[:, :T], lhsT=wkvb_sb[:, kc, j * 128:(j + 1) * 128], rhs=cn_sb[:, 6 + kc, :T],
                                                                      start=(kc == 0), stop=(kc == 1)), reads=[wkvb_b, cn_b], writes=[pa_b])
                store_fm(lambda pa=pa: pa[:, :T], [pa_b], 768 + j * 128, 128, t0, T)
            pa, pa_b = pring.next()
            pb, pb_b = pring.next()
            mm_fm(pa, pa_b, 1024, 32, T)
            mm_fm(pb, pb_b, 1824, 32, T)
            rope_store(pa, pa_b, pb, pb_b, 32, 768 + 512, t0, T, rc3_sb, rc3_b, rs3_sb, rs3_b)
            for j in range(5):
                col = 1056 + j * 128
                swc = 1856 + j * 128
                pa, pa_b = pring.next()
                pb, pb_b = pring.next()
                mm_fm(pa, pa_b, col, 128, T)
                mm_fm(pb, pb_b, swc, 128, T)
                S.op("act", lambda e, pa=pa: e.activation(out=sq_sb[:, 0, :T], in_=pa[:, :T], func=AF.Square), reads=[pa_b], writes=[sq_b])
                pn, pn_b = pring.next()
                S.op("pe", lambda e, pn=pn: e.matmul(pn[:, :T], lhsT=blk_sb[:, :], rhs=sq_sb[:, 0, :T], start=True, stop=True), reads=[sq_b, blk_b], writes=[pn_b])
                rsqrt_from_ps(pn, pn_b, nrm_sb, nrm_b, T, 1.0 / 64)
                gc = 0 if j < 4 else 2
                o_sb, o_b = oring.next()
                S.op("dve", lambda e, pa=pa, gc=gc: e.scalar_tensor_tensor(out=u1_sb[:, :T], in0=pa[:, :T], scalar=dg_sb[:, gc:gc + 1], in1=rc_sb[:, :T], op0=ALU.mult, op1=ALU.mult),
                     reads=[pa_b, dg_b, rc_b], writes=[u1_b])
                S.op("dve", lambda e, pb=pb, gc=gc: e.scalar_tensor_tensor(out=u2_sb[:, :T], in0=pb[:, :T], scalar=dg_sb[:, gc + 1:gc + 2], in1=rs_sb[:, :T], op0=ALU.mult, op1=ALU.mult),
                     reads=[pb_b, dg_b, rs_b], writes=[u2_b])
                S.op("pool", lambda e: e.tensor_tensor(out=u1_sb[:, :T], in0=u1_sb[:, :T], in1=u2_sb[:, :T], op=ALU.add), reads=[u1_b, u2_b], writes=[u1_b])
                S.op("dve", lambda e, o_sb=o_sb: e.tensor_tensor(out=o_sb[:, :T], in0=u1_sb[:, :T], in1=nrm_sb[:, :T], op=ALU.mult), reads=[u1_b, nrm_b], writes=[o_b])
                S.dma("sp", fm_out[1312 + j * 128:1312 + (j + 1) * 128, t0:t0 + T], o_sb[:, :T], reads=[o_b])
            tmcols = [(1696, 128, 512)]
        for s in range(T // 128):
            vo_sb, vo_b = vo_ring.next()
            for (wc, n, oc) in tmcols:
                pa, pa_b = pring.next()
                for kc in range(8):
                    S.op("pe", lambda e, kc=kc, pa=pa, wc=wc, n=n, s=s: e.matmul(pa[:, :n], lhsT=h_sb[:, kc, s * 128:(s + 1) * 128], rhs=w_sb[:, kc, wc:wc + n],
                                                                               start=(kc == 0), stop=(kc == 7)), reads=[h_b, w_b], writes=[pa_b])
                S.op("act", lambda e, pa=pa, n=n, oc=oc, vo_sb=vo_sb: e.copy(out=vo_sb[:, oc:oc + n], in_=pa[:, :n]), reads=[pa_b], writes=[vo_b])
            if odd:
                pa, pa_b = pring.next()
                for kc in range(2):
                    S.op("pe", lambda e, kc=kc, pa=pa, s=s: e.matmul(pa[:, :512], lhsT=cn_sb[:, 6 + kc, s * 128:(s + 1) * 128], rhs=wkvb_sb[:, kc, 512:1024],
                                                                      start=(kc == 0), stop=(kc == 1)), reads=[cn_b, wkvb_b], writes=[pa_b])
                S.op("act", lambda e, pa=pa, vo_sb=vo_sb: e.copy(out=vo_sb[:, 0:512], in_=pa[:, :512]), reads=[pa_b], writes=[vo_b])
            S.dma("sp", tm_out[t0 + s * 128:t0 + (s + 1) * 128, :], vo_sb[:, :], reads=[vo_b], final=True)
    if own:
        return k.finish()
    k.end_phase()


def _maskA_np():
    m = np.zeros((128, 6, 512), np.float32)
    kl = np.arange(128)[:, None]
    ql = np.arange(128)[None, :]
    for kbrel in range(6):
        for qb in range(4):
            rel = (kbrel - 1) - qb
            if rel == -1:
                m[:, kbrel, qb * 128:(qb + 1) * 128] = (kl >= ql)
            elif rel == 0:
                m[:, kbrel, qb * 128:(qb + 1) * 128] = 1.0
            elif rel == 1:
                m[:, kbrel, qb * 128:(qb + 1) * 128] = (kl <= ql)
    return m


def _nbr_index():
    rows = SEQ // GRID_W
    out = []
    for v, tile in enumerate((0, 1, rows // 8 - 1)):
        r0 = tile * 8
        kp = np.arange(128)
        kr2, kc = kp // 64, kp % 64
        q = np.arange(512)
        qr, c = r0 + q // 64, q % 64
        rs = np.clip(qr - 4, 0, rows - 8)
        cs = np.clip(c - 8, 0, GRID_W - 16)
        valid = np.zeros((128, 8, 512), bool)
        dr = np.zeros((128, 8, 512), np.int64)
        dc = np.zeros((128, 8, 512), np.int64)
        for kbrel in range(8):
            krow = r0 - 4 + 2 * kbrel + kr2
            okr = (krow[:, None] >= rs[None, :]) & (krow[:, None] < rs[None, :] + 8) & (krow[:, None] >= 0) & (krow[:, None] < rows)
            okc = (kc[:, None] >= cs[None, :]) & (kc[:, None] < cs[None, :] + 16)
            valid[:, kbrel, :] = okr & okc
            dr[:, kbrel, :] = krow[:, None] - qr[None, :] + 7
            dc[:, kbrel, :] = kc[:, None] - c[None, :] + 15
        out.append((valid, np.clip(dr, 0, 14), np.clip(dc, 0, 30)))
    return out


_NBR = None


def nbr_index():
    global _NBR
    if _NBR is None:
        _NBR = _nbr_index()
    return _NBR


def build_p2(odd):
    k = K("p2o" if odd else "p2e")
    S = k.S
    dk2 = 96 if odd else 64
    q1T = k.dram("q1T", [2, 64, NTOK], BF16, "ExternalInput")
    k1T = k.dram("k1T", [64, NTOK], BF16, "ExternalInput")
    v1 = k.dram("v1", [NTOK, 64], BF16, "ExternalInput")
    q2T = k.dram("q2T", [2, dk2, NTOK], BF16, "ExternalInput")
    k2T = k.dram("k2T", [2, dk2, NTOK], BF16, "ExternalInput")
    v2 = k.dram("v2", [NTOK, 128], BF16, "ExternalInput")
    ident_d = k.dram("ident", [128, 128], F32, "ExternalInput")
    yT = k.dram("yT", [2, 128, NTOK], BF16, "ExternalOutput")
    identb, identb_b, onesb, ones_b, eps_sb, eps_b = load_consts(k, S, ident_d)
    if not odd:
        maskA_d = k.dram("maskA", [128, 6, 512], F32, "ExternalInput")
        sink_d = k.dram("sink", [128, 2], F32, "ExternalInput")
        rpbx_d = k.dram("rpbx", [2, 3, 128, 8, 512], F32, "ExternalInput")
        maskA, maskA_b = k.sb([128, 6, 512], BF16, "maskA_sb")
        S.dma("pool", maskA[:], maskA_d[:, :, :], writes=[maskA_b])
        esink, esink_b = k.sb([128, 2], F32, "esink_sb")
        S.dma("sp", esink[:], sink_d[:, :], writes=[esink_b])
        S.op("act", lambda e: e.activation(out=esink[:], in_=esink[:], func=AF.Exp), reads=[esink_b], writes=[esink_b])
        MB = {}
        stg, stg_b = k.sb([128, 512], F32, "stg")
        for h in range(2):
            for v in range(3):
                m_sb, m_b = k.sb([128, 8, 512], BF16, f"MB{h}{v}")
                for kb in range(8):
                    S.dma("sp", stg[:], rpbx_d[h, v, :, kb, :], writes=[stg_b])
                    S.op("act", lambda e: e.activation(out=m_sb[:, kb, :], in_=stg[:], func=AF.Exp), reads=[stg_b], writes=[m_b])
                MB[(h, v)] = (m_sb, m_b)
        nbr = nbr_index()
        needB = [[[bool(nbr[v][0][:, kb, qb * 128:(qb + 1) * 128].any()) for qb in range(4)] for kb in range(8)] for v in range(3)]
        mA = _maskA_np()
        needA = [[bool(mA[:, kb, qb * 128:(qb + 1) * 128].any()) for qb in range(4)] for kb in range(6)]

    qT_sb, qT_b = k.sb([dk2, NTOK], BF16, "qT_sb")
    kT_sb, kT_b = k.sb([dk2, NTOK], BF16, "kT_sb")
    va_sb, va_b = k.sb([128, NKB, 65], BF16, "va_sb")
    S.op("dve", lambda e: e.memset(va_sb[:, :, 64:65], 1.0), writes=[va_b])
    sring = Ring([k.ps([128, 512], F32, f"s{i}") for i in range(2)])
    O = [k.ps([128, 512], F32, f"o{i}") for i in range(4)]
    pt_ps, pt_b = k.ps([128, 512], BF16, "ptp")
    pring = Ring([k.sb([128, 512], BF16, f"pT{i}") for i in range(3)])
    den_sb, den_b = k.sb([128, 4], F32, "den")
    y_sb, y_b = k.sb([128, 4, 64], BF16, "y_sb")
    yTring = Ring([k.sb([64, 512], BF16, f"yT{i}") for i in range(2)])

    jobs = [(0, 0), (0, 1), (1, 0), (1, 1)]
    for (mx, h) in jobs:
        dk = 64 if mx == 0 else dk2
        scale = float(dk) ** -0.5
        qsrc = q1T[h] if mx == 0 else q2T[h]
        ksrc = k1T if mx == 0 else k2T[h]
        for c0 in range(0, NTOK, 4160):
            S.dma("sp", qT_sb[:dk, c0:c0 + 4160], qsrc[:, c0:c0 + 4160], writes=[qT_b])
            S.dma("sp", kT_sb[:dk, c0:c0 + 4160], ksrc[:, c0:c0 + 4160], writes=[kT_b])
        if mx == 0:
            if h == 0:
                S.dma("sp", va_sb[:, :, 0:64], v1.rearrange("(kb p) d -> p kb d", p=128), writes=[va_b])
        else:
            S.dma("sp", va_sb[:, :, 0:64], v2[:, h * 64:(h + 1) * 64].rearrange("(kb p) d -> p kb d", p=128), writes=[va_b])
        tiles = [(0, 256, None)] + [(CTX + 512 * i, 512, i) for i in range(SEQ // 512)]
        for (q0, nq, ti) in tiles:
            nqb = nq // 128
            kbl = []
            if ti is None:
                kbl = [(0, None, [True] * nqb), (1, None, [True] * nqb)]
            elif odd:
                kbl = [(kb, None, [True] * 4) for kb in range(NKB)]
            elif mx == 0:
                for kbrel in range(6):
                    lb = 4 * ti + kbrel - 1
                    if 0 <= lb < SEQ // 128:
                        kbl.append((2 + lb, (maskA, maskA_b, kbrel), needA[kbrel]))
                kbl += [(0, None, [True] * 4), (1, None, [True] * 4)]
            else:
                v = 0 if ti == 0 else (2 if ti == SEQ // 512 - 1 else 1)
                for kbrel in range(8):
                    lb = 4 * ti - 2 + kbrel
                    if 0 <= lb < SEQ // 128 and any(needB[v][kbrel]):
                        m_sb, m_b = MB[(h, v)]
                        kbl.append((2 + lb, (m_sb, m_b, kbrel), needB[v][kbrel]))
                kbl += [(0, None, [True] * 4), (1, None, [True] * 4)]
            first = [min(i for i, (_, _, nd) in enumerate(kbl) if nd[qb]) for qb in range(nqb)]
            last = [max(i for i, (_, _, nd) in enumerate(kbl) if nd[qb]) for qb in range(nqb)]
            for i, (kb, msk, nd) in enumerate(kbl):
                s_ps, s_b = sring.next()
                S.op("pe", lambda e: e.matmul(s_ps[:, :nq], lhsT=kT_sb[:dk, kb * 128:(kb + 1) * 128], rhs=qT_sb[:dk, q0:q0 + nq], start=True, stop=True),
                     reads=[kT_b, qT_b], writes=[s_b])
                p_sb, p_b = pring.next()
                S.op("act", lambda e: e.activation(out=p_sb[:, :nq], in_=s_ps[:, :nq], func=AF.Exp, scale=scale), reads=[s_b], writes=[p_b])
                if msk is not None:
                    m_sb, m_b, kbrel = msk
                    S.op("dve" if i % 2 == 0 else "pool", lambda e: e.tensor_tensor(out=p_sb[:, :nq], in0=p_sb[:, :nq], in1=m_sb[:, kbrel, :nq], op=ALU.mult),
                         reads=[p_b, m_b], writes=[p_b])
                for qb in range(nqb):
                    if nd[qb]:
                        o_ps, o_b = O[qb]
                        S.op("pe", lambda e: e.matmul(o_ps[:, 0:65], lhsT=p_sb[:, qb * 128:(qb + 1) * 128], rhs=va_sb[:, kb, :],
                                                      start=(i == first[qb]), stop=(i == last[qb])), reads=[p_b, va_b], writes=[o_b])
            yt_sb, yt_b = yTring.next()
            for qb in range(nqb):
                o_ps, o_b = O[qb]
                if (not odd) and mx == 0:
                    S.op("dve", lambda e: e.tensor_tensor(out=den_sb[:, qb:qb + 1], in0=o_ps[:, 64:65], in1=esink[:, h:h + 1], op=ALU.add), reads=[o_b, esink_b], writes=[den_b])
                    S.op("dve", lambda e: e.reciprocal(out=den_sb[:, qb:qb + 1], in_=den_sb[:, qb:qb + 1]), reads=[den_b], writes=[den_b])
                else:
                    S.op("dve", lambda e: e.reciprocal(out=den_sb[:, qb:qb + 1], in_=o_ps[:, 64:65]), reads=[o_b], writes=[den_b])
                S.op("dve", lambda e: e.tensor_scalar(out=y_sb[:, qb, :], in0=o_ps[:, 0:64], scalar1=den_sb[:, qb:qb + 1], scalar2=None, op0=ALU.mult),
                     reads=[o_b, den_b], writes=[y_b])
                S.op("pe", lambda e: e.transpose(out=pt_ps[:64, qb * 128:(qb + 1) * 128], in_=y_sb[:, qb, :], identity=identb[:, :]), reads=[y_b, identb_b], writes=[pt_b])
            S.op("dve", lambda e: e.tensor_copy(out=yt_sb[:, :nq], in_=pt_ps[:64, :nq]), reads=[pt_b], writes=[yt_b])
            S.dma("sp", yT[mx, h * 64:(h + 1) * 64, q0:q0 + nq], yt_sb[:, :nq], reads=[yt_b], final=True)
    return k.finish()


PASSES = [[0, 1, 2], [3, 4, 5], [6, 7, 8]]


def build_p3(k=None, io=None, do_final=True):
    own = k is None
    if own:
        k = K("p3")
    else:
        k.begin_phase(io)
    S = k.S
    nc = k.nc
    xT = k.dram("xT", [DM, NT], F32, "ExternalInput")
    yT = k.dram("yT", [8, 128, NT], BF16, "ExternalInput")
    cond = k.dram("cond", [128, 8, 2], F32, "ExternalInput")
    modw = k.dram("modw", [DM, 4096], F32, "ExternalInput")
    modb = k.dram("modb", [128, 32], F32, "ExternalInput")
    gain = k.dram("gain", [128, 8], F32, "ExternalInput")
    fgain = k.dram("fgain", [128, 8], F32, "ExternalInput")
    wout = k.dram("wout", [DM, DM], F32, "ExternalInput")
    rw = k.dram("rw", [DM, NEXP], F32, "ExternalInput")
    rb = k.dram("rb", [1, NEXP], F32, "ExternalInput")
    ewin = k.dram("ewin", [NEXP, DM, 2048], F32, "ExternalInput")
    ebin = k.dram("ebin", [128, NEXP, 16], F32, "ExternalInput")
    ewout = k.dram("ewout", [NEXP, DM, DM], F32, "ExternalInput")
    ebout = k.dram("ebout", [NEXP, DM], F32, "ExternalInput")
    ident_d = k.dram("ident", [128, 128], F32, "ExternalInput")
    x2T = k.dram("x2T", [DM, NT], F32, "ExternalOutput")
    xfT = k.dram("xfT", [DM, NT], F32, "ExternalOutput") if do_final else None
    x1T = k.dram("x1T", [DM, NT], F32, "Internal")
    h2T = k.dram("h2T", [DM, NT], BF16, "Internal")
    gT_h = nc.dram_tensor(f"gTd_ph{k.phase}", [NEXP, NT], F32, kind="Internal")
    gT = gT_h.ap()
    x1T_b, h2T_b, gT_b = Buf("x1T"), Buf("h2T"), Buf("gT")

    identb, identb_b, onesb, ones_b, eps_sb, eps_b = load_consts(k, S, ident_d)
    identf, identf_b = k.sb([128, 128], F32, "identf")
    S.dma("sp", identf[:], ident_d[:, :], writes=[identf_b])
    onesf, onesf_b = k.sb([1, 128], F32, "onesf")
    S.op("dve", lambda e: e.memset(onesf[:], 1.0), writes=[onesf_b])
    cond_sb, cond_b = k.sb([128, 8, 2], F32, "cond_sb")
    S.dma("sp", cond_sb[:], cond[:, :, :], writes=[cond_b])
    S.op("act", lambda e: e.activation(out=cond_sb[:], in_=cond_sb[:], func=AF.Silu), reads=[cond_b], writes=[cond_b])
    modb_sb, modb_b = k.sb([128, 32], F32, "modb_sb")
    S.dma("sp", modb_sb[:], modb[:, :], writes=[modb_b])
    gain_sb, gain_b = k.sb([128, 8], F32, "gain_sb")
    S.dma("sp", gain_sb[:], gain[:, :], writes=[gain_b])
    fg_sb, fg_b = k.sb([128, 8], F32, "fg_sb")
    S.dma("sp", fg_sb[:], fgain[:, :], writes=[fg_b])
    mv_sb, mv_b = k.sb([128, 4, 8, 2], F32, "mv_sb")
    A_sb, A_b = k.sb([128, 8, 2], F32, "A_sb")
    ebin_sb, ebin_b = k.sb([128, NEXP, 16], F32, "ebin_sb")
    S.dma("sp", ebin_sb[:], ebin[:, :, :], writes=[ebin_b])
    ebout_sb, ebout_b = k.sb([NEXP, DM], F32, "ebout_sb")
    S.dma("sp", ebout_sb[:], ebout[:, :], writes=[ebout_b])
    rstd_sb, rstd_b = k.sb([128, 512], F32, "rstd_sb")
    sq_sb, sq_b = k.sb([128, 8, 512], BF16, "sq_sb")
    x_sb, x_b = k.sb([128, 8, 512], F32, "x_sb")
    t_sb, t_b = k.sb([128, 512], F32, "t_sb")
    ss_ps, ss_b = k.ps([128, 512], F32, "ss_ps")
    pring = Ring([k.ps([128, 512], F32, f"pp{i}") for i in range(6)])
    tiles = token_tiles()

    stA = contextlib.ExitStack()
    main_stack = k.stack
    k.stack = stA
    wm, wm_b = k.sb([128, 8, 512], F32, "modw_sb")
    emit_mod_vectors(k, S, modw, modb_sb, modb_b, cond_sb, cond_b, 4, mv_sb, mv_b, Ring([(wm, wm_b)]))
    for c in range(2):
        S.op("dve", lambda e: e.scalar_tensor_tensor(out=A_sb[:, :, c], in0=mv_sb[:, 2, :, c], scalar=1.0, in1=gain_sb[:, :], op0=ALU.add, op1=ALU.mult),
             reads=[mv_b, gain_b], writes=[A_b])
    wo_sb, wo_b = k.sb([128, 8, DM], BF16, "wo_sb")
    for kc in range(8):
        S.dma("pool", wo_sb[:, kc, :], wout[kc * 128:(kc + 1) * 128, :], writes=[wo_b])
    rw_sb, rw_b = k.sb([128, 8, NEXP], F32, "rw_sb")
    S.dma("sp", rw_sb[:], rw.rearrange("(kc p) e -> p kc e", p=128), writes=[rw_b])
    rb_sb, rb_b = k.sb([1, NEXP], F32, "rb_sb")
    S.dma("sp", rb_sb[:], rb[:, :], writes=[rb_b])
    y_sb, y_b = k.sb([128, 8, 512], BF16, "y_sb")
    hf_sb, hf_b = k.sb([128, 8, 512], F32, "hf_sb")
    hb_sb, hb_b = k.sb([128, 8, 512], BF16, "hb_sb")
    lg_sb, lg_b = k.sb([128, NEXP], F32, "lg_sb")
    m8_sb, m8_b = k.sb([128, 8], F32, "m8_sb")
    mk_sb, mk_b = k.sb([128, NEXP], F32, "mk_sb")
    ex_sb, ex_b = k.sb([128, NEXP], F32, "ex_sb")
    sm_sb, sm_b = k.sb([128, 2], F32, "sm_sb")
    gt_sb, gt_b = k.sb([NEXP, 512], F32, "gt_sb")
    for (t0, T, is_ctx) in tiles:
        c = 1 if is_ctx else 0
        S.dma("sp", x_sb[:, :, :T], xT[:, t0:t0 + T].rearrange("(kc p) t -> p kc t", p=128), writes=[x_b])
        S.dma("sp", y_sb[:, :, :T], yT[:, :, t0:t0 + T].rearrange("kc p t -> p kc t"), writes=[y_b])
        for o in range(8):
            pa, pa_b = pring.next()
            for kc in range(8):
                S.op("pe", lambda e: e.matmul(pa[:, :T], lhsT=wo_sb[:, kc, o * 128:(o + 1) * 128], rhs=y_sb[:, kc, :T], start=(kc == 0), stop=(kc == 7)),
                     reads=[wo_b, y_b], writes=[pa_b])
            S.op("dve", lambda e: e.scalar_tensor_tensor(out=x_sb[:, o, :T], in0=pa[:, :T], scalar=mv_sb[:, 0, o, c:c + 1], in1=x_sb[:, o, :T], op0=ALU.mult, op1=ALU.add),
                 reads=[pa_b, mv_b, x_b], writes=[x_b])
        S.dma("sp", x1T[:, t0:t0 + T].rearrange("(kc p) t -> p kc t", p=128), x_sb[:, :, :T], reads=[x_b], writes=[x1T_b])
        emit_norm_tile(k, S, x_sb, x_b, T, onesb, ones_b, sq_sb, sq_b, ss_ps, ss_b, rstd_sb, rstd_b, eps_sb, eps_b)
        for kc in range(8):
            S.op("dve", lambda e: e.scalar_tensor_tensor(out=t_sb[:, :T], in0=x_sb[:, kc, :T], scalar=A_sb[:, kc, c:c + 1], in1=rstd_sb[:, :T], op0=ALU.mult, op1=ALU.mult),
                 reads=[x_b, A_b, rstd_b], writes=[t_b])
            S.op("act", lambda e: e.activation(out=hf_sb[:, kc, :T], in_=t_sb[:, :T], func=AF.Identity, bias=mv_sb[:, 1, kc, c:c + 1], scale=1.0),
                 reads=[t_b, mv_b], writes=[hf_b])
            S.op("pool", lambda e: e.tensor_copy(out=hb_sb[:, kc, :T], in_=hf_sb[:, kc, :T]), reads=[hf_b], writes=[hb_b])
        S.dma("sp", h2T[:, t0:t0 + T].rearrange("(kc p) t -> p kc t", p=128), hb_sb[:, :, :T], reads=[hb_b], writes=[h2T_b])
        for s in range(T // 128):
            pr, pr_b = pring.next()
            for kc in range(8):
                S.op("pe", lambda e: e.matmul(pr[:, :NEXP], lhsT=hf_sb[:, kc, s * 128:(s + 1) * 128], rhs=rw_sb[:, kc, :], start=(kc == 0), stop=False),
                     reads=[hf_b, rw_b], writes=[pr_b])
            S.op("pe", lambda e: e.matmul(pr[:, :NEXP], lhsT=onesf[0:1, :], rhs=rb_sb[0:1, :], start=False, stop=True), reads=[onesf_b, rb_b], writes=[pr_b])
            S.op("dve", lambda e: e.tensor_copy(out=lg_sb[:, :], in_=pr[:, :NEXP]), reads=[pr_b], writes=[lg_b])
            S.op("dve", lambda e: e.max(out=m8_sb[:, :], in_=lg_sb[:, :]), reads=[lg_b], writes=[m8_b])
            S.op("dve", lambda e: e.tensor_scalar(out=mk_sb[:, :], in0=lg_sb[:, :], scalar1=m8_sb[:, 3:4], scalar2=None, op0=ALU.is_ge), reads=[lg_b, m8_b], writes=[mk_b])
            S.op("dve", lambda e: e.tensor_scalar(out=sm_sb[:, 0:1], in0=m8_sb[:, 0:1], scalar1=-1.0, scalar2=None, op0=ALU.mult), reads=[m8_b], writes=[sm_b])
            S.op("act", lambda e: e.activation(out=ex_sb[:, :], in_=lg_sb[:, :], func=AF.Exp, bias=sm_sb[:, 0:1], scale=1.0), reads=[lg_b, sm_b], writes=[ex_b])
            S.op("dve", lambda e: e.tensor_tensor(out=ex_sb[:, :], in0=ex_sb[:, :], in1=mk_sb[:, :], op=ALU.mult), reads=[ex_b, mk_b], writes=[ex_b])
            S.op("dve", lambda e: e.reduce_sum(out=sm_sb[:, 1:2], in_=ex_sb[:, :], axis=AX.X), reads=[ex_b], writes=[sm_b])
            S.op("dve", lambda e: e.reciprocal(out=sm_sb[:, 1:2], in_=sm_sb[:, 1:2]), reads=[sm_b], writes=[sm_b])
            S.op("dve", lambda e: e.tensor_scalar(out=ex_sb[:, :], in0=ex_sb[:, :], scalar1=sm_sb[:, 1:2], scalar2=None, op0=ALU.mult), reads=[ex_b, sm_b], writes=[ex_b])
            pt, pt_b = pring.next()
            S.op("pe", lambda e: e.transpose(out=pt[:NEXP, :128], in_=ex_sb[:, :], identity=identf[:, :]), reads=[ex_b, identf_b], writes=[pt_b])
            S.op("act", lambda e: e.copy(out=gt_sb[:, s * 128:(s + 1) * 128], in_=pt[:NEXP, :128]), reads=[pt_b], writes=[gt_b])
        S.dma("sp", gT[:, t0:t0 + T], gt_sb[:, :T], reads=[gt_b], writes=[gT_b])
    k.stack = main_stack
    S.barrier()
    stA.close()

    for tl in PASSES:
        c0 = tiles[tl[0]][0]
        Np = sum(tiles[i][1] for i in tl)
        stB = contextlib.ExitStack()
        k.stack = stB
        hp_sb, hp_b = k.sb([128, 8, Np], BF16, "hp_sb")
        acc_sb, acc_b = k.sb([128, 8, Np], F32, "acc_sb")
        S.dma("sp", hp_sb[:], h2T[:, c0:c0 + Np].rearrange("(kc p) t -> p kc t", p=128), reads=[h2T_b], writes=[hp_b])
        gq_sb, gq_b = k.sb([NEXP, 512], F32, "gq_sb")
        for i in tl:
            t0, T, _ = tiles[i]
            S.dma("sp", gq_sb[:, :T], gT[:, t0:t0 + T], reads=[gT_b], writes=[gq_b])
            for o in range(8):
                pa, pa_b = pring.next()
                S.op("pe", lambda e: e.matmul(pa[:, :T], lhsT=ebout_sb[:, o * 128:(o + 1) * 128], rhs=gq_sb[:, :T], start=True, stop=True), reads=[ebout_b, gq_b], writes=[pa_b])
                S.op("act", lambda e: e.copy(out=acc_sb[:, o, t0 - c0:t0 - c0 + T], in_=pa[:, :T]), reads=[pa_b], writes=[acc_b])
        stE = contextlib.ExitStack()
        k.stack = stE
        wi_ring = Ring([k.sb([128, 8, 1024], BF16, f"wi{i}") for i in range(2)])
        wo_ring = Ring([k.sb([128, 4, 1024], BF16, f"wo{i}") for i in range(2)])
        act_ring = Ring([k.sb([128, 4, 512], BF16, f"ac{i}") for i in range(2)])
        g1r = Ring([k.sb([128, 512], F32, f"g1{i}") for i in range(2)])
        sgr = Ring([k.sb([128, 512], F32, f"sg{i}") for i in range(2)])
        l1r = Ring([k.sb([128, 512], F32, f"l1{i}") for i in range(2)])
        gbr = Ring([k.sb([128, 512], F32, f"gb{i}") for i in range(2)])

        wstg = Ring([k.sb([128, 1024], F32, "wstg") for i in range(4)])

        def load_pieces(he):
            e_, hf = he // 2, he % 2
            wi, wi_b = wi_ring.next()
            wo, wo_b = wo_ring.next()
            pieces = []
            for kc in range(8):
                def p_in(kc=kc):
                    st, st_b = wstg.next()
                    S.dma("sp", st[:, 0:512], ewin[e_, kc * 128:(kc + 1) * 128, hf * 512:(hf + 1) * 512], writes=[st_b])
                    S.dma("sp", st[:, 512:1024], ewin[e_, kc * 128:(kc + 1) * 128, 1024 + hf * 512:1024 + (hf + 1) * 512], writes=[st_b])
                    S.op("pool", lambda e: e.tensor_copy(out=wi[:, kc, :], in_=st[:, :]), reads=[st_b], writes=[wi_b])
                pieces.append(p_in)
            for kc in range(4):
                def p_out(kc=kc):
                    st, st_b = wstg.next()
                    r0 = hf * 512 + kc * 128
                    S.dma("sp", st[:, :], ewout[e_, r0:r0 + 128, :], writes=[st_b])
                    S.op("pool", lambda e: e.tensor_copy(out=wo[:, kc, :], in_=st[:, :]), reads=[st_b], writes=[wo_b])
                pieces.append(p_out)
            return (wi, wi_b, wo, wo_b), pieces

        def load_w(he):
            bufs, pieces = load_pieces(he)
            for p in pieces:
                p()
            return bufs

        nxt = load_w(0)
        for he in range(2 * NEXP):
            e_, hf = he // 2, he % 2
            wi, wi_b, wo, wo_b = nxt
            pend = []
            if he + 1 < 2 * NEXP:
                nxt, pend = load_pieces(he + 1)
            def emit_down(ac, ac_b, lo, T):
                for o in range(8):
                    py, py_b = pring.next()
                    for kc in range(4):
                        S.op("pe", lambda e: e.matmul(py[:, :T], lhsT=wo[:, kc, o * 128:(o + 1) * 128], rhs=ac[:, kc, :T], start=(kc == 0), stop=(kc == 3)),
                             reads=[wo_b, ac_b], writes=[py_b])
                    S.op("dve", lambda e: e.tensor_tensor(out=acc_sb[:, o, lo:lo + T], in0=py[:, :T], in1=acc_sb[:, o, lo:lo + T], op=ALU.add), reads=[py_b, acc_b], writes=[acc_b])
            prev = None
            for i in tl:
                t0, T, _ = tiles[i]
                lo = t0 - c0
                gb, gb_b = gbr.next()
                S.dma("sp", gb[:, :T], bass.AP(gT_h, e_ * NT + t0, [[0, 128], [1, T]]), reads=[gT_b], writes=[gb_b])
                ac, ac_b = act_ring.next()
                for dc in range(4):
                    pg, pg_b = pring.next()
                    pl, pl_b = pring.next()
                    for kc in range(8):
                        S.op("pe", lambda e: e.matmul(pg[:, :T], lhsT=wi[:, kc, dc * 128:(dc + 1) * 128], rhs=hp_sb[:, kc, lo:lo + T], start=(kc == 0), stop=(kc == 7)),
                             reads=[wi_b, hp_b], writes=[pg_b])
                    for kc in range(8):
                        S.op("pe", lambda e: e.matmul(pl[:, :T], lhsT=wi[:, kc, 512 + dc * 128:512 + (dc + 1) * 128], rhs=hp_sb[:, kc, lo:lo + T], start=(kc == 0), stop=(kc == 7)),
                             reads=[wi_b, hp_b], writes=[pl_b])
                    gi = hf * 4 + dc
                    li = 8 + hf * 4 + dc
                    g1, g1_b = g1r.next()
                    sg, sg_b = sgr.next()
                    l1, l1_b = l1r.next()
                    S.op("dve", lambda e: e.tensor_scalar(out=g1[:, :T], in0=pg[:, :T], scalar1=ebin_sb[:, e_, gi:gi + 1], scalar2=7.0, op0=ALU.add, op1=ALU.min), reads=[pg_b, ebin_b], writes=[g1_b])
                    S.op("act", lambda e: e.activation(out=sg[:, :T], in_=g1[:, :T], func=AF.Sigmoid, scale=1.702), reads=[g1_b], writes=[sg_b])
                    S.op("dve", lambda e: e.tensor_scalar(out=l1[:, :T], in0=pl[:, :T], scalar1=ebin_sb[:, e_, li:li + 1], scalar2=None, op0=ALU.add), reads=[pl_b, ebin_b], writes=[l1_b])
                    S.op("pool", lambda e: e.tensor_scalar(out=l1[:, :T], in0=l1[:, :T], scalar1=7.0, scalar2=-7.0, op0=ALU.min, op1=ALU.max), reads=[l1_b], writes=[l1_b])
                    S.op("pool", lambda e: e.tensor_tensor(out=g1[:, :T], in0=g1[:, :T], in1=sg[:, :T], op=ALU.mult), reads=[g1_b, sg_b], writes=[g1_b])
                    S.op("dve", lambda e: e.scalar_tensor_tensor(out=g1[:, :T], in0=l1[:, :T], scalar=1.0, in1=g1[:, :T], op0=ALU.add, op1=ALU.mult), reads=[g1_b, l1_b], writes=[g1_b])
                    S.op("dve", lambda e: e.tensor_tensor(out=ac[:, dc, :T], in0=g1[:, :T], in1=gb[:, :T], op=ALU.mult), reads=[g1_b, gb_b], writes=[ac_b])
                    if pend:
                        pend.pop(0)()
                if prev is not None:
                    emit_down(*prev)
                prev = (ac, ac_b, lo, T)
            emit_down(*prev)
            while pend:
                pend.pop(0)()
        k.stack = stB
        S.barrier()
        stE.close()
        ob_ring = Ring([k.sb([128, 8, 512], F32, f"ob{i}") for i in range(2)])
        for i in tl:
            t0, T, is_ctx = tiles[i]
            c = 1 if is_ctx else 0
            lo = t0 - c0
            S.dma("sp", x_sb[:, :, :T], x1T[:, t0:t0 + T].rearrange("(kc p) t -> p kc t", p=128), reads=[x1T_b], writes=[x_b])
            for o in range(8):
                S.op("dve", lambda e: e.scalar_tensor_tensor(out=x_sb[:, o, :T], in0=acc_sb[:, o, lo:lo + T], scalar=mv_sb[:, 3, o, c:c + 1], in1=x_sb[:, o, :T], op0=ALU.mult, op1=ALU.add),
                     reads=[acc_b, mv_b, x_b], writes=[x_b])
            S.dma("sp", x2T[:, t0:t0 + T].rearrange("(kc p) t -> p kc t", p=128), x_sb[:, :, :T], reads=[x_b], final=True)
            if not do_final:
                continue
            emit_norm_tile(k, S, x_sb, x_b, T, onesb, ones_b, sq_sb, sq_b, ss_ps, ss_b, rstd_sb, rstd_b, eps_sb, eps_b)
            ob, ob_b = ob_ring.next()
            for o in range(8):
                S.op("dve", lambda e: e.scalar_tensor_tensor(out=ob[:, o, :T], in0=x_sb[:, o, :T], scalar=fg_sb[:, o:o + 1], in1=rstd_sb[:, :T], op0=ALU.mult, op1=ALU.mult),
                     reads=[x_b, fg_b, rstd_b], writes=[ob_b])
            S.dma("sp", xfT[:, t0:t0 + T].rearrange("(kc p) t -> p kc t", p=128), ob[:, :, :T], reads=[ob_b], final=True)
        k.stack = main_stack
        S.barrier()
        stB.close()
    if own:
        return k.finish()
    k.end_phase()


def emit_p2f(k, io, odd):
    k.begin_phase(io)
    S = k.S
    FR = 1952 if odd else 1664
    fm = io["fm"]
    GK = io["GK"]
    GV = io["GV"]
    tm = io["tm"]
    yT = io["yT"]
    ident_d = io["ident"]
    identb, identb_b, onesb, ones_b, eps_sb, eps_b = load_consts(k, S, ident_d)
    NB = 130 if odd else 50
    NCOL = NB * 128
    dkmax = 96 if odd else 64
    qT_sb, qT_b = k.sb([dkmax, NT], BF16, "qT_sb")
    kT_sb, kT_b = k.sb([dkmax, NCOL], BF16, "kT_sb")
    va_sb, va_b = k.sb([128, NB, 65], BF16, "va_sb")
    S.op("dve", lambda e: e.memset(va_sb[:, :, 64:65], 1.0), writes=[va_b])
    sring = Ring([k.ps([128, 512], F32, "s") for i in range(2)])
    O = [k.ps([128, 512], F32, "o") for i in range(4)]
    pt_ps, pt_b = k.ps([128, 512], BF16, "ptp")
    pring = Ring([k.sb([128, 512], BF16, "pT") for i in range(3)])
    den_sb, den_b = k.sb([128, 4], F32, "den")
    y_sb, y_b = k.sb([128, 4, 64], BF16, "y_sb")
    yTring = Ring([k.sb([64, 512], BF16, "yT") for i in range(2)])
    if not odd:
        maskA, maskA_b = k.sb([128, 6, 512], BF16, "maskA_sb")
        S.dma("pool", maskA[:], io["maskA"][:, :, :], writes=[maskA_b])
        candA, candA_b = k.sb([128, 8, 512], BF16, "candA_sb")
        S.dma("pool", candA[:], io["candA"][:, :, :], writes=[candA_b])
        selB, selB_b = k.sb([128, 8], F32, "selB_sb")
        S.dma("sp", selB[:], io["selB"][:, :], writes=[selB_b])
        wvar, wvar_b = k.sb([128, 4], F32, "wvar_sb")
        S.dma("sp", wvar[:], io["wvar"][:, :], writes=[wvar_b])
        esink, esink_b = k.sb([128, 8], F32, "esink_sb")
        S.dma("sp", esink[:], io["sink"][:, :], writes=[esink_b])
        S.op("act", lambda e: e.activation(out=esink[:], in_=esink[:], func=AF.Exp), reads=[esink_b], writes=[esink_b])
        stg_ring = Ring([k.sb([128, 512], F32, "stg") for i in range(2)])
        Mv = [k.sb([128, 8, 512], BF16, f"Mv{v}") for v in range(3)]
        MT0, MT0_b = k.sb([128, 8, 512], BF16, "MT0")
        MT7, MT7_b = k.sb([128, 8, 512], BF16, "MT7")
        tmpM, tmpM_b = k.sb([128, 8, 512], BF16, "tmpM")
        nbr = nbr_index()
        needB = [[any(bool(nbr[v][0][:, kb, qb * 128:(qb + 1) * 128].any()) for v in range(3)) for qb in range(4)] for kb in range(8)]
        mA = _maskA_np()
        needA = [[bool(mA[:, kb, qb * 128:(qb + 1) * 128].any()) for qb in range(4)] for kb in range(6)]

    def hb(r, which, b):
        return 34 + r * 4 + which * 2 + b

    def gv_rows(r, t0, n):
        out = []
        for (c0, cn, ap) in GV:
            a, b = max(t0, c0), min(t0 + n, c0 + cn)
            if a < b:
                out.append((ap[r * cn + (a - c0):r * cn + (b - c0), :], a, b - a))
        return out

    def load_v(dst_blk0, r, t0, n, vcol):
        for (ap, a, m) in gv_rows(r, t0, n):
            b0 = dst_blk0 + (a - t0) // 128
            S.dma("sp", va_sb[:, b0:b0 + m // 128, 0:64], ap[:, vcol:vcol + 64].rearrange("(kb p) d -> p kb d", p=128), writes=[va_b])

    def load_kv(krows, dk_parts, vcol):
        for (row0, nr, p0) in krows:
            gap, gn = GK[row0]
            assert gn == nr
            S.dma("sp", kT_sb[p0:p0 + nr, 0:CTX], fm[row0:row0 + nr, 0:CTX], writes=[kT_b])
            if odd:
                for r in range(4):
                    S.dma("sp", kT_sb[p0:p0 + nr, CTX + r * LAT_PC:CTX + (r + 1) * LAT_PC], gap[r * nr:(r + 1) * nr, CTX:NT], writes=[kT_b])
            else:
                S.dma("sp", kT_sb[p0:p0 + nr, CTX:NT], fm[row0:row0 + nr, CTX:NT], writes=[kT_b])
                for r in range(4):
                    S.dma("sp", kT_sb[p0:p0 + nr, NT + r * 512:NT + r * 512 + 256], gap[r * nr:(r + 1) * nr, NT - 256:NT], writes=[kT_b])
                    S.dma("sp", kT_sb[p0:p0 + nr, NT + r * 512 + 256:NT + r * 512 + 512], gap[r * nr:(r + 1) * nr, CTX:CTX + 256], writes=[kT_b])
        S.dma("sp", va_sb[:, 0:2, 0:64], tm[0:CTX, vcol:vcol + 64].rearrange("(kb p) d -> p kb d", p=128), writes=[va_b])
        if odd:
            for r in range(4):
                load_v(2 + r * 32, r, CTX, LAT_PC, vcol)
        else:
            S.dma("sp", va_sb[:, 2:34, 0:64], tm[CTX:NT, vcol:vcol + 64].rearrange("(kb p) d -> p kb d", p=128), writes=[va_b])
            for r in range(4):
                load_v(34 + r * 4, r, NT - 256, 256, vcol)
                load_v(36 + r * 4, r, CTX, 256, vcol)

    if odd:
        jobs = [("C", h) for h in range(8)] + [("D", h) for h in range(8)]
    else:
        jobs = [("A", h) for h in range(8)] + [("B", h) for h in range(8)]
    for (kind, h) in jobs:
        g = h // 4
        if kind == "A":
            dk, qrow, ychunk = 64, h * 64, h // 2
            if h % 4 == 0:
                load_kv([(512 + g * 64, 64, 0)], 64, g * 64)
        elif kind == "B":
            dk, qrow, ychunk = 64, 640 + h * 64, 4 + h // 2
            load_kv([(1152 + h * 64, 64, 0)], 64, 128 + h * 64)
            for v in range(3):
                for kb in range(8):
                    stg, stg_b = stg_ring.next()
                    S.dma("sp", stg[:], io["rpbx"][h, v, :, kb, :], writes=[stg_b])
                    S.op("act", lambda e: e.activation(out=Mv[v][0][:, kb, :], in_=stg[:], func=AF.Exp), reads=[stg_b], writes=[Mv[v][1]])
            S.op("dve", lambda e: e.tensor_scalar(out=tmpM[:], in0=Mv[1][0][:], scalar1=wvar[:, 1:2], scalar2=None, op0=ALU.mult), reads=[Mv[1][1], wvar_b], writes=[tmpM_b])
            S.op("dve", lambda e: e.scalar_tensor_tensor(out=MT0[:], in0=Mv[0][0][:], scalar=wvar[:, 0:1], in1=tmpM[:], op0=ALU.mult, op1=ALU.add), reads=[Mv[0][1], wvar_b, tmpM_b], writes=[MT0_b])
            S.op("dve", lambda e: e.tensor_scalar(out=tmpM[:], in0=Mv[1][0][:], scalar1=wvar[:, 3:4], scalar2=None, op0=ALU.mult), reads=[Mv[1][1], wvar_b], writes=[tmpM_b])
            S.op("dve", lambda e: e.scalar_tensor_tensor(out=MT7[:], in0=Mv[2][0][:], scalar=wvar[:, 2:3], in1=tmpM[:], op0=ALU.mult, op1=ALU.add), reads=[Mv[2][1], wvar_b, tmpM_b], writes=[MT7_b])
        elif kind == "C":
            dk, qrow, ychunk = 96, h * 96, h // 2
            load_kv([(768 + h * 64, 64, 0), (1280, 32, 64)], 96, h * 64)
        else:
            dk, qrow, ychunk = 64, 1312 + h * 64, 4 + h // 2
            if h % 4 == 0:
                load_kv([(1824 + g * 64, 64, 0)], 64, 512 + g * 64)
        scale = float(dk) ** -0.5
        S.dma("sp", qT_sb[:dk, :], fm[qrow:qrow + dk, :], writes=[qT_b])
        tiles = [(0, 256, None)] + [(CTX + 512 * i, 512, i) for i in range(LAT_PC // 512)]
        for (q0, nq, ti) in tiles:
            nqb = nq // 128
            kbl = []
            allq = [True] * nqb
            if ti is None:
                kbl = [(0, None, None, None, allq), (1, None, None, None, allq)]
            elif odd:
                kbl = [(kb, None, None, None, allq) for kb in range(NB)]
            elif kind == "A":
                for kbrel in range(6):
                    lb = 4 * ti + kbrel - 1
                    if 0 <= lb < 32:
                        kbl.append((2 + lb, maskA[:, kbrel, :], maskA_b, None, needA[kbrel]))
                    elif lb < 0:
                        for r in range(4):
                            kbl.append((hb(r, 0, 1), candA[:, r, :], candA_b, None, needA[kbrel]))
                    else:
                        for r in range(4):
                            kbl.append((hb(r, 1, 0), candA[:, 4 + r, :], candA_b, None, needA[kbrel]))
                kbl += [(0, None, None, None, allq), (1, None, None, None, allq)]
            else:
                M, M_b = (MT0, MT0_b) if ti == 0 else ((MT7, MT7_b) if ti == 7 else Mv[1])
                for kbrel in range(8):
                    lb = 4 * ti - 2 + kbrel
                    if not any(needB[kbrel]):
                        continue
                    if 0 <= lb < 32:
                        kbl.append((2 + lb, M[:, kbrel, :], M_b, None, needB[kbrel]))
                    elif lb < 0:
                        for r in range(4):
                            kbl.append((hb(r, 0, lb + 2), M[:, kbrel, :], M_b, selB[:, r:r + 1], needB[kbrel]))
                    else:
                        for r in range(4):
                            kbl.append((hb(r, 1, lb - 32), M[:, kbrel, :], M_b, selB[:, 4 + r:5 + r], needB[kbrel]))
                kbl += [(0, None, None, None, allq), (1, None, None, None, allq)]
            first = [min(i for i, ent in enumerate(kbl) if ent[4][qb]) for qb in range(nqb)]
            last = [max(i for i, ent in enumerate(kbl) if ent[4][qb]) for qb in range(nqb)]
            def emit_S(ii):
                kb_ = kbl[ii][0]
                sp_, sb_ = sring.next()
                S.op("pe", lambda e: e.matmul(sp_[:, :nq], lhsT=kT_sb[:dk, kb_ * 128:(kb_ + 1) * 128], rhs=qT_sb[:dk, q0:q0 + nq], start=True, stop=True),
                     reads=[kT_b, qT_b], writes=[sb_])
                return sp_, sb_
            cur_s = emit_S(0)
            for i, (kb, mask_ap, mask_b, scal, nd) in enumerate(kbl):
                s_ps, s_b = cur_s
                if i + 1 < len(kbl):
                    cur_s = emit_S(i + 1)
                p_sb, p_b = pring.next()
                S.op("act", lambda e: e.activation(out=p_sb[:, :nq], in_=s_ps[:, :nq], func=AF.Exp, scale=scale), reads=[s_b], writes=[p_b])
                if mask_ap is not None:
                    if scal is None:
                        S.op("dve" if i % 2 == 0 else "pool", lambda e: e.tensor_tensor(out=p_sb[:, :nq], in0=p_sb[:, :nq], in1=mask_ap, op=ALU.mult),
                             reads=[p_b, mask_b], writes=[p_b])
                    else:
                        S.op("dve", lambda e: e.scalar_tensor_tensor(out=p_sb[:, :nq], in0=p_sb[:, :nq], scalar=scal, in1=mask_ap, op0=ALU.mult, op1=ALU.mult),
                             reads=[p_b, mask_b, selB_b], writes=[p_b])
                for qb in range(nqb):
                    if nd[qb]:
                        o_ps, o_b = O[qb]
                        S.op("pe", lambda e: e.matmul(o_ps[:, 0:65], lhsT=p_sb[:, qb * 128:(qb + 1) * 128], rhs=va_sb[:, kb, :],
                                                      start=(i == first[qb]), stop=(i == last[qb])), reads=[p_b, va_b], writes=[o_b])
            yt_sb, yt_b = yTring.next()
            for qb in range(nqb):
                o_ps, o_b = O[qb]
                if kind == "A":
                    S.op("dve", lambda e: e.tensor_tensor(out=den_sb[:, qb:qb + 1], in0=o_ps[:, 64:65], in1=esink[:, h:h + 1], op=ALU.add), reads=[o_b, esink_b], writes=[den_b])
                    S.op("dve", lambda e: e.reciprocal(out=den_sb[:, qb:qb + 1], in_=den_sb[:, qb:qb + 1]), reads=[den_b], writes=[den_b])
                else:
                    S.op("dve", lambda e: e.reciprocal(out=den_sb[:, qb:qb + 1], in_=o_ps[:, 64:65]), reads=[o_b], writes=[den_b])
                S.op("dve", lambda e: e.tensor_scalar(out=y_sb[:, qb, :], in0=o_ps[:, 0:64], scalar1=den_sb[:, qb:qb + 1], scalar2=None, op0=ALU.mult),
                     reads=[o_b, den_b], writes=[y_b])
                S.op("pe", lambda e: e.transpose(out=pt_ps[:64, qb * 128:(qb + 1) * 128], in_=y_sb[:, qb, :], identity=identb[:, :]), reads=[y_b, identb_b], writes=[pt_b])
            S.op("dve", lambda e: e.tensor_copy(out=yt_sb[:, :nq], in_=pt_ps[:64, :nq]), reads=[pt_b], writes=[yt_b])
            S.dma("sp", yT[ychunk, (h % 2) * 64:(h % 2) * 64 + 64, q0:q0 + nq], yt_sb[:, :nq], reads=[yt_b])
    k.end_phase()


def build_fused(depth=DEPTH):
    k = K("fused")
    k.fused = True
    S = k.S
    nc = k.nc
    EI = "ExternalInput"
    g = {}
    g["xT0"] = k.dram("xT0", [DM, NT], F32, EI)
    for nm, shp in (("cond", [128, 8, 2]), ("ident", [128, 128]), ("blk", [128, 128]), ("ropeC", [128, NT]), ("ropeS", [128, NT]),
                    ("ropeC2", [96, NT]), ("ropeS2", [96, NT]), ("ropeC3", [32, NT]), ("ropeS3", [32, NT]), ("fgain", [128, 8]),
                    ("maskA", [128, 6, 512]), ("candA", [128, 8, 512]), ("selB", [128, 8]), ("wvar", [128, 4])):
        g[nm] = k.dram(nm, shp, F32, EI)
    L = []
    for l in range(depth):
        odd = l % 2 == 1
        d = {}
        d["modw"] = k.dram(f"modw{l}", [DM, 6144], F32, EI)
        d["modb"] = k.dram(f"modb{l}", [128, 48], F32, EI)
        d["gmix"] = k.dram(f"gmix{l}", [128, 8], F32, EI)
        d["gffn"] = k.dram(f"gffn{l}", [128, 8], F32, EI)
        d["w"] = k.dram(f"w{l}", [DM, (1824 + 32 + 640) if odd else (2304 + 640)], F32, EI)
        if odd:
            d["wqb"] = k.dram(f"wqb{l}", [768, 1536], F32, EI)
            d["wkvb"] = k.dram(f"wkvb{l}", [256, 1024], F32, EI)
            d["qn"] = k.dram(f"qn{l}", [128, 6], F32, EI)
            d["kvn"] = k.dram(f"kvn{l}", [128, 2], F32, EI)
            d["dgq"] = k.dram(f"dgq{l}", [128, 4], F32, EI)
        else:
            d["sink"] = k.dram(f"sink{l}", [128, 8], F32, EI)
            d["rpbx"] = k.dram(f"rpbx{l}", [8, 3, 128, 8, 512], F32, EI)
        d["wout"] = k.dram(f"wout{l}", [DM, DM], F32, EI)
        d["rw"] = k.dram(f"rw{l}", [DM, NEXP], F32, EI)
        d["rb"] = k.dram(f"rb{l}", [1, NEXP], F32, EI)
        d["ewin"] = k.dram(f"ewin{l}", [NEXP, DM, 2048], F32, EI)
        d["ebin"] = k.dram(f"ebin{l}", [128, NEXP, 16], F32, EI)
        d["ewout"] = k.dram(f"ewout{l}", [NEXP, DM, DM], F32, EI)
        d["ebout"] = k.dram(f"ebout{l}", [NEXP, DM], F32, EI)
        L.append(d)
    out = k.dram("out", [DM, NT], F32, "ExternalOutput")
    XA = k.dram("XA", [DM, NT], F32, "Internal")
    XB = k.dram("XB", [DM, NT], F32, "Internal")
    fmE = k.dram("fmE", [1664, NT], BF16, "Internal")
    fmO = k.dram("fmO", [1952, NT], BF16, "Internal")
    tmE = k.dram("tmE", [NT, 640], BF16, "Internal")
    tmO = k.dram("tmO", [NT, 640], BF16, "Internal")
    def kpieces(odd):
        rows = [(768 + 64 * i, 64) for i in range(8)] + [(1280, 32)] + [(1824, 64), (1888, 64)] if odd else \
               [(512, 64), (576, 64)] + [(1152 + 64 * i, 64) for i in range(8)]
        return rows
    vpieces = [(c * 512, min(512, NT - c * 512)) for c in range((NT + 511) // 512)]
    GKs, GVs = {}, {}
    for par, tag in ((False, "E"), (True, "O")):
        GKs[par] = {r0: (k.dram(f"gk{tag}{r0}", [4 * n, NT], BF16, "Internal"), n) for (r0, n) in kpieces(par)}
        GVs[par] = [(t0, n, k.dram(f"gv{tag}{t0}", [4 * n, 640], BF16, "Internal")) for (t0, n) in vpieces]
    yTd = k.dram("yTd", [8, 128, NT], BF16, "Internal")
    groups = [[0, 1, 2, 3], [4, 5, 6, 7]]
    ccscr, _ = k.sb([1, 4], F32, "ccscr")
    xs = [g["xT0"], XA, XB]
    for l in range(depth):
        odd = l % 2 == 1
        d = L[l]
        xin = xs[0] if l == 0 else xs[1 + (l - 1) % 2]
        xout = xs[1 + l % 2]
        fm_, tm_ = (fmO, tmO) if odd else (fmE, tmE)
        io = {"xT": xin, "cond": g["cond"], "modw": d["modw"][:, 0:2048], "modb": d["modb"][:, 0:16], "gain": d["gmix"],
              "ropeC": g["ropeC"], "ropeS": g["ropeS"], "w": d["w"], "fm": fm_, "tm": tm_, "ident": g["ident"]}
        if odd:
            io.update({"wqb": d["wqb"], "wkvb": d["wkvb"], "qn": d["qn"], "kvn": d["kvn"], "dgq": d["dgq"], "ropeC2": g["ropeC2"],
                       "ropeS2": g["ropeS2"], "ropeC3": g["ropeC3"], "ropeS3": g["ropeS3"], "blk": g["blk"]})
        build_p1(odd, k, io)
        for r0, (gap, n) in GKs[odd].items():
            S.cc(k.stack, lambda e: e.collective_compute("AllGather", ALU.bypass, replica_groups=groups, ins=[fm_[r0:r0 + n, :]], outs=[gap]), ccscr[0:1, 0:4])
        for (t0, n, gap) in GVs[odd]:
            S.cc(k.stack, lambda e: e.collective_compute("AllGather", ALU.bypass, replica_groups=groups, ins=[tm_[t0:t0 + n, :]], outs=[gap]), ccscr[0:1, 0:4])
        S.barrier()
        io2 = {"fm": fm_, "tm": tm_, "GK": GKs[odd], "GV": GVs[odd], "yT": yTd, "ident": g["ident"]}
        if not odd:
            io2.update({"maskA": g["maskA"], "candA": g["candA"], "selB": g["selB"], "wvar": g["wvar"], "sink": d["sink"], "rpbx": d["rpbx"]})
        emit_p2f(k, io2, odd)
        last = l == depth - 1
        io3 = {"xT": xin, "yT": yTd, "cond": g["cond"], "modw": d["modw"][:, 2048:6144], "modb": d["modb"][:, 16:48], "gain": d["gffn"],
               "fgain": g["fgain"], "wout": d["wout"], "rw": d["rw"], "rb": d["rb"], "ewin": d["ewin"], "ebin": d["ebin"],
               "ewout": d["ewout"], "ebout": d["ebout"], "ident": g["ident"], "x2T": xout, "xfT": out}
        build_p3(k, io3, do_final=last)
    return k.finish()


def kernel(x, c, ctx, c_ctx, mod_w, mod_b, norm_mix, norm_ffn, ab_w_in, ab_w_out, a_sink, b_rpb,
                 cd_w_in, c_q_norm, c_w_q_b, c_kv_norm, c_w_kv_b, d_q_norm, d_k_norm, cd_w_out,
                 router_w, router_b, exp_w_in, exp_b_in, exp_w_out, exp_b_out, final_norm, _depth=DEPTH):
    f32 = lambda a: np.ascontiguousarray(np.asarray(a, np.float32))
    x, c, ctx, c_ctx = f32(x), f32(c), f32(ctx), f32(c_ctx)
    if ("fused", _depth) not in _PROGS:
        _PROGS[("fused", _depth)] = build_fused(_depth)
    nc = _PROGS[("fused", _depth)]
    Ch, Sh = _rope_tables(64, [d for _ in range(2) for d in range(64)])
    Cm, Sm = _rope_tables(32, [-1] * 64 + list(range(32)))
    C3, S3 = _rope_tables(32, list(range(32)))
    mA = _maskA_np()
    shared = {"ident": np.eye(128, dtype=np.float32), "blk": np.kron(np.eye(2, dtype=np.float32), np.ones((64, 64), np.float32)),
              "fgain": fm(final_norm, 8), "maskA": mA}
    for l in range(_depth):
        i = l // 2
        odd = l % 2 == 1
        shared[f"modw{l}"] = f32(mod_w[l])
        shared[f"modb{l}"] = fm(f32(mod_b[l]), 48)
        shared[f"gmix{l}"] = fm(norm_mix[l], 8)
        shared[f"gffn{l}"] = fm(norm_ffn[l], 8)
        if not odd:
            w = f32(ab_w_in[i])
            shared[f"w{l}"] = np.ascontiguousarray(np.concatenate([w, _swap_cols(w, 0, 10, 64, 0, 64)], axis=1))
            shared[f"sink{l}"] = np.ascontiguousarray(np.tile(f32(a_sink[i])[None, :], (128, 1)))
            rp = f32(b_rpb[i])
            rx = np.empty((8, 3, 128, 8, 512), np.float32)
            for hh in range(8):
                for v, (valid, dr, dc) in enumerate(nbr_index()):
                    rx[hh, v] = np.where(valid, rp[hh][dr, dc], np.float32(-30000.0))
            shared[f"rpbx{l}"] = rx
            shared[f"wout{l}"] = f32(ab_w_out[i])
        else:
            w = f32(cd_w_in[i])
            shared[f"w{l}"] = np.ascontiguousarray(np.concatenate([w, _swap_cols(w, 1024, 1, 32, 0, 32), _swap_cols(w, 1056, 10, 64, 0, 64)], axis=1))
            wq = f32(c_w_q_b[i])
            shared[f"wqb{l}"] = np.ascontiguousarray(np.concatenate([wq, _swap_cols(wq, 0, 8, 96, 64, 32)], axis=1))
            wkv = f32(c_w_kv_b[i]).reshape(256, 8, 128)
            shared[f"wkvb{l}"] = np.ascontiguousarray(np.concatenate([wkv[:, :, :64].reshape(256, 512), wkv[:, :, 64:].reshape(256, 512)], axis=1))
            shared[f"qn{l}"] = fm(c_q_norm[i], 6)
            shared[f"kvn{l}"] = fm(c_kv_norm[i], 2)
            gq, gk = f32(d_q_norm[i]), f32(d_k_norm[i])
            sw = (np.arange(64) + 32) % 64
            shared[f"dgq{l}"] = np.ascontiguousarray(np.stack([np.tile(gq, 2), np.tile(gq[sw], 2), np.tile(gk, 2), np.tile(gk[sw], 2)], axis=1))
            shared[f"wout{l}"] = f32(cd_w_out[i])
        shared[f"rw{l}"] = f32(router_w[l])
        shared[f"rb{l}"] = f32(router_b[l])[None, :]
        shared[f"ewin{l}"] = f32(exp_w_in[l])
        shared[f"ebin{l}"] = np.ascontiguousarray(f32(exp_b_in[l]).reshape(NEXP, 16, 128).transpose(2, 0, 1))
        shared[f"ewout{l}"] = f32(exp_w_out[l])
        shared[f"ebout{l}"] = f32(exp_b_out[l])
    ins = []
    for core in range(NCORES):
        b, r = core // 4, core % 4
        d = dict(shared)
        tok = np.concatenate([ctx[b], x[b, r * LAT_PC:(r + 1) * LAT_PC]], axis=0)
        d["xT0"] = np.ascontiguousarray(tok.T)
        d["cond"] = np.ascontiguousarray(np.stack([fm(c[b], 8), fm(c_ctx, 8)], axis=-1))
        d["ropeC"], d["ropeS"] = _tabs_for_core(Ch, Sh, r)
        d["ropeC2"], d["ropeS2"] = _tabs_for_core(Cm, Sm, r)
        d["ropeC3"], d["ropeS3"] = _tabs_for_core(C3, S3, r)
        cand = np.zeros((128, 8, 512), np.float32)
        sel = np.zeros((128, 8), np.float32)
        for rr in range(4):
            if rr == r - 1:
                cand[:, rr, :] = mA[:, 0, :]
                sel[:, rr] = 1.0
            if rr == r + 1:
                cand[:, 4 + rr, :] = mA[:, 5, :]
                sel[:, 4 + rr] = 1.0
        d["candA"], d["selB"] = cand, sel
        wv = np.zeros((128, 4), np.float32)
        wv[:, 0] = 1.0 if r == 0 else 0.0
        wv[:, 1] = 0.0 if r == 0 else 1.0
        wv[:, 2] = 1.0 if r == 3 else 0.0
        wv[:, 3] = 0.0 if r == 3 else 1.0
        d["wvar"] = wv
        ins.append(d)
    res = run_bass_kernel_spmd(nc, ins, core_ids=list(range(NCORES))).results
    out = np.empty((BATCH, SEQ, DM), np.float32)
    for core in range(NCORES):
        b, r = core // 4, core % 4
        out[b, r * LAT_PC:(r + 1) * LAT_PC] = res[core]["out"][:, CTX:].T
    return out


_PROGS = {}
BF = ml_dtypes.bfloat16


def _prog(name):
    if name not in _PROGS:
        if name == "p1e":
            _PROGS[name] = build_p1(False)
        elif name == "p1o":
            _PROGS[name] = build_p1(True)
        elif name == "p2e":
            _PROGS[name] = build_p2(False)
        elif name == "p2o":
            _PROGS[name] = build_p2(True)
        else:
            _PROGS[name] = build_p3()
    return _PROGS[name]


def _run(name, in_maps):
    res = run_bass_kernel_spmd(_prog(name), in_maps, core_ids=list(range(NCORES)))
    return res.results


def fm(v, n):
    return np.ascontiguousarray(np.asarray(v, np.float32).reshape(n, 128).T)


def _rope_tables(dim, rows_pattern):
    t = np.arange(SEQ, dtype=np.int32)
    row = (t // GRID_W).astype(np.float32)
    col = (t % GRID_W).astype(np.float32)
    quarter = dim // 4
    inv = (np.float32(10000.0) ** (-np.arange(quarter, dtype=np.float32) / np.float32(quarter))).astype(np.float32)
    ang = np.concatenate([row[:, None] * inv, col[:, None] * inv], axis=-1).astype(np.float32)
    cos, sin = np.cos(ang).astype(np.float32), np.sin(ang).astype(np.float32)
    half = dim // 2
    C = np.ones((len(rows_pattern), SEQ), np.float32)
    Sn = np.zeros((len(rows_pattern), SEQ), np.float32)
    for r, d in enumerate(rows_pattern):
        if d < 0:
            continue
        C[r] = cos[:, d % half]
        Sn[r] = -sin[:, d % half] if d < half else sin[:, d % half]
    return C, Sn


def _core_table(tab, r):
    out = np.empty((tab.shape[0], NT), np.float32)
    out[:, :CTX] = tab[:, :1] * 0 + (1.0 if tab is None else 0.0)
    return out


def _tabs_for_core(C, Sn, r):
    Cc = np.ones((C.shape[0], NT), np.float32)
    Sc = np.zeros((C.shape[0], NT), np.float32)
    Cc[:, CTX:] = C[:, r * LAT_PC:(r + 1) * LAT_PC]
    Sc[:, CTX:] = Sn[:, r * LAT_PC:(r + 1) * LAT_PC]
    return Cc, Sc


def _swap_cols(w, c0, nheads, hd, rot0, rotd):
    blk = w[:, c0:c0 + nheads * hd].copy()
    idx = np.arange(nheads * hd)
    h, d = idx // hd, idx % hd
    src = idx.copy()
    inrot = (d >= rot0) & (d < rot0 + rotd)
    src[inrot] = h[inrot] * hd + rot0 + ((d[inrot] - rot0 + rotd // 2) % rotd)
    return blk[:, src]


def kernel_unfused(x, c, ctx, c_ctx, mod_w, mod_b, norm_mix, norm_ffn, ab_w_in, ab_w_out, a_sink, b_rpb,
           cd_w_in, c_q_norm, c_w_q_b, c_kv_norm, c_w_kv_b, d_q_norm, d_k_norm, cd_w_out,
           router_w, router_b, exp_w_in, exp_b_in, exp_w_out, exp_b_out, final_norm, _depth=DEPTH):
    f32 = lambda a: np.asarray(a, np.float32)
    x, c, ctx, c_ctx = f32(x), f32(c), f32(ctx), f32(c_ctx)
    ident = np.eye(128, dtype=np.float32)
    xT = []
    for core in range(NCORES):
        b, r = core // 4, core % 4
        tok = np.concatenate([ctx[b], x[b, r * LAT_PC:(r + 1) * LAT_PC]], axis=0)
        xT.append(np.ascontiguousarray(tok.T))
    conds = [np.ascontiguousarray(np.stack([fm(c[core // 4], 8), fm(c_ctx, 8)], axis=-1)) for core in range(NCORES)]
    Ch, Sh = _rope_tables(64, [d for _ in range(2) for d in range(64)])
    Cm, Sm = _rope_tables(32, [-1] * 64 + list(range(32)))
    C3, S3 = _rope_tables(32, list(range(32)))
    blk = np.kron(np.eye(2, dtype=np.float32), np.ones((64, 64), np.float32))
    maskA = _maskA_np()
    xf = None
    for l in range(_depth):
        i = l // 2
        odd = l % 2 == 1
        mw, mb = f32(mod_w[l]), f32(mod_b[l])
        ins = []
        for core in range(NCORES):
            r = core % 4
            Cc, Sc = _tabs_for_core(Ch, Sh, r)
            d = {"xT": xT[core], "cond": conds[core], "modw": np.ascontiguousarray(mw[:, :2048]), "modb": fm(mb[:2048], 16),
                 "gain": fm(norm_mix[l], 8), "ropeC": Cc, "ropeS": Sc, "ident": ident}
            if not odd:
                w = f32(ab_w_in[i])
                d["w"] = np.ascontiguousarray(np.concatenate([w, _swap_cols(w, 0, 10, 64, 0, 64)], axis=1))
            else:
                w = f32(cd_w_in[i])
                d["w"] = np.ascontiguousarray(np.concatenate([w, _swap_cols(w, 1024, 1, 32, 0, 32), _swap_cols(w, 1056, 10, 64, 0, 64)], axis=1))
                wq = f32(c_w_q_b[i])
                d["wqb"] = np.ascontiguousarray(np.concatenate([wq, _swap_cols(wq, 0, 8, 96, 64, 32)], axis=1))
                wkv = f32(c_w_kv_b[i]).reshape(256, 8, 128)
                d["wkvb"] = np.ascontiguousarray(np.concatenate([wkv[:, :, :64].reshape(256, 512), wkv[:, :, 64:].reshape(256, 512)], axis=1))
                d["qn"] = fm(c_q_norm[i], 6)
                d["kvn"] = fm(c_kv_norm[i], 2)
                gq, gk = f32(d_q_norm[i]), f32(d_k_norm[i])
                sw = (np.arange(64) + 32) % 64
                d["dgq"] = np.ascontiguousarray(np.stack([np.tile(gq, 2), np.tile(gq[sw], 2), np.tile(gk, 2), np.tile(gk[sw], 2)], axis=1))
                d["ropeC2"], d["ropeS2"] = _tabs_for_core(Cm, Sm, r)
                d["ropeC3"], d["ropeS3"] = _tabs_for_core(C3, S3, r)
                d["blk"] = blk
            ins.append(d)
        res = _run("p1o" if odd else "p1e", ins)
        ins2 = []
        for b in range(BATCH):
            FM = np.concatenate([res[4 * b]["fm"][:, :CTX]] + [res[4 * b + r]["fm"][:, CTX:] for r in range(4)], axis=1)
            TM = np.concatenate([res[4 * b]["tm"][:CTX]] + [res[4 * b + r]["tm"][CTX:] for r in range(4)], axis=0)
            for j in range(4):
                g = j // 2
                if not odd:
                    d = {"q1T": np.stack([FM[(2 * j + hh) * 64:(2 * j + hh + 1) * 64] for hh in range(2)]),
                         "k1T": FM[512 + g * 64:512 + (g + 1) * 64], "v1": TM[:, g * 64:(g + 1) * 64],
                         "q2T": np.stack([FM[640 + (2 * j + hh) * 64:640 + (2 * j + hh + 1) * 64] for hh in range(2)]),
                         "k2T": np.stack([FM[1152 + (2 * j + hh) * 64:1152 + (2 * j + hh + 1) * 64] for hh in range(2)]),
                         "v2": TM[:, 128 + 2 * j * 64:128 + (2 * j + 2) * 64], "maskA": maskA}
                    d["sink"] = np.ascontiguousarray(np.tile(f32(a_sink[i])[None, 2 * j:2 * j + 2], (128, 1)))
                    rp = f32(b_rpb[i])
                    rx = np.empty((2, 3, 128, 8, 512), np.float32)
                    for hh in range(2):
                        for v, (valid, dr, dc) in enumerate(nbr_index()):
                            rx[hh, v] = np.where(valid, rp[2 * j + hh][dr, dc], np.float32(-30000.0))
                    d["rpbx"] = rx
                else:
                    d = {"q1T": np.stack([FM[1312 + (2 * j + hh) * 64:1312 + (2 * j + hh + 1) * 64] for hh in range(2)]),
                         "k1T": FM[1824 + g * 64:1824 + (g + 1) * 64], "v1": TM[:, 512 + g * 64:512 + (g + 1) * 64],
                         "q2T": np.stack([FM[(2 * j + hh) * 96:(2 * j + hh + 1) * 96] for hh in range(2)]),
                         "k2T": np.stack([np.concatenate([FM[768 + (2 * j + hh) * 64:768 + (2 * j + hh + 1) * 64], FM[1280:1312]], axis=0) for hh in range(2)]),
                         "v2": TM[:, 2 * j * 64:(2 * j + 2) * 64]}
                d = {kk: np.ascontiguousarray(vv) for kk, vv in d.items()}
                d["ident"] = ident
                ins2.append(d)
        del res
        res2 = _run("p2o" if odd else "p2e", ins2)
        ins3 = []
        for core in range(NCORES):
            b, r = core // 4, core % 4
            yin = np.empty((8, 128, NT), BF)
            for j in range(4):
                y = res2[4 * b + j]["yT"]
                for mx in range(2):
                    ci = (mx * 4 + j) if not odd else ((1 - mx) * 4 + j)
                    yin[ci, :, :CTX] = y[mx][:, :CTX]
                    yin[ci, :, CTX:] = y[mx][:, CTX + r * LAT_PC:CTX + (r + 1) * LAT_PC]
            d = {"xT": xT[core], "yT": yin, "cond": conds[core], "modw": np.ascontiguousarray(mw[:, 2048:]), "modb": fm(mb[2048:], 32),
                 "gain": fm(norm_ffn[l], 8), "fgain": fm(final_norm, 8), "wout": f32(cd_w_out[i] if odd else ab_w_out[i]),
                 "rw": f32(router_w[l]), "rb": f32(router_b[l])[None, :], "ewin": f32(exp_w_in[l]),
                 "ebin": np.ascontiguousarray(f32(exp_b_in[l]).reshape(NEXP, 16, 128).transpose(2, 0, 1)),
                 "ewout": f32(exp_w_out[l]), "ebout": f32(exp_b_out[l]), "ident": ident}
            ins3.append(d)
        del res2
        res3 = _run("p3", ins3)
        xT = [res3[core]["x2T"] for core in range(NCORES)]
        xf = [res3[core]["xfT"] for core in range(NCORES)]
        del res3
    out = np.empty((BATCH, SEQ, DM), np.float32)
    for core in range(NCORES):
        b, r = core // 4, core % 4
        out[b, r * LAT_PC:(r + 1) * LAT_PC] = xf[core][:, CTX:].T
    return out
```
